# Optimizing a Trainium2 kernel written in Bass

```python
import math
import jax, jax.numpy as jnp
from jax import lax
import numpy as np

D_MODEL = 1024
BATCH = 2
SEQ = 8192
DEPTH = 4

N_A_LAYERS = DEPTH // 2
N_B_LAYERS = DEPTH - N_A_LAYERS

DN_HEADS = 8
DN_HEAD_DIM = D_MODEL // DN_HEADS
DN_WIDTH = DN_HEADS * DN_HEAD_DIM
CONV_K = 4
DN_CHUNK = 64

DA_HEADS = 8
DA_HEAD_DIM = D_MODEL // (2 * DA_HEADS)
DA_V_DIM = 2 * DA_HEAD_DIM
DA_QK_WIDTH = 2 * DA_HEADS * DA_HEAD_DIM
DA_WIDTH = DA_HEADS * DA_V_DIM
Q_BLOCK = 128

N_GROUPS = 4
EXPERTS_PER_GROUP = 8
N_EXPERTS = N_GROUPS * EXPERTS_PER_GROUP
TOP_K = 2
EXPERT_FF = D_MODEL // 2
MOE_BLOCK = 128

ALPHA = (2 * DEPTH) ** 0.25
BETA_INIT = (8 * DEPTH) ** -0.25
LN_EPS = 1e-5
RMS_EPS = 1e-6

kernel_name = 'yoco_deltanet_diffattn_hier_moe'


def layer_norm(x, g, b):
    xf = x.astype(jnp.float32)
    mu = jnp.mean(xf, axis=-1, keepdims=True)
    var = jnp.mean(jnp.square(xf - mu), axis=-1, keepdims=True)
    return ((xf - mu) * lax.rsqrt(var + LN_EPS) * g + b).astype(x.dtype)


def rms_norm(x, w):
    xf = x.astype(jnp.float32)
    return xf * lax.rsqrt(jnp.mean(jnp.square(xf), axis=-1, keepdims=True) + RMS_EPS) * w.astype(jnp.float32)


def l2_normalize(x):
    xf = x.astype(jnp.float32)
    return xf * lax.rsqrt(jnp.sum(jnp.square(xf), axis=-1, keepdims=True) + RMS_EPS)


def causal_depthwise_conv(x, w):
    k_width, t = w.shape[0], x.shape[1]
    xp = jnp.pad(x, ((0, 0), (k_width - 1, 0), (0, 0)))
    y = xp[:, 0:t] * w[0]
    for j in range(1, k_width):
        y = y + xp[:, j:j + t] * w[j]
    return y


def chunked_gated_delta_rule(q, k, v, g, beta):
    b, t, h, dk = q.shape
    dv = v.shape[-1]
    n, c = t // DN_CHUNK, DN_CHUNK

    def to_chunks(a):
        return jnp.moveaxis(a.reshape((b, n, c, h) + a.shape[3:]), 3, 1)

    q = to_chunks(q * dk ** -0.5)
    k = to_chunks(k)
    v = to_chunks(v.astype(jnp.float32))
    gc = jnp.cumsum(to_chunks(g), axis=-1)
    beta = to_chunks(beta)
    idx = jnp.arange(c)
    causal = idx[:, None] >= idx[None, :]
    strict = idx[:, None] > idx[None, :]
    decay = jnp.exp(jnp.where(causal, gc[..., :, None] - gc[..., None, :], -jnp.inf))
    k_beta = k * beta[..., None]
    a_mat = jnp.where(strict, jnp.einsum('bhnid,bhnjd->bhnij', k_beta, k) * decay, 0.0)
    rhs = jnp.concatenate([v * beta[..., None], k_beta * jnp.exp(gc)[..., None]], axis=-1)
    sol = lax.linalg.triangular_solve(a_mat + jnp.eye(c, dtype=jnp.float32), rhs,
                                      left_side=True, lower=True, unit_diagonal=True)
    u, w = sol[..., :dv], sol[..., dv:]
    attn = jnp.where(causal, jnp.einsum('bhnid,bhnjd->bhnij', q, k) * decay, 0.0)
    q_dec = q * jnp.exp(gc)[..., None]
    k_dec = k * jnp.exp(gc[..., -1:] - gc)[..., None]
    g_last = jnp.exp(gc[..., -1])

    def step(state, xs):
        u_i, w_i, q_i, k_i, attn_i, gl_i = xs
        v_new = u_i - jnp.einsum('bhcd,bhdv->bhcv', w_i, state)
        o_i = jnp.einsum('bhcd,bhdv->bhcv', q_i, state) + jnp.einsum('bhij,bhjv->bhiv', attn_i, v_new)
        state = state * gl_i[..., None, None] + jnp.einsum('bhcd,bhcv->bhdv', k_i, v_new)
        return state, o_i

    xs = tuple(jnp.moveaxis(a, 2, 0) for a in (u, w, q_dec, k_dec, attn, g_last))
    state0 = jnp.zeros((b, h, dk, dv), jnp.float32)
    _, o = lax.scan(step, state0, xs)
    return o.transpose(1, 0, 3, 2, 4).reshape(b, t, h, dv)


def gated_deltanet(x, w_in, conv_w, a_log, dt_bias, norm_w, w_out):
    b, t, _ = x.shape
    proj = x @ w_in
    qkv = jax.nn.silu(causal_depthwise_conv(proj[..., :3 * DN_WIDTH], conv_w))
    qkv = qkv.reshape(b, t, 3, DN_HEADS, DN_HEAD_DIM)
    q = l2_normalize(qkv[:, :, 0])
    k = l2_normalize(qkv[:, :, 1])
    v = qkv[:, :, 2]
    z = proj[..., 3 * DN_WIDTH:4 * DN_WIDTH].reshape(b, t, DN_HEADS, DN_HEAD_DIM)
    beta = jax.nn.sigmoid(proj[..., 4 * DN_WIDTH:4 * DN_WIDTH + DN_HEADS].astype(jnp.float32))
    alpha_logit = proj[..., 4 * DN_WIDTH + DN_HEADS:].astype(jnp.float32)
    g = -jnp.exp(a_log.astype(jnp.float32)) * jax.nn.softplus(alpha_logit + dt_bias.astype(jnp.float32))
    o = chunked_gated_delta_rule(q, k, v, g, beta)
    o = rms_norm(o, norm_w) * jax.nn.silu(z.astype(jnp.float32))
    return o.reshape(b, t, DN_WIDTH).astype(x.dtype) @ w_out


def shared_kv(x, kv_w):
    b, t, _ = x.shape
    kv = x @ kv_w
    keys = kv[..., :DA_QK_WIDTH].reshape(b, t, DA_HEADS, 2, DA_HEAD_DIM).transpose(3, 0, 2, 1, 4)
    vals = kv[..., DA_QK_WIDTH:].reshape(b, t, DA_HEADS, DA_V_DIM).transpose(0, 2, 1, 3)
    return keys, vals


def differential_attention(x, keys, vals, w_q, lam_p, subln_w, w_out, lam_init):
    b, t, _ = x.shape
    nb = t // Q_BLOCK
    q = (x @ w_q).reshape(b, nb, Q_BLOCK, DA_HEADS, 2, DA_HEAD_DIM).transpose(1, 4, 0, 3, 2, 5)
    lp = lam_p.astype(jnp.float32)
    lam = jnp.exp(jnp.sum(lp[0] * lp[1])) - jnp.exp(jnp.sum(lp[2] * lp[3])) + lam_init
    key_pos = jnp.arange(t)
    scale = DA_HEAD_DIM ** -0.5

    def block(args):
        q_blk, blk = args
        query_pos = blk * Q_BLOCK + jnp.arange(Q_BLOCK)
        mask = key_pos[None, :] <= query_pos[:, None]
        s = jnp.einsum('sbhqd,sbhkd->sbhqk', q_blk, keys).astype(jnp.float32) * scale
        p = jax.nn.softmax(jnp.where(mask, s, -jnp.inf), axis=-1)
        a = p[0] - lam * p[1]
        return jnp.einsum('bhqk,bhkv->bhqv', a.astype(vals.dtype), vals)

    o = lax.map(block, (q, jnp.arange(nb)))
    o = o.transpose(1, 0, 3, 2, 4).reshape(b, t, DA_HEADS, DA_V_DIM)
    o = rms_norm(o, subln_w) * (1.0 - lam_init)
    return o.reshape(b, t, DA_WIDTH).astype(x.dtype) @ w_out


def hierarchical_moe(x, w_group, w_expert, w13, w2):
    b, t, d = x.shape
    n = b * t
    m = n * TOP_K
    xf = x.reshape(n, d)
    group_prob = jax.nn.softmax((xf @ w_group).astype(jnp.float32), axis=-1)
    p_group, group_idx = lax.top_k(group_prob, 1)
    expert_logits = (xf @ w_expert).astype(jnp.float32).reshape(n, N_GROUPS, EXPERTS_PER_GROUP)
    in_group = expert_logits[jnp.arange(n), group_idx[:, 0]]
    top_p, top_i = lax.top_k(jax.nn.softmax(in_group, axis=-1), TOP_K)
    gate = (p_group * top_p / jnp.sum(top_p, axis=-1, keepdims=True)).reshape(m)
    expert_id = (group_idx * EXPERTS_PER_GROUP + top_i).reshape(m)
    token_id = jnp.repeat(jnp.arange(n), TOP_K)
    order = jnp.argsort(expert_id)
    eid_s, tok_s, gate_s = expert_id[order], token_id[order], gate[order]
    counts = jnp.bincount(expert_id, length=N_EXPERTS)
    start = jnp.cumsum(counts) - counts
    padded = (counts + MOE_BLOCK - 1) // MOE_BLOCK * MOE_BLOCK
    pad_end = jnp.cumsum(padded)
    pad_start = pad_end - padded
    dest = pad_start[eid_s] + jnp.arange(m) - start[eid_s]
    n_rows = (m + N_EXPERTS * (MOE_BLOCK - 1) + MOE_BLOCK - 1) // MOE_BLOCK * MOE_BLOCK
    n_blocks = n_rows // MOE_BLOCK
    row_tok = jnp.zeros((n_rows,), jnp.int32).at[dest].set(tok_s)
    row_gate = jnp.zeros((n_rows,), x.dtype).at[dest].set(gate_s.astype(x.dtype))
    block_eid = jnp.minimum(jnp.searchsorted(pad_end, jnp.arange(n_blocks) * MOE_BLOCK, side='right'),
                            N_EXPERTS - 1)
    x_rows = xf[row_tok].reshape(n_blocks, MOE_BLOCK, d)

    def expert_block(args):
        xb, e = args
        hcat = xb @ w13[e]
        hid = jax.nn.silu(hcat[:, :EXPERT_FF]) * hcat[:, EXPERT_FF:]
        return hid @ w2[e]

    y_rows = lax.map(expert_block, (x_rows, block_eid)).reshape(n_rows, d)
    y = jax.ops.segment_sum(y_rows * row_gate[:, None], row_tok, num_segments=n)
    return y.reshape(b, t, d)


def lambda_init(layer_idx):
    return 0.8 - 0.6 * math.exp(-0.3 * layer_idx)


def setup_inputs(seed: int = 0) -> dict:
    key = jax.random.key(seed)
    ks = jax.random.split(key, 24)
    f32 = jnp.float32
    D = D_MODEL

    def nrm(k, shape, scale):
        return jax.random.normal(k, shape, f32) * scale

    in_a = 4 * DN_WIDTH + 2 * DN_HEADS
    x = nrm(ks[0], (BATCH, SEQ, D), 1.0)
    a_w_in = nrm(ks[1], (N_A_LAYERS, D, in_a), D ** -0.5)
    a_conv_w = nrm(ks[2], (N_A_LAYERS, CONV_K, 3 * DN_WIDTH), CONV_K ** -0.5)
    a_a_log = jnp.log(jax.random.uniform(ks[3], (N_A_LAYERS, DN_HEADS), f32, 1.0, 16.0))
    dt = jnp.exp(jax.random.uniform(ks[4], (N_A_LAYERS, DN_HEADS), f32, math.log(1e-3), math.log(1e-1)))
    a_dt_bias = dt + jnp.log(-jnp.expm1(-dt))
    a_norm_w = 1.0 + nrm(ks[5], (N_A_LAYERS, DN_HEAD_DIM), 0.02)
    a_w_out = nrm(ks[6], (N_A_LAYERS, DN_WIDTH, D), DN_WIDTH ** -0.5 * BETA_INIT)
    kv_w = nrm(ks[7], (D, DA_QK_WIDTH + DA_WIDTH), D ** -0.5)
    b_w_q = nrm(ks[8], (N_B_LAYERS, D, DA_QK_WIDTH), D ** -0.5)
    b_lambda = nrm(ks[9], (N_B_LAYERS, 4, DA_HEAD_DIM), 0.1)
    b_subln_w = 1.0 + nrm(ks[10], (N_B_LAYERS, DA_V_DIM), 0.02)
    b_w_out = nrm(ks[11], (N_B_LAYERS, DA_WIDTH, D), DA_WIDTH ** -0.5 * BETA_INIT)
    ln_mix_g = 1.0 + nrm(ks[12], (DEPTH, D), 0.02)
    ln_mix_b = nrm(ks[13], (DEPTH, D), 0.02)
    ln_ffn_g = 1.0 + nrm(ks[14], (DEPTH, D), 0.02)
    ln_ffn_b = nrm(ks[15], (DEPTH, D), 0.02)
    moe_w_group = nrm(ks[16], (DEPTH, D, N_GROUPS), D ** -0.5)
    moe_w_expert = nrm(ks[17], (DEPTH, D, N_EXPERTS), D ** -0.5)
    moe_w13 = nrm(ks[18], (DEPTH, N_EXPERTS, D, 2 * EXPERT_FF), D ** -0.5)
    moe_w2 = nrm(ks[19], (DEPTH, N_EXPERTS, EXPERT_FF, D), EXPERT_FF ** -0.5 * BETA_INIT)
    return {'x': x, 'a_w_in': a_w_in, 'a_conv_w': a_conv_w, 'a_a_log': a_a_log, 'a_dt_bias': a_dt_bias,
            'a_norm_w': a_norm_w, 'a_w_out': a_w_out, 'kv_w': kv_w, 'b_w_q': b_w_q, 'b_lambda': b_lambda,
            'b_subln_w': b_subln_w, 'b_w_out': b_w_out, 'ln_mix_g': ln_mix_g, 'ln_mix_b': ln_mix_b,
            'ln_ffn_g': ln_ffn_g, 'ln_ffn_b': ln_ffn_b, 'moe_w_group': moe_w_group,
            'moe_w_expert': moe_w_expert, 'moe_w13': moe_w13, 'moe_w2': moe_w2}


def reference(x, a_w_in, a_conv_w, a_a_log, a_dt_bias, a_norm_w, a_w_out, kv_w, b_w_q, b_lambda,
              b_subln_w, b_w_out, ln_mix_g, ln_mix_b, ln_ffn_g, ln_ffn_b, moe_w_group, moe_w_expert,
              moe_w13, moe_w2):
    h = x
    keys, vals = None, None
    for layer in range(DEPTH):
        if layer < N_A_LAYERS:
            mix = gated_deltanet(h, a_w_in[layer], a_conv_w[layer], a_a_log[layer], a_dt_bias[layer],
                                 a_norm_w[layer], a_w_out[layer])
        else:
            j = layer - N_A_LAYERS
            if j == 0:
                keys, vals = shared_kv(h, kv_w)
            mix = differential_attention(h, keys, vals, b_w_q[j], b_lambda[j], b_subln_w[j], b_w_out[j],
                                         lambda_init(layer))
        h = layer_norm(ALPHA * h + mix, ln_mix_g[layer], ln_mix_b[layer])
        ffn = hierarchical_moe(h, moe_w_group[layer], moe_w_expert[layer], moe_w13[layer], moe_w2[layer])
        h = layer_norm(ALPHA * h + ffn, ln_ffn_g[layer], ln_ffn_b[layer])
    return h
```

```python
import math
from contextlib import ExitStack
import numpy as np
import concourse.bass as bass
import concourse.mybir as mybir
from concourse.bass_utils import run_bass_kernel_spmd

F32 = mybir.dt.float32
BF16 = mybir.dt.bfloat16
I32 = mybir.dt.int32
AF = mybir.ActivationFunctionType
ALU = mybir.AluOpType
AX = mybir.AxisListType

ENGS = ['tensor', 'vector', 'scalar', 'gpsimd', 'sync']
SEM_EPOCH = 30000
N_DMA_SEMS = 16


class Buf:
    __slots__ = ('name', 'w', 'r', 'excl')

    def __init__(self, name='', excl=False):
        self.name = name
        self.excl = excl
        self.w = None
        self.r = {}


class _Op:
    __slots__ = ('fn', 'waits', 'inc', 'incval', 'dma')

    def __init__(self, fn, waits, dma):
        self.fn = fn
        self.waits = waits
        self.inc = False
        self.incval = 0
        self.dma = dma


class K:
    def __init__(self, nc):
        self.nc = nc
        self.ops = {e: [] for e in ENGS}
        self.waited = {e: {} for e in ENGS}
        self.dma_rr = {e: 0 for e in ENGS}
        self.dma_cnt = {}

    def _need_wait(self, eng, t):
        key = (t[0], t[1])
        if self.waited[eng].get(key, -1) >= t[2]:
            return False
        self.waited[eng][key] = t[2]
        if t[0] == 'e':
            self.ops[t[1]][t[2]].inc = True
        return True

    def op(self, eng, fn, reads=(), writes=(), dma=False):
        idx = len(self.ops[eng])
        writes = list(writes) + [b for b in reads if b.excl]
        reads = [b for b in reads if not b.excl]
        deps = []
        for b in reads:
            if b.w is not None:
                deps.append(b.w)
        for b in writes:
            if b.w is not None:
                deps.append(b.w)
            deps.extend(b.r.values())
        waits = []
        for t in deps:
            if t[0] == 'e' and t[1] == eng and eng == 'tensor':
                continue
            if self._need_wait(eng, t):
                waits.append(t)
        dm = None
        if dma:
            slot = self.dma_rr[eng]
            self.dma_rr[eng] = (slot + 1) % N_DMA_SEMS
            cnt = self.dma_cnt.get((eng, slot), 0) + 1
            self.dma_cnt[(eng, slot)] = cnt
            dm = ((eng, slot), cnt * 16)
            if cnt > 1:
                t = ('d', (eng, slot), (cnt - 1) * 16)
                if self._need_wait(eng, t):
                    waits.append(t)
            tok = ('d', (eng, slot), cnt * 16)
        else:
            tok = ('e', eng, idx)
        self.ops[eng].append(_Op(fn, waits, dm))
        kk = (tok[0], tok[1])
        for b in reads:
            b.r[kk] = tok
        for b in writes:
            b.w = tok
            b.r = {}
        return tok

    def dma(self, eng, out, in_, reads=(), writes=(), **kw):
        return self.op(eng, lambda e: e.dma_start(out=out, in_=in_, **kw), reads=reads, writes=writes, dma=True)

    def wait_all(self, eng, tokens):
        waits = [t for t in tokens if self._need_wait(eng, t)]
        if waits:
            self.ops[eng].append(_Op(None, waits, None))

    def barrier(self):
        toks = []
        for e in ENGS:
            for i in range(len(self.ops[e]) - 1, -1, -1):
                o = self.ops[e][i]
                if o.fn is not None and o.dma is None:
                    toks.append(('e', e, i))
                    break
        for key, cnt in self.dma_cnt.items():
            toks.append(('d', key, cnt * 16))
        for e in ENGS:
            self.wait_all(e, toks)
        self.flush()

    def flush(self):
        nc = self.nc
        if not hasattr(self, 'flushed'):
            self.flushed = {e: 0 for e in ENGS}
            self.inccnt = {e: 0 for e in ENGS}
            self.esems = {e: [] for e in ENGS}
            self.dsems = {}
        start = dict(self.flushed)
        for e in ENGS:
            for o in self.ops[e][start[e]:]:
                if o.inc:
                    self.inccnt[e] += 1
                    o.incval = self.inccnt[e]
            need = (self.inccnt[e] + SEM_EPOCH - 1) // SEM_EPOCH
            while len(self.esems[e]) < max(need, 1):
                self.esems[e].append(nc.alloc_semaphore(f"es_{e}_{len(self.esems[e])}"))
        for key in self.dma_cnt:
            if key not in self.dsems:
                self.dsems[key] = nc.alloc_semaphore(f"ds_{key[0]}_{key[1]}")
        esems, dsems, ops = self.esems, self.dsems, self.ops

        def semval(e2, incval):
            return esems[e2][(incval - 1) // SEM_EPOCH], (incval - 1) % SEM_EPOCH + 1

        with nc.Block() as block:
            for eng in ENGS:
                todo = ops[eng][start[eng]:]

                def body(e, eng=eng, todo=todo):
                    for o in todo:
                        for t in o.waits:
                            if t[0] == 'e':
                                p = ops[t[1]][t[2]]
                                assert p.incval > 0, (eng, t)
                                s, v = semval(t[1], p.incval)
                                e.wait_ge(s, v)
                            else:
                                e.wait_ge(dsems[t[1]], t[2])
                        if o.fn is None:
                            continue
                        ins = o.fn(e)
                        if o.dma is not None:
                            ins.then_inc(dsems[o.dma[0]], 16)
                        elif o.inc:
                            s, v = semval(eng, o.incval)
                            ins.then_inc(s, 1)
                if todo:
                    getattr(block, eng)(body)
                self.flushed[eng] = len(ops[eng])

    def finish(self):
        self.flush()


_uid = [0]


def _un(name):
    _uid[0] += 1
    return f"{name}_u{_uid[0]}"


class Ring:
    def __init__(self, es, nc, name, shape, dt, n):
        self.t = [es.enter_context(nc.sbuf_tensor(_un(f"{name}_{i}"), shape, dt)) for i in range(n)]
        self.b = [Buf(f"{name}_{i}") for i in range(n)]
        self.i = 0

    def next(self):
        i = self.i
        self.i = (i + 1) % len(self.t)
        return self.t[i], self.b[i]


D = 1024
NH = 8
ALPHA = 8 ** 0.25
LN_EPS = 1e-5
RMS_EPS = 1e-6
NEG = -1.0e30


def lambda_init(layer_idx):
    return 0.8 - 0.6 * math.exp(-0.3 * layer_idx)


class _Stop(Exception):
    pass


def build(T, CAP, stop=0, ksub=0, dumpflag=False):
    NT = T // 128
    NCH = T // 64
    NB = T // 512
    nc = bass.Bass("TRN2", target_bir_lowering=False)

    def din(name, shape, dt=F32):
        return nc.dram_tensor(name, shape, dt, kind="ExternalInput").ap()

    x = din("x", [T, D])
    a_w_in = din("a_w_in", [2, D, 4112])
    a_conv_w = din("a_conv_w", [2, 4, 3072])
    a_a_log = din("a_a_log", [2, 8])
    a_dt_bias = din("a_dt_bias", [2, 8])
    a_norm_w = din("a_norm_w", [2, 128])
    a_w_out = din("a_w_out", [2, D, D])
    kv_w = din("kv_w", [D, 2048])
    b_w_q = din("b_w_q", [2, D, D])
    b_lambda = din("b_lambda", [2, 4, 64])
    b_subln_w = din("b_subln_w", [2, 128])
    b_w_out = din("b_w_out", [2, D, D])
    ln_mix_g = din("ln_mix_g", [4, D])
    ln_mix_b = din("ln_mix_b", [4, D])
    ln_ffn_g = din("ln_ffn_g", [4, D])
    ln_ffn_b = din("ln_ffn_b", [4, D])
    moe_w_group = din("moe_w_group", [4, D, 4])
    moe_w_expert = din("moe_w_expert", [4, D, 32])
    moe_w13 = din("moe_w13", [4, 32, D, 1024])
    moe_w2 = din("moe_w2", [4, 32, 512, D])
    out = nc.dram_tensor("out", [T, D], F32, kind="ExternalOutput").ap()
    dbg = nc.dram_tensor("dbg", [T, D], BF16, kind="ExternalOutput").ap() if stop else None
    stage = [0]

    def checkpoint(noraise=False):
        stage[0] += 1
        if stop and stage[0] >= stop:
            if noraise:
                return True
            raise _Stop()
        return False

    h_d = nc.dram_tensor("h_d", [T, D], F32).ap()
    hT_d = nc.dram_tensor("hT_d", [D, T], BF16).ap()
    om_d = nc.dram_tensor("om_d", [T, D], BF16).ap()
    xb_d = nc.dram_tensor("xb_d", [T, D], BF16).ap()
    kT_d = nc.dram_tensor("kT_d", [NH, 2, 64, T], BF16).ap()
    va_d = nc.dram_tensor("va_d", [NH, T, 129], BF16).ap()
    NS = 32 * CAP
    xs_d = nc.dram_tensor("xs_d", [NS + 128, D], BF16).ap()
    ys_h = [nc.dram_tensor(f"ys_d{i}", [NS + 128, 512], F32).ap() for i in range(2)]
    hT_v = hT_d.rearrange("(k p) t -> p k t", p=128)

    k = K(nc)
    out_toks = []
    dumped = set()

    def dump(name, ap, B):
        if not dumpflag or name in dumped:
            return
        dumped.add(name)
        t = nc.dram_tensor("dump_" + name, list(ap.shape), F32, kind="ExternalOutput").ap()
        out_toks.append(k.dma('gpsimd', t, ap, reads=[B]))
    with ExitStack() as top:
        def sbt(es, name, shape, dt):
            return es.enter_context(nc.sbuf_tensor(_un(name), shape, dt))

        ps = [top.enter_context(nc.psum_tensor(f"ps{i}", [128, 512], F32)) for i in range(7)]
        psB = [Buf(f"ps{i}", excl=True) for i in range(7)]
        pb = top.enter_context(nc.psum_tensor("pb", [128, 1024], BF16))
        pbB = Buf("pb", excl=True)

        ident = sbt(top, "ident", [128, 128], F32)
        identb = sbt(top, "identb", [128, 128], BF16)
        ones_f = sbt(top, "ones_f", [128, 128], F32)
        ones_b = sbt(top, "ones_b", [128, 128], BF16)
        triu = sbt(top, "triu", [128, 128], F32)
        triub = sbt(top, "triub", [128, 128], BF16)
        sutri = sbt(top, "sutri", [128, 128], BF16)
        zero_b = sbt(top, "zero_b", [128, 1024], BF16)
        cB = Buf("consts")
        k.op('gpsimd', lambda e: e.iota(ones_f[:], [[1, 128]], base=0, channel_multiplier=-1,
                                        allow_small_or_imprecise_dtypes=True), writes=[cB])
        k.op('vector', lambda e: e.tensor_single_scalar(ident[:], ones_f[:], 0.0, ALU.is_equal), reads=[cB], writes=[cB])
        k.op('vector', lambda e: e.tensor_single_scalar(identb[:], ones_f[:], 0.0, ALU.is_equal), reads=[cB], writes=[cB])
        k.op('vector', lambda e: e.tensor_single_scalar(triu[:], ones_f[:], 0.0, ALU.is_ge), reads=[cB], writes=[cB])
        k.op('vector', lambda e: e.tensor_single_scalar(triub[:], ones_f[:], 0.0, ALU.is_ge), reads=[cB], writes=[cB])
        k.op('vector', lambda e: e.tensor_single_scalar(sutri[:], ones_f[:], 0.0, ALU.is_gt), reads=[cB], writes=[cB])
        k.op('vector', lambda e: e.memset(ones_f[:], 1.0), reads=[cB], writes=[cB])
        k.op('vector', lambda e: e.memset(ones_b[:], 1.0), writes=[cB])
        k.op('vector', lambda e: e.memset(zero_b[:], 0.0), writes=[cB])
        zero_f = sbt(top, "zero_f", [128, 512], F32)
        k.op('vector', lambda e: e.memset(zero_f[:], 0.0), writes=[cB])
        for r0 in range(0, NS + 128, 128):
            k.dma('sync', xs_d[r0:r0 + 128, :], zero_b[:], reads=[cB])
        for hf in range(2):
            k.dma('sync', ys_h[hf][NS:NS + 128, :], zero_f[:], reads=[cB])
        k.barrier()

        def layer_norm(es_ring, t, tB, gbc, bbc, wBs, y, yB):
            st, stB = es_ring['st'].next()
            jk, jkB = es_ring['junk'].next()
            k.op('scalar', lambda e: e.activation(jk[:], t[:], AF.Copy, accum_out=st[:, 0:1]), reads=[tB], writes=[jkB, stB])
            k.op('scalar', lambda e: e.activation(jk[:], t[:], AF.Square, accum_out=st[:, 1:2]), reads=[tB], writes=[jkB, stB])
            k.op('vector', lambda e: e.tensor_scalar_mul(st[:, 2:3], st[:, 0:1], 1.0 / D), reads=[stB], writes=[stB])
            k.op('vector', lambda e: e.tensor_tensor(st[:, 3:4], st[:, 2:3], st[:, 2:3], ALU.mult), reads=[stB], writes=[stB])
            k.op('vector', lambda e: e.scalar_tensor_tensor(st[:, 4:5], st[:, 1:2], 1.0 / D, st[:, 3:4], ALU.mult, ALU.subtract),
                 reads=[stB], writes=[stB])
            k.op('scalar', lambda e: e.activation(st[:, 5:6], st[:, 4:5], AF.Sqrt, bias=LN_EPS), reads=[stB], writes=[stB])
            k.op('vector', lambda e: e.reciprocal(st[:, 6:7], st[:, 5:6]), reads=[stB], writes=[stB])
            k.op('vector', lambda e: e.tensor_scalar(t[:], t[:], st[:, 2:3], st[:, 6:7], ALU.subtract, ALU.mult), reads=[tB, stB], writes=[tB])
            k.op('gpsimd', lambda e: e.tensor_tensor(t[:], t[:], gbc[:], ALU.mult), reads=[tB] + wBs, writes=[tB])
            k.op('vector', lambda e: e.tensor_tensor(y[:], t[:], bbc[:], ALU.add), reads=[tB] + wBs, writes=[yB])

        def make_hT(rings, y, yB, tile):
            hb, hbB = rings['hTs'].next()
            for half in range(2):
                pp, ppB = ps[5 + half], psB[5 + half]
                for j in range(4):
                    kk = half * 4 + j
                    k.op('tensor', lambda e, pp=pp, j=j, kk=kk: e.transpose(pp[:, j * 128:(j + 1) * 128], y[:, kk * 128:(kk + 1) * 128], ident[:]),
                         reads=[yB, cB], writes=[ppB])
                eng = 'vector' if half == 0 else 'scalar'
                if eng == 'vector':
                    k.op('vector', lambda e, pp=pp, half=half: e.tensor_copy(hb[:, half * 4:(half + 1) * 4, :], pp[:].rearrange("p (a b) -> p a b", a=4)),
                         reads=[ppB], writes=[hbB])
                else:
                    k.op('scalar', lambda e, pp=pp, half=half: e.copy(hb[:, half * 4:(half + 1) * 4, :], pp[:].rearrange("p (a b) -> p a b", a=4)),
                         reads=[ppB], writes=[hbB])
            k.dma('sync', hT_v[:, :, tile * 128:(tile + 1) * 128], hb[:], reads=[hbB])

        with ExitStack() as es:
            rings = {'hTs': Ring(es, nc, "hTs", [128, 8, 128], BF16, 2)}
            xr = Ring(es, nc, "xin", [128, D], F32, 2)
            for t in range(NT):
                xt, xB = xr.next()
                k.dma('sync', xt[:], x[t * 128:(t + 1) * 128, :], writes=[xB])
                k.dma('gpsimd', h_d[t * 128:(t + 1) * 128, :], xt[:], reads=[xB])
                make_hT(rings, xt, xB, t)
            k.barrier()

        def load_w_bf16(es, name, src, cols):
            w = sbt(es, name, [128, 8, cols], BF16)
            wB = Buf(name)
            k.dma('gpsimd', w[:], src.rearrange("(k p) c -> p k c", p=128), writes=[wB])
            return w, wB

        def bcast_row(es, name, src_row, n, dt=F32):
            w = sbt(es, name, [128, n], dt)
            wB = Buf(name)
            k.dma('sync', w[:], src_row.partition_broadcast(128), writes=[wB])
            return w, wB

        def deltanet(l):
            with ExitStack() as es:
                cw = sbt(es, "cw", [128, 24, 4], F32)
                cwB = Buf("cw")
                cwn = sbt(es, "cwn", [4, 3072], F32)
                k.dma('sync', cwn[:], a_conv_w[l], writes=[cwB])
                for part in range(24):
                    k.op('tensor', lambda e, part=part: e.transpose(ps[0][:, part * 4:(part + 1) * 4], cwn[:, part * 128:(part + 1) * 128], ident[0:4, 0:4]),
                         reads=[cwB, cB], writes=[psB[0]])
                k.op('vector', lambda e: e.tensor_copy(cw[:].rearrange("p a b -> p (a b)"), ps[0][:, 0:96]), reads=[psB[0]], writes=[cwB])
                nw, nwB = bcast_row(es, "nw", a_norm_w[l:l + 1, :], 128)
                alog, alB = bcast_row(es, "alog", a_a_log[l:l + 1, :], 8)
                dtb, dtB = bcast_row(es, "dtb", a_dt_bias[l:l + 1, :], 8)
                nea = sbt(es, "nea", [128, 8], F32)
                k.op('scalar', lambda e: e.activation(nea[:], alog[:], AF.Exp), reads=[alB], writes=[alB])
                k.op('vector', lambda e: e.tensor_scalar_mul(nea[:], nea[:], -1.0), reads=[alB], writes=[alB])
                qT = sbt(es, "qT", [128, T], BF16)
                kT = sbt(es, "kT", [128, T], BF16)
                vT = sbt(es, "vT", [128, T], BF16)
                qkvB = [Buf("qT"), Buf("kT"), Buf("vT")]
                qkv = [qT, kT, vT]
                zs = sbt(es, "zs", [64, NCH, 128], BF16)
                zsB = Buf("zs")
                gl = sbt(es, "gl", [64, 8, NCH], F32)
                glB = Buf("gl")
                egl = sbt(es, "egl", [128, NCH], F32)
                eglB = Buf("egl")
                S = sbt(es, "S", [128, 128], F32)
                SB = Buf("S")
                hbr = Ring(es, nc, "hblk", [128, 8, 512], BF16, 2)
                raw = sbt(es, "raw", [128, 3, 515], F32)
                rawB = [Buf("raw0"), Buf("raw1"), Buf("raw2")]
                cvr = Ring(es, nc, "cv", [128, 512], F32, 2)
                sqr = Ring(es, nc, "sq", [128, 512], F32, 2)
                R = 3
                r_kg = Ring(es, nc, "kg", [64, 256], F32, R)
                r_kdec = Ring(es, nc, "kdec", [64, 128], F32, R)
                r_dm = Ring(es, nc, "dm", [64, 64], F32, R)
                r_dg = Ring(es, nc, "dg", [64, 64], F32, R)
                r_egb = Ring(es, nc, "egb", [128, 64], F32, R)
                r_qg = Ring(es, nc, "qg", [128, 64], F32, R)
                r_at = Ring(es, nc, "at", [64, 64], F32, R)
                r_X = Ring(es, nc, "X", [64, 64], F32, 4)
                r_Y = Ring(es, nc, "Y", [64, 64], F32, 4)
                r_P = Ring(es, nc, "P", [64, 64], F32, R)
                r_uw = Ring(es, nc, "uw", [64, 256], F32, R)
                r_wT = Ring(es, nc, "wT", [128, 64], F32, R)
                r_vn = Ring(es, nc, "vn", [64, 128], F32, 2)
                r_o = Ring(es, nc, "o", [64, 128], F32, 2)
                r_ob = Ring(es, nc, "ob", [64, 128], BF16, 2)
                r_st = Ring(es, nc, "dst", [64, 4], F32, 2)
                r_jk = Ring(es, nc, "djk", [64, 128], F32, 2)
                for h in range(NH):
                    wq3, wq3B = [], []
                    wcat = sbt(es, f"wcat{h}", [128, 8, 384], BF16) if h == 0 else wcat_keep[0]
                    wzg = sbt(es, f"wzg{h}", [128, 8, 128], BF16) if h == 0 else wcat_keep[1]
                    if h == 0:
                        wcat_keep = [wcat, wzg]
                        wcB = Buf("wcat")
                        wzB = Buf("wzg")
                    for part in range(3):
                        k.dma('gpsimd', wcat[:, :, part * 128:(part + 1) * 128],
                              a_w_in[l, :, part * 1024 + h * 128: part * 1024 + (h + 1) * 128].rearrange("(k p) c -> p k c", p=128), writes=[wcB])
                    k.dma('gpsimd', wzg[:], a_w_in[l, :, 3072 + h * 128:3072 + (h + 1) * 128].rearrange("(k p) c -> p k c", p=128), writes=[wzB])
                    if h == 0:
                        wg16 = sbt(es, "wg16", [128, 8, 16], BF16)
                        k.dma('gpsimd', wg16[:], a_w_in[l, :, 4096:4112].rearrange("(k p) c -> p k c", p=128), writes=[wzB])
                    for part in range(3):
                        k.op('vector', lambda e, part=part: e.memset(raw[:, part, 0:3], 0.0), writes=[rawB[part]])
                    if ksub == 5:
                        k.barrier()
                        return
                    for blk in range(NB):
                        hb, hbB = hbr.next()
                        k.dma('sync', hb[:], hT_v[:, :, blk * 512:(blk + 1) * 512], writes=[hbB])
                        for part in range(3):
                            pp, ppB = ps[part % 2], psB[part % 2]
                            for kc in range(8):
                                k.op('tensor', lambda e, pp=pp, kc=kc, part=part, hb=hb: e.matmul(pp[:, :], wcat[:, kc, part * 128:(part + 1) * 128], hb[:, kc, :],
                                                                                            start=(kc == 0), stop=(kc == 7)),
                                     reads=[wcB, hbB], writes=[ppB])
                            k.op('scalar', lambda e, pp=pp, part=part: e.copy(raw[:, part, 3:515], pp[:, :]), reads=[ppB], writes=[rawB[part]])
                            if ksub == 11:
                                k.barrier()
                                return
                            cv, cvB = cvr.next()
                            ci = part * 8 + h
                            k.op('vector', lambda e, cv=cv, part=part, ci=ci: e.tensor_scalar_mul(cv[:], raw[:, part, 0:512], cw[:, ci, 0:1]),
                                 reads=[rawB[part], cwB], writes=[cvB])
                            for j in range(1, 4):
                                k.op('vector', lambda e, cv=cv, part=part, ci=ci, j=j: e.scalar_tensor_tensor(cv[:], raw[:, part, j:j + 512], cw[:, ci, j:j + 1], cv[:],
                                                                                                        ALU.mult, ALU.add),
                                     reads=[rawB[part], cwB, cvB], writes=[cvB])
                            k.op('vector', lambda e, part=part: e.tensor_copy(raw[:, part, 0:3], raw[:, part, 512:515]), reads=[rawB[part]], writes=[rawB[part]])
                            if ksub == 12:
                                k.barrier()
                                return
                            dst = qkv[part][:, blk * 512:(blk + 1) * 512]
                            if part == 2:
                                k.op('scalar', lambda e, cv=cv, dst=dst: e.activation(dst, cv[:], AF.Silu), reads=[cvB], writes=[qkvB[part]])
                            else:
                                k.op('scalar', lambda e, cv=cv: e.activation(cv[:], cv[:], AF.Silu), reads=[cvB], writes=[cvB])
                                sq, sqB = sqr.next()
                                k.op('gpsimd', lambda e, cv=cv, sq=sq: e.tensor_tensor(sq[:], cv[:], cv[:], ALU.mult), reads=[cvB], writes=[sqB])
                                p2, p2B = ps[2], psB[2]
                                for hf in range(2):
                                    k.op('tensor', lambda e, sq=sq, p2=p2, hf=hf: e.matmul(p2[:, hf * 256:(hf + 1) * 256], ones_f[:], sq[:, hf * 256:(hf + 1) * 256], start=True, stop=True), reads=[sqB, cB], writes=[p2B])
                                k.op('scalar', lambda e, sq=sq, p2=p2: e.activation(sq[:], p2[:, :], AF.Sqrt, bias=RMS_EPS), reads=[p2B], writes=[sqB])
                                k.op('vector', lambda e, sq=sq: e.reciprocal(sq[:], sq[:]), reads=[sqB], writes=[sqB])
                                sc = (128 ** -0.5) if part == 0 else 1.0
                                k.op('vector', lambda e, cv=cv, sq=sq, dst=dst, sc=sc: e.scalar_tensor_tensor(dst, cv[:], sc, sq[:], ALU.mult, ALU.mult),
                                     reads=[cvB, sqB], writes=[qkvB[part]])
                            if ksub == 13:
                                k.barrier()
                                return
                        if ksub == 14:
                            k.barrier()
                            return
                        for cc in range(8):
                            c = blk * 8 + cc
                            pp, ppB = ps[3 + cc % 2], psB[3 + cc % 2]
                            for kc in range(8):
                                k.op('tensor', lambda e, pp=pp, kc=kc, cc=cc, hb=hb: e.matmul(pp[0:64, 0:128], hb[:, kc, cc * 64:(cc + 1) * 64], wzg[:, kc, :],
                                                                                       start=(kc == 0), stop=(kc == 7)),
                                     reads=[wzB, hbB], writes=[ppB])
                            for kc in range(8):
                                k.op('tensor', lambda e, pp=pp, kc=kc, cc=cc, hb=hb: e.matmul(pp[0:64, 128:144], hb[:, kc, cc * 64:(cc + 1) * 64], wg16[:, kc, :],
                                                                                       start=(kc == 0), stop=(kc == 7)),
                                     reads=[wzB, hbB], writes=[ppB])
                            k.op('scalar', lambda e, pp=pp, c=c: e.activation(zs[:, c, :], pp[0:64, 0:128], AF.Silu), reads=[ppB], writes=[zsB])
                            k.op('vector', lambda e, pp=pp, c=c, h=h: e.tensor_copy(gl[:, 0, c:c + 1], pp[0:64, 128 + h:129 + h]), reads=[ppB], writes=[glB])
                            k.op('vector', lambda e, pp=pp, c=c, h=h: e.tensor_copy(gl[:, 1, c:c + 1], pp[0:64, 136 + h:137 + h]), reads=[ppB], writes=[glB])
                    if ksub == 1:
                        k.barrier()
                        return
                    k.op('scalar', lambda e: e.activation(gl[:, 0, :], gl[:, 0, :], AF.Sigmoid), reads=[glB], writes=[glB])
                    k.op('vector', lambda e: e.tensor_scalar_mul(gl[:, 5, :], gl[:, 0, :], -1.0), reads=[glB], writes=[glB])
                    k.op('scalar', lambda e, h=h: e.activation(gl[:, 1, :], gl[:, 1, :], AF.Exp, bias=dtb[0:64, h:h + 1]), reads=[glB, dtB], writes=[glB])
                    k.op('scalar', lambda e: e.activation(gl[:, 1, :], gl[:, 1, :], AF.Ln, bias=1.0), reads=[glB], writes=[glB])
                    k.op('vector', lambda e, h=h: e.tensor_scalar_mul(gl[:, 1, :], gl[:, 1, :], nea[0:64, h:h + 1]), reads=[glB, alB], writes=[glB])
                    for c0 in range(0, NCH, 512):
                        n = min(512, NCH - c0)
                        k.op('tensor', lambda e, c0=c0, n=n: e.matmul(ps[0][0:64, 0:n], triu[0:64, 0:64], gl[:, 1, c0:c0 + n], start=True, stop=True),
                             reads=[glB, cB], writes=[psB[0]])
                        k.op('vector', lambda e, c0=c0, n=n: e.tensor_copy(gl[:, 2, c0:c0 + n], ps[0][0:64, 0:n]), reads=[psB[0]], writes=[glB])
                        k.op('tensor', lambda e, c0=c0, n=n: e.matmul(ps[1][:, 0:n], ones_f[0:64, :], gl[:, 1, c0:c0 + n], start=True, stop=True),
                             reads=[glB, cB], writes=[psB[1]])
                        k.op('scalar', lambda e, c0=c0, n=n: e.activation(egl[:, c0:c0 + n], ps[1][:, 0:n], AF.Exp), reads=[psB[1]], writes=[eglB])
                        k.op('vector', lambda e, c0=c0, n=n: e.tensor_copy(gl[:, 6, c0:c0 + n], ps[1][0:64, 0:n]), reads=[psB[1]], writes=[glB])
                    k.op('scalar', lambda e: e.activation(gl[:, 3, :], gl[:, 2, :], AF.Exp), reads=[glB], writes=[glB])
                    k.op('vector', lambda e: e.tensor_tensor(gl[:, 7, :], gl[:, 6, :], gl[:, 2, :], ALU.subtract), reads=[glB], writes=[glB])
                    k.op('scalar', lambda e: e.activation(gl[:, 4, :], gl[:, 7, :], AF.Exp), reads=[glB], writes=[glB])
                    k.op('vector', lambda e: e.memset(S[:], 0.0), writes=[SB])
                    if h == 0 and l == 0:
                        dump("gl", gl[:], glB)
                        dump("egl", egl[:], eglB)
                        dump("qT", qT[:, 0:128], qkvB[0])
                        dump("kT", kT[:, 0:128], qkvB[1])
                        dump("vT", vT[:, 0:128], qkvB[2])
                        dump("zs", zs[:, 0:2, :], zsB)
                    if ksub == 2:
                        k.barrier()
                        return

                    def bulk(c):
                        cs = slice(c * 64, (c + 1) * 64)
                        res = {}
                        k.op('tensor', lambda e: e.transpose(pb[0:64, 0:128], kT[:, cs], identb[:]), reads=[qkvB[1], cB], writes=[pbB])
                        k.op('tensor', lambda e: e.transpose(pb[0:64, 128:256], vT[:, cs], identb[:]), reads=[qkvB[2], cB], writes=[pbB])
                        kg, kgB = r_kg.next()
                        kd, kdB = r_kdec.next()
                        k.op('vector', lambda e: e.tensor_scalar_mul(kg[:, 128:256], pb[0:64, 0:128], gl[:, 3, c:c + 1]), reads=[pbB, glB], writes=[kgB])
                        k.op('vector', lambda e: e.tensor_scalar_mul(kd[:], pb[0:64, 0:128], gl[:, 4, c:c + 1]), reads=[pbB, glB], writes=[kdB])
                        k.op('vector', lambda e: e.tensor_copy(kg[:, 0:128], pb[0:64, 128:256]), reads=[pbB], writes=[kgB])
                        p0, p0B = ps[0], psB[0]
                        k.op('tensor', lambda e: e.matmul(p0[0:64, 0:64], kT[:, cs], kT[:, cs], start=True, stop=True), reads=[qkvB[1]], writes=[p0B])
                        k.op('tensor', lambda e: e.matmul(p0[0:64, 64:128], kT[:, cs], qT[:, cs], start=True, stop=True), reads=[qkvB[1], qkvB[0]], writes=[p0B])
                        dg, dgB = r_dg.next()
                        k.op('vector', lambda e: e.tensor_scalar_mul(dg[:], ident[0:64, 0:64], gl[:, 2, c:c + 1]), reads=[glB, cB], writes=[dgB])
                        p1, p1B = ps[1], psB[1]
                        k.op('tensor', lambda e: e.matmul(p1[:, 0:64], ones_f[0:64, :], dg[:], start=True, stop=True), reads=[dgB, cB], writes=[p1B])
                        dm, dmB = r_dm.next()
                        k.op('vector', lambda e: e.tensor_scalar(dm[:], p1[0:64, 0:64], gl[:, 2, c:c + 1], 0.0, ALU.subtract, ALU.min), reads=[p1B, glB], writes=[dmB])
                        k.op('scalar', lambda e: e.activation(dm[:], dm[:], AF.Exp), reads=[dmB], writes=[dmB])
                        k.op('vector', lambda e: e.tensor_tensor(dm[:], dm[:], triu[0:64, 0:64], ALU.mult), reads=[dmB, cB], writes=[dmB])
                        egb, egbB = r_egb.next()
                        k.op('scalar', lambda e: e.activation(egb[:], p1[:, 0:64], AF.Exp), reads=[p1B], writes=[egbB])
                        qg, qgB = r_qg.next()
                        k.op('gpsimd', lambda e: e.tensor_tensor(qg[:], qT[:, cs], egb[:], ALU.mult), reads=[qkvB[0], egbB], writes=[qgB])
                        at, atB = r_at.next()
                        k.op('vector', lambda e: e.tensor_tensor(at[:], p0[0:64, 64:128], dm[:], ALU.mult), reads=[p0B, dmB], writes=[atB])
                        k.op('vector', lambda e: e.tensor_tensor(dm[:], dm[:], ident[0:64, 0:64], ALU.subtract), reads=[dmB, cB], writes=[dmB])
                        X, XB = r_X.next()
                        k.op('vector', lambda e, X=X: e.scalar_tensor_tensor(X[:], p0[0:64, 0:64], gl[:, 5, c:c + 1], dm[:], ALU.mult, ALU.mult),
                             reads=[p0B, glB, dmB], writes=[XB])
                        p2, p2B = ps[2], psB[2]
                        k.op('tensor', lambda e, X=X: e.transpose(p2[0:64, 0:64], X[:], ident[0:64, 0:64]), reads=[XB, cB], writes=[p2B])
                        Y, YB = r_Y.next()
                        k.op('scalar', lambda e, Y=Y: e.copy(Y[:], p2[0:64, 0:64]), reads=[p2B], writes=[YB])
                        P, PB = r_P.next()
                        k.op('vector', lambda e, X=X: e.tensor_tensor(P[:], X[:], ident[0:64, 0:64], ALU.add), reads=[XB, cB], writes=[PB])
                        if h == 0 and l == 0 and c == 0:
                            dump("X0", X[:], XB)
                            dump("Y0", Y[:], YB)
                            dump("Pinit", P[:], PB)
                        for s in range(1, 6):
                            Xn, XnB = (None, None)
                            if s < 5:
                                k.op('tensor', lambda e, X=X, Y=Y: e.matmul(p2[0:64, 64:128], Y[:], X[:], start=True, stop=True), reads=[XB, YB], writes=[p2B])
                            k.op('tensor', lambda e, X=X, Y=Y: e.matmul(p2[0:64, 128:192], X[:], Y[:], start=True, stop=True), reads=[XB, YB], writes=[p2B])
                            Yn, YnB = r_Y.next()
                            k.op('scalar', lambda e, Yn=Yn: e.copy(Yn[:], p2[0:64, 128:192]), reads=[p2B], writes=[YnB])
                            if s < 5:
                                Xn, XnB = r_X.next()
                                k.op('vector', lambda e, Xn=Xn: e.tensor_copy(Xn[:], p2[0:64, 64:128]), reads=[p2B], writes=[XnB])
                            p3, p3B = ps[3], psB[3]
                            k.op('tensor', lambda e, Yn=Yn, P=P: e.matmul(p3[0:64, 0:64], Yn[:], P[:], start=True, stop=True), reads=[YnB, PB], writes=[p3B])
                            k.op('vector', lambda e, P=P: e.tensor_tensor(P[:], P[:], p3[0:64, 0:64], ALU.add), reads=[p3B, PB], writes=[PB])
                            if h == 0 and l == 0 and c == 0 and s == 1:
                                dump("Y1", Yn[:], YnB)
                                dump("X1", Xn[:], XnB)
                                dump("Pafter1", P[:], PB)
                            Y, YB = Yn, YnB
                            if s < 5:
                                X, XB = Xn, XnB
                        p4, p4B = ps[4], psB[4]
                        k.op('tensor', lambda e: e.matmul(p4[0:64, 0:256], P[:], kg[:], start=True, stop=True), reads=[PB, kgB], writes=[p4B])
                        uw, uwB = r_uw.next()
                        k.op('vector', lambda e: e.tensor_scalar_mul(uw[:], p4[0:64, 0:256], gl[:, 0, c:c + 1]), reads=[p4B, glB], writes=[uwB])
                        k.op('tensor', lambda e: e.transpose(p4[:, 256:320], uw[:, 128:256], ident[0:64, 0:64]), reads=[uwB, cB], writes=[p4B])
                        wT, wTB = r_wT.next()
                        k.op('vector', lambda e: e.tensor_copy(wT[:], p4[:, 256:320]), reads=[p4B], writes=[wTB])
                        if h == 0 and l == 0 and c <= 1:
                            dump(f"kg{c}", kg[:], kgB)
                            dump(f"kd{c}", kd[:], kdB)
                            dump(f"at{c}", at[:], atB)
                            dump(f"P{c}", P[:], PB)
                            dump(f"uw{c}", uw[:], uwB)
                            dump(f"wT{c}", wT[:], wTB)
                            dump(f"qg{c}", qg[:], qgB)
                        return dict(uw=(uw, uwB), wT=(wT, wTB), qg=(qg, qgB), at=(at, atB), kd=(kd, kdB))

                    def scan(c, r):
                        uw, uwB = r['uw']
                        wT, wTB = r['wT']
                        qg, qgB = r['qg']
                        at, atB = r['at']
                        kd, kdB = r['kd']
                        p5, p5B = ps[5], psB[5]
                        k.op('tensor', lambda e: e.matmul(p5[0:64, 0:128], wT[:], S[:], start=True, stop=True), reads=[wTB, SB], writes=[p5B])
                        vn, vnB = r_vn.next()
                        k.op('vector', lambda e: e.tensor_tensor(vn[:], uw[:, 0:128], p5[0:64, 0:128], ALU.subtract), reads=[uwB, p5B], writes=[vnB])
                        k.op('tensor', lambda e: e.matmul(p5[0:64, 128:256], qg[:], S[:], start=True, stop=False), reads=[qgB, SB], writes=[p5B])
                        k.op('tensor', lambda e: e.matmul(p5[0:64, 128:256], at[:], vn[:], start=False, stop=True), reads=[atB, vnB], writes=[p5B])
                        p6, p6B = ps[6], psB[6]
                        k.op('tensor', lambda e: e.matmul(p6[:, 0:128], kd[:], vn[:], start=True, stop=True), reads=[kdB, vnB], writes=[p6B])
                        k.op('vector', lambda e: e.scalar_tensor_tensor(S[:], S[:], egl[:, c:c + 1], p6[:, 0:128], ALU.mult, ALU.add),
                             reads=[SB, eglB, p6B], writes=[SB])
                        o, oB = r_o.next()
                        st, stB = r_st.next()
                        jk, jkB = r_jk.next()
                        k.op('scalar', lambda e: e.copy(o[:], p5[0:64, 128:256]), reads=[p5B], writes=[oB])
                        if h == 0 and l == 0 and c <= 1:
                            dump(f"vn{c}", vn[:], vnB)
                            dump(f"o{c}", o[:], oB)
                            dump(f"S{c}", S[:], SB)
                        k.op('scalar', lambda e: e.activation(jk[:], o[:], AF.Square, accum_out=st[:, 0:1]), reads=[oB], writes=[jkB, stB])
                        k.op('scalar', lambda e: e.activation(st[:, 1:2], st[:, 0:1], AF.Sqrt, bias=RMS_EPS, scale=1.0 / 128), reads=[stB], writes=[stB])
                        k.op('vector', lambda e: e.reciprocal(st[:, 2:3], st[:, 1:2]), reads=[stB], writes=[stB])
                        k.op('vector', lambda e: e.scalar_tensor_tensor(o[:], o[:], st[:, 2:3], nw[0:64, :], ALU.mult, ALU.mult), reads=[oB, stB, nwB], writes=[oB])
                        ob, obB = r_ob.next()
                        k.op('gpsimd', lambda e: e.tensor_tensor(ob[:], o[:], zs[:, c, :], ALU.mult), reads=[oB, zsB], writes=[obB])
                        k.dma('sync', om_d[c * 64:(c + 1) * 64, h * 128:(h + 1) * 128], ob[:], reads=[obB])

                    pend = bulk(0)
                    if ksub == 3:
                        k.barrier()
                        return
                    for c in range(NCH):
                        nxt = bulk(c + 1) if c + 1 < NCH else None
                        scan(c, pend)
                        if ksub == 4:
                            k.barrier()
                            return
                        pend = nxt
                k.barrier()

        def shared_kv():
            with ExitStack() as es:
                hbr = Ring(es, nc, "khb", [128, 8, 512], BF16, 2)
                kr = Ring(es, nc, "kst", [64, 512], BF16, 3)
                vr = Ring(es, nc, "vst", [128, 129], BF16, 3)
                for h in range(NH):
                    wk, wkB = load_w_bf16(es, f"wk{h}", kv_w[:, h * 128:(h + 1) * 128], 128) if h == 0 else (wk_keep, wkB_keep)
                    wv, wvB = load_w_bf16(es, f"wv{h}", kv_w[:, 1024 + h * 128:1024 + (h + 1) * 128], 128) if h == 0 else (wv_keep, wvB_keep)
                    if h == 0:
                        wk_keep, wkB_keep, wv_keep, wvB_keep = wk, wkB, wv, wvB
                    else:
                        k.dma('gpsimd', wk[:], kv_w[:, h * 128:(h + 1) * 128].rearrange("(k p) c -> p k c", p=128), writes=[wkB])
                        k.dma('gpsimd', wv[:], kv_w[:, 1024 + h * 128:1024 + (h + 1) * 128].rearrange("(k p) c -> p k c", p=128), writes=[wvB])
                    for blk in range(NB):
                        hb, hbB = hbr.next()
                        k.dma('sync', hb[:], hT_v[:, :, blk * 512:(blk + 1) * 512], writes=[hbB])
                        for s in range(2):
                            pp, ppB = ps[s], psB[s]
                            for kc in range(8):
                                k.op('tensor', lambda e, pp=pp, kc=kc, s=s, hb=hb: e.matmul(pp[0:64, :], wk[:, kc, s * 64:(s + 1) * 64], hb[:, kc, :],
                                                                                     start=(kc == 0), stop=(kc == 7)), reads=[wkB, hbB], writes=[ppB])
                            kt, ktB = kr.next()
                            k.op('scalar' if s else 'vector', (lambda e, kt=kt, pp=pp: e.copy(kt[:], pp[0:64, :])) if s else
                                 (lambda e, kt=kt, pp=pp: e.tensor_copy(kt[:], pp[0:64, :])), reads=[ppB], writes=[ktB])
                            k.dma('sync', kT_d[h, s, :, blk * 512:(blk + 1) * 512], kt[:], reads=[ktB])
                        for tt in range(4):
                            pp, ppB = ps[2 + tt % 2], psB[2 + tt % 2]
                            for kc in range(8):
                                k.op('tensor', lambda e, pp=pp, kc=kc, tt=tt, hb=hb: e.matmul(pp[:, 0:128], hb[:, kc, tt * 128:(tt + 1) * 128], wv[:, kc, :],
                                                                                       start=(kc == 0), stop=(kc == 7)), reads=[wvB, hbB], writes=[ppB])
                            vt, vtB = vr.next()
                            k.op('vector', lambda e, vt=vt, pp=pp: e.tensor_copy(vt[:, 0:128], pp[:, 0:128]), reads=[ppB], writes=[vtB])
                            k.op('gpsimd', lambda e, vt=vt: e.memset(vt[:, 128:129], 1.0), writes=[vtB])
                            tok0 = blk * 512 + tt * 128
                            k.dma('sync', va_d[h, tok0:tok0 + 128, :], vt[:], reads=[vtB])
                k.barrier()

        def diffattn(j, layer):
            lam_init = lambda_init(layer)
            with ExitStack() as es:
                lp, lpB = bcast_row(es, "lp", b_lambda[j:j + 1].rearrange("o a b -> o (a b)"), 256)
                sw, swB = bcast_row(es, "sw", b_subln_w[j:j + 1, :], 128)
                lam = sbt(es, "lam", [128, 8], F32)
                lamB = Buf("lam")
                pr = sbt(es, "lpr", [128, 128], F32)
                k.op('vector', lambda e: e.tensor_tensor(pr[:, 0:64], lp[:, 0:64], lp[:, 64:128], ALU.mult), reads=[lpB], writes=[lamB])
                k.op('vector', lambda e: e.tensor_tensor(pr[:, 64:128], lp[:, 128:192], lp[:, 192:256], ALU.mult), reads=[lpB], writes=[lamB])
                k.op('vector', lambda e: e.reduce_sum(lam[:, 0:1], pr[:, 0:64], AX.X), reads=[lamB], writes=[lamB])
                k.op('vector', lambda e: e.reduce_sum(lam[:, 1:2], pr[:, 64:128], AX.X), reads=[lamB], writes=[lamB])
                k.op('scalar', lambda e: e.activation(lam[:, 2:4], lam[:, 0:2], AF.Exp), reads=[lamB], writes=[lamB])
                k.op('vector', lambda e: e.tensor_tensor(lam[:, 4:5], lam[:, 2:3], lam[:, 3:4], ALU.subtract), reads=[lamB], writes=[lamB])
                k.op('vector', lambda e: e.tensor_scalar(lam[:, 5:6], lam[:, 4:5], lam_init, -1.0, ALU.add, ALU.mult), reads=[lamB], writes=[lamB])
                k.op('vector', lambda e: e.tensor_scalar_mul(sw[:], sw[:], 1.0 - lam_init), reads=[swB], writes=[swB])
                qs = [sbt(es, f"qs{s}", [64, T], BF16) for s in range(2)]
                qsB = [Buf("qs0"), Buf("qs1")]
                kTs = [sbt(es, f"kTs{s}", [64, T], BF16) for s in range(2)]
                kTB = [Buf("kT0"), Buf("kT1")]
                va = sbt(es, "va", [128, NT, 144], BF16)
                vaB = Buf("va")
                hbr = Ring(es, nc, "ahb", [128, 8, 512], BF16, 2)
                ptr = Ring(es, nc, "pt", [128, 512], BF16, 4)
                r_o1 = Ring(es, nc, "ao1", [128, 128], F32, 2)
                r_st = Ring(es, nc, "ast", [128, 8], F32, 2)
                r_jk = Ring(es, nc, "ajk", [128, 128], F32, 2)
                r_ob = Ring(es, nc, "aob", [128, 128], BF16, 2)
                wq = sbt(es, "wq", [128, 8, 128], BF16)
                wqB = Buf("wq")
                acc = {}
                slots = [(4, 0), (4, 144), (4, 288), (5, 0), (5, 144), (5, 288), (6, 0), (6, 144)]
                for s in range(2):
                    for i in range(4):
                        acc[(s, i)] = slots[s * 4 + i]
                for h in range(NH):
                    k.dma('gpsimd', wq[:], b_w_q[j, :, h * 128:(h + 1) * 128].rearrange("(k p) c -> p k c", p=128), writes=[wqB])
                    for s in range(2):
                        k.dma('sync', kTs[s][:], kT_d[h, s], writes=[kTB[s]])
                    k.dma('sync', va[:, :, 0:129], va_d[h].rearrange("(t p) c -> p t c", p=128), writes=[vaB])
                    for blk in range(NB):
                        hb, hbB = hbr.next()
                        k.dma('sync', hb[:], hT_v[:, :, blk * 512:(blk + 1) * 512], writes=[hbB])
                        for s in range(2):
                            pp, ppB = ps[s], psB[s]
                            for kc in range(8):
                                k.op('tensor', lambda e, pp=pp, kc=kc, s=s, hb=hb: e.matmul(pp[0:64, :], wq[:, kc, s * 64:(s + 1) * 64], hb[:, kc, :],
                                                                                     start=(kc == 0), stop=(kc == 7)), reads=[wqB, hbB], writes=[ppB])
                            k.op('scalar', lambda e, pp=pp, s=s, blk=blk: e.activation(qs[s][:, blk * 512:(blk + 1) * 512], pp[0:64, :], AF.Copy, scale=0.125),
                                 reads=[ppB], writes=[qsB[s]])
                    for qb in range(NB):
                        nkt = 4 * qb + 4
                        for kt in range(nkt):
                            jd = kt - 4 * qb
                            lo = 0 if jd < 0 else jd * 128
                            n = 512 - lo
                            for s in range(2):
                                pp, ppB = ps[(kt * 2 + s) % 4], psB[(kt * 2 + s) % 4]
                                k.op('tensor', lambda e, pp=pp, s=s, kt=kt, qb=qb, lo=lo, n=n: e.matmul(pp[:, 0:n], kTs[s][:, kt * 128:(kt + 1) * 128],
                                                                                                 qs[s][:, qb * 512 + lo:(qb + 1) * 512], start=True, stop=True),
                                     reads=[kTB[s], qsB[s]], writes=[ppB])
                                pt, ptB = ptr.next()
                                k.op('scalar', lambda e, pt=pt, pp=pp, n=n: e.activation(pt[:, 0:n], pp[:, 0:n], AF.Exp), reads=[ppB], writes=[ptB])
                                if jd >= 0:
                                    k.op('vector', lambda e, pt=pt: e.tensor_tensor(pt[:, 0:128], pt[:, 0:128], triub[:], ALU.mult), reads=[ptB, cB], writes=[ptB])
                                for i in range(max(jd, 0), 4):
                                    bank, off = acc[(s, i)]
                                    c0 = i * 128 - lo
                                    st_flag = (kt == 0 and off == 0)
                                    k.op('tensor', lambda e, pt=pt, bank=bank, off=off, c0=c0, kt=kt, i=i, qb=qb, st_flag=st_flag: e.matmul(
                                        ps[bank][:, off:off + 129], pt[:, c0:c0 + 128], va[:, kt, 0:129], start=st_flag, stop=(kt == 4 * qb + i),
                                        skip_group_check=True),
                                        reads=[ptB, vaB], writes=[psB[bank]])
                        for i in range(4):
                            b1, f1 = acc[(0, i)]
                            b2, f2 = acc[(1, i)]
                            st, stB = r_st.next()
                            o1, o1B = r_o1.next()
                            jk, jkB = r_jk.next()
                            ob, obB = r_ob.next()
                            k.op('vector', lambda e, st=st, b1=b1, f1=f1: e.reciprocal(st[:, 0:1], ps[b1][:, f1 + 128:f1 + 129]), reads=[psB[b1]], writes=[stB])
                            k.op('vector', lambda e, st=st, b2=b2, f2=f2: e.reciprocal(st[:, 1:2], ps[b2][:, f2 + 128:f2 + 129]), reads=[psB[b2]], writes=[stB])
                            k.op('vector', lambda e, st=st: e.tensor_tensor(st[:, 2:3], st[:, 1:2], lam[:, 5:6], ALU.mult), reads=[stB, lamB], writes=[stB])
                            k.op('vector', lambda e, st=st, o1=o1, b1=b1, f1=f1: e.tensor_scalar_mul(o1[:], ps[b1][:, f1:f1 + 128], st[:, 0:1]),
                                 reads=[psB[b1], stB], writes=[o1B])
                            k.op('vector', lambda e, st=st, o1=o1, b2=b2, f2=f2: e.scalar_tensor_tensor(o1[:], ps[b2][:, f2:f2 + 128], st[:, 2:3], o1[:], ALU.mult, ALU.add),
                                 reads=[psB[b2], stB, o1B], writes=[o1B])
                            k.op('scalar', lambda e, st=st, o1=o1, jk=jk: e.activation(jk[:], o1[:], AF.Square, accum_out=st[:, 3:4]), reads=[o1B], writes=[jkB, stB])
                            k.op('scalar', lambda e, st=st: e.activation(st[:, 4:5], st[:, 3:4], AF.Sqrt, bias=RMS_EPS, scale=1.0 / 128), reads=[stB], writes=[stB])
                            k.op('vector', lambda e, st=st: e.reciprocal(st[:, 5:6], st[:, 4:5]), reads=[stB], writes=[stB])
                            k.op('vector', lambda e, st=st, o1=o1, ob=ob: e.scalar_tensor_tensor(ob[:], o1[:], st[:, 5:6], sw[:], ALU.mult, ALU.mult),
                                 reads=[o1B, stB, swB], writes=[obB])
                            t0 = qb * 512 + i * 128
                            k.dma('sync', om_d[t0:t0 + 128, h * 128:(h + 1) * 128], ob[:], reads=[obB])
                k.barrier()

        def tok_moe(layer, w_out_src, last):
            with ExitStack() as es:
                wo, woB = load_w_bf16(es, "wo", w_out_src, 1024)
                wr = sbt(es, "wr", [128, 8, 40], F32)
                wrB = Buf("wr")
                k.dma('sync', wr[:, :, 0:4], moe_w_group[layer].rearrange("(k p) c -> p k c", p=128), writes=[wrB])
                k.dma('sync', wr[:, :, 4:36], moe_w_expert[layer].rearrange("(k p) c -> p k c", p=128), writes=[wrB])
                g1, g1B = bcast_row(es, "lng1", ln_mix_g[layer:layer + 1, :], D)
                b1, b1B = bcast_row(es, "lnb1", ln_mix_b[layer:layer + 1, :], D)
                lnB1 = Buf("ln1")
                oh = [sbt(es, f"oh{i}", [128, NT, 32], F32) for i in range(2)]
                ohB = Buf("oh")
                gates = sbt(es, "gates", [128, NT, 2], F32)
                gB = Buf("gates")
                rings = {'hTs': Ring(es, nc, "hTs2", [128, 8, 128], BF16, 2), 'st': Ring(es, nc, "lst", [128, 8], F32, 2),
                         'junk': Ring(es, nc, "ljk", [128, D], F32, 1)}
                with ExitStack() as e1:
                    r_om = Ring(e1, nc, "om", [128, D], BF16, 2)
                    r_omT = Ring(e1, nc, "omT", [128, 8, 128], BF16, 2)
                    r_h = Ring(e1, nc, "hh", [128, D], F32, 2)
                    r_t = Ring(e1, nc, "tt", [128, D], F32, 2)
                    r_y = Ring(e1, nc, "yy", [128, D], F32, 2)
                    r_xb = Ring(e1, nc, "xb", [128, D], BF16, 2)
                    r_xT = Ring(e1, nc, "xT", [128, 8, 128], F32, 2)
                    r_rt = Ring(e1, nc, "rt", [128, 160], F32, 2)
                    for t in range(NT):
                        rows = slice(t * 128, (t + 1) * 128)
                        om, omB = r_om.next()
                        k.dma('sync', om[:], om_d[rows, :], writes=[omB])
                        hh, hhB = r_h.next()
                        k.dma('sync', hh[:], h_d[rows, :], writes=[hhB])
                        for kc in range(8):
                            k.op('tensor', lambda e, om=om, kc=kc: e.transpose(pb[:, kc * 128:(kc + 1) * 128], om[:, kc * 128:(kc + 1) * 128], identb[:]),
                                 reads=[omB, cB], writes=[pbB])
                        omT, omTB = r_omT.next()
                        k.op('vector', lambda e, omT=omT: e.tensor_copy(omT[:], pb[:].rearrange("p (a b) -> p a b", a=8)), reads=[pbB], writes=[omTB])
                        tt, ttB = r_t.next()
                        for half in range(2):
                            pp, ppB = ps[half], psB[half]
                            for kc in range(8):
                                k.op('tensor', lambda e, pp=pp, kc=kc, half=half, omT=omT: e.matmul(pp[:, :], omT[:, kc, :], wo[:, kc, half * 512:(half + 1) * 512],
                                                                                             start=(kc == 0), stop=(kc == 7)), reads=[omTB, woB], writes=[ppB])
                            k.op('vector', lambda e, pp=pp, half=half, tt=tt, hh=hh: e.scalar_tensor_tensor(tt[:, half * 512:(half + 1) * 512], hh[:, half * 512:(half + 1) * 512],
                                                                                                      ALPHA, pp[:, :], ALU.mult, ALU.add),
                                 reads=[ppB, hhB], writes=[ttB])
                        yy, yyB = r_y.next()
                        layer_norm(rings, tt, ttB, g1, b1, [g1B, b1B], yy, yyB)
                        k.dma('sync', h_d[rows, :], yy[:], reads=[yyB, hhB])
                        xb, xbB = r_xb.next()
                        k.op('scalar', lambda e, xb=xb, yy=yy: e.copy(xb[:], yy[:]), reads=[yyB], writes=[xbB])
                        k.dma('sync', xb_d[rows, :], xb[:], reads=[xbB])
                        xT, xTB = r_xT.next()
                        for half in range(2):
                            pp, ppB = ps[2 + half], psB[2 + half]
                            for jj in range(4):
                                kc = half * 4 + jj
                                k.op('tensor', lambda e, pp=pp, jj=jj, kc=kc, yy=yy: e.transpose(pp[:, jj * 128:(jj + 1) * 128], yy[:, kc * 128:(kc + 1) * 128], ident[:]),
                                     reads=[yyB, cB], writes=[ppB])
                            if half == 0:
                                k.op('vector', lambda e, pp=pp, xT=xT: e.tensor_copy(xT[:, 0:4, :], pp[:].rearrange("p (a b) -> p a b", a=4)), reads=[ppB], writes=[xTB])
                            else:
                                k.op('scalar', lambda e, pp=pp, xT=xT: e.copy(xT[:, 4:8, :], pp[:].rearrange("p (a b) -> p a b", a=4)), reads=[ppB], writes=[xTB])
                        p4, p4B = ps[4], psB[4]
                        for kc in range(8):
                            k.op('tensor', lambda e, kc=kc, xT=xT: e.matmul(p4[:, 0:36], xT[:, kc, :], wr[:, kc, 0:36], start=(kc == 0), stop=(kc == 7)),
                                 reads=[xTB, wrB], writes=[p4B])
                        rt, rtB = r_rt.next()
                        V = lambda fn, rt=rt, rtB=rtB, extra_r=(), extra_w=(): k.op('vector', fn, reads=[rtB] + list(extra_r), writes=[rtB] + list(extra_w))
                        k.op('vector', lambda e, rt=rt: e.tensor_copy(rt[:, 0:36], p4[:, 0:36]), reads=[p4B], writes=[rtB])
                        V(lambda e, rt=rt: e.reduce_max(rt[:, 36:37], rt[:, 0:4], AX.X))
                        V(lambda e, rt=rt: e.tensor_scalar_mul(rt[:, 37:38], rt[:, 36:37], -1.0))
                        k.op('scalar', lambda e, rt=rt: e.activation(rt[:, 84:88], rt[:, 0:4], AF.Exp, bias=rt[:, 37:38], accum_out=rt[:, 38:39]), reads=[rtB], writes=[rtB])
                        V(lambda e, rt=rt: e.reciprocal(rt[:, 39:40], rt[:, 38:39]))
                        V(lambda e, rt=rt: e.tensor_scalar(rt[:, 40:44], rt[:, 0:4], rt[:, 36:37], None, ALU.is_ge))
                        V(lambda e, rt=rt: e.tensor_scalar(rt[:, 40:44], rt[:, 40:44], -NEG, NEG, ALU.mult, ALU.add))
                        for g in range(4):
                            V(lambda e, rt=rt, g=g: e.tensor_scalar(rt[:, 44 + g * 8:52 + g * 8], rt[:, 4 + g * 8:12 + g * 8], rt[:, 40 + g:41 + g], None, ALU.add))
                        V(lambda e, rt=rt: e.reduce_max(rt[:, 76:77], rt[:, 44:76], AX.X))
                        V(lambda e, rt=rt, t=t: e.tensor_scalar(oh[0][:, t, :], rt[:, 44:76], rt[:, 76:77], None, ALU.is_ge), extra_w=[ohB])
                        V(lambda e, rt=rt, t=t: e.scalar_tensor_tensor(rt[:, 96:128], oh[0][:, t, :], NEG, rt[:, 44:76], ALU.mult, ALU.add), extra_r=[ohB])
                        V(lambda e, rt=rt: e.reduce_max(rt[:, 77:78], rt[:, 96:128], AX.X))
                        V(lambda e, rt=rt, t=t: e.tensor_scalar(oh[1][:, t, :], rt[:, 96:128], rt[:, 77:78], None, ALU.is_ge), extra_w=[ohB])
                        V(lambda e, rt=rt: e.tensor_tensor(rt[:, 78:79], rt[:, 77:78], rt[:, 76:77], ALU.subtract))
                        k.op('scalar', lambda e, rt=rt: e.activation(rt[:, 79:80], rt[:, 78:79], AF.Exp), reads=[rtB], writes=[rtB])
                        V(lambda e, rt=rt: e.tensor_scalar_add(rt[:, 80:81], rt[:, 79:80], 1.0))
                        V(lambda e, rt=rt: e.reciprocal(rt[:, 81:82], rt[:, 80:81]))
                        V(lambda e, rt=rt, t=t: e.tensor_tensor(gates[:, t, 0:1], rt[:, 39:40], rt[:, 81:82], ALU.mult), extra_w=[gB])
                        V(lambda e, rt=rt, t=t: e.tensor_tensor(gates[:, t, 1:2], rt[:, 39:40], gates[:, t, 0:1], ALU.subtract), extra_r=[gB], extra_w=[gB])
                    k.barrier()
                if checkpoint(noraise=True):
                    return True
                dest = sbt(es, "dest", [128, 2, NT], I32)
                destB = Buf("dest")
                with ExitStack() as e2:
                    selb = sbt(e2, "selb", [128, NT, 32], BF16)
                    cnt = sbt(e2, "cnt", [128, NT, 32], F32)
                    pref = sbt(e2, "pref", [128, NT, 32], F32)
                    slot = sbt(e2, "slot", [128, NT, 32], F32)
                    ebase = sbt(e2, "ebase", [128, 32], F32)
                    tmp = sbt(e2, "ptmp", [128, NT, 32], F32)
                    dfl = sbt(e2, "dfl", [128, 2, NT], F32)
                    pB = Buf("pos")
                    k.op('gpsimd', lambda e: e.iota(ebase[:], [[CAP, 32]], base=0, channel_multiplier=0, allow_small_or_imprecise_dtypes=True), writes=[pB])
                    k.op('vector', lambda e: e.tensor_tensor(selb[:], oh[0][:], oh[1][:], ALU.add), reads=[ohB], writes=[pB])
                    TPB = 16
                    for t0 in range(0, NT, TPB):
                        n = min(TPB, NT - t0)
                        k.op('tensor', lambda e, t0=t0, n=n: e.matmul(ps[0][:, 0:n * 32], ones_b[:], selb[:, t0:t0 + n, :].rearrange("p a b -> p (a b)"), start=True, stop=True),
                             reads=[pB, cB], writes=[psB[0]])
                        k.op('vector', lambda e, t0=t0, n=n: e.tensor_copy(cnt[:, t0:t0 + n, :].rearrange("p a b -> p (a b)"), ps[0][:, 0:n * 32]), reads=[psB[0]], writes=[pB])
                        k.op('tensor', lambda e, t0=t0, n=n: e.matmul(ps[1][:, 0:n * 32], sutri[:], selb[:, t0:t0 + n, :].rearrange("p a b -> p (a b)"), start=True, stop=True),
                             reads=[pB, cB], writes=[psB[1]])
                        k.op('vector', lambda e, t0=t0, n=n: e.tensor_copy(slot[:, t0:t0 + n, :].rearrange("p a b -> p (a b)"), ps[1][:, 0:n * 32]), reads=[psB[1]], writes=[pB])
                    k.op('vector', lambda e: e.memset(pref[:, 0, :], 0.0), reads=[pB], writes=[pB])
                    for t in range(1, NT):
                        k.op('vector', lambda e, t=t: e.tensor_tensor(pref[:, t, :], pref[:, t - 1, :], cnt[:, t - 1, :], ALU.add), reads=[pB], writes=[pB])
                    k.op('vector', lambda e: e.tensor_tensor(slot[:], slot[:], pref[:], ALU.add), reads=[pB], writes=[pB])
                    k.op('vector', lambda e: e.tensor_scalar(tmp[:], slot[:], float(CAP), 1.0e6, ALU.is_ge, ALU.mult), reads=[pB], writes=[pB])
                    k.op('vector', lambda e: e.tensor_tensor(slot[:], slot[:], tmp[:], ALU.add), reads=[pB], writes=[pB])
                    for t in range(NT):
                        k.op('gpsimd', lambda e, t=t: e.tensor_tensor(slot[:, t, :], slot[:, t, :], ebase[:], ALU.add), reads=[pB], writes=[pB])
                    for i in range(2):
                        k.op('vector', lambda e, i=i: e.tensor_tensor(tmp[:], slot[:], oh[i][:], ALU.mult), reads=[pB, ohB], writes=[pB])
                        k.op('vector', lambda e, i=i: e.reduce_sum(dfl[:, i, :], tmp[:], AX.X), reads=[pB], writes=[pB])
                    k.op('vector', lambda e: e.tensor_scalar_min(dfl[:], dfl[:], float(NS)), reads=[pB], writes=[pB])
                    k.op('vector', lambda e: e.tensor_copy(dest[:], dfl[:]), reads=[pB], writes=[destB])
                    r_xb2 = Ring(e2, nc, "xb2", [128, D], BF16, 3)
                    for t in range(NT):
                        xb, xbB = r_xb2.next()
                        k.dma('sync', xb[:], xb_d[t * 128:(t + 1) * 128, :], writes=[xbB])
                        for i in range(2):
                            k.op('gpsimd', lambda e, xb=xb, i=i, t=t: e.indirect_dma_start(
                                out=xs_d, out_offset=bass.IndirectOffsetOnAxis(ap=dest[:, i, t:t + 1], axis=0), in_=xb[:], in_offset=None),
                                reads=[xbB, destB], dma=True)
                    k.barrier()
                with ExitStack() as e3:
                    NG = (CAP + 127) // 128
                    r_w13 = Ring(e3, nc, "w13", [128, 8, 1024], BF16, 2)
                    r_w2 = Ring(e3, nc, "w2", [128, 4, 1024], BF16, 2)
                    r_xs = Ring(e3, nc, "xs", [128, NG, D], BF16, 2)
                    r_xsT = Ring(e3, nc, "xsT", [128, 8, CAP], BF16, 2)
                    r_sg = Ring(e3, nc, "sg", [128, 4, CAP], F32, 1)
                    r_hid = Ring(e3, nc, "hid", [128, 4, CAP], BF16, 2)
                    r_ys = Ring(e3, nc, "ys", [128, D], F32, 2)
                    for ex in range(32):
                        w13, w13B = r_w13.next()
                        k.dma('gpsimd', w13[:], moe_w13[layer, ex].rearrange("(k p) c -> p k c", p=128), writes=[w13B])
                        w2, w2B = r_w2.next()
                        k.dma('gpsimd', w2[:], moe_w2[layer, ex].rearrange("(k p) c -> p k c", p=128), writes=[w2B])
                        xs, xsB = r_xs.next()
                        xsT, xsTB = r_xsT.next()
                        for g in range(NG):
                            r0 = ex * CAP + g * 128
                            nr = min(128, CAP - g * 128)
                            k.dma('sync', xs[0:nr, g, :], xs_d[r0:r0 + nr, :], writes=[xsB])
                        for g in range(NG):
                            nr = min(128, CAP - g * 128)
                            for kc in range(8):
                                k.op('tensor', lambda e, xs=xs, g=g, kc=kc, nr=nr: e.transpose(pb[:, kc * 128:kc * 128 + nr], xs[0:nr, g, kc * 128:(kc + 1) * 128], identb[0:nr, 0:nr]),
                                     reads=[xsB, cB], writes=[pbB])
                            k.op('vector', lambda e, xsT=xsT, g=g, nr=nr: e.tensor_copy(xsT[:, :, g * 128:g * 128 + nr], pb[:].rearrange("p (a b) -> p a b", a=8)[:, :, 0:nr]),
                                 reads=[pbB], writes=[xsTB])
                        sg, sgB = r_sg.next()
                        hid, hidB = r_hid.next()
                        for n0 in range(0, CAP, 512):
                            n = min(512, CAP - n0)
                            for m in range(8):
                                pp, ppB = ps[m % 4], psB[m % 4]
                                for kc in range(8):
                                    k.op('tensor', lambda e, pp=pp, kc=kc, m=m, w13=w13, xsT=xsT, n0=n0, n=n: e.matmul(pp[:, 0:n], w13[:, kc, m * 128:(m + 1) * 128], xsT[:, kc, n0:n0 + n],
                                                                                                           start=(kc == 0), stop=(kc == 7)),
                                         reads=[w13B, xsTB], writes=[ppB])
                                if m < 4:
                                    k.op('scalar', lambda e, pp=pp, m=m, sg=sg, n0=n0, n=n: e.activation(sg[:, m, n0:n0 + n], pp[:, 0:n], AF.Silu), reads=[ppB], writes=[sgB])
                                else:
                                    k.op('vector', lambda e, pp=pp, m=m, sg=sg, hid=hid, n0=n0, n=n: e.tensor_tensor(hid[:, m - 4, n0:n0 + n], sg[:, m - 4, n0:n0 + n], pp[:, 0:n], ALU.mult),
                                         reads=[ppB, sgB], writes=[hidB])
                        for g in range(NG):
                            nr = min(128, CAP - g * 128)
                            ys, ysB = r_ys.next()
                            for half in range(2):
                                pp, ppB = ps[4 + half], psB[4 + half]
                                for f in range(4):
                                    k.op('tensor', lambda e, pp=pp, f=f, half=half, hid=hid, w2=w2, g=g, nr=nr: e.matmul(pp[0:nr, :], hid[:, f, g * 128:g * 128 + nr], w2[:, f, half * 512:(half + 1) * 512],
                                                                                                             start=(f == 0), stop=(f == 3)),
                                         reads=[hidB, w2B], writes=[ppB])
                                if half == 0:
                                    k.op('vector', lambda e, pp=pp, ys=ys, nr=nr: e.tensor_copy(ys[0:nr, 0:512], pp[0:nr, :]), reads=[ppB], writes=[ysB])
                                else:
                                    k.op('scalar', lambda e, pp=pp, ys=ys, nr=nr: e.copy(ys[0:nr, 512:1024], pp[0:nr, :]), reads=[ppB], writes=[ysB])
                            r0 = ex * CAP + g * 128
                            for hf in range(2):
                                k.dma('sync', ys_h[hf][r0:r0 + nr, :], ys[0:nr, hf * 512:(hf + 1) * 512], reads=[ysB])
                    k.barrier()
                with ExitStack() as e4:
                    g2, g2B = bcast_row(e4, "lng2", ln_ffn_g[layer:layer + 1, :], D)
                    b2, b2B = bcast_row(e4, "lnb2", ln_ffn_b[layer:layer + 1, :], D)
                    r_yq = [Ring(e4, nc, f"yq{q}", [128, 512], F32, 2) for q in range(4)]
                    r_h = Ring(e4, nc, "ch", [128, D], F32, 2)
                    r_o = Ring(e4, nc, "co", [128, D], F32, 2)
                    for t in range(NT):
                        rows = slice(t * 128, (t + 1) * 128)
                        hh, hhB = r_h.next()
                        k.dma('sync', hh[:], h_d[rows, :], writes=[hhB])
                        k.op('scalar', lambda e, hh=hh: e.mul(hh[:], hh[:], ALPHA), reads=[hhB], writes=[hhB])
                        for i in range(2):
                            for hf in range(2):
                                yq, yqB = r_yq[i * 2 + hf].next()
                                k.op('gpsimd', lambda e, yq=yq, i=i, t=t, hf=hf: e.indirect_dma_start(
                                    out=yq[:], out_offset=None, in_=ys_h[hf],
                                    in_offset=bass.IndirectOffsetOnAxis(ap=dest[:, i, t:t + 1], axis=0)), reads=[destB], writes=[yqB], dma=True)
                                k.op('vector', lambda e, hh=hh, yq=yq, t=t, i=i, hf=hf: e.scalar_tensor_tensor(
                                    hh[:, hf * 512:(hf + 1) * 512], yq[:], gates[:, t, i:i + 1], hh[:, hf * 512:(hf + 1) * 512], ALU.mult, ALU.add),
                                    reads=[hhB, yqB, gB], writes=[hhB])
                        oo, ooB = r_o.next()
                        layer_norm(rings, hh, hhB, g2, b2, [g2B, b2B], oo, ooB)
                        if last:
                            out_toks.append(k.dma('sync', out[rows, :], oo[:], reads=[ooB]))
                        else:
                            k.dma('sync', h_d[rows, :], oo[:], reads=[ooB, hhB])
                            make_hT(rings, oo, ooB, t)
                    k.barrier()

        try:
            checkpoint()
            for layer in range(4):
                if layer < 2:
                    deltanet(layer)
                    checkpoint()
                    if tok_moe(layer, a_w_out[layer], last=False):
                        raise _Stop()
                    checkpoint()
                else:
                    j = layer - 2
                    if j == 0:
                        shared_kv()
                    diffattn(j, layer)
                    checkpoint()
                    if tok_moe(layer, b_w_out[j], last=(layer == 3)):
                        raise _Stop()
                    checkpoint()
        except _Stop:
            out_toks.append(k.dma('sync', out, h_d))
            out_toks.append(k.dma('sync', dbg, om_d))
        k.wait_all('sync', out_toks)
        k.finish()
    return nc, k


SEQ = 8192
CAP_FULL = 640
_cache = {}


def kernel(**inputs):
    x = np.asarray(inputs['x'])
    B, T, _ = x.shape
    cap = CAP_FULL if T == SEQ else max(64, int(T / 16 + 6 * math.sqrt(T / 16) + 16) // 32 * 32 + 32)
    key = (T, cap)
    if key not in _cache:
        _cache[key] = build(T, cap)[0]
    nc = _cache[key]
    shared = {n: np.ascontiguousarray(np.asarray(v, dtype=np.float32)) for n, v in inputs.items() if n != 'x'}
    in_maps = []
    for b in range(B):
        m = dict(shared)
        m['x'] = np.ascontiguousarray(x[b])
        in_maps.append(m)
    res = run_bass_kernel_spmd(nc, in_maps, core_ids=list(range(B)))
    return np.stack([np.asarray(res.results[b]['out']) for b in range(B)], axis=0).astype(np.float32)
```

```python
import math
from contextlib import ExitStack
import numpy as np
import concourse.bass as bass
import concourse.mybir as mybir
from concourse.bass_utils import run_bass_kernel_spmd

F32 = mybir.dt.float32
BF16 = mybir.dt.bfloat16
I32 = mybir.dt.int32
AF = mybir.ActivationFunctionType
ALU = mybir.AluOpType
AX = mybir.AxisListType

ENGS = ['tensor', 'vector', 'scalar', 'gpsimd', 'sync']
SEM_EPOCH = 30000
N_DMA_SEMS = 16


class Buf:
    __slots__ = ('name', 'w', 'r', 'excl')

    def __init__(self, name='', excl=False):
        self.name = name
        self.excl = excl
        self.w = None
        self.r = {}


class _Op:
    __slots__ = ('fn', 'waits', 'inc', 'incval', 'dma')

    def __init__(self, fn, waits, dma):
        self.fn = fn
        self.waits = waits
        self.inc = False
        self.incval = 0
        self.dma = dma


class K:
    def __init__(self, nc):
        self.nc = nc
        self.ops = {e: [] for e in ENGS}
        self.waited = {e: {} for e in ENGS}
        self.dma_rr = {e: 0 for e in ENGS}
        self.dma_cnt = {}

    def _need_wait(self, eng, t):
        key = (t[0], t[1])
        if self.waited[eng].get(key, -1) >= t[2]:
            return False
        self.waited[eng][key] = t[2]
        if t[0] == 'e':
            self.ops[t[1]][t[2]].inc = True
        return True

    def op(self, eng, fn, reads=(), writes=(), dma=False):
        idx = len(self.ops[eng])
        writes = list(writes) + [b for b in reads if b.excl]
        reads = [b for b in reads if not b.excl]
        deps = []
        for b in reads:
            if b.w is not None:
                deps.append(b.w)
        for b in writes:
            if b.w is not None:
                deps.append(b.w)
            deps.extend(b.r.values())
        waits = []
        for t in deps:
            if t[0] == 'e' and t[1] == eng and eng == 'tensor':
                continue
            if self._need_wait(eng, t):
                waits.append(t)
        dm = None
        if dma:
            slot = self.dma_rr[eng]
            self.dma_rr[eng] = (slot + 1) % N_DMA_SEMS
            cnt = self.dma_cnt.get((eng, slot), 0) + 1
            self.dma_cnt[(eng, slot)] = cnt
            dm = ((eng, slot), cnt * 16)
            if cnt > 1:
                t = ('d', (eng, slot), (cnt - 1) * 16)
                if self._need_wait(eng, t):
                    waits.append(t)
            tok = ('d', (eng, slot), cnt * 16)
        else:
            tok = ('e', eng, idx)
        self.ops[eng].append(_Op(fn, waits, dm))
        kk = (tok[0], tok[1])
        for b in reads:
            b.r[kk] = tok
        for b in writes:
            b.w = tok
            b.r = {}
        return tok

    def dma(self, eng, out, in_, reads=(), writes=(), **kw):
        return self.op(eng, lambda e: e.dma_start(out=out, in_=in_, **kw), reads=reads, writes=writes, dma=True)

    def wait_all(self, eng, tokens):
        waits = [t for t in tokens if self._need_wait(eng, t)]
        if waits:
            self.ops[eng].append(_Op(None, waits, None))

    def barrier(self):
        toks = []
        for e in ENGS:
            for i in range(len(self.ops[e]) - 1, -1, -1):
                o = self.ops[e][i]
                if o.fn is not None and o.dma is None:
                    toks.append(('e', e, i))
                    break
        for key, cnt in self.dma_cnt.items():
            toks.append(('d', key, cnt * 16))
        for e in ENGS:
            self.wait_all(e, toks)
        self.flush()

    def flush(self):
        nc = self.nc
        if not hasattr(self, 'flushed'):
            self.flushed = {e: 0 for e in ENGS}
            self.inccnt = {e: 0 for e in ENGS}
            self.esems = {e: [] for e in ENGS}
            self.dsems = {}
        start = dict(self.flushed)
        for e in ENGS:
            for o in self.ops[e][start[e]:]:
                if o.inc:
                    self.inccnt[e] += 1
                    o.incval = self.inccnt[e]
            need = (self.inccnt[e] + SEM_EPOCH - 1) // SEM_EPOCH
            while len(self.esems[e]) < max(need, 1):
                self.esems[e].append(nc.alloc_semaphore(f"es_{e}_{len(self.esems[e])}"))
        for key in self.dma_cnt:
            if key not in self.dsems:
                self.dsems[key] = nc.alloc_semaphore(f"ds_{key[0]}_{key[1]}")
        esems, dsems, ops = self.esems, self.dsems, self.ops

        def semval(e2, incval):
            return esems[e2][(incval - 1) // SEM_EPOCH], (incval - 1) % SEM_EPOCH + 1

        with nc.Block() as block:
            for eng in ENGS:
                todo = ops[eng][start[eng]:]

                def body(e, eng=eng, todo=todo):
                    for o in todo:
                        for t in o.waits:
                            if t[0] == 'e':
                                p = ops[t[1]][t[2]]
                                assert p.incval > 0, (eng, t)
                                s, v = semval(t[1], p.incval)
                                e.wait_ge(s, v)
                            else:
                                e.wait_ge(dsems[t[1]], t[2])
                        if o.fn is None:
                            continue
                        ins = o.fn(e)
                        if o.dma is not None:
                            ins.then_inc(dsems[o.dma[0]], 16)
                        elif o.inc:
                            s, v = semval(eng, o.incval)
                            ins.then_inc(s, 1)
                if todo:
                    getattr(block, eng)(body)
                self.flushed[eng] = len(ops[eng])

    def finish(self):
        self.flush()


_uid = [0]


def _un(name):
    _uid[0] += 1
    return f"{name}_u{_uid[0]}"


class Ring:
    def __init__(self, es, nc, name, shape, dt, n):
        self.t = [es.enter_context(nc.sbuf_tensor(_un(f"{name}_{i}"), shape, dt)) for i in range(n)]
        self.b = [Buf(f"{name}_{i}") for i in range(n)]
        self.i = 0

    def next(self):
        i = self.i
        self.i = (i + 1) % len(self.t)
        return self.t[i], self.b[i]


D = 1024
NH = 8
ALPHA = 8 ** 0.25
LN_EPS = 1e-5
RMS_EPS = 1e-6
NEG = -1.0e30


def lambda_init(layer_idx):
    return 0.8 - 0.6 * math.exp(-0.3 * layer_idx)


class _Stop(Exception):
    pass


def build(T, CAP, stop=0, ksub=0, dumpflag=False):
    NT = T // 128
    NCH = T // 64
    NB = T // 512
    nc = bass.Bass("TRN2", target_bir_lowering=False)

    def din(name, shape, dt=F32):
        return nc.dram_tensor(name, shape, dt, kind="ExternalInput").ap()

    x = din("x", [T, D])
    a_w_in = din("a_w_in", [2, D, 4112])
    a_conv_w = din("a_conv_w", [2, 4, 3072])
    a_a_log = din("a_a_log", [2, 8])
    a_dt_bias = din("a_dt_bias", [2, 8])
    a_norm_w = din("a_norm_w", [2, 128])
    a_w_out = din("a_w_out", [2, D, D])
    kv_w = din("kv_w", [D, 2048])
    b_w_q = din("b_w_q", [2, D, D])
    b_lambda = din("b_lambda", [2, 4, 64])
    b_subln_w = din("b_subln_w", [2, 128])
    b_w_out = din("b_w_out", [2, D, D])
    ln_mix_g = din("ln_mix_g", [4, D])
    ln_mix_b = din("ln_mix_b", [4, D])
    ln_ffn_g = din("ln_ffn_g", [4, D])
    ln_ffn_b = din("ln_ffn_b", [4, D])
    moe_w_group = din("moe_w_group", [4, D, 4])
    moe_w_expert = din("moe_w_expert", [4, D, 32])
    moe_w13 = din("moe_w13", [4, 32, D, 1024])
    moe_w2 = din("moe_w2", [4, 32, 512, D])
    out = nc.dram_tensor("out", [T, D], F32, kind="ExternalOutput").ap()
    dbg = nc.dram_tensor("dbg", [T, D], BF16, kind="ExternalOutput").ap() if stop else None
    stage = [0]

    def checkpoint(noraise=False):
        stage[0] += 1
        if stop and stage[0] >= stop:
            if noraise:
                return True
            raise _Stop()
        return False

    h_d = nc.dram_tensor("h_d", [T, D], F32).ap()
    hT_d = nc.dram_tensor("hT_d", [D, T], BF16).ap()
    om_d = nc.dram_tensor("om_d", [T, D], BF16).ap()
    xb_d = nc.dram_tensor("xb_d", [T, D], BF16).ap()
    kT_d = nc.dram_tensor("kT_d", [NH, 2, 64, T], BF16).ap()
    va_d = nc.dram_tensor("va_d", [NH, T, 129], BF16).ap()
    NS = 32 * CAP
    xs_d = nc.dram_tensor("xs_d", [NS + 128, D], BF16).ap()
    ys_h = [nc.dram_tensor(f"ys_d{i}", [NS + 128, 512], F32).ap() for i in range(2)]
    hT_v = hT_d.rearrange("(k p) t -> p k t", p=128)

    k = K(nc)
    out_toks = []
    dumped = set()

    def dump(name, ap, B):
        if not dumpflag or name in dumped:
            return
        dumped.add(name)
        t = nc.dram_tensor("dump_" + name, list(ap.shape), F32, kind="ExternalOutput").ap()
        out_toks.append(k.dma('gpsimd', t, ap, reads=[B]))
    with ExitStack() as top:
        def sbt(es, name, shape, dt):
            return es.enter_context(nc.sbuf_tensor(_un(name), shape, dt))

        ps = [top.enter_context(nc.psum_tensor(f"ps{i}", [128, 512], F32)) for i in range(7)]
        psB = [Buf(f"ps{i}", excl=True) for i in range(7)]
        pb = top.enter_context(nc.psum_tensor("pb", [128, 1024], BF16))
        pbB = Buf("pb", excl=True)

        ident = sbt(top, "ident", [128, 128], F32)
        identb = sbt(top, "identb", [128, 128], BF16)
        ones_f = sbt(top, "ones_f", [128, 128], F32)
        ones_b = sbt(top, "ones_b", [128, 128], BF16)
        triu = sbt(top, "triu", [128, 128], F32)
        triub = sbt(top, "triub", [128, 128], BF16)
        sutri = sbt(top, "sutri", [128, 128], BF16)
        zero_b = sbt(top, "zero_b", [128, 1024], BF16)
        triu4 = sbt(top, "triu4", [64, 4, 64], F32)
        ident4 = sbt(top, "ident4", [64, 4, 64], F32)
        cB = Buf("consts")
        k.op('gpsimd', lambda e: e.iota(ones_f[:], [[1, 128]], base=0, channel_multiplier=-1,
                                        allow_small_or_imprecise_dtypes=True), writes=[cB])
        k.op('vector', lambda e: e.tensor_single_scalar(ident[:], ones_f[:], 0.0, ALU.is_equal), reads=[cB], writes=[cB])
        k.op('vector', lambda e: e.tensor_single_scalar(identb[:], ones_f[:], 0.0, ALU.is_equal), reads=[cB], writes=[cB])
        k.op('vector', lambda e: e.tensor_single_scalar(triu[:], ones_f[:], 0.0, ALU.is_ge), reads=[cB], writes=[cB])
        k.op('vector', lambda e: e.tensor_single_scalar(triub[:], ones_f[:], 0.0, ALU.is_ge), reads=[cB], writes=[cB])
        k.op('vector', lambda e: e.tensor_single_scalar(sutri[:], ones_f[:], 0.0, ALU.is_gt), reads=[cB], writes=[cB])
        for g4 in range(4):
            k.op('vector', lambda e, g4=g4: e.tensor_copy(triu4[:, g4, :], triu[0:64, 0:64]), reads=[cB], writes=[cB])
            k.op('vector', lambda e, g4=g4: e.tensor_copy(ident4[:, g4, :], ident[0:64, 0:64]), reads=[cB], writes=[cB])
        k.op('vector', lambda e: e.memset(ones_f[:], 1.0), reads=[cB], writes=[cB])
        k.op('vector', lambda e: e.memset(ones_b[:], 1.0), writes=[cB])
        k.op('vector', lambda e: e.memset(zero_b[:], 0.0), writes=[cB])
        zero_f = sbt(top, "zero_f", [128, 512], F32)
        k.op('vector', lambda e: e.memset(zero_f[:], 0.0), writes=[cB])
        for r0 in range(0, NS + 128, 128):
            k.dma('sync', xs_d[r0:r0 + 128, :], zero_b[:], reads=[cB])
        for hf in range(2):
            k.dma('sync', ys_h[hf][NS:NS + 128, :], zero_f[:], reads=[cB])
        k.barrier()

        def layer_norm(es_ring, t, tB, gbc, bbc, wBs, y, yB):
            st, stB = es_ring['st'].next()
            jk, jkB = es_ring['junk'].next()
            k.op('scalar', lambda e: e.activation(jk[:], t[:], AF.Copy, accum_out=st[:, 0:1]), reads=[tB], writes=[jkB, stB])
            k.op('scalar', lambda e: e.activation(jk[:], t[:], AF.Square, accum_out=st[:, 1:2]), reads=[tB], writes=[jkB, stB])
            k.op('vector', lambda e: e.tensor_scalar_mul(st[:, 2:3], st[:, 0:1], 1.0 / D), reads=[stB], writes=[stB])
            k.op('vector', lambda e: e.tensor_tensor(st[:, 3:4], st[:, 2:3], st[:, 2:3], ALU.mult), reads=[stB], writes=[stB])
            k.op('vector', lambda e: e.scalar_tensor_tensor(st[:, 4:5], st[:, 1:2], 1.0 / D, st[:, 3:4], ALU.mult, ALU.subtract),
                 reads=[stB], writes=[stB])
            k.op('scalar', lambda e: e.activation(st[:, 5:6], st[:, 4:5], AF.Sqrt, bias=LN_EPS), reads=[stB], writes=[stB])
            k.op('vector', lambda e: e.reciprocal(st[:, 6:7], st[:, 5:6]), reads=[stB], writes=[stB])
            k.op('vector', lambda e: e.tensor_scalar(t[:], t[:], st[:, 2:3], st[:, 6:7], ALU.subtract, ALU.mult), reads=[tB, stB], writes=[tB])
            k.op('gpsimd', lambda e: e.tensor_tensor(t[:], t[:], gbc[:], ALU.mult), reads=[tB] + wBs, writes=[tB])
            k.op('vector', lambda e: e.tensor_tensor(y[:], t[:], bbc[:], ALU.add), reads=[tB] + wBs, writes=[yB])

        def make_hT(rings, y, yB, tile):
            hb, hbB = rings['hTs'].next()
            for half in range(2):
                pp, ppB = ps[5 + half], psB[5 + half]
                for j in range(4):
                    kk = half * 4 + j
                    k.op('tensor', lambda e, pp=pp, j=j, kk=kk: e.transpose(pp[:, j * 128:(j + 1) * 128], y[:, kk * 128:(kk + 1) * 128], ident[:]),
                         reads=[yB, cB], writes=[ppB])
                eng = 'vector' if half == 0 else 'scalar'
                if eng == 'vector':
                    k.op('vector', lambda e, pp=pp, half=half: e.tensor_copy(hb[:, half * 4:(half + 1) * 4, :], pp[:].rearrange("p (a b) -> p a b", a=4)),
                         reads=[ppB], writes=[hbB])
                else:
                    k.op('scalar', lambda e, pp=pp, half=half: e.copy(hb[:, half * 4:(half + 1) * 4, :], pp[:].rearrange("p (a b) -> p a b", a=4)),
                         reads=[ppB], writes=[hbB])
            k.dma('sync', hT_v[:, :, tile * 128:(tile + 1) * 128], hb[:], reads=[hbB])

        with ExitStack() as es:
            rings = {'hTs': Ring(es, nc, "hTs", [128, 8, 128], BF16, 2)}
            xr = Ring(es, nc, "xin", [128, D], F32, 2)
            for t in range(NT):
                xt, xB = xr.next()
                k.dma('sync', xt[:], x[t * 128:(t + 1) * 128, :], writes=[xB])
                k.dma('gpsimd', h_d[t * 128:(t + 1) * 128, :], xt[:], reads=[xB])
                make_hT(rings, xt, xB, t)
            k.barrier()

        def load_w_bf16(es, name, src, cols):
            w = sbt(es, name, [128, 8, cols], BF16)
            wB = Buf(name)
            k.dma('gpsimd', w[:], src.rearrange("(k p) c -> p k c", p=128), writes=[wB])
            return w, wB

        def bcast_row(es, name, src_row, n, dt=F32):
            w = sbt(es, name, [128, n], dt)
            wB = Buf(name)
            k.dma('sync', w[:], src_row.partition_broadcast(128), writes=[wB])
            return w, wB

        def deltanet(l):
            with ExitStack() as es:
                cw = sbt(es, "cw", [128, 24, 4], F32)
                cwB = Buf("cw")
                cwn = sbt(es, "cwn", [4, 3072], F32)
                k.dma('sync', cwn[:], a_conv_w[l], writes=[cwB])
                for part in range(24):
                    k.op('tensor', lambda e, part=part: e.transpose(ps[0][:, part * 4:(part + 1) * 4], cwn[:, part * 128:(part + 1) * 128], ident[0:4, 0:4]),
                         reads=[cwB, cB], writes=[psB[0]])
                k.op('vector', lambda e: e.tensor_copy(cw[:].rearrange("p a b -> p (a b)"), ps[0][:, 0:96]), reads=[psB[0]], writes=[cwB])
                nw, nwB = bcast_row(es, "nw", a_norm_w[l:l + 1, :], 128)
                alog, alB = bcast_row(es, "alog", a_a_log[l:l + 1, :], 8)
                dtb, dtB = bcast_row(es, "dtb", a_dt_bias[l:l + 1, :], 8)
                nea = sbt(es, "nea", [128, 8], F32)
                k.op('scalar', lambda e: e.activation(nea[:], alog[:], AF.Exp), reads=[alB], writes=[alB])
                k.op('vector', lambda e: e.tensor_scalar_mul(nea[:], nea[:], -1.0), reads=[alB], writes=[alB])
                qT = sbt(es, "qT", [128, T], BF16)
                kT = sbt(es, "kT", [128, T], BF16)
                vT = sbt(es, "vT", [128, T], BF16)
                qkvB = [Buf("qT"), Buf("kT"), Buf("vT")]
                qkv = [qT, kT, vT]
                zs = sbt(es, "zs", [64, NCH, 128], BF16)
                zsB = Buf("zs")
                gl = sbt(es, "gl", [64, 8, NCH], F32)
                glB = Buf("gl")
                egl = sbt(es, "egl", [128, NCH], F32)
                eglB = Buf("egl")
                S = sbt(es, "S", [128, 128], F32)
                SB = Buf("S")
                hbr = Ring(es, nc, "hblk", [128, 8, 512], BF16, 2)
                raw = sbt(es, "raw", [128, 3, 515], F32)
                rawB = [Buf("raw0"), Buf("raw1"), Buf("raw2")]
                cvr = Ring(es, nc, "cv", [128, 512], F32, 2)
                sqr = Ring(es, nc, "sq", [128, 512], F32, 2)
                G = 4
                R = 2
                r_kg = Ring(es, nc, "kg", [64, G, 256], F32, R)
                r_kdec = Ring(es, nc, "kdec", [64, G, 128], F32, R)
                r_dm = Ring(es, nc, "dm", [64, G, 64], F32, R)
                r_dg = Ring(es, nc, "dg", [64, G, 64], F32, R)
                r_egb = Ring(es, nc, "egb", [128, G, 64], F32, R)
                r_qg = Ring(es, nc, "qg", [128, G, 64], F32, R)
                r_at = Ring(es, nc, "at", [64, G, 64], F32, R)
                r_X = Ring(es, nc, "X", [64, G, 64], F32, 4)
                r_Y = Ring(es, nc, "Y", [64, G, 64], F32, 4)
                r_P = Ring(es, nc, "P", [64, G, 64], F32, R)
                r_uw = Ring(es, nc, "uw", [64, G, 256], F32, R)
                r_wT = Ring(es, nc, "wT", [128, G, 64], F32, R)
                r_vn = Ring(es, nc, "vn", [64, 128], F32, 2)
                r_o = Ring(es, nc, "o", [64, 128], F32, 2)
                r_ob = Ring(es, nc, "ob", [64, 128], BF16, 2)
                r_st = Ring(es, nc, "dst", [64, 4], F32, 2)
                r_jk = Ring(es, nc, "djk", [64, 128], F32, 2)
                for h in range(NH):
                    wq3, wq3B = [], []
                    wcat = sbt(es, f"wcat{h}", [128, 8, 384], BF16) if h == 0 else wcat_keep[0]
                    wzg = sbt(es, f"wzg{h}", [128, 8, 128], BF16) if h == 0 else wcat_keep[1]
                    if h == 0:
                        wcat_keep = [wcat, wzg]
                        wcB = Buf("wcat")
                        wzB = Buf("wzg")
                    for part in range(3):
                        k.dma('gpsimd', wcat[:, :, part * 128:(part + 1) * 128],
                              a_w_in[l, :, part * 1024 + h * 128: part * 1024 + (h + 1) * 128].rearrange("(k p) c -> p k c", p=128), writes=[wcB])
                    k.dma('gpsimd', wzg[:], a_w_in[l, :, 3072 + h * 128:3072 + (h + 1) * 128].rearrange("(k p) c -> p k c", p=128), writes=[wzB])
                    if h == 0:
                        wg16 = sbt(es, "wg16", [128, 8, 16], BF16)
                        k.dma('gpsimd', wg16[:], a_w_in[l, :, 4096:4112].rearrange("(k p) c -> p k c", p=128), writes=[wzB])
                    for part in range(3):
                        k.op('vector', lambda e, part=part: e.memset(raw[:, part, 0:3], 0.0), writes=[rawB[part]])
                    if ksub == 5:
                        k.barrier()
                        return
                    for blk in range(NB):
                        hb, hbB = hbr.next()
                        k.dma('sync', hb[:], hT_v[:, :, blk * 512:(blk + 1) * 512], writes=[hbB])
                        for part in range(3):
                            pp, ppB = ps[part % 2], psB[part % 2]
                            for kc in range(8):
                                k.op('tensor', lambda e, pp=pp, kc=kc, part=part, hb=hb: e.matmul(pp[:, :], wcat[:, kc, part * 128:(part + 1) * 128], hb[:, kc, :],
                                                                                            start=(kc == 0), stop=(kc == 7)),
                                     reads=[wcB, hbB], writes=[ppB])
                            k.op('scalar', lambda e, pp=pp, part=part: e.copy(raw[:, part, 3:515], pp[:, :]), reads=[ppB], writes=[rawB[part]])
                            if ksub == 11:
                                k.barrier()
                                return
                            cv, cvB = cvr.next()
                            ci = part * 8 + h
                            k.op('vector', lambda e, cv=cv, part=part, ci=ci: e.tensor_scalar_mul(cv[:], raw[:, part, 0:512], cw[:, ci, 0:1]),
                                 reads=[rawB[part], cwB], writes=[cvB])
                            for j in range(1, 4):
                                k.op('vector', lambda e, cv=cv, part=part, ci=ci, j=j: e.scalar_tensor_tensor(cv[:], raw[:, part, j:j + 512], cw[:, ci, j:j + 1], cv[:],
                                                                                                        ALU.mult, ALU.add),
                                     reads=[rawB[part], cwB, cvB], writes=[cvB])
                            k.op('vector', lambda e, part=part: e.tensor_copy(raw[:, part, 0:3], raw[:, part, 512:515]), reads=[rawB[part]], writes=[rawB[part]])
                            if ksub == 12:
                                k.barrier()
                                return
                            dst = qkv[part][:, blk * 512:(blk + 1) * 512]
                            if part == 2:
                                k.op('scalar', lambda e, cv=cv, dst=dst: e.activation(dst, cv[:], AF.Silu), reads=[cvB], writes=[qkvB[part]])
                            else:
                                k.op('scalar', lambda e, cv=cv: e.activation(cv[:], cv[:], AF.Silu), reads=[cvB], writes=[cvB])
                                sq, sqB = sqr.next()
                                k.op('gpsimd', lambda e, cv=cv, sq=sq: e.tensor_tensor(sq[:], cv[:], cv[:], ALU.mult), reads=[cvB], writes=[sqB])
                                p2, p2B = ps[2], psB[2]
                                for hf in range(2):
                                    k.op('tensor', lambda e, sq=sq, p2=p2, hf=hf: e.matmul(p2[:, hf * 256:(hf + 1) * 256], ones_f[:], sq[:, hf * 256:(hf + 1) * 256], start=True, stop=True), reads=[sqB, cB], writes=[p2B])
                                k.op('scalar', lambda e, sq=sq, p2=p2: e.activation(sq[:], p2[:, :], AF.Sqrt, bias=RMS_EPS), reads=[p2B], writes=[sqB])
                                k.op('vector', lambda e, sq=sq: e.reciprocal(sq[:], sq[:]), reads=[sqB], writes=[sqB])
                                sc = (128 ** -0.5) if part == 0 else 1.0
                                k.op('vector', lambda e, cv=cv, sq=sq, dst=dst, sc=sc: e.scalar_tensor_tensor(dst, cv[:], sc, sq[:], ALU.mult, ALU.mult),
                                     reads=[cvB, sqB], writes=[qkvB[part]])
                            if ksub == 13:
                                k.barrier()
                                return
                        if ksub == 14:
                            k.barrier()
                            return
                        for cc in range(8):
                            c = blk * 8 + cc
                            pp, ppB = ps[3 + cc % 2], psB[3 + cc % 2]
                            for kc in range(8):
                                k.op('tensor', lambda e, pp=pp, kc=kc, cc=cc, hb=hb: e.matmul(pp[0:64, 0:128], hb[:, kc, cc * 64:(cc + 1) * 64], wzg[:, kc, :],
                                                                                       start=(kc == 0), stop=(kc == 7)),
                                     reads=[wzB, hbB], writes=[ppB])
                            for kc in range(8):
                                k.op('tensor', lambda e, pp=pp, kc=kc, cc=cc, hb=hb: e.matmul(pp[0:64, 128:144], hb[:, kc, cc * 64:(cc + 1) * 64], wg16[:, kc, :],
                                                                                       start=(kc == 0), stop=(kc == 7)),
                                     reads=[wzB, hbB], writes=[ppB])
                            k.op('scalar', lambda e, pp=pp, c=c: e.activation(zs[:, c, :], pp[0:64, 0:128], AF.Silu), reads=[ppB], writes=[zsB])
                            k.op('vector', lambda e, pp=pp, c=c, h=h: e.tensor_copy(gl[:, 0, c:c + 1], pp[0:64, 128 + h:129 + h]), reads=[ppB], writes=[glB])
                            k.op('vector', lambda e, pp=pp, c=c, h=h: e.tensor_copy(gl[:, 1, c:c + 1], pp[0:64, 136 + h:137 + h]), reads=[ppB], writes=[glB])
                    if ksub == 1:
                        k.barrier()
                        return
                    k.op('scalar', lambda e: e.activation(gl[:, 0, :], gl[:, 0, :], AF.Sigmoid), reads=[glB], writes=[glB])
                    k.op('vector', lambda e: e.tensor_scalar_mul(gl[:, 5, :], gl[:, 0, :], -1.0), reads=[glB], writes=[glB])
                    k.op('scalar', lambda e, h=h: e.activation(gl[:, 1, :], gl[:, 1, :], AF.Exp, bias=dtb[0:64, h:h + 1]), reads=[glB, dtB], writes=[glB])
                    k.op('scalar', lambda e: e.activation(gl[:, 1, :], gl[:, 1, :], AF.Ln, bias=1.0), reads=[glB], writes=[glB])
                    k.op('vector', lambda e, h=h: e.tensor_scalar_mul(gl[:, 1, :], gl[:, 1, :], nea[0:64, h:h + 1]), reads=[glB, alB], writes=[glB])
                    for c0 in range(0, NCH, 512):
                        n = min(512, NCH - c0)
                        k.op('tensor', lambda e, c0=c0, n=n: e.matmul(ps[0][0:64, 0:n], triu[0:64, 0:64], gl[:, 1, c0:c0 + n], start=True, stop=True),
                             reads=[glB, cB], writes=[psB[0]])
                        k.op('vector', lambda e, c0=c0, n=n: e.tensor_copy(gl[:, 2, c0:c0 + n], ps[0][0:64, 0:n]), reads=[psB[0]], writes=[glB])
                        k.op('tensor', lambda e, c0=c0, n=n: e.matmul(ps[1][:, 0:n], ones_f[0:64, :], gl[:, 1, c0:c0 + n], start=True, stop=True),
                             reads=[glB, cB], writes=[psB[1]])
                        k.op('scalar', lambda e, c0=c0, n=n: e.activation(egl[:, c0:c0 + n], ps[1][:, 0:n], AF.Exp), reads=[psB[1]], writes=[eglB])
                        k.op('vector', lambda e, c0=c0, n=n: e.tensor_copy(gl[:, 6, c0:c0 + n], ps[1][0:64, 0:n]), reads=[psB[1]], writes=[glB])
                    k.op('scalar', lambda e: e.activation(gl[:, 3, :], gl[:, 2, :], AF.Exp), reads=[glB], writes=[glB])
                    k.op('vector', lambda e: e.tensor_tensor(gl[:, 7, :], gl[:, 6, :], gl[:, 2, :], ALU.subtract), reads=[glB], writes=[glB])
                    k.op('scalar', lambda e: e.activation(gl[:, 4, :], gl[:, 7, :], AF.Exp), reads=[glB], writes=[glB])
                    k.op('vector', lambda e: e.memset(S[:], 0.0), writes=[SB])
                    if h == 0 and l == 0:
                        dump("gl", gl[:], glB)
                        dump("egl", egl[:], eglB)
                        dump("qT", qT[:, 0:128], qkvB[0])
                        dump("kT", kT[:, 0:128], qkvB[1])
                        dump("vT", vT[:, 0:128], qkvB[2])
                        dump("zs", zs[:, 0:2, :], zsB)
                    if ksub == 2:
                        k.barrier()
                        return

                    def bulk(gi):
                        c0 = gi * G
                        gcols = slice(c0 * 64, (c0 + G) * 64)
                        csl = [slice((c0 + g) * 64, (c0 + g + 1) * 64) for g in range(G)]
                        kg, kgB = r_kg.next()
                        kd, kdB = r_kdec.next()
                        for g in range(G):
                            k.op('tensor', lambda e, g=g: e.transpose(pb[0:64, g * 256:g * 256 + 128], kT[:, csl[g]], identb[:]), reads=[qkvB[1], cB], writes=[pbB])
                            k.op('tensor', lambda e, g=g: e.transpose(pb[0:64, g * 256 + 128:g * 256 + 256], vT[:, csl[g]], identb[:]), reads=[qkvB[2], cB], writes=[pbB])
                        for g in range(G):
                            c = c0 + g
                            k.op('vector', lambda e, g=g, c=c: e.tensor_scalar_mul(kg[:, g, 128:256], pb[0:64, g * 256:g * 256 + 128], gl[:, 3, c:c + 1]), reads=[pbB, glB], writes=[kgB])
                            k.op('vector', lambda e, g=g, c=c: e.tensor_scalar_mul(kd[:, g, :], pb[0:64, g * 256:g * 256 + 128], gl[:, 4, c:c + 1]), reads=[pbB, glB], writes=[kdB])
                        k.op('scalar', lambda e: e.copy(kg[:, :, 0:128], pb[0:64, :].rearrange("p (g c) -> p g c", g=G)[:, :, 128:256]), reads=[pbB], writes=[kgB])
                        p0, p0B = ps[0], psB[0]
                        for g in range(G):
                            k.op('tensor', lambda e, g=g: e.matmul(p0[0:64, g * 128:g * 128 + 64], kT[:, csl[g]], kT[:, csl[g]], start=True, stop=True), reads=[qkvB[1]], writes=[p0B])
                            k.op('tensor', lambda e, g=g: e.matmul(p0[0:64, g * 128 + 64:g * 128 + 128], kT[:, csl[g]], qT[:, csl[g]], start=True, stop=True), reads=[qkvB[1], qkvB[0]], writes=[p0B])
                        p0v = p0[0:64, :].rearrange("p (g c) -> p g c", g=G)
                        dg, dgB = r_dg.next()
                        for g in range(G):
                            c = c0 + g
                            k.op('gpsimd', lambda e, g=g, c=c: e.tensor_scalar_mul(dg[:, g, :], ident[0:64, 0:64], gl[:, 2, c:c + 1]), reads=[glB, cB], writes=[dgB])
                        p1, p1B = ps[1], psB[1]
                        for g in range(G):
                            k.op('tensor', lambda e, g=g: e.matmul(p1[:, g * 64:(g + 1) * 64], ones_f[0:64, :], dg[:, g, :], start=True, stop=True), reads=[dgB, cB], writes=[p1B])
                        dm, dmB = r_dm.next()
                        for g in range(G):
                            c = c0 + g
                            k.op('vector', lambda e, g=g, c=c: e.tensor_scalar(dm[:, g, :], p1[0:64, g * 64:(g + 1) * 64], gl[:, 2, c:c + 1], 0.0, ALU.subtract, ALU.min), reads=[p1B, glB], writes=[dmB])
                        egb, egbB = r_egb.next()
                        k.op('scalar', lambda e: e.activation(egb[:].rearrange("p g c -> p (g c)"), p1[:, 0:G * 64], AF.Exp), reads=[p1B], writes=[egbB])
                        k.op('scalar', lambda e: e.activation(dm[:], dm[:], AF.Exp), reads=[dmB], writes=[dmB])
                        k.op('vector', lambda e: e.tensor_tensor(dm[:], dm[:], triu4[:], ALU.mult), reads=[dmB, cB], writes=[dmB])
                        qg, qgB = r_qg.next()
                        k.op('gpsimd', lambda e: e.tensor_tensor(qg[:].rearrange("p g c -> p (g c)"), qT[:, gcols], egb[:].rearrange("p g c -> p (g c)"), ALU.mult), reads=[qkvB[0], egbB], writes=[qgB])
                        at, atB = r_at.next()
                        k.op('vector', lambda e: e.tensor_tensor(at[:], p0v[:, :, 64:128], dm[:], ALU.mult), reads=[p0B, dmB], writes=[atB])
                        k.op('vector', lambda e: e.tensor_tensor(dm[:], dm[:], ident4[:], ALU.subtract), reads=[dmB, cB], writes=[dmB])
                        X, XB = r_X.next()
                        for g in range(G):
                            c = c0 + g
                            k.op('vector', lambda e, X=X, g=g, c=c: e.scalar_tensor_tensor(X[:, g, :], p0[0:64, g * 128:g * 128 + 64], gl[:, 5, c:c + 1], dm[:, g, :], ALU.mult, ALU.mult),
                                 reads=[p0B, glB, dmB], writes=[XB])
                        p2, p2B = ps[2], psB[2]
                        p3, p3B = ps[3], psB[3]
                        p4, p4B = ps[4], psB[4]
                        for g in range(G):
                            k.op('tensor', lambda e, X=X, g=g: e.transpose(p3[0:64, g * 64:(g + 1) * 64], X[:, g, :], ident[0:64, 0:64]), reads=[XB, cB], writes=[p3B])
                        Y, YB = r_Y.next()
                        k.op('scalar', lambda e, Y=Y: e.copy(Y[:].rearrange("p g c -> p (g c)"), p3[0:64, 0:G * 64]), reads=[p3B], writes=[YB])
                        P, PB = r_P.next()
                        k.op('vector', lambda e, X=X: e.tensor_tensor(P[:], X[:], ident4[:], ALU.add), reads=[XB, cB], writes=[PB])
                        for s in range(1, 6):
                            if s < 5:
                                for g in range(G):
                                    k.op('tensor', lambda e, X=X, Y=Y, g=g: e.matmul(p2[0:64, g * 64:(g + 1) * 64], Y[:, g, :], X[:, g, :], start=True, stop=True), reads=[XB, YB], writes=[p2B])
                            for g in range(G):
                                k.op('tensor', lambda e, X=X, Y=Y, g=g: e.matmul(p3[0:64, g * 64:(g + 1) * 64], X[:, g, :], Y[:, g, :], start=True, stop=True), reads=[XB, YB], writes=[p3B])
                            Yn, YnB = r_Y.next()
                            k.op('scalar', lambda e, Yn=Yn: e.copy(Yn[:].rearrange("p g c -> p (g c)"), p3[0:64, 0:G * 64]), reads=[p3B], writes=[YnB])
                            if s < 5:
                                Xn, XnB = r_X.next()
                                k.op('vector', lambda e, Xn=Xn: e.tensor_copy(Xn[:].rearrange("p g c -> p (g c)"), p2[0:64, 0:G * 64]), reads=[p2B], writes=[XnB])
                            for g in range(G):
                                k.op('tensor', lambda e, Yn=Yn, g=g: e.matmul(p4[0:64, g * 64:(g + 1) * 64], Yn[:, g, :], P[:, g, :], start=True, stop=True), reads=[YnB, PB], writes=[p4B])
                            k.op('vector', lambda e: e.tensor_tensor(P[:].rearrange("p g c -> p (g c)"), P[:].rearrange("p g c -> p (g c)"), p4[0:64, 0:G * 64], ALU.add), reads=[p4B, PB], writes=[PB])
                            Y, YB = Yn, YnB
                            if s < 5:
                                X, XB = Xn, XnB
                        uw, uwB = r_uw.next()
                        for rr in range(G // 2):
                            for g2 in range(2):
                                g = rr * 2 + g2
                                k.op('tensor', lambda e, g=g, g2=g2: e.matmul(p4[0:64, g2 * 256:(g2 + 1) * 256], P[:, g, :], kg[:, g, :], start=True, stop=True), reads=[PB, kgB], writes=[p4B])
                            for g2 in range(2):
                                g = rr * 2 + g2
                                c = c0 + g
                                k.op('vector', lambda e, g=g, g2=g2, c=c: e.tensor_scalar_mul(uw[:, g, :], p4[0:64, g2 * 256:(g2 + 1) * 256], gl[:, 0, c:c + 1]), reads=[p4B, glB], writes=[uwB])
                        for g in range(G):
                            k.op('tensor', lambda e, g=g: e.transpose(p1[:, g * 64:(g + 1) * 64], uw[:, g, 128:256], ident[0:64, 0:64]), reads=[uwB, cB], writes=[p1B])
                        wT, wTB = r_wT.next()
                        k.op('vector', lambda e: e.tensor_copy(wT[:].rearrange("p g c -> p (g c)"), p1[:, 0:G * 64]), reads=[p1B], writes=[wTB])
                        return dict(uw=(uw, uwB), wT=(wT, wTB), qg=(qg, qgB), at=(at, atB), kd=(kd, kdB))

                    def scan(c, r, g):
                        uw_, uwB = r['uw']
                        wT_, wTB = r['wT']
                        qg_, qgB = r['qg']
                        at_, atB = r['at']
                        kd_, kdB = r['kd']
                        uw, wT, qg, at, kd = uw_[:, g, :], wT_[:, g, :], qg_[:, g, :], at_[:, g, :], kd_[:, g, :]
                        p5, p5B = ps[5], psB[5]
                        k.op('tensor', lambda e: e.matmul(p5[0:64, 0:128], wT, S[:], start=True, stop=True), reads=[wTB, SB], writes=[p5B])
                        vn, vnB = r_vn.next()
                        k.op('vector', lambda e: e.tensor_tensor(vn[:], uw[:, 0:128], p5[0:64, 0:128], ALU.subtract), reads=[uwB, p5B], writes=[vnB])
                        k.op('tensor', lambda e: e.matmul(p5[0:64, 128:256], qg, S[:], start=True, stop=False), reads=[qgB, SB], writes=[p5B])
                        k.op('tensor', lambda e: e.matmul(p5[0:64, 128:256], at, vn[:], start=False, stop=True), reads=[atB, vnB], writes=[p5B])
                        p6, p6B = ps[6], psB[6]
                        k.op('tensor', lambda e: e.matmul(p6[:, 0:128], kd, vn[:], start=True, stop=True), reads=[kdB, vnB], writes=[p6B])
                        k.op('vector', lambda e: e.scalar_tensor_tensor(S[:], S[:], egl[:, c:c + 1], p6[:, 0:128], ALU.mult, ALU.add),
                             reads=[SB, eglB, p6B], writes=[SB])
                        o, oB = r_o.next()
                        st, stB = r_st.next()
                        jk, jkB = r_jk.next()
                        k.op('scalar', lambda e: e.copy(o[:], p5[0:64, 128:256]), reads=[p5B], writes=[oB])
                        if h == 0 and l == 0 and c <= 1:
                            dump(f"vn{c}", vn[:], vnB)
                            dump(f"o{c}", o[:], oB)
                            dump(f"S{c}", S[:], SB)
                        k.op('scalar', lambda e: e.activation(jk[:], o[:], AF.Square, accum_out=st[:, 0:1]), reads=[oB], writes=[jkB, stB])
                        k.op('scalar', lambda e: e.activation(st[:, 1:2], st[:, 0:1], AF.Sqrt, bias=RMS_EPS, scale=1.0 / 128), reads=[stB], writes=[stB])
                        k.op('vector', lambda e: e.reciprocal(st[:, 2:3], st[:, 1:2]), reads=[stB], writes=[stB])
                        k.op('vector', lambda e: e.scalar_tensor_tensor(o[:], o[:], st[:, 2:3], nw[0:64, :], ALU.mult, ALU.mult), reads=[oB, stB, nwB], writes=[oB])
                        ob, obB = r_ob.next()
                        k.op('gpsimd', lambda e: e.tensor_tensor(ob[:], o[:], zs[:, c, :], ALU.mult), reads=[oB, zsB], writes=[obB])
                        k.dma('sync', om_d[c * 64:(c + 1) * 64, h * 128:(h + 1) * 128], ob[:], reads=[obB])

                    assert NCH % G == 0
                    pend = bulk(0)
                    for gi in range(NCH // G):
                        nxt = bulk(gi + 1) if gi + 1 < NCH // G else None
                        for g in range(G):
                            scan(gi * G + g, pend, g)
                        pend = nxt
                k.barrier()

        def shared_kv():
            with ExitStack() as es:
                hbr = Ring(es, nc, "khb", [128, 8, 512], BF16, 2)
                kr = Ring(es, nc, "kst", [64, 512], BF16, 3)
                vr = Ring(es, nc, "vst", [128, 129], BF16, 3)
                for h in range(NH):
                    wk, wkB = load_w_bf16(es, f"wk{h}", kv_w[:, h * 128:(h + 1) * 128], 128) if h == 0 else (wk_keep, wkB_keep)
                    wv, wvB = load_w_bf16(es, f"wv{h}", kv_w[:, 1024 + h * 128:1024 + (h + 1) * 128], 128) if h == 0 else (wv_keep, wvB_keep)
                    if h == 0:
                        wk_keep, wkB_keep, wv_keep, wvB_keep = wk, wkB, wv, wvB
                    else:
                        k.dma('gpsimd', wk[:], kv_w[:, h * 128:(h + 1) * 128].rearrange("(k p) c -> p k c", p=128), writes=[wkB])
                        k.dma('gpsimd', wv[:], kv_w[:, 1024 + h * 128:1024 + (h + 1) * 128].rearrange("(k p) c -> p k c", p=128), writes=[wvB])
                    for blk in range(NB):
                        hb, hbB = hbr.next()
                        k.dma('sync', hb[:], hT_v[:, :, blk * 512:(blk + 1) * 512], writes=[hbB])
                        for s in range(2):
                            pp, ppB = ps[s], psB[s]
                            for kc in range(8):
                                k.op('tensor', lambda e, pp=pp, kc=kc, s=s, hb=hb: e.matmul(pp[0:64, :], wk[:, kc, s * 64:(s + 1) * 64], hb[:, kc, :],
                                                                                     start=(kc == 0), stop=(kc == 7)), reads=[wkB, hbB], writes=[ppB])
                            kt, ktB = kr.next()
                            k.op('scalar' if s else 'vector', (lambda e, kt=kt, pp=pp: e.copy(kt[:], pp[0:64, :])) if s else
                                 (lambda e, kt=kt, pp=pp: e.tensor_copy(kt[:], pp[0:64, :])), reads=[ppB], writes=[ktB])
                            k.dma('sync', kT_d[h, s, :, blk * 512:(blk + 1) * 512], kt[:], reads=[ktB])
                        for tt in range(4):
                            pp, ppB = ps[2 + tt % 2], psB[2 + tt % 2]
                            for kc in range(8):
                                k.op('tensor', lambda e, pp=pp, kc=kc, tt=tt, hb=hb: e.matmul(pp[:, 0:128], hb[:, kc, tt * 128:(tt + 1) * 128], wv[:, kc, :],
                                                                                       start=(kc == 0), stop=(kc == 7)), reads=[wvB, hbB], writes=[ppB])
                            vt, vtB = vr.next()
                            k.op('vector', lambda e, vt=vt, pp=pp: e.tensor_copy(vt[:, 0:128], pp[:, 0:128]), reads=[ppB], writes=[vtB])
                            k.op('gpsimd', lambda e, vt=vt: e.memset(vt[:, 128:129], 1.0), writes=[vtB])
                            tok0 = blk * 512 + tt * 128
                            k.dma('sync', va_d[h, tok0:tok0 + 128, :], vt[:], reads=[vtB])
                k.barrier()

        def diffattn(j, layer):
            lam_init = lambda_init(layer)
            with ExitStack() as es:
                lp, lpB = bcast_row(es, "lp", b_lambda[j:j + 1].rearrange("o a b -> o (a b)"), 256)
                sw, swB = bcast_row(es, "sw", b_subln_w[j:j + 1, :], 128)
                lam = sbt(es, "lam", [128, 8], F32)
                lamB = Buf("lam")
                pr = sbt(es, "lpr", [128, 128], F32)
                k.op('vector', lambda e: e.tensor_tensor(pr[:, 0:64], lp[:, 0:64], lp[:, 64:128], ALU.mult), reads=[lpB], writes=[lamB])
                k.op('vector', lambda e: e.tensor_tensor(pr[:, 64:128], lp[:, 128:192], lp[:, 192:256], ALU.mult), reads=[lpB], writes=[lamB])
                k.op('vector', lambda e: e.reduce_sum(lam[:, 0:1], pr[:, 0:64], AX.X), reads=[lamB], writes=[lamB])
                k.op('vector', lambda e: e.reduce_sum(lam[:, 1:2], pr[:, 64:128], AX.X), reads=[lamB], writes=[lamB])
                k.op('scalar', lambda e: e.activation(lam[:, 2:4], lam[:, 0:2], AF.Exp), reads=[lamB], writes=[lamB])
                k.op('vector', lambda e: e.tensor_tensor(lam[:, 4:5], lam[:, 2:3], lam[:, 3:4], ALU.subtract), reads=[lamB], writes=[lamB])
                k.op('vector', lambda e: e.tensor_scalar(lam[:, 5:6], lam[:, 4:5], lam_init, -1.0, ALU.add, ALU.mult), reads=[lamB], writes=[lamB])
                k.op('vector', lambda e: e.tensor_scalar_mul(sw[:], sw[:], 1.0 - lam_init), reads=[swB], writes=[swB])
                qs = [sbt(es, f"qs{s}", [64, T], BF16) for s in range(2)]
                qsB = [Buf("qs0"), Buf("qs1")]
                kTs = [sbt(es, f"kTs{s}", [64, T], BF16) for s in range(2)]
                kTB = [Buf("kT0"), Buf("kT1")]
                va = sbt(es, "va", [128, NT, 144], BF16)
                vaB = Buf("va")
                hbr = Ring(es, nc, "ahb", [128, 8, 512], BF16, 2)
                ptr = Ring(es, nc, "pt", [128, 512], BF16, 4)
                r_o1 = Ring(es, nc, "ao1", [128, 128], F32, 2)
                r_st = Ring(es, nc, "ast", [128, 8], F32, 2)
                r_jk = Ring(es, nc, "ajk", [128, 128], F32, 2)
                r_ob = Ring(es, nc, "aob", [128, 128], BF16, 2)
                wq = sbt(es, "wq", [128, 8, 128], BF16)
                wqB = Buf("wq")
                acc = {}
                slots = [(4, 0), (4, 144), (4, 288), (5, 0), (5, 144), (5, 288), (6, 0), (6, 144)]
                for s in range(2):
                    for i in range(4):
                        acc[(s, i)] = slots[s * 4 + i]
                for h in range(NH):
                    k.dma('gpsimd', wq[:], b_w_q[j, :, h * 128:(h + 1) * 128].rearrange("(k p) c -> p k c", p=128), writes=[wqB])
                    for s in range(2):
                        k.dma('sync', kTs[s][:], kT_d[h, s], writes=[kTB[s]])
                    k.dma('sync', va[:, :, 0:129], va_d[h].rearrange("(t p) c -> p t c", p=128), writes=[vaB])
                    for blk in range(NB):
                        hb, hbB = hbr.next()
                        k.dma('sync', hb[:], hT_v[:, :, blk * 512:(blk + 1) * 512], writes=[hbB])
                        for s in range(2):
                            pp, ppB = ps[s], psB[s]
                            for kc in range(8):
                                k.op('tensor', lambda e, pp=pp, kc=kc, s=s, hb=hb: e.matmul(pp[0:64, :], wq[:, kc, s * 64:(s + 1) * 64], hb[:, kc, :],
                                                                                     start=(kc == 0), stop=(kc == 7)), reads=[wqB, hbB], writes=[ppB])
                            k.op('scalar', lambda e, pp=pp, s=s, blk=blk: e.activation(qs[s][:, blk * 512:(blk + 1) * 512], pp[0:64, :], AF.Copy, scale=0.125),
                                 reads=[ppB], writes=[qsB[s]])
                    for qb in range(NB):
                        nkt = 4 * qb + 4
                        for kt in range(nkt):
                            jd = kt - 4 * qb
                            lo = 0 if jd < 0 else jd * 128
                            n = 512 - lo
                            for s in range(2):
                                pp, ppB = ps[(kt * 2 + s) % 4], psB[(kt * 2 + s) % 4]
                                k.op('tensor', lambda e, pp=pp, s=s, kt=kt, qb=qb, lo=lo, n=n: e.matmul(pp[:, 0:n], kTs[s][:, kt * 128:(kt + 1) * 128],
                                                                                                 qs[s][:, qb * 512 + lo:(qb + 1) * 512], start=True, stop=True),
                                     reads=[kTB[s], qsB[s]], writes=[ppB])
                                pt, ptB = ptr.next()
                                k.op('scalar', lambda e, pt=pt, pp=pp, n=n: e.activation(pt[:, 0:n], pp[:, 0:n], AF.Exp), reads=[ppB], writes=[ptB])
                                if jd >= 0:
                                    k.op('vector', lambda e, pt=pt: e.tensor_tensor(pt[:, 0:128], pt[:, 0:128], triub[:], ALU.mult), reads=[ptB, cB], writes=[ptB])
                                for i in range(max(jd, 0), 4):
                                    bank, off = acc[(s, i)]
                                    c0 = i * 128 - lo
                                    st_flag = (kt == 0 and off == 0)
                                    k.op('tensor', lambda e, pt=pt, bank=bank, off=off, c0=c0, kt=kt, i=i, qb=qb, st_flag=st_flag: e.matmul(
                                        ps[bank][:, off:off + 129], pt[:, c0:c0 + 128], va[:, kt, 0:129], start=st_flag, stop=(kt == 4 * qb + i),
                                        skip_group_check=True),
                                        reads=[ptB, vaB], writes=[psB[bank]])
                        for i in range(4):
                            b1, f1 = acc[(0, i)]
                            b2, f2 = acc[(1, i)]
                            st, stB = r_st.next()
                            o1, o1B = r_o1.next()
                            jk, jkB = r_jk.next()
                            ob, obB = r_ob.next()
                            k.op('vector', lambda e, st=st, b1=b1, f1=f1: e.reciprocal(st[:, 0:1], ps[b1][:, f1 + 128:f1 + 129]), reads=[psB[b1]], writes=[stB])
                            k.op('vector', lambda e, st=st, b2=b2, f2=f2: e.reciprocal(st[:, 1:2], ps[b2][:, f2 + 128:f2 + 129]), reads=[psB[b2]], writes=[stB])
                            k.op('vector', lambda e, st=st: e.tensor_tensor(st[:, 2:3], st[:, 1:2], lam[:, 5:6], ALU.mult), reads=[stB, lamB], writes=[stB])
                            k.op('vector', lambda e, st=st, o1=o1, b1=b1, f1=f1: e.tensor_scalar_mul(o1[:], ps[b1][:, f1:f1 + 128], st[:, 0:1]),
                                 reads=[psB[b1], stB], writes=[o1B])
                            k.op('vector', lambda e, st=st, o1=o1, b2=b2, f2=f2: e.scalar_tensor_tensor(o1[:], ps[b2][:, f2:f2 + 128], st[:, 2:3], o1[:], ALU.mult, ALU.add),
                                 reads=[psB[b2], stB, o1B], writes=[o1B])
                            k.op('scalar', lambda e, st=st, o1=o1, jk=jk: e.activation(jk[:], o1[:], AF.Square, accum_out=st[:, 3:4]), reads=[o1B], writes=[jkB, stB])
                            k.op('scalar', lambda e, st=st: e.activation(st[:, 4:5], st[:, 3:4], AF.Sqrt, bias=RMS_EPS, scale=1.0 / 128), reads=[stB], writes=[stB])
                            k.op('vector', lambda e, st=st: e.reciprocal(st[:, 5:6], st[:, 4:5]), reads=[stB], writes=[stB])
                            k.op('vector', lambda e, st=st, o1=o1, ob=ob: e.scalar_tensor_tensor(ob[:], o1[:], st[:, 5:6], sw[:], ALU.mult, ALU.mult),
                                 reads=[o1B, stB, swB], writes=[obB])
                            t0 = qb * 512 + i * 128
                            k.dma('sync', om_d[t0:t0 + 128, h * 128:(h + 1) * 128], ob[:], reads=[obB])
                k.barrier()

        def tok_moe(layer, w_out_src, last):
            with ExitStack() as es:
                wo, woB = load_w_bf16(es, "wo", w_out_src, 1024)
                wr = sbt(es, "wr", [128, 8, 40], F32)
                wrB = Buf("wr")
                k.dma('sync', wr[:, :, 0:4], moe_w_group[layer].rearrange("(k p) c -> p k c", p=128), writes=[wrB])
                k.dma('sync', wr[:, :, 4:36], moe_w_expert[layer].rearrange("(k p) c -> p k c", p=128), writes=[wrB])
                g1, g1B = bcast_row(es, "lng1", ln_mix_g[layer:layer + 1, :], D)
                b1, b1B = bcast_row(es, "lnb1", ln_mix_b[layer:layer + 1, :], D)
                lnB1 = Buf("ln1")
                oh = [sbt(es, f"oh{i}", [128, NT, 32], F32) for i in range(2)]
                ohB = Buf("oh")
                gates = sbt(es, "gates", [128, NT, 2], F32)
                gB = Buf("gates")
                rings = {'hTs': Ring(es, nc, "hTs2", [128, 8, 128], BF16, 2), 'st': Ring(es, nc, "lst", [128, 8], F32, 2),
                         'junk': Ring(es, nc, "ljk", [128, D], F32, 1)}
                with ExitStack() as e1:
                    r_om = Ring(e1, nc, "om", [128, D], BF16, 2)
                    r_omT = Ring(e1, nc, "omT", [128, 8, 128], BF16, 2)
                    r_h = Ring(e1, nc, "hh", [128, D], F32, 2)
                    r_t = Ring(e1, nc, "tt", [128, D], F32, 2)
                    r_y = Ring(e1, nc, "yy", [128, D], F32, 2)
                    r_xb = Ring(e1, nc, "xb", [128, D], BF16, 2)
                    r_xT = Ring(e1, nc, "xT", [128, 8, 128], F32, 2)
                    r_rt = Ring(e1, nc, "rt", [128, 160], F32, 2)
                    for t in range(NT):
                        rows = slice(t * 128, (t + 1) * 128)
                        om, omB = r_om.next()
                        k.dma('sync', om[:], om_d[rows, :], writes=[omB])
                        hh, hhB = r_h.next()
                        k.dma('sync', hh[:], h_d[rows, :], writes=[hhB])
                        for kc in range(8):
                            k.op('tensor', lambda e, om=om, kc=kc: e.transpose(pb[:, kc * 128:(kc + 1) * 128], om[:, kc * 128:(kc + 1) * 128], identb[:]),
                                 reads=[omB, cB], writes=[pbB])
                        omT, omTB = r_omT.next()
                        k.op('vector', lambda e, omT=omT: e.tensor_copy(omT[:], pb[:].rearrange("p (a b) -> p a b", a=8)), reads=[pbB], writes=[omTB])
                        tt, ttB = r_t.next()
                        for half in range(2):
                            pp, ppB = ps[half], psB[half]
                            for kc in range(8):
                                k.op('tensor', lambda e, pp=pp, kc=kc, half=half, omT=omT: e.matmul(pp[:, :], omT[:, kc, :], wo[:, kc, half * 512:(half + 1) * 512],
                                                                                             start=(kc == 0), stop=(kc == 7)), reads=[omTB, woB], writes=[ppB])
                            k.op('vector', lambda e, pp=pp, half=half, tt=tt, hh=hh: e.scalar_tensor_tensor(tt[:, half * 512:(half + 1) * 512], hh[:, half * 512:(half + 1) * 512],
                                                                                                      ALPHA, pp[:, :], ALU.mult, ALU.add),
                                 reads=[ppB, hhB], writes=[ttB])
                        yy, yyB = r_y.next()
                        layer_norm(rings, tt, ttB, g1, b1, [g1B, b1B], yy, yyB)
                        k.dma('sync', h_d[rows, :], yy[:], reads=[yyB, hhB])
                        xb, xbB = r_xb.next()
                        k.op('scalar', lambda e, xb=xb, yy=yy: e.copy(xb[:], yy[:]), reads=[yyB], writes=[xbB])
                        k.dma('sync', xb_d[rows, :], xb[:], reads=[xbB])
                        xT, xTB = r_xT.next()
                        for half in range(2):
                            pp, ppB = ps[2 + half], psB[2 + half]
                            for jj in range(4):
                                kc = half * 4 + jj
                                k.op('tensor', lambda e, pp=pp, jj=jj, kc=kc, yy=yy: e.transpose(pp[:, jj * 128:(jj + 1) * 128], yy[:, kc * 128:(kc + 1) * 128], ident[:]),
                                     reads=[yyB, cB], writes=[ppB])
                            if half == 0:
                                k.op('vector', lambda e, pp=pp, xT=xT: e.tensor_copy(xT[:, 0:4, :], pp[:].rearrange("p (a b) -> p a b", a=4)), reads=[ppB], writes=[xTB])
                            else:
                                k.op('scalar', lambda e, pp=pp, xT=xT: e.copy(xT[:, 4:8, :], pp[:].rearrange("p (a b) -> p a b", a=4)), reads=[ppB], writes=[xTB])
                        p4, p4B = ps[4], psB[4]
                        for kc in range(8):
                            k.op('tensor', lambda e, kc=kc, xT=xT: e.matmul(p4[:, 0:36], xT[:, kc, :], wr[:, kc, 0:36], start=(kc == 0), stop=(kc == 7)),
                                 reads=[xTB, wrB], writes=[p4B])
                        rt, rtB = r_rt.next()
                        V = lambda fn, rt=rt, rtB=rtB, extra_r=(), extra_w=(): k.op('vector', fn, reads=[rtB] + list(extra_r), writes=[rtB] + list(extra_w))
                        k.op('vector', lambda e, rt=rt: e.tensor_copy(rt[:, 0:36], p4[:, 0:36]), reads=[p4B], writes=[rtB])
                        V(lambda e, rt=rt: e.reduce_max(rt[:, 36:37], rt[:, 0:4], AX.X))
                        V(lambda e, rt=rt: e.tensor_scalar_mul(rt[:, 37:38], rt[:, 36:37], -1.0))
                        k.op('scalar', lambda e, rt=rt: e.activation(rt[:, 84:88], rt[:, 0:4], AF.Exp, bias=rt[:, 37:38], accum_out=rt[:, 38:39]), reads=[rtB], writes=[rtB])
                        V(lambda e, rt=rt: e.reciprocal(rt[:, 39:40], rt[:, 38:39]))
                        V(lambda e, rt=rt: e.tensor_scalar(rt[:, 40:44], rt[:, 0:4], rt[:, 36:37], None, ALU.is_ge))
                        V(lambda e, rt=rt: e.tensor_scalar(rt[:, 40:44], rt[:, 40:44], -NEG, NEG, ALU.mult, ALU.add))
                        for g in range(4):
                            V(lambda e, rt=rt, g=g: e.tensor_scalar(rt[:, 44 + g * 8:52 + g * 8], rt[:, 4 + g * 8:12 + g * 8], rt[:, 40 + g:41 + g], None, ALU.add))
                        V(lambda e, rt=rt: e.reduce_max(rt[:, 76:77], rt[:, 44:76], AX.X))
                        V(lambda e, rt=rt, t=t: e.tensor_scalar(oh[0][:, t, :], rt[:, 44:76], rt[:, 76:77], None, ALU.is_ge), extra_w=[ohB])
                        V(lambda e, rt=rt, t=t: e.scalar_tensor_tensor(rt[:, 96:128], oh[0][:, t, :], NEG, rt[:, 44:76], ALU.mult, ALU.add), extra_r=[ohB])
                        V(lambda e, rt=rt: e.reduce_max(rt[:, 77:78], rt[:, 96:128], AX.X))
                        V(lambda e, rt=rt, t=t: e.tensor_scalar(oh[1][:, t, :], rt[:, 96:128], rt[:, 77:78], None, ALU.is_ge), extra_w=[ohB])
                        V(lambda e, rt=rt: e.tensor_tensor(rt[:, 78:79], rt[:, 77:78], rt[:, 76:77], ALU.subtract))
                        k.op('scalar', lambda e, rt=rt: e.activation(rt[:, 79:80], rt[:, 78:79], AF.Exp), reads=[rtB], writes=[rtB])
                        V(lambda e, rt=rt: e.tensor_scalar_add(rt[:, 80:81], rt[:, 79:80], 1.0))
                        V(lambda e, rt=rt: e.reciprocal(rt[:, 81:82], rt[:, 80:81]))
                        V(lambda e, rt=rt, t=t: e.tensor_tensor(gates[:, t, 0:1], rt[:, 39:40], rt[:, 81:82], ALU.mult), extra_w=[gB])
                        V(lambda e, rt=rt, t=t: e.tensor_tensor(gates[:, t, 1:2], rt[:, 39:40], gates[:, t, 0:1], ALU.subtract), extra_r=[gB], extra_w=[gB])
                    k.barrier()
                if checkpoint(noraise=True):
                    return True
                dest = sbt(es, "dest", [128, 2, NT], I32)
                destB = Buf("dest")
                with ExitStack() as e2:
                    selb = sbt(e2, "selb", [128, NT, 32], BF16)
                    cnt = sbt(e2, "cnt", [128, NT, 32], F32)
                    pref = sbt(e2, "pref", [128, NT, 32], F32)
                    slot = sbt(e2, "slot", [128, NT, 32], F32)
                    ebase = sbt(e2, "ebase", [128, 32], F32)
                    tmp = sbt(e2, "ptmp", [128, NT, 32], F32)
                    dfl = sbt(e2, "dfl", [128, 2, NT], F32)
                    pB = Buf("pos")
                    k.op('gpsimd', lambda e: e.iota(ebase[:], [[CAP, 32]], base=0, channel_multiplier=0, allow_small_or_imprecise_dtypes=True), writes=[pB])
                    k.op('vector', lambda e: e.tensor_tensor(selb[:], oh[0][:], oh[1][:], ALU.add), reads=[ohB], writes=[pB])
                    TPB = 16
                    for t0 in range(0, NT, TPB):
                        n = min(TPB, NT - t0)
                        k.op('tensor', lambda e, t0=t0, n=n: e.matmul(ps[0][:, 0:n * 32], ones_b[:], selb[:, t0:t0 + n, :].rearrange("p a b -> p (a b)"), start=True, stop=True),
                             reads=[pB, cB], writes=[psB[0]])
                        k.op('vector', lambda e, t0=t0, n=n: e.tensor_copy(cnt[:, t0:t0 + n, :].rearrange("p a b -> p (a b)"), ps[0][:, 0:n * 32]), reads=[psB[0]], writes=[pB])
                        k.op('tensor', lambda e, t0=t0, n=n: e.matmul(ps[1][:, 0:n * 32], sutri[:], selb[:, t0:t0 + n, :].rearrange("p a b -> p (a b)"), start=True, stop=True),
                             reads=[pB, cB], writes=[psB[1]])
                        k.op('vector', lambda e, t0=t0, n=n: e.tensor_copy(slot[:, t0:t0 + n, :].rearrange("p a b -> p (a b)"), ps[1][:, 0:n * 32]), reads=[psB[1]], writes=[pB])
                    k.op('vector', lambda e: e.memset(pref[:, 0, :], 0.0), reads=[pB], writes=[pB])
                    for t in range(1, NT):
                        k.op('vector', lambda e, t=t: e.tensor_tensor(pref[:, t, :], pref[:, t - 1, :], cnt[:, t - 1, :], ALU.add), reads=[pB], writes=[pB])
                    k.op('vector', lambda e: e.tensor_tensor(slot[:], slot[:], pref[:], ALU.add), reads=[pB], writes=[pB])
                    k.op('vector', lambda e: e.tensor_scalar(tmp[:], slot[:], float(CAP), 1.0e6, ALU.is_ge, ALU.mult), reads=[pB], writes=[pB])
                    k.op('vector', lambda e: e.tensor_tensor(slot[:], slot[:], tmp[:], ALU.add), reads=[pB], writes=[pB])
                    for t in range(NT):
                        k.op('gpsimd', lambda e, t=t: e.tensor_tensor(slot[:, t, :], slot[:, t, :], ebase[:], ALU.add), reads=[pB], writes=[pB])
                    for i in range(2):
                        k.op('vector', lambda e, i=i: e.tensor_tensor(tmp[:], slot[:], oh[i][:], ALU.mult), reads=[pB, ohB], writes=[pB])
                        k.op('vector', lambda e, i=i: e.reduce_sum(dfl[:, i, :], tmp[:], AX.X), reads=[pB], writes=[pB])
                    k.op('vector', lambda e: e.tensor_scalar_min(dfl[:], dfl[:], float(NS)), reads=[pB], writes=[pB])
                    k.op('vector', lambda e: e.tensor_copy(dest[:], dfl[:]), reads=[pB], writes=[destB])
                    r_xb2 = Ring(e2, nc, "xb2", [128, D], BF16, 3)
                    for t in range(NT):
                        xb, xbB = r_xb2.next()
                        k.dma('sync', xb[:], xb_d[t * 128:(t + 1) * 128, :], writes=[xbB])
                        for i in range(2):
                            k.op('gpsimd', lambda e, xb=xb, i=i, t=t: e.indirect_dma_start(
                                out=xs_d, out_offset=bass.IndirectOffsetOnAxis(ap=dest[:, i, t:t + 1], axis=0), in_=xb[:], in_offset=None),
                                reads=[xbB, destB], dma=True)
                    k.barrier()
                with ExitStack() as e3:
                    NG = (CAP + 127) // 128
                    r_w13 = Ring(e3, nc, "w13", [128, 8, 1024], BF16, 2)
                    r_w2 = Ring(e3, nc, "w2", [128, 4, 1024], BF16, 2)
                    r_xs = Ring(e3, nc, "xs", [128, NG, D], BF16, 2)
                    r_xsT = Ring(e3, nc, "xsT", [128, 8, CAP], BF16, 2)
                    r_sg = Ring(e3, nc, "sg", [128, 4, CAP], F32, 1)
                    r_hid = Ring(e3, nc, "hid", [128, 4, CAP], BF16, 2)
                    r_ys = Ring(e3, nc, "ys", [128, D], F32, 2)
                    for ex in range(32):
                        w13, w13B = r_w13.next()
                        k.dma('gpsimd', w13[:], moe_w13[layer, ex].rearrange("(k p) c -> p k c", p=128), writes=[w13B])
                        w2, w2B = r_w2.next()
                        k.dma('gpsimd', w2[:], moe_w2[layer, ex].rearrange("(k p) c -> p k c", p=128), writes=[w2B])
                        xs, xsB = r_xs.next()
                        xsT, xsTB = r_xsT.next()
                        for g in range(NG):
                            r0 = ex * CAP + g * 128
                            nr = min(128, CAP - g * 128)
                            k.dma('sync', xs[0:nr, g, :], xs_d[r0:r0 + nr, :], writes=[xsB])
                        for g in range(NG):
                            nr = min(128, CAP - g * 128)
                            for kc in range(8):
                                k.op('tensor', lambda e, xs=xs, g=g, kc=kc, nr=nr: e.transpose(pb[:, kc * 128:kc * 128 + nr], xs[0:nr, g, kc * 128:(kc + 1) * 128], identb[0:nr, 0:nr]),
                                     reads=[xsB, cB], writes=[pbB])
                            k.op('vector', lambda e, xsT=xsT, g=g, nr=nr: e.tensor_copy(xsT[:, :, g * 128:g * 128 + nr], pb[:].rearrange("p (a b) -> p a b", a=8)[:, :, 0:nr]),
                                 reads=[pbB], writes=[xsTB])
                        sg, sgB = r_sg.next()
                        hid, hidB = r_hid.next()
                        for n0 in range(0, CAP, 512):
                            n = min(512, CAP - n0)
                            for m in range(8):
                                pp, ppB = ps[m % 4], psB[m % 4]
                                for kc in range(8):
                                    k.op('tensor', lambda e, pp=pp, kc=kc, m=m, w13=w13, xsT=xsT, n0=n0, n=n: e.matmul(pp[:, 0:n], w13[:, kc, m * 128:(m + 1) * 128], xsT[:, kc, n0:n0 + n],
                                                                                                           start=(kc == 0), stop=(kc == 7)),
                                         reads=[w13B, xsTB], writes=[ppB])
                                if m < 4:
                                    k.op('scalar', lambda e, pp=pp, m=m, sg=sg, n0=n0, n=n: e.activation(sg[:, m, n0:n0 + n], pp[:, 0:n], AF.Silu), reads=[ppB], writes=[sgB])
                                else:
                                    k.op('vector', lambda e, pp=pp, m=m, sg=sg, hid=hid, n0=n0, n=n: e.tensor_tensor(hid[:, m - 4, n0:n0 + n], sg[:, m - 4, n0:n0 + n], pp[:, 0:n], ALU.mult),
                                         reads=[ppB, sgB], writes=[hidB])
                        for g in range(NG):
                            nr = min(128, CAP - g * 128)
                            ys, ysB = r_ys.next()
                            for half in range(2):
                                pp, ppB = ps[4 + half], psB[4 + half]
                                for f in range(4):
                                    k.op('tensor', lambda e, pp=pp, f=f, half=half, hid=hid, w2=w2, g=g, nr=nr: e.matmul(pp[0:nr, :], hid[:, f, g * 128:g * 128 + nr], w2[:, f, half * 512:(half + 1) * 512],
                                                                                                             start=(f == 0), stop=(f == 3)),
                                         reads=[hidB, w2B], writes=[ppB])
                                if half == 0:
                                    k.op('vector', lambda e, pp=pp, ys=ys, nr=nr: e.tensor_copy(ys[0:nr, 0:512], pp[0:nr, :]), reads=[ppB], writes=[ysB])
                                else:
                                    k.op('scalar', lambda e, pp=pp, ys=ys, nr=nr: e.copy(ys[0:nr, 512:1024], pp[0:nr, :]), reads=[ppB], writes=[ysB])
                            r0 = ex * CAP + g * 128
                            for hf in range(2):
                                k.dma('sync', ys_h[hf][r0:r0 + nr, :], ys[0:nr, hf * 512:(hf + 1) * 512], reads=[ysB])
                    k.barrier()
                with ExitStack() as e4:
                    g2, g2B = bcast_row(e4, "lng2", ln_ffn_g[layer:layer + 1, :], D)
                    b2, b2B = bcast_row(e4, "lnb2", ln_ffn_b[layer:layer + 1, :], D)
                    r_yq = [Ring(e4, nc, f"yq{q}", [128, 512], F32, 2) for q in range(4)]
                    r_h = Ring(e4, nc, "ch", [128, D], F32, 2)
                    r_o = Ring(e4, nc, "co", [128, D], F32, 2)
                    for t in range(NT):
                        rows = slice(t * 128, (t + 1) * 128)
                        hh, hhB = r_h.next()
                        k.dma('sync', hh[:], h_d[rows, :], writes=[hhB])
                        k.op('scalar', lambda e, hh=hh: e.mul(hh[:], hh[:], ALPHA), reads=[hhB], writes=[hhB])
                        for i in range(2):
                            for hf in range(2):
                                yq, yqB = r_yq[i * 2 + hf].next()
                                k.op('gpsimd', lambda e, yq=yq, i=i, t=t, hf=hf: e.indirect_dma_start(
                                    out=yq[:], out_offset=None, in_=ys_h[hf],
                                    in_offset=bass.IndirectOffsetOnAxis(ap=dest[:, i, t:t + 1], axis=0)), reads=[destB], writes=[yqB], dma=True)
                                k.op('vector', lambda e, hh=hh, yq=yq, t=t, i=i, hf=hf: e.scalar_tensor_tensor(
                                    hh[:, hf * 512:(hf + 1) * 512], yq[:], gates[:, t, i:i + 1], hh[:, hf * 512:(hf + 1) * 512], ALU.mult, ALU.add),
                                    reads=[hhB, yqB, gB], writes=[hhB])
                        oo, ooB = r_o.next()
                        layer_norm(rings, hh, hhB, g2, b2, [g2B, b2B], oo, ooB)
                        if last:
                            out_toks.append(k.dma('sync', out[rows, :], oo[:], reads=[ooB]))
                        else:
                            k.dma('sync', h_d[rows, :], oo[:], reads=[ooB, hhB])
                            make_hT(rings, oo, ooB, t)
                    k.barrier()

        try:
            checkpoint()
            for layer in range(4):
                if layer < 2:
                    deltanet(layer)
                    checkpoint()
                    if tok_moe(layer, a_w_out[layer], last=False):
                        raise _Stop()
                    checkpoint()
                else:
                    j = layer - 2
                    if j == 0:
                        shared_kv()
                    diffattn(j, layer)
                    checkpoint()
                    if tok_moe(layer, b_w_out[j], last=(layer == 3)):
                        raise _Stop()
                    checkpoint()
        except _Stop:
            out_toks.append(k.dma('sync', out, h_d))
            out_toks.append(k.dma('sync', dbg, om_d))
        k.wait_all('sync', out_toks)
        k.finish()
    return nc, k


SEQ = 8192
CAP_FULL = 640
_cache = {}


def kernel(**inputs):
    x = np.asarray(inputs['x'])
    B, T, _ = x.shape
    cap = CAP_FULL if T == SEQ else max(64, int(T / 16 + 6 * math.sqrt(T / 16) + 16) // 32 * 32 + 32)
    key = (T, cap)
    if key not in _cache:
        _cache[key] = build(T, cap)[0]
    nc = _cache[key]
    shared = {n: np.ascontiguousarray(np.asarray(v, dtype=np.float32)) for n, v in inputs.items() if n != 'x'}
    in_maps = []
    for b in range(B):
        m = dict(shared)
        m['x'] = np.ascontiguousarray(x[b])
        in_maps.append(m)
    res = run_bass_kernel_spmd(nc, in_maps, core_ids=list(range(B)))
    return np.stack([np.asarray(res.results[b]['out']) for b in range(B)], axis=0).astype(np.float32)
```

```python
import math
from contextlib import ExitStack
import numpy as np
import concourse.bass as bass
import concourse.mybir as mybir
from concourse.bass_utils import run_bass_kernel_spmd

F32 = mybir.dt.float32
BF16 = mybir.dt.bfloat16
I32 = mybir.dt.int32
AF = mybir.ActivationFunctionType
ALU = mybir.AluOpType
AX = mybir.AxisListType

ENGS = ['tensor', 'vector', 'scalar', 'gpsimd', 'sync']
SEM_EPOCH = 30000
N_DMA_SEMS = 16


class Buf:
    __slots__ = ('name', 'w', 'r', 'excl')

    def __init__(self, name='', excl=False):
        self.name = name
        self.excl = excl
        self.w = None
        self.r = {}


class _Op:
    __slots__ = ('fn', 'waits', 'inc', 'incval', 'dma')

    def __init__(self, fn, waits, dma):
        self.fn = fn
        self.waits = waits
        self.inc = False
        self.incval = 0
        self.dma = dma


class K:
    def __init__(self, nc):
        self.nc = nc
        self.ops = {e: [] for e in ENGS}
        self.waited = {e: {} for e in ENGS}
        self.dma_rr = {e: 0 for e in ENGS}
        self.dma_cnt = {}

    def _need_wait(self, eng, t):
        key = (t[0], t[1])
        if self.waited[eng].get(key, -1) >= t[2]:
            return False
        self.waited[eng][key] = t[2]
        if t[0] == 'e':
            self.ops[t[1]][t[2]].inc = True
        return True

    def op(self, eng, fn, reads=(), writes=(), dma=False):
        idx = len(self.ops[eng])
        writes = list(writes) + [b for b in reads if b.excl]
        reads = [b for b in reads if not b.excl]
        deps = []
        for b in reads:
            if b.w is not None:
                deps.append(b.w)
        for b in writes:
            if b.w is not None:
                deps.append(b.w)
            deps.extend(b.r.values())
        waits = []
        for t in deps:
            if t[0] == 'e' and t[1] == eng and eng == 'tensor':
                continue
            if self._need_wait(eng, t):
                waits.append(t)
        dm = None
        if dma:
            slot = self.dma_rr[eng]
            self.dma_rr[eng] = (slot + 1) % N_DMA_SEMS
            cnt = self.dma_cnt.get((eng, slot), 0) + 1
            self.dma_cnt[(eng, slot)] = cnt
            dm = ((eng, slot), cnt * 16)
            if cnt > 1:
                t = ('d', (eng, slot), (cnt - 1) * 16)
                if self._need_wait(eng, t):
                    waits.append(t)
            tok = ('d', (eng, slot), cnt * 16)
        else:
            tok = ('e', eng, idx)
        self.ops[eng].append(_Op(fn, waits, dm))
        kk = (tok[0], tok[1])
        for b in reads:
            b.r[kk] = tok
        for b in writes:
            b.w = tok
            b.r = {}
        return tok

    def dma(self, eng, out, in_, reads=(), writes=(), **kw):
        return self.op(eng, lambda e: e.dma_start(out=out, in_=in_, **kw), reads=reads, writes=writes, dma=True)

    def wait_all(self, eng, tokens):
        waits = [t for t in tokens if self._need_wait(eng, t)]
        if waits:
            self.ops[eng].append(_Op(None, waits, None))

    def barrier(self):
        toks = []
        for e in ENGS:
            for i in range(len(self.ops[e]) - 1, -1, -1):
                o = self.ops[e][i]
                if o.fn is not None and o.dma is None:
                    toks.append(('e', e, i))
                    break
        for key, cnt in self.dma_cnt.items():
            toks.append(('d', key, cnt * 16))
        for e in ENGS:
            self.wait_all(e, toks)
        self.flush()

    def flush(self):
        nc = self.nc
        if not hasattr(self, 'flushed'):
            self.flushed = {e: 0 for e in ENGS}
            self.inccnt = {e: 0 for e in ENGS}
            self.esems = {e: [] for e in ENGS}
            self.dsems = {}
        start = dict(self.flushed)
        for e in ENGS:
            for o in self.ops[e][start[e]:]:
                if o.inc:
                    self.inccnt[e] += 1
                    o.incval = self.inccnt[e]
            need = (self.inccnt[e] + SEM_EPOCH - 1) // SEM_EPOCH
            while len(self.esems[e]) < max(need, 1):
                self.esems[e].append(nc.alloc_semaphore(f"es_{e}_{len(self.esems[e])}"))
        for key in self.dma_cnt:
            if key not in self.dsems:
                self.dsems[key] = nc.alloc_semaphore(f"ds_{key[0]}_{key[1]}")
        esems, dsems, ops = self.esems, self.dsems, self.ops

        def semval(e2, incval):
            return esems[e2][(incval - 1) // SEM_EPOCH], (incval - 1) % SEM_EPOCH + 1

        with nc.Block() as block:
            for eng in ENGS:
                todo = ops[eng][start[eng]:]

                def body(e, eng=eng, todo=todo):
                    for o in todo:
                        for t in o.waits:
                            if t[0] == 'e':
                                p = ops[t[1]][t[2]]
                                assert p.incval > 0, (eng, t)
                                s, v = semval(t[1], p.incval)
                                e.wait_ge(s, v)
                            else:
                                e.wait_ge(dsems[t[1]], t[2])
                        if o.fn is None:
                            continue
                        ins = o.fn(e)
                        if o.dma is not None:
                            ins.then_inc(dsems[o.dma[0]], 16)
                        elif o.inc:
                            s, v = semval(eng, o.incval)
                            ins.then_inc(s, 1)
                if todo:
                    getattr(block, eng)(body)
                self.flushed[eng] = len(ops[eng])

    def finish(self):
        self.flush()


_uid = [0]


def _un(name):
    _uid[0] += 1
    return f"{name}_u{_uid[0]}"


class Ring:
    def __init__(self, es, nc, name, shape, dt, n):
        self.t = [es.enter_context(nc.sbuf_tensor(_un(f"{name}_{i}"), shape, dt)) for i in range(n)]
        self.b = [Buf(f"{name}_{i}") for i in range(n)]
        self.i = 0

    def next(self):
        i = self.i
        self.i = (i + 1) % len(self.t)
        return self.t[i], self.b[i]


D = 1024
NH = 8
ALPHA = 8 ** 0.25
LN_EPS = 1e-5
RMS_EPS = 1e-6
NEG = -1.0e30


def lambda_init(layer_idx):
    return 0.8 - 0.6 * math.exp(-0.3 * layer_idx)


class _Stop(Exception):
    pass


def build(T, CAP, stop=0, ksub=0, dumpflag=False):
    NT = T // 128
    NCH = T // 64
    NB = T // 512
    nc = bass.Bass("TRN2", target_bir_lowering=False)

    def din(name, shape, dt=F32):
        return nc.dram_tensor(name, shape, dt, kind="ExternalInput").ap()

    x = din("x", [T, D])
    a_w_in = din("a_w_in", [2, D, 4112])
    a_conv_w = din("a_conv_w", [2, 4, 3072])
    a_a_log = din("a_a_log", [2, 8])
    a_dt_bias = din("a_dt_bias", [2, 8])
    a_norm_w = din("a_norm_w", [2, 128])
    a_w_out = din("a_w_out", [2, D, D])
    kv_w = din("kv_w", [D, 2048])
    b_w_q = din("b_w_q", [2, D, D])
    b_lambda = din("b_lambda", [2, 4, 64])
    b_subln_w = din("b_subln_w", [2, 128])
    b_w_out = din("b_w_out", [2, D, D])
    ln_mix_g = din("ln_mix_g", [4, D])
    ln_mix_b = din("ln_mix_b", [4, D])
    ln_ffn_g = din("ln_ffn_g", [4, D])
    ln_ffn_b = din("ln_ffn_b", [4, D])
    moe_w_group = din("moe_w_group", [4, D, 4])
    moe_w_expert = din("moe_w_expert", [4, D, 32])
    moe_w13 = din("moe_w13", [4, 32, D, 1024])
    moe_w2 = din("moe_w2", [4, 32, 512, D])
    out = nc.dram_tensor("out", [T, D], F32, kind="ExternalOutput").ap()
    dbg = nc.dram_tensor("dbg", [T, D], BF16, kind="ExternalOutput").ap() if stop else None
    stage = [0]

    def checkpoint(noraise=False):
        stage[0] += 1
        if stop and stage[0] >= stop:
            if noraise:
                return True
            raise _Stop()
        return False

    h_d = nc.dram_tensor("h_d", [T, D], F32).ap()
    hT_d = nc.dram_tensor("hT_d", [D, T], BF16).ap()
    om_d = nc.dram_tensor("om_d", [T, D], BF16).ap()
    xb_d = nc.dram_tensor("xb_d", [T, D], BF16).ap()
    kT_d = nc.dram_tensor("kT_d", [NH, 2, 64, T], BF16).ap()
    va_d = nc.dram_tensor("va_d", [NH, T, 129], BF16).ap()
    NS = 32 * CAP
    xs_d = nc.dram_tensor("xs_d", [NS + 128, D], BF16).ap()
    ys_h = [nc.dram_tensor(f"ys_d{i}", [NS + 128, 512], F32).ap() for i in range(2)]
    hT_v = hT_d.rearrange("(k p) t -> p k t", p=128)

    k = K(nc)
    out_toks = []
    dumped = set()

    def dump(name, ap, B):
        if not dumpflag or name in dumped:
            return
        dumped.add(name)
        t = nc.dram_tensor("dump_" + name, list(ap.shape), F32, kind="ExternalOutput").ap()
        out_toks.append(k.dma('gpsimd', t, ap, reads=[B]))
    with ExitStack() as top:
        def sbt(es, name, shape, dt):
            return es.enter_context(nc.sbuf_tensor(_un(name), shape, dt))

        ps = [top.enter_context(nc.psum_tensor(f"ps{i}", [128, 512], F32)) for i in range(7)]
        psB = [Buf(f"ps{i}", excl=True) for i in range(7)]
        pb = top.enter_context(nc.psum_tensor("pb", [128, 1024], BF16))
        pbB = Buf("pb", excl=True)

        ident = sbt(top, "ident", [128, 128], F32)
        identb = sbt(top, "identb", [128, 128], BF16)
        ones_f = sbt(top, "ones_f", [128, 128], F32)
        ones_b = sbt(top, "ones_b", [128, 128], BF16)
        triu = sbt(top, "triu", [128, 128], F32)
        triub = sbt(top, "triub", [128, 128], BF16)
        sutri = sbt(top, "sutri", [128, 128], BF16)
        zero_b = sbt(top, "zero_b", [128, 1024], BF16)
        triu4 = sbt(top, "triu4", [64, 4, 64], F32)
        ident4 = sbt(top, "ident4", [64, 4, 64], F32)
        cB = Buf("consts")
        k.op('gpsimd', lambda e: e.iota(ones_f[:], [[1, 128]], base=0, channel_multiplier=-1,
                                        allow_small_or_imprecise_dtypes=True), writes=[cB])
        k.op('vector', lambda e: e.tensor_single_scalar(ident[:], ones_f[:], 0.0, ALU.is_equal), reads=[cB], writes=[cB])
        k.op('vector', lambda e: e.tensor_single_scalar(identb[:], ones_f[:], 0.0, ALU.is_equal), reads=[cB], writes=[cB])
        k.op('vector', lambda e: e.tensor_single_scalar(triu[:], ones_f[:], 0.0, ALU.is_ge), reads=[cB], writes=[cB])
        k.op('vector', lambda e: e.tensor_single_scalar(triub[:], ones_f[:], 0.0, ALU.is_ge), reads=[cB], writes=[cB])
        k.op('vector', lambda e: e.tensor_single_scalar(sutri[:], ones_f[:], 0.0, ALU.is_gt), reads=[cB], writes=[cB])
        for g4 in range(4):
            k.op('vector', lambda e, g4=g4: e.tensor_copy(triu4[:, g4, :], triu[0:64, 0:64]), reads=[cB], writes=[cB])
            k.op('vector', lambda e, g4=g4: e.tensor_copy(ident4[:, g4, :], ident[0:64, 0:64]), reads=[cB], writes=[cB])
        k.op('vector', lambda e: e.memset(ones_f[:], 1.0), reads=[cB], writes=[cB])
        k.op('vector', lambda e: e.memset(ones_b[:], 1.0), writes=[cB])
        k.op('vector', lambda e: e.memset(zero_b[:], 0.0), writes=[cB])
        zero_f = sbt(top, "zero_f", [128, 512], F32)
        k.op('vector', lambda e: e.memset(zero_f[:], 0.0), writes=[cB])
        for r0 in range(0, NS + 128, 128):
            k.dma('sync', xs_d[r0:r0 + 128, :], zero_b[:], reads=[cB])
        for hf in range(2):
            k.dma('sync', ys_h[hf][NS:NS + 128, :], zero_f[:], reads=[cB])
        k.barrier()

        def layer_norm(es_ring, t, tB, gbc, bbc, wBs, y, yB):
            st, stB = es_ring['st'].next()
            jk, jkB = es_ring['junk'].next()
            k.op('scalar', lambda e: e.activation(jk[:], t[:], AF.Copy, accum_out=st[:, 0:1]), reads=[tB], writes=[jkB, stB])
            k.op('scalar', lambda e: e.activation(jk[:], t[:], AF.Square, accum_out=st[:, 1:2]), reads=[tB], writes=[jkB, stB])
            k.op('vector', lambda e: e.tensor_scalar_mul(st[:, 2:3], st[:, 0:1], 1.0 / D), reads=[stB], writes=[stB])
            k.op('vector', lambda e: e.tensor_tensor(st[:, 3:4], st[:, 2:3], st[:, 2:3], ALU.mult), reads=[stB], writes=[stB])
            k.op('vector', lambda e: e.scalar_tensor_tensor(st[:, 4:5], st[:, 1:2], 1.0 / D, st[:, 3:4], ALU.mult, ALU.subtract),
                 reads=[stB], writes=[stB])
            k.op('scalar', lambda e: e.activation(st[:, 5:6], st[:, 4:5], AF.Sqrt, bias=LN_EPS), reads=[stB], writes=[stB])
            k.op('vector', lambda e: e.reciprocal(st[:, 6:7], st[:, 5:6]), reads=[stB], writes=[stB])
            k.op('vector', lambda e: e.tensor_scalar(t[:], t[:], st[:, 2:3], st[:, 6:7], ALU.subtract, ALU.mult), reads=[tB, stB], writes=[tB])
            k.op('gpsimd', lambda e: e.tensor_tensor(t[:], t[:], gbc[:], ALU.mult), reads=[tB] + wBs, writes=[tB])
            k.op('vector', lambda e: e.tensor_tensor(y[:], t[:], bbc[:], ALU.add), reads=[tB] + wBs, writes=[yB])

        def make_hT(rings, y, yB, tile):
            hb, hbB = rings['hTs'].next()
            for half in range(2):
                pp, ppB = ps[5 + half], psB[5 + half]
                for j in range(4):
                    kk = half * 4 + j
                    k.op('tensor', lambda e, pp=pp, j=j, kk=kk: e.transpose(pp[:, j * 128:(j + 1) * 128], y[:, kk * 128:(kk + 1) * 128], ident[:]),
                         reads=[yB, cB], writes=[ppB])
                eng = 'vector' if half == 0 else 'scalar'
                if eng == 'vector':
                    k.op('vector', lambda e, pp=pp, half=half: e.tensor_copy(hb[:, half * 4:(half + 1) * 4, :], pp[:].rearrange("p (a b) -> p a b", a=4)),
                         reads=[ppB], writes=[hbB])
                else:
                    k.op('scalar', lambda e, pp=pp, half=half: e.copy(hb[:, half * 4:(half + 1) * 4, :], pp[:].rearrange("p (a b) -> p a b", a=4)),
                         reads=[ppB], writes=[hbB])
            k.dma('sync', hT_v[:, :, tile * 128:(tile + 1) * 128], hb[:], reads=[hbB])

        with ExitStack() as es:
            rings = {'hTs': Ring(es, nc, "hTs", [128, 8, 128], BF16, 2)}
            xr = Ring(es, nc, "xin", [128, D], F32, 2)
            for t in range(NT):
                xt, xB = xr.next()
                k.dma('sync', xt[:], x[t * 128:(t + 1) * 128, :], writes=[xB])
                k.dma('gpsimd', h_d[t * 128:(t + 1) * 128, :], xt[:], reads=[xB])
                make_hT(rings, xt, xB, t)
            k.barrier()

        def load_w_bf16(es, name, src, cols):
            w = sbt(es, name, [128, 8, cols], BF16)
            wB = Buf(name)
            k.dma('gpsimd', w[:], src.rearrange("(k p) c -> p k c", p=128), writes=[wB])
            return w, wB

        def bcast_row(es, name, src_row, n, dt=F32):
            w = sbt(es, name, [128, n], dt)
            wB = Buf(name)
            k.dma('sync', w[:], src_row.partition_broadcast(128), writes=[wB])
            return w, wB

        def deltanet(l):
            with ExitStack() as es:
                cw = sbt(es, "cw", [128, 24, 4], F32)
                cwB = Buf("cw")
                cwn = sbt(es, "cwn", [4, 3072], F32)
                k.dma('sync', cwn[:], a_conv_w[l], writes=[cwB])
                for part in range(24):
                    k.op('tensor', lambda e, part=part: e.transpose(ps[0][:, part * 4:(part + 1) * 4], cwn[:, part * 128:(part + 1) * 128], ident[0:4, 0:4]),
                         reads=[cwB, cB], writes=[psB[0]])
                k.op('vector', lambda e: e.tensor_copy(cw[:].rearrange("p a b -> p (a b)"), ps[0][:, 0:96]), reads=[psB[0]], writes=[cwB])
                nw, nwB = bcast_row(es, "nw", a_norm_w[l:l + 1, :], 128)
                alog, alB = bcast_row(es, "alog", a_a_log[l:l + 1, :], 8)
                dtb, dtB = bcast_row(es, "dtb", a_dt_bias[l:l + 1, :], 8)
                nea = sbt(es, "nea", [128, 8], F32)
                k.op('scalar', lambda e: e.activation(nea[:], alog[:], AF.Exp), reads=[alB], writes=[alB])
                k.op('vector', lambda e: e.tensor_scalar_mul(nea[:], nea[:], -1.0), reads=[alB], writes=[alB])
                qT = sbt(es, "qT", [128, T], BF16)
                kT = sbt(es, "kT", [128, T], BF16)
                vT = sbt(es, "vT", [128, T], BF16)
                qkvB = [Buf("qT"), Buf("kT"), Buf("vT")]
                qkv = [qT, kT, vT]
                zs = sbt(es, "zs", [64, NCH, 128], BF16)
                zsB = Buf("zs")
                gl = sbt(es, "gl", [64, 8, NCH], F32)
                glB = Buf("gl")
                egl = sbt(es, "egl", [128, NCH], F32)
                eglB = Buf("egl")
                S = sbt(es, "S", [128, 128], F32)
                SB = Buf("S")
                hbr = Ring(es, nc, "hblk", [128, 8, 512], BF16, 2)
                raw = sbt(es, "raw", [128, 3, 515], F32)
                rawB = [Buf("raw0"), Buf("raw1"), Buf("raw2")]
                cvr = Ring(es, nc, "cv", [128, 512], F32, 2)
                sqr = Ring(es, nc, "sq", [128, 512], F32, 2)
                G = 4
                R = 2
                r_kg = Ring(es, nc, "kg", [64, G, 256], F32, R)
                r_kdec = Ring(es, nc, "kdec", [64, G, 128], F32, R)
                r_dm = Ring(es, nc, "dm", [64, G, 64], F32, R)
                r_dg = Ring(es, nc, "dg", [64, G, 64], F32, R)
                r_egb = Ring(es, nc, "egb", [128, G, 64], F32, R)
                r_qg = Ring(es, nc, "qg", [128, G, 64], F32, R)
                r_at = Ring(es, nc, "at", [64, G, 64], F32, R)
                r_X = Ring(es, nc, "X", [64, G, 64], F32, 4)
                r_Y = Ring(es, nc, "Y", [64, G, 64], F32, 4)
                r_P = Ring(es, nc, "P", [64, G, 64], F32, R)
                r_uw = Ring(es, nc, "uw", [64, G, 256], F32, R)
                r_wT = Ring(es, nc, "wT", [128, G, 64], F32, R)
                r_vn = Ring(es, nc, "vn", [64, 128], F32, 2)
                r_o = Ring(es, nc, "o", [64, 128], F32, 2)
                r_ob = Ring(es, nc, "ob", [64, 128], BF16, 2)
                r_st = Ring(es, nc, "dst", [64, 4], F32, 2)
                r_jk = Ring(es, nc, "djk", [64, 128], F32, 2)
                for h in range(NH):
                    wq3, wq3B = [], []
                    wcat = sbt(es, f"wcat{h}", [128, 8, 384], BF16) if h == 0 else wcat_keep[0]
                    wzg = sbt(es, f"wzg{h}", [128, 8, 128], BF16) if h == 0 else wcat_keep[1]
                    if h == 0:
                        wcat_keep = [wcat, wzg]
                        wcB = Buf("wcat")
                        wzB = Buf("wzg")
                    for part in range(3):
                        k.dma('gpsimd', wcat[:, :, part * 128:(part + 1) * 128],
                              a_w_in[l, :, part * 1024 + h * 128: part * 1024 + (h + 1) * 128].rearrange("(k p) c -> p k c", p=128), writes=[wcB])
                    k.dma('gpsimd', wzg[:], a_w_in[l, :, 3072 + h * 128:3072 + (h + 1) * 128].rearrange("(k p) c -> p k c", p=128), writes=[wzB])
                    if h == 0:
                        wg16 = sbt(es, "wg16", [128, 8, 16], BF16)
                        k.dma('gpsimd', wg16[:], a_w_in[l, :, 4096:4112].rearrange("(k p) c -> p k c", p=128), writes=[wzB])
                    for part in range(3):
                        k.op('vector', lambda e, part=part: e.memset(raw[:, part, 0:3], 0.0), writes=[rawB[part]])
                    if ksub == 5:
                        k.barrier()
                        return
                    for blk in range(NB):
                        hb, hbB = hbr.next()
                        k.dma('sync', hb[:], hT_v[:, :, blk * 512:(blk + 1) * 512], writes=[hbB])
                        for part in range(3):
                            pp, ppB = ps[part % 2], psB[part % 2]
                            for kc in range(8):
                                k.op('tensor', lambda e, pp=pp, kc=kc, part=part, hb=hb: e.matmul(pp[:, :], wcat[:, kc, part * 128:(part + 1) * 128], hb[:, kc, :],
                                                                                            start=(kc == 0), stop=(kc == 7)),
                                     reads=[wcB, hbB], writes=[ppB])
                            k.op('scalar', lambda e, pp=pp, part=part: e.copy(raw[:, part, 3:515], pp[:, :]), reads=[ppB], writes=[rawB[part]])
                            if ksub == 11:
                                k.barrier()
                                return
                            cv, cvB = cvr.next()
                            ci = part * 8 + h
                            k.op('vector', lambda e, cv=cv, part=part, ci=ci: e.tensor_scalar_mul(cv[:], raw[:, part, 0:512], cw[:, ci, 0:1]),
                                 reads=[rawB[part], cwB], writes=[cvB])
                            for j in range(1, 4):
                                k.op('vector', lambda e, cv=cv, part=part, ci=ci, j=j: e.scalar_tensor_tensor(cv[:], raw[:, part, j:j + 512], cw[:, ci, j:j + 1], cv[:],
                                                                                                        ALU.mult, ALU.add),
                                     reads=[rawB[part], cwB, cvB], writes=[cvB])
                            k.op('vector', lambda e, part=part: e.tensor_copy(raw[:, part, 0:3], raw[:, part, 512:515]), reads=[rawB[part]], writes=[rawB[part]])
                            if ksub == 12:
                                k.barrier()
                                return
                            dst = qkv[part][:, blk * 512:(blk + 1) * 512]
                            if part == 2:
                                k.op('scalar', lambda e, cv=cv, dst=dst: e.activation(dst, cv[:], AF.Silu), reads=[cvB], writes=[qkvB[part]])
                            else:
                                k.op('scalar', lambda e, cv=cv: e.activation(cv[:], cv[:], AF.Silu), reads=[cvB], writes=[cvB])
                                sq, sqB = sqr.next()
                                k.op('gpsimd', lambda e, cv=cv, sq=sq: e.tensor_tensor(sq[:], cv[:], cv[:], ALU.mult), reads=[cvB], writes=[sqB])
                                p2, p2B = ps[2], psB[2]
                                for hf in range(2):
                                    k.op('tensor', lambda e, sq=sq, p2=p2, hf=hf: e.matmul(p2[:, hf * 256:(hf + 1) * 256], ones_f[:], sq[:, hf * 256:(hf + 1) * 256], start=True, stop=True), reads=[sqB, cB], writes=[p2B])
                                k.op('scalar', lambda e, sq=sq, p2=p2: e.activation(sq[:], p2[:, :], AF.Sqrt, bias=RMS_EPS), reads=[p2B], writes=[sqB])
                                k.op('vector', lambda e, sq=sq: e.reciprocal(sq[:], sq[:]), reads=[sqB], writes=[sqB])
                                sc = (128 ** -0.5) if part == 0 else 1.0
                                k.op('vector', lambda e, cv=cv, sq=sq, dst=dst, sc=sc: e.scalar_tensor_tensor(dst, cv[:], sc, sq[:], ALU.mult, ALU.mult),
                                     reads=[cvB, sqB], writes=[qkvB[part]])
                            if ksub == 13:
                                k.barrier()
                                return
                        if ksub == 14:
                            k.barrier()
                            return
                        for cc in range(8):
                            c = blk * 8 + cc
                            pp, ppB = ps[3 + cc % 2], psB[3 + cc % 2]
                            for kc in range(8):
                                k.op('tensor', lambda e, pp=pp, kc=kc, cc=cc, hb=hb: e.matmul(pp[0:64, 0:128], hb[:, kc, cc * 64:(cc + 1) * 64], wzg[:, kc, :],
                                                                                       start=(kc == 0), stop=(kc == 7)),
                                     reads=[wzB, hbB], writes=[ppB])
                            for kc in range(8):
                                k.op('tensor', lambda e, pp=pp, kc=kc, cc=cc, hb=hb: e.matmul(pp[0:64, 128:144], hb[:, kc, cc * 64:(cc + 1) * 64], wg16[:, kc, :],
                                                                                       start=(kc == 0), stop=(kc == 7)),
                                     reads=[wzB, hbB], writes=[ppB])
                            k.op('scalar', lambda e, pp=pp, c=c: e.activation(zs[:, c, :], pp[0:64, 0:128], AF.Silu), reads=[ppB], writes=[zsB])
                            k.op('vector', lambda e, pp=pp, c=c, h=h: e.tensor_copy(gl[:, 0, c:c + 1], pp[0:64, 128 + h:129 + h]), reads=[ppB], writes=[glB])
                            k.op('vector', lambda e, pp=pp, c=c, h=h: e.tensor_copy(gl[:, 1, c:c + 1], pp[0:64, 136 + h:137 + h]), reads=[ppB], writes=[glB])
                    if ksub == 1:
                        k.barrier()
                        return
                    k.op('scalar', lambda e: e.activation(gl[:, 0, :], gl[:, 0, :], AF.Sigmoid), reads=[glB], writes=[glB])
                    k.op('vector', lambda e: e.tensor_scalar_mul(gl[:, 5, :], gl[:, 0, :], -1.0), reads=[glB], writes=[glB])
                    k.op('scalar', lambda e, h=h: e.activation(gl[:, 1, :], gl[:, 1, :], AF.Exp, bias=dtb[0:64, h:h + 1]), reads=[glB, dtB], writes=[glB])
                    k.op('scalar', lambda e: e.activation(gl[:, 1, :], gl[:, 1, :], AF.Ln, bias=1.0), reads=[glB], writes=[glB])
                    k.op('vector', lambda e, h=h: e.tensor_scalar_mul(gl[:, 1, :], gl[:, 1, :], nea[0:64, h:h + 1]), reads=[glB, alB], writes=[glB])
                    for c0 in range(0, NCH, 512):
                        n = min(512, NCH - c0)
                        k.op('tensor', lambda e, c0=c0, n=n: e.matmul(ps[0][0:64, 0:n], triu[0:64, 0:64], gl[:, 1, c0:c0 + n], start=True, stop=True),
                             reads=[glB, cB], writes=[psB[0]])
                        k.op('vector', lambda e, c0=c0, n=n: e.tensor_copy(gl[:, 2, c0:c0 + n], ps[0][0:64, 0:n]), reads=[psB[0]], writes=[glB])
                        k.op('tensor', lambda e, c0=c0, n=n: e.matmul(ps[1][:, 0:n], ones_f[0:64, :], gl[:, 1, c0:c0 + n], start=True, stop=True),
                             reads=[glB, cB], writes=[psB[1]])
                        k.op('scalar', lambda e, c0=c0, n=n: e.activation(egl[:, c0:c0 + n], ps[1][:, 0:n], AF.Exp), reads=[psB[1]], writes=[eglB])
                        k.op('vector', lambda e, c0=c0, n=n: e.tensor_copy(gl[:, 6, c0:c0 + n], ps[1][0:64, 0:n]), reads=[psB[1]], writes=[glB])
                    k.op('scalar', lambda e: e.activation(gl[:, 3, :], gl[:, 2, :], AF.Exp), reads=[glB], writes=[glB])
                    k.op('vector', lambda e: e.tensor_tensor(gl[:, 7, :], gl[:, 6, :], gl[:, 2, :], ALU.subtract), reads=[glB], writes=[glB])
                    k.op('scalar', lambda e: e.activation(gl[:, 4, :], gl[:, 7, :], AF.Exp), reads=[glB], writes=[glB])
                    k.op('vector', lambda e: e.memset(S[:], 0.0), writes=[SB])
                    if h == 0 and l == 0:
                        dump("gl", gl[:], glB)
                        dump("egl", egl[:], eglB)
                        dump("qT", qT[:, 0:128], qkvB[0])
                        dump("kT", kT[:, 0:128], qkvB[1])
                        dump("vT", vT[:, 0:128], qkvB[2])
                        dump("zs", zs[:, 0:2, :], zsB)
                    if ksub == 2:
                        k.barrier()
                        return

                    def bulk(gi):
                        c0 = gi * G
                        gcols = slice(c0 * 64, (c0 + G) * 64)
                        csl = [slice((c0 + g) * 64, (c0 + g + 1) * 64) for g in range(G)]
                        kg, kgB = r_kg.next()
                        kd, kdB = r_kdec.next()
                        for g in range(G):
                            k.op('tensor', lambda e, g=g: e.transpose(pb[0:64, g * 256:g * 256 + 128], kT[:, csl[g]], identb[:]), reads=[qkvB[1], cB], writes=[pbB])
                            k.op('tensor', lambda e, g=g: e.transpose(pb[0:64, g * 256 + 128:g * 256 + 256], vT[:, csl[g]], identb[:]), reads=[qkvB[2], cB], writes=[pbB])
                        for g in range(G):
                            c = c0 + g
                            k.op('vector', lambda e, g=g, c=c: e.tensor_scalar_mul(kg[:, g, 128:256], pb[0:64, g * 256:g * 256 + 128], gl[:, 3, c:c + 1]), reads=[pbB, glB], writes=[kgB])
                            k.op('vector', lambda e, g=g, c=c: e.tensor_scalar_mul(kd[:, g, :], pb[0:64, g * 256:g * 256 + 128], gl[:, 4, c:c + 1]), reads=[pbB, glB], writes=[kdB])
                        k.op('scalar', lambda e: e.copy(kg[:, :, 0:128], pb[0:64, :].rearrange("p (g c) -> p g c", g=G)[:, :, 128:256]), reads=[pbB], writes=[kgB])
                        p0, p0B = ps[0], psB[0]
                        for g in range(G):
                            k.op('tensor', lambda e, g=g: e.matmul(p0[0:64, g * 128:g * 128 + 64], kT[:, csl[g]], kT[:, csl[g]], start=True, stop=True), reads=[qkvB[1]], writes=[p0B])
                            k.op('tensor', lambda e, g=g: e.matmul(p0[0:64, g * 128 + 64:g * 128 + 128], kT[:, csl[g]], qT[:, csl[g]], start=True, stop=True), reads=[qkvB[1], qkvB[0]], writes=[p0B])
                        p0v = p0[0:64, :].rearrange("p (g c) -> p g c", g=G)
                        dg, dgB = r_dg.next()
                        for g in range(G):
                            c = c0 + g
                            k.op('gpsimd', lambda e, g=g, c=c: e.tensor_scalar_mul(dg[:, g, :], ident[0:64, 0:64], gl[:, 2, c:c + 1]), reads=[glB, cB], writes=[dgB])
                        p1, p1B = ps[1], psB[1]
                        for g in range(G):
                            k.op('tensor', lambda e, g=g: e.matmul(p1[:, g * 64:(g + 1) * 64], ones_f[0:64, :], dg[:, g, :], start=True, stop=True), reads=[dgB, cB], writes=[p1B])
                        dm, dmB = r_dm.next()
                        for g in range(G):
                            c = c0 + g
                            k.op('vector', lambda e, g=g, c=c: e.tensor_scalar(dm[:, g, :], p1[0:64, g * 64:(g + 1) * 64], gl[:, 2, c:c + 1], 0.0, ALU.subtract, ALU.min), reads=[p1B, glB], writes=[dmB])
                        egb, egbB = r_egb.next()
                        k.op('scalar', lambda e: e.activation(egb[:].rearrange("p g c -> p (g c)"), p1[:, 0:G * 64], AF.Exp), reads=[p1B], writes=[egbB])
                        k.op('scalar', lambda e: e.activation(dm[:], dm[:], AF.Exp), reads=[dmB], writes=[dmB])
                        k.op('vector', lambda e: e.tensor_tensor(dm[:], dm[:], triu4[:], ALU.mult), reads=[dmB, cB], writes=[dmB])
                        qg, qgB = r_qg.next()
                        k.op('gpsimd', lambda e: e.tensor_tensor(qg[:].rearrange("p g c -> p (g c)"), qT[:, gcols], egb[:].rearrange("p g c -> p (g c)"), ALU.mult), reads=[qkvB[0], egbB], writes=[qgB])
                        at, atB = r_at.next()
                        k.op('vector', lambda e: e.tensor_tensor(at[:], p0v[:, :, 64:128], dm[:], ALU.mult), reads=[p0B, dmB], writes=[atB])
                        k.op('vector', lambda e: e.tensor_tensor(dm[:], dm[:], ident4[:], ALU.subtract), reads=[dmB, cB], writes=[dmB])
                        X, XB = r_X.next()
                        for g in range(G):
                            c = c0 + g
                            k.op('vector', lambda e, X=X, g=g, c=c: e.scalar_tensor_tensor(X[:, g, :], p0[0:64, g * 128:g * 128 + 64], gl[:, 5, c:c + 1], dm[:, g, :], ALU.mult, ALU.mult),
                                 reads=[p0B, glB, dmB], writes=[XB])
                        p2, p2B = ps[2], psB[2]
                        p3, p3B = ps[3], psB[3]
                        p4, p4B = ps[4], psB[4]
                        for g in range(G):
                            k.op('tensor', lambda e, X=X, g=g: e.transpose(p3[0:64, g * 64:(g + 1) * 64], X[:, g, :], ident[0:64, 0:64]), reads=[XB, cB], writes=[p3B])
                        Y, YB = r_Y.next()
                        k.op('scalar', lambda e, Y=Y: e.copy(Y[:].rearrange("p g c -> p (g c)"), p3[0:64, 0:G * 64]), reads=[p3B], writes=[YB])
                        P, PB = r_P.next()
                        k.op('vector', lambda e, X=X: e.tensor_tensor(P[:], X[:], ident4[:], ALU.add), reads=[XB, cB], writes=[PB])
                        for s in range(1, 6):
                            if s < 5:
                                for g in range(G):
                                    k.op('tensor', lambda e, X=X, Y=Y, g=g: e.matmul(p2[0:64, g * 64:(g + 1) * 64], Y[:, g, :], X[:, g, :], start=True, stop=True), reads=[XB, YB], writes=[p2B])
                            for g in range(G):
                                k.op('tensor', lambda e, X=X, Y=Y, g=g: e.matmul(p3[0:64, g * 64:(g + 1) * 64], X[:, g, :], Y[:, g, :], start=True, stop=True), reads=[XB, YB], writes=[p3B])
                            Yn, YnB = r_Y.next()
                            k.op('scalar', lambda e, Yn=Yn: e.copy(Yn[:].rearrange("p g c -> p (g c)"), p3[0:64, 0:G * 64]), reads=[p3B], writes=[YnB])
                            if s < 5:
                                Xn, XnB = r_X.next()
                                k.op('vector', lambda e, Xn=Xn: e.tensor_copy(Xn[:].rearrange("p g c -> p (g c)"), p2[0:64, 0:G * 64]), reads=[p2B], writes=[XnB])
                            for g in range(G):
                                k.op('tensor', lambda e, Yn=Yn, g=g: e.matmul(p4[0:64, g * 64:(g + 1) * 64], Yn[:, g, :], P[:, g, :], start=True, stop=True), reads=[YnB, PB], writes=[p4B])
                            k.op('vector', lambda e: e.tensor_tensor(P[:].rearrange("p g c -> p (g c)"), P[:].rearrange("p g c -> p (g c)"), p4[0:64, 0:G * 64], ALU.add), reads=[p4B, PB], writes=[PB])
                            Y, YB = Yn, YnB
                            if s < 5:
                                X, XB = Xn, XnB
                        uw, uwB = r_uw.next()
                        for rr in range(G // 2):
                            for g2 in range(2):
                                g = rr * 2 + g2
                                k.op('tensor', lambda e, g=g, g2=g2: e.matmul(p4[0:64, g2 * 256:(g2 + 1) * 256], P[:, g, :], kg[:, g, :], start=True, stop=True), reads=[PB, kgB], writes=[p4B])
                            for g2 in range(2):
                                g = rr * 2 + g2
                                c = c0 + g
                                k.op('vector', lambda e, g=g, g2=g2, c=c: e.tensor_scalar_mul(uw[:, g, :], p4[0:64, g2 * 256:(g2 + 1) * 256], gl[:, 0, c:c + 1]), reads=[p4B, glB], writes=[uwB])
                        for g in range(G):
                            k.op('tensor', lambda e, g=g: e.transpose(p1[:, g * 64:(g + 1) * 64], uw[:, g, 128:256], ident[0:64, 0:64]), reads=[uwB, cB], writes=[p1B])
                        wT, wTB = r_wT.next()
                        k.op('vector', lambda e: e.tensor_copy(wT[:].rearrange("p g c -> p (g c)"), p1[:, 0:G * 64]), reads=[p1B], writes=[wTB])
                        return dict(uw=(uw, uwB), wT=(wT, wTB), qg=(qg, qgB), at=(at, atB), kd=(kd, kdB))

                    def scan(c, r, g):
                        uw_, uwB = r['uw']
                        wT_, wTB = r['wT']
                        qg_, qgB = r['qg']
                        at_, atB = r['at']
                        kd_, kdB = r['kd']
                        uw, wT, qg, at, kd = uw_[:, g, :], wT_[:, g, :], qg_[:, g, :], at_[:, g, :], kd_[:, g, :]
                        p5, p5B = ps[5], psB[5]
                        k.op('tensor', lambda e: e.matmul(p5[0:64, 0:128], wT, S[:], start=True, stop=True), reads=[wTB, SB], writes=[p5B])
                        vn, vnB = r_vn.next()
                        k.op('vector', lambda e: e.tensor_tensor(vn[:], uw[:, 0:128], p5[0:64, 0:128], ALU.subtract), reads=[uwB, p5B], writes=[vnB])
                        k.op('tensor', lambda e: e.matmul(p5[0:64, 128:256], qg, S[:], start=True, stop=False), reads=[qgB, SB], writes=[p5B])
                        k.op('tensor', lambda e: e.matmul(p5[0:64, 128:256], at, vn[:], start=False, stop=True), reads=[atB, vnB], writes=[p5B])
                        p6, p6B = ps[6], psB[6]
                        k.op('tensor', lambda e: e.matmul(p6[:, 0:128], kd, vn[:], start=True, stop=True), reads=[kdB, vnB], writes=[p6B])
                        k.op('vector', lambda e: e.scalar_tensor_tensor(S[:], S[:], egl[:, c:c + 1], p6[:, 0:128], ALU.mult, ALU.add),
                             reads=[SB, eglB, p6B], writes=[SB])
                        o, oB = r_o.next()
                        st, stB = r_st.next()
                        jk, jkB = r_jk.next()
                        k.op('scalar', lambda e: e.copy(o[:], p5[0:64, 128:256]), reads=[p5B], writes=[oB])
                        if h == 0 and l == 0 and c <= 1:
                            dump(f"vn{c}", vn[:], vnB)
                            dump(f"o{c}", o[:], oB)
                            dump(f"S{c}", S[:], SB)
                        k.op('scalar', lambda e: e.activation(jk[:], o[:], AF.Square, accum_out=st[:, 0:1]), reads=[oB], writes=[jkB, stB])
                        k.op('scalar', lambda e: e.activation(st[:, 1:2], st[:, 0:1], AF.Sqrt, bias=RMS_EPS, scale=1.0 / 128), reads=[stB], writes=[stB])
                        k.op('vector', lambda e: e.reciprocal(st[:, 2:3], st[:, 1:2]), reads=[stB], writes=[stB])
                        k.op('vector', lambda e: e.scalar_tensor_tensor(o[:], o[:], st[:, 2:3], nw[0:64, :], ALU.mult, ALU.mult), reads=[oB, stB, nwB], writes=[oB])
                        ob, obB = r_ob.next()
                        k.op('gpsimd', lambda e: e.tensor_tensor(ob[:], o[:], zs[:, c, :], ALU.mult), reads=[oB, zsB], writes=[obB])
                        k.dma('sync', om_d[c * 64:(c + 1) * 64, h * 128:(h + 1) * 128], ob[:], reads=[obB])

                    assert NCH % G == 0
                    pend = bulk(0)
                    for gi in range(NCH // G):
                        nxt = bulk(gi + 1) if gi + 1 < NCH // G else None
                        for g in range(G):
                            scan(gi * G + g, pend, g)
                        pend = nxt
                k.barrier()

        def shared_kv():
            with ExitStack() as es:
                hbr = Ring(es, nc, "khb", [128, 8, 512], BF16, 2)
                kr = Ring(es, nc, "kst", [64, 512], BF16, 3)
                vr = Ring(es, nc, "vst", [128, 129], BF16, 3)
                for h in range(NH):
                    wk, wkB = load_w_bf16(es, f"wk{h}", kv_w[:, h * 128:(h + 1) * 128], 128) if h == 0 else (wk_keep, wkB_keep)
                    wv, wvB = load_w_bf16(es, f"wv{h}", kv_w[:, 1024 + h * 128:1024 + (h + 1) * 128], 128) if h == 0 else (wv_keep, wvB_keep)
                    if h == 0:
                        wk_keep, wkB_keep, wv_keep, wvB_keep = wk, wkB, wv, wvB
                    else:
                        k.dma('gpsimd', wk[:], kv_w[:, h * 128:(h + 1) * 128].rearrange("(k p) c -> p k c", p=128), writes=[wkB])
                        k.dma('gpsimd', wv[:], kv_w[:, 1024 + h * 128:1024 + (h + 1) * 128].rearrange("(k p) c -> p k c", p=128), writes=[wvB])
                    for blk in range(NB):
                        hb, hbB = hbr.next()
                        k.dma('sync', hb[:], hT_v[:, :, blk * 512:(blk + 1) * 512], writes=[hbB])
                        for s in range(2):
                            pp, ppB = ps[s], psB[s]
                            for kc in range(8):
                                k.op('tensor', lambda e, pp=pp, kc=kc, s=s, hb=hb: e.matmul(pp[0:64, :], wk[:, kc, s * 64:(s + 1) * 64], hb[:, kc, :],
                                                                                     start=(kc == 0), stop=(kc == 7)), reads=[wkB, hbB], writes=[ppB])
                            kt, ktB = kr.next()
                            k.op('scalar' if s else 'vector', (lambda e, kt=kt, pp=pp: e.copy(kt[:], pp[0:64, :])) if s else
                                 (lambda e, kt=kt, pp=pp: e.tensor_copy(kt[:], pp[0:64, :])), reads=[ppB], writes=[ktB])
                            k.dma('sync', kT_d[h, s, :, blk * 512:(blk + 1) * 512], kt[:], reads=[ktB])
                        for tt in range(4):
                            pp, ppB = ps[2 + tt % 2], psB[2 + tt % 2]
                            for kc in range(8):
                                k.op('tensor', lambda e, pp=pp, kc=kc, tt=tt, hb=hb: e.matmul(pp[:, 0:128], hb[:, kc, tt * 128:(tt + 1) * 128], wv[:, kc, :],
                                                                                       start=(kc == 0), stop=(kc == 7)), reads=[wvB, hbB], writes=[ppB])
                            vt, vtB = vr.next()
                            k.op('vector', lambda e, vt=vt, pp=pp: e.tensor_copy(vt[:, 0:128], pp[:, 0:128]), reads=[ppB], writes=[vtB])
                            k.op('gpsimd', lambda e, vt=vt: e.memset(vt[:, 128:129], 1.0), writes=[vtB])
                            tok0 = blk * 512 + tt * 128
                            k.dma('sync', va_d[h, tok0:tok0 + 128, :], vt[:], reads=[vtB])
                k.barrier()

        def diffattn(j, layer):
            lam_init = lambda_init(layer)
            with ExitStack() as es:
                lp, lpB = bcast_row(es, "lp", b_lambda[j:j + 1].rearrange("o a b -> o (a b)"), 256)
                sw, swB = bcast_row(es, "sw", b_subln_w[j:j + 1, :], 128)
                lam = sbt(es, "lam", [128, 8], F32)
                lamB = Buf("lam")
                pr = sbt(es, "lpr", [128, 128], F32)
                k.op('vector', lambda e: e.tensor_tensor(pr[:, 0:64], lp[:, 0:64], lp[:, 64:128], ALU.mult), reads=[lpB], writes=[lamB])
                k.op('vector', lambda e: e.tensor_tensor(pr[:, 64:128], lp[:, 128:192], lp[:, 192:256], ALU.mult), reads=[lpB], writes=[lamB])
                k.op('vector', lambda e: e.reduce_sum(lam[:, 0:1], pr[:, 0:64], AX.X), reads=[lamB], writes=[lamB])
                k.op('vector', lambda e: e.reduce_sum(lam[:, 1:2], pr[:, 64:128], AX.X), reads=[lamB], writes=[lamB])
                k.op('scalar', lambda e: e.activation(lam[:, 2:4], lam[:, 0:2], AF.Exp), reads=[lamB], writes=[lamB])
                k.op('vector', lambda e: e.tensor_tensor(lam[:, 4:5], lam[:, 2:3], lam[:, 3:4], ALU.subtract), reads=[lamB], writes=[lamB])
                k.op('vector', lambda e: e.tensor_scalar(lam[:, 5:6], lam[:, 4:5], lam_init, -1.0, ALU.add, ALU.mult), reads=[lamB], writes=[lamB])
                k.op('vector', lambda e: e.tensor_scalar_mul(sw[:], sw[:], 1.0 - lam_init), reads=[swB], writes=[swB])
                qs = [sbt(es, f"qs{s}", [64, T], BF16) for s in range(2)]
                qsB = [Buf("qs0"), Buf("qs1")]
                kTs = [sbt(es, f"kTs{s}", [64, T], BF16) for s in range(2)]
                kTB = [Buf("kT0"), Buf("kT1")]
                va = sbt(es, "va", [128, NT, 144], BF16)
                vaB = Buf("va")
                hbr = Ring(es, nc, "ahb", [128, 8, 512], BF16, 2)
                ptr = Ring(es, nc, "pt", [128, 512], BF16, 6)
                r_o1 = Ring(es, nc, "ao1", [128, 128], F32, 2)
                r_st = Ring(es, nc, "ast", [128, 8], F32, 2)
                r_jk = Ring(es, nc, "ajk", [128, 128], F32, 2)
                r_ob = Ring(es, nc, "aob", [128, 128], BF16, 2)
                wq = sbt(es, "wq", [128, 8, 128], BF16)
                wqB = Buf("wq")
                acc = {}
                slots = [(4, 0), (4, 144), (4, 288), (5, 0), (5, 144), (5, 288), (6, 0), (6, 144)]
                for s in range(2):
                    for i in range(4):
                        acc[(s, i)] = slots[s * 4 + i]
                for h in range(NH):
                    k.dma('gpsimd', wq[:], b_w_q[j, :, h * 128:(h + 1) * 128].rearrange("(k p) c -> p k c", p=128), writes=[wqB])
                    for s in range(2):
                        k.dma('sync', kTs[s][:], kT_d[h, s], writes=[kTB[s]])
                    k.dma('sync', va[:, :, 0:129], va_d[h].rearrange("(t p) c -> p t c", p=128), writes=[vaB])
                    for blk in range(NB):
                        hb, hbB = hbr.next()
                        k.dma('sync', hb[:], hT_v[:, :, blk * 512:(blk + 1) * 512], writes=[hbB])
                        for s in range(2):
                            pp, ppB = ps[s], psB[s]
                            for kc in range(8):
                                k.op('tensor', lambda e, pp=pp, kc=kc, s=s, hb=hb: e.matmul(pp[0:64, :], wq[:, kc, s * 64:(s + 1) * 64], hb[:, kc, :],
                                                                                     start=(kc == 0), stop=(kc == 7)), reads=[wqB, hbB], writes=[ppB])
                            k.op('scalar', lambda e, pp=pp, s=s, blk=blk: e.activation(qs[s][:, blk * 512:(blk + 1) * 512], pp[0:64, :], AF.Copy, scale=0.125),
                                 reads=[ppB], writes=[qsB[s]])
                    for qb in range(NB):
                        nkt = 4 * qb + 4
                        steps = [(kt, s) for kt in range(nkt) for s in range(2)]
                        LA = 2

                        def front(idx, qb=qb):
                            kt, s = steps[idx]
                            jd = kt - 4 * qb
                            lo = 0 if jd < 0 else jd * 128
                            n = 512 - lo
                            pp, ppB = ps[idx % 4], psB[idx % 4]
                            k.op('tensor', lambda e, pp=pp, s=s, kt=kt, qb=qb, lo=lo, n=n: e.matmul(pp[:, 0:n], kTs[s][:, kt * 128:(kt + 1) * 128],
                                                                                             qs[s][:, qb * 512 + lo:(qb + 1) * 512], start=True, stop=True),
                                 reads=[kTB[s], qsB[s]], writes=[ppB])
                            pt, ptB = ptr.next()
                            k.op('scalar', lambda e, pt=pt, pp=pp, n=n: e.activation(pt[:, 0:n], pp[:, 0:n], AF.Exp), reads=[ppB], writes=[ptB])
                            if jd >= 0:
                                k.op('vector', lambda e, pt=pt: e.tensor_tensor(pt[:, 0:128], pt[:, 0:128], triub[:], ALU.mult), reads=[ptB, cB], writes=[ptB])
                            return pt, ptB, jd, lo

                        def back(idx, fr, qb=qb):
                            kt, s = steps[idx]
                            pt, ptB, jd, lo = fr
                            for i in range(max(jd, 0), 4):
                                bank, off = acc[(s, i)]
                                c0 = i * 128 - lo
                                st_flag = (kt == 0 and off == 0)
                                k.op('tensor', lambda e, pt=pt, bank=bank, off=off, c0=c0, kt=kt, i=i, qb=qb, st_flag=st_flag: e.matmul(
                                    ps[bank][:, off:off + 129], pt[:, c0:c0 + 128], va[:, kt, 0:129], start=st_flag, stop=(kt == 4 * qb + i),
                                    skip_group_check=True),
                                    reads=[ptB, vaB], writes=[psB[bank]])

                        fronts = {}
                        for idx in range(len(steps) + LA):
                            if idx < len(steps):
                                fronts[idx] = front(idx)
                            if idx - LA >= 0:
                                back(idx - LA, fronts.pop(idx - LA))
                        for i in range(4):
                            b1, f1 = acc[(0, i)]
                            b2, f2 = acc[(1, i)]
                            st, stB = r_st.next()
                            o1, o1B = r_o1.next()
                            jk, jkB = r_jk.next()
                            ob, obB = r_ob.next()
                            k.op('vector', lambda e, st=st, b1=b1, f1=f1: e.reciprocal(st[:, 0:1], ps[b1][:, f1 + 128:f1 + 129]), reads=[psB[b1]], writes=[stB])
                            k.op('vector', lambda e, st=st, b2=b2, f2=f2: e.reciprocal(st[:, 1:2], ps[b2][:, f2 + 128:f2 + 129]), reads=[psB[b2]], writes=[stB])
                            k.op('vector', lambda e, st=st: e.tensor_tensor(st[:, 2:3], st[:, 1:2], lam[:, 5:6], ALU.mult), reads=[stB, lamB], writes=[stB])
                            k.op('vector', lambda e, st=st, o1=o1, b1=b1, f1=f1: e.tensor_scalar_mul(o1[:], ps[b1][:, f1:f1 + 128], st[:, 0:1]),
                                 reads=[psB[b1], stB], writes=[o1B])
                            k.op('vector', lambda e, st=st, o1=o1, b2=b2, f2=f2: e.scalar_tensor_tensor(o1[:], ps[b2][:, f2:f2 + 128], st[:, 2:3], o1[:], ALU.mult, ALU.add),
                                 reads=[psB[b2], stB, o1B], writes=[o1B])
                            k.op('scalar', lambda e, st=st, o1=o1, jk=jk: e.activation(jk[:], o1[:], AF.Square, accum_out=st[:, 3:4]), reads=[o1B], writes=[jkB, stB])
                            k.op('scalar', lambda e, st=st: e.activation(st[:, 4:5], st[:, 3:4], AF.Sqrt, bias=RMS_EPS, scale=1.0 / 128), reads=[stB], writes=[stB])
                            k.op('vector', lambda e, st=st: e.reciprocal(st[:, 5:6], st[:, 4:5]), reads=[stB], writes=[stB])
                            k.op('vector', lambda e, st=st, o1=o1, ob=ob: e.scalar_tensor_tensor(ob[:], o1[:], st[:, 5:6], sw[:], ALU.mult, ALU.mult),
                                 reads=[o1B, stB, swB], writes=[obB])
                            t0 = qb * 512 + i * 128
                            k.dma('sync', om_d[t0:t0 + 128, h * 128:(h + 1) * 128], ob[:], reads=[obB])
                k.barrier()

        def tok_moe(layer, w_out_src, last):
            with ExitStack() as es:
                wo, woB = load_w_bf16(es, "wo", w_out_src, 1024)
                wr = sbt(es, "wr", [128, 8, 40], F32)
                wrB = Buf("wr")
                k.dma('sync', wr[:, :, 0:4], moe_w_group[layer].rearrange("(k p) c -> p k c", p=128), writes=[wrB])
                k.dma('sync', wr[:, :, 4:36], moe_w_expert[layer].rearrange("(k p) c -> p k c", p=128), writes=[wrB])
                g1, g1B = bcast_row(es, "lng1", ln_mix_g[layer:layer + 1, :], D)
                b1, b1B = bcast_row(es, "lnb1", ln_mix_b[layer:layer + 1, :], D)
                lnB1 = Buf("ln1")
                oh = [sbt(es, f"oh{i}", [128, NT, 32], F32) for i in range(2)]
                ohB = Buf("oh")
                gates = sbt(es, "gates", [128, NT, 2], F32)
                gB = Buf("gates")
                rings = {'hTs': Ring(es, nc, "hTs2", [128, 8, 128], BF16, 2), 'st': Ring(es, nc, "lst", [128, 8], F32, 2),
                         'junk': Ring(es, nc, "ljk", [128, D], F32, 1)}
                with ExitStack() as e1:
                    r_om = Ring(e1, nc, "om", [128, D], BF16, 2)
                    r_omT = Ring(e1, nc, "omT", [128, 8, 128], BF16, 2)
                    r_h = Ring(e1, nc, "hh", [128, D], F32, 2)
                    r_t = Ring(e1, nc, "tt", [128, D], F32, 2)
                    r_y = Ring(e1, nc, "yy", [128, D], F32, 2)
                    r_xb = Ring(e1, nc, "xb", [128, D], BF16, 2)
                    r_xT = Ring(e1, nc, "xT", [128, 8, 128], F32, 2)
                    r_rt = Ring(e1, nc, "rt", [128, 160], F32, 2)
                    for t in range(NT):
                        rows = slice(t * 128, (t + 1) * 128)
                        om, omB = r_om.next()
                        k.dma('sync', om[:], om_d[rows, :], writes=[omB])
                        hh, hhB = r_h.next()
                        k.dma('sync', hh[:], h_d[rows, :], writes=[hhB])
                        for kc in range(8):
                            k.op('tensor', lambda e, om=om, kc=kc: e.transpose(pb[:, kc * 128:(kc + 1) * 128], om[:, kc * 128:(kc + 1) * 128], identb[:]),
                                 reads=[omB, cB], writes=[pbB])
                        omT, omTB = r_omT.next()
                        k.op('vector', lambda e, omT=omT: e.tensor_copy(omT[:], pb[:].rearrange("p (a b) -> p a b", a=8)), reads=[pbB], writes=[omTB])
                        tt, ttB = r_t.next()
                        for half in range(2):
                            pp, ppB = ps[half], psB[half]
                            for kc in range(8):
                                k.op('tensor', lambda e, pp=pp, kc=kc, half=half, omT=omT: e.matmul(pp[:, :], omT[:, kc, :], wo[:, kc, half * 512:(half + 1) * 512],
                                                                                             start=(kc == 0), stop=(kc == 7)), reads=[omTB, woB], writes=[ppB])
                            k.op('vector', lambda e, pp=pp, half=half, tt=tt, hh=hh: e.scalar_tensor_tensor(tt[:, half * 512:(half + 1) * 512], hh[:, half * 512:(half + 1) * 512],
                                                                                                      ALPHA, pp[:, :], ALU.mult, ALU.add),
                                 reads=[ppB, hhB], writes=[ttB])
                        yy, yyB = r_y.next()
                        layer_norm(rings, tt, ttB, g1, b1, [g1B, b1B], yy, yyB)
                        k.dma('sync', h_d[rows, :], yy[:], reads=[yyB, hhB])
                        xb, xbB = r_xb.next()
                        k.op('scalar', lambda e, xb=xb, yy=yy: e.copy(xb[:], yy[:]), reads=[yyB], writes=[xbB])
                        k.dma('sync', xb_d[rows, :], xb[:], reads=[xbB])
                        xT, xTB = r_xT.next()
                        for half in range(2):
                            pp, ppB = ps[2 + half], psB[2 + half]
                            for jj in range(4):
                                kc = half * 4 + jj
                                k.op('tensor', lambda e, pp=pp, jj=jj, kc=kc, yy=yy: e.transpose(pp[:, jj * 128:(jj + 1) * 128], yy[:, kc * 128:(kc + 1) * 128], ident[:]),
                                     reads=[yyB, cB], writes=[ppB])
                            if half == 0:
                                k.op('vector', lambda e, pp=pp, xT=xT: e.tensor_copy(xT[:, 0:4, :], pp[:].rearrange("p (a b) -> p a b", a=4)), reads=[ppB], writes=[xTB])
                            else:
                                k.op('scalar', lambda e, pp=pp, xT=xT: e.copy(xT[:, 4:8, :], pp[:].rearrange("p (a b) -> p a b", a=4)), reads=[ppB], writes=[xTB])
                        p4, p4B = ps[4], psB[4]
                        for kc in range(8):
                            k.op('tensor', lambda e, kc=kc, xT=xT: e.matmul(p4[:, 0:36], xT[:, kc, :], wr[:, kc, 0:36], start=(kc == 0), stop=(kc == 7)),
                                 reads=[xTB, wrB], writes=[p4B])
                        rt, rtB = r_rt.next()
                        V = lambda fn, rt=rt, rtB=rtB, extra_r=(), extra_w=(): k.op('vector', fn, reads=[rtB] + list(extra_r), writes=[rtB] + list(extra_w))
                        k.op('vector', lambda e, rt=rt: e.tensor_copy(rt[:, 0:36], p4[:, 0:36]), reads=[p4B], writes=[rtB])
                        V(lambda e, rt=rt: e.reduce_max(rt[:, 36:37], rt[:, 0:4], AX.X))
                        V(lambda e, rt=rt: e.tensor_scalar_mul(rt[:, 37:38], rt[:, 36:37], -1.0))
                        k.op('scalar', lambda e, rt=rt: e.activation(rt[:, 84:88], rt[:, 0:4], AF.Exp, bias=rt[:, 37:38], accum_out=rt[:, 38:39]), reads=[rtB], writes=[rtB])
                        V(lambda e, rt=rt: e.reciprocal(rt[:, 39:40], rt[:, 38:39]))
                        V(lambda e, rt=rt: e.tensor_scalar(rt[:, 40:44], rt[:, 0:4], rt[:, 36:37], None, ALU.is_ge))
                        V(lambda e, rt=rt: e.tensor_scalar(rt[:, 40:44], rt[:, 40:44], -NEG, NEG, ALU.mult, ALU.add))
                        for g in range(4):
                            V(lambda e, rt=rt, g=g: e.tensor_scalar(rt[:, 44 + g * 8:52 + g * 8], rt[:, 4 + g * 8:12 + g * 8], rt[:, 40 + g:41 + g], None, ALU.add))
                        V(lambda e, rt=rt: e.reduce_max(rt[:, 76:77], rt[:, 44:76], AX.X))
                        V(lambda e, rt=rt, t=t: e.tensor_scalar(oh[0][:, t, :], rt[:, 44:76], rt[:, 76:77], None, ALU.is_ge), extra_w=[ohB])
                        V(lambda e, rt=rt, t=t: e.scalar_tensor_tensor(rt[:, 96:128], oh[0][:, t, :], NEG, rt[:, 44:76], ALU.mult, ALU.add), extra_r=[ohB])
                        V(lambda e, rt=rt: e.reduce_max(rt[:, 77:78], rt[:, 96:128], AX.X))
                        V(lambda e, rt=rt, t=t: e.tensor_scalar(oh[1][:, t, :], rt[:, 96:128], rt[:, 77:78], None, ALU.is_ge), extra_w=[ohB])
                        V(lambda e, rt=rt: e.tensor_tensor(rt[:, 78:79], rt[:, 77:78], rt[:, 76:77], ALU.subtract))
                        k.op('scalar', lambda e, rt=rt: e.activation(rt[:, 79:80], rt[:, 78:79], AF.Exp), reads=[rtB], writes=[rtB])
                        V(lambda e, rt=rt: e.tensor_scalar_add(rt[:, 80:81], rt[:, 79:80], 1.0))
                        V(lambda e, rt=rt: e.reciprocal(rt[:, 81:82], rt[:, 80:81]))
                        V(lambda e, rt=rt, t=t: e.tensor_tensor(gates[:, t, 0:1], rt[:, 39:40], rt[:, 81:82], ALU.mult), extra_w=[gB])
                        V(lambda e, rt=rt, t=t: e.tensor_tensor(gates[:, t, 1:2], rt[:, 39:40], gates[:, t, 0:1], ALU.subtract), extra_r=[gB], extra_w=[gB])
                    k.barrier()
                if checkpoint(noraise=True):
                    return True
                dest = sbt(es, "dest", [128, 2, NT], I32)
                destB = Buf("dest")
                with ExitStack() as e2:
                    selb = sbt(e2, "selb", [128, NT, 32], BF16)
                    cnt = sbt(e2, "cnt", [128, NT, 32], F32)
                    pref = sbt(e2, "pref", [128, NT, 32], F32)
                    slot = sbt(e2, "slot", [128, NT, 32], F32)
                    ebase = sbt(e2, "ebase", [128, 32], F32)
                    tmp = sbt(e2, "ptmp", [128, NT, 32], F32)
                    dfl = sbt(e2, "dfl", [128, 2, NT], F32)
                    pB = Buf("pos")
                    k.op('gpsimd', lambda e: e.iota(ebase[:], [[CAP, 32]], base=0, channel_multiplier=0, allow_small_or_imprecise_dtypes=True), writes=[pB])
                    k.op('vector', lambda e: e.tensor_tensor(selb[:], oh[0][:], oh[1][:], ALU.add), reads=[ohB], writes=[pB])
                    TPB = 16
                    for t0 in range(0, NT, TPB):
                        n = min(TPB, NT - t0)
                        k.op('tensor', lambda e, t0=t0, n=n: e.matmul(ps[0][:, 0:n * 32], ones_b[:], selb[:, t0:t0 + n, :].rearrange("p a b -> p (a b)"), start=True, stop=True),
                             reads=[pB, cB], writes=[psB[0]])
                        k.op('vector', lambda e, t0=t0, n=n: e.tensor_copy(cnt[:, t0:t0 + n, :].rearrange("p a b -> p (a b)"), ps[0][:, 0:n * 32]), reads=[psB[0]], writes=[pB])
                        k.op('tensor', lambda e, t0=t0, n=n: e.matmul(ps[1][:, 0:n * 32], sutri[:], selb[:, t0:t0 + n, :].rearrange("p a b -> p (a b)"), start=True, stop=True),
                             reads=[pB, cB], writes=[psB[1]])
                        k.op('vector', lambda e, t0=t0, n=n: e.tensor_copy(slot[:, t0:t0 + n, :].rearrange("p a b -> p (a b)"), ps[1][:, 0:n * 32]), reads=[psB[1]], writes=[pB])
                    k.op('vector', lambda e: e.memset(pref[:, 0, :], 0.0), reads=[pB], writes=[pB])
                    for t in range(1, NT):
                        k.op('vector', lambda e, t=t: e.tensor_tensor(pref[:, t, :], pref[:, t - 1, :], cnt[:, t - 1, :], ALU.add), reads=[pB], writes=[pB])
                    k.op('vector', lambda e: e.tensor_tensor(slot[:], slot[:], pref[:], ALU.add), reads=[pB], writes=[pB])
                    k.op('vector', lambda e: e.tensor_scalar(tmp[:], slot[:], float(CAP), 1.0e6, ALU.is_ge, ALU.mult), reads=[pB], writes=[pB])
                    k.op('vector', lambda e: e.tensor_tensor(slot[:], slot[:], tmp[:], ALU.add), reads=[pB], writes=[pB])
                    for t in range(NT):
                        k.op('gpsimd', lambda e, t=t: e.tensor_tensor(slot[:, t, :], slot[:, t, :], ebase[:], ALU.add), reads=[pB], writes=[pB])
                    for i in range(2):
                        k.op('vector', lambda e, i=i: e.tensor_tensor(tmp[:], slot[:], oh[i][:], ALU.mult), reads=[pB, ohB], writes=[pB])
                        k.op('vector', lambda e, i=i: e.reduce_sum(dfl[:, i, :], tmp[:], AX.X), reads=[pB], writes=[pB])
                    k.op('vector', lambda e: e.tensor_scalar_min(dfl[:], dfl[:], float(NS)), reads=[pB], writes=[pB])
                    k.op('vector', lambda e: e.tensor_copy(dest[:], dfl[:]), reads=[pB], writes=[destB])
                    r_xb2 = Ring(e2, nc, "xb2", [128, D], BF16, 3)
                    for t in range(NT):
                        xb, xbB = r_xb2.next()
                        k.dma('sync', xb[:], xb_d[t * 128:(t + 1) * 128, :], writes=[xbB])
                        for i in range(2):
                            k.op('gpsimd', lambda e, xb=xb, i=i, t=t: e.indirect_dma_start(
                                out=xs_d, out_offset=bass.IndirectOffsetOnAxis(ap=dest[:, i, t:t + 1], axis=0), in_=xb[:], in_offset=None),
                                reads=[xbB, destB], dma=True)
                    k.barrier()
                with ExitStack() as e3:
                    NG = (CAP + 127) // 128
                    r_w13 = Ring(e3, nc, "w13", [128, 8, 1024], BF16, 2)
                    r_w2 = Ring(e3, nc, "w2", [128, 4, 1024], BF16, 2)
                    r_xs = Ring(e3, nc, "xs", [128, NG, D], BF16, 2)
                    r_xsT = Ring(e3, nc, "xsT", [128, 8, CAP], BF16, 2)
                    r_sg = Ring(e3, nc, "sg", [128, 4, CAP], F32, 1)
                    r_hid = Ring(e3, nc, "hid", [128, 4, CAP], BF16, 2)
                    r_ys = Ring(e3, nc, "ys", [128, D], F32, 2)
                    for ex in range(32):
                        w13, w13B = r_w13.next()
                        k.dma('gpsimd', w13[:], moe_w13[layer, ex].rearrange("(k p) c -> p k c", p=128), writes=[w13B])
                        w2, w2B = r_w2.next()
                        k.dma('gpsimd', w2[:], moe_w2[layer, ex].rearrange("(k p) c -> p k c", p=128), writes=[w2B])
                        xs, xsB = r_xs.next()
                        xsT, xsTB = r_xsT.next()
                        for g in range(NG):
                            r0 = ex * CAP + g * 128
                            nr = min(128, CAP - g * 128)
                            k.dma('sync', xs[0:nr, g, :], xs_d[r0:r0 + nr, :], writes=[xsB])
                        for g in range(NG):
                            nr = min(128, CAP - g * 128)
                            for kc in range(8):
                                k.op('tensor', lambda e, xs=xs, g=g, kc=kc, nr=nr: e.transpose(pb[:, kc * 128:kc * 128 + nr], xs[0:nr, g, kc * 128:(kc + 1) * 128], identb[0:nr, 0:nr]),
                                     reads=[xsB, cB], writes=[pbB])
                            k.op('vector', lambda e, xsT=xsT, g=g, nr=nr: e.tensor_copy(xsT[:, :, g * 128:g * 128 + nr], pb[:].rearrange("p (a b) -> p a b", a=8)[:, :, 0:nr]),
                                 reads=[pbB], writes=[xsTB])
                        sg, sgB = r_sg.next()
                        hid, hidB = r_hid.next()
                        for n0 in range(0, CAP, 512):
                            n = min(512, CAP - n0)
                            for m in range(8):
                                pp, ppB = ps[m % 4], psB[m % 4]
                                for kc in range(8):
                                    k.op('tensor', lambda e, pp=pp, kc=kc, m=m, w13=w13, xsT=xsT, n0=n0, n=n: e.matmul(pp[:, 0:n], w13[:, kc, m * 128:(m + 1) * 128], xsT[:, kc, n0:n0 + n],
                                                                                                           start=(kc == 0), stop=(kc == 7)),
                                         reads=[w13B, xsTB], writes=[ppB])
                                if m < 4:
                                    k.op('scalar', lambda e, pp=pp, m=m, sg=sg, n0=n0, n=n: e.activation(sg[:, m, n0:n0 + n], pp[:, 0:n], AF.Silu), reads=[ppB], writes=[sgB])
                                else:
                                    k.op('vector', lambda e, pp=pp, m=m, sg=sg, hid=hid, n0=n0, n=n: e.tensor_tensor(hid[:, m - 4, n0:n0 + n], sg[:, m - 4, n0:n0 + n], pp[:, 0:n], ALU.mult),
                                         reads=[ppB, sgB], writes=[hidB])
                        for g in range(NG):
                            nr = min(128, CAP - g * 128)
                            ys, ysB = r_ys.next()
                            for half in range(2):
                                pp, ppB = ps[4 + half], psB[4 + half]
                                for f in range(4):
                                    k.op('tensor', lambda e, pp=pp, f=f, half=half, hid=hid, w2=w2, g=g, nr=nr: e.matmul(pp[0:nr, :], hid[:, f, g * 128:g * 128 + nr], w2[:, f, half * 512:(half + 1) * 512],
                                                                                                             start=(f == 0), stop=(f == 3)),
                                         reads=[hidB, w2B], writes=[ppB])
                                if half == 0:
                                    k.op('vector', lambda e, pp=pp, ys=ys, nr=nr: e.tensor_copy(ys[0:nr, 0:512], pp[0:nr, :]), reads=[ppB], writes=[ysB])
                                else:
                                    k.op('scalar', lambda e, pp=pp, ys=ys, nr=nr: e.copy(ys[0:nr, 512:1024], pp[0:nr, :]), reads=[ppB], writes=[ysB])
                            r0 = ex * CAP + g * 128
                            for hf in range(2):
                                k.dma('sync', ys_h[hf][r0:r0 + nr, :], ys[0:nr, hf * 512:(hf + 1) * 512], reads=[ysB])
                    k.barrier()
                with ExitStack() as e4:
                    g2, g2B = bcast_row(e4, "lng2", ln_ffn_g[layer:layer + 1, :], D)
                    b2, b2B = bcast_row(e4, "lnb2", ln_ffn_b[layer:layer + 1, :], D)
                    r_yq = [Ring(e4, nc, f"yq{q}", [128, 512], F32, 2) for q in range(4)]
                    r_h = Ring(e4, nc, "ch", [128, D], F32, 2)
                    r_o = Ring(e4, nc, "co", [128, D], F32, 2)
                    for t in range(NT):
                        rows = slice(t * 128, (t + 1) * 128)
                        hh, hhB = r_h.next()
                        k.dma('sync', hh[:], h_d[rows, :], writes=[hhB])
                        k.op('scalar', lambda e, hh=hh: e.mul(hh[:], hh[:], ALPHA), reads=[hhB], writes=[hhB])
                        for i in range(2):
                            for hf in range(2):
                                yq, yqB = r_yq[i * 2 + hf].next()
                                k.op('gpsimd', lambda e, yq=yq, i=i, t=t, hf=hf: e.indirect_dma_start(
                                    out=yq[:], out_offset=None, in_=ys_h[hf],
                                    in_offset=bass.IndirectOffsetOnAxis(ap=dest[:, i, t:t + 1], axis=0)), reads=[destB], writes=[yqB], dma=True)
                                k.op('vector', lambda e, hh=hh, yq=yq, t=t, i=i, hf=hf: e.scalar_tensor_tensor(
                                    hh[:, hf * 512:(hf + 1) * 512], yq[:], gates[:, t, i:i + 1], hh[:, hf * 512:(hf + 1) * 512], ALU.mult, ALU.add),
                                    reads=[hhB, yqB, gB], writes=[hhB])
                        oo, ooB = r_o.next()
                        layer_norm(rings, hh, hhB, g2, b2, [g2B, b2B], oo, ooB)
                        if last:
                            out_toks.append(k.dma('sync', out[rows, :], oo[:], reads=[ooB]))
                        else:
                            k.dma('sync', h_d[rows, :], oo[:], reads=[ooB, hhB])
                            make_hT(rings, oo, ooB, t)
                    k.barrier()

        try:
            checkpoint()
            for layer in range(4):
                if layer < 2:
                    deltanet(layer)
                    checkpoint()
                    if tok_moe(layer, a_w_out[layer], last=False):
                        raise _Stop()
                    checkpoint()
                else:
                    j = layer - 2
                    if j == 0:
                        shared_kv()
                    diffattn(j, layer)
                    checkpoint()
                    if tok_moe(layer, b_w_out[j], last=(layer == 3)):
                        raise _Stop()
                    checkpoint()
        except _Stop:
            out_toks.append(k.dma('sync', out, h_d))
            out_toks.append(k.dma('sync', dbg, om_d))
        k.wait_all('sync', out_toks)
        k.finish()
    return nc, k


SEQ = 8192
CAP_FULL = 640
_cache = {}


def kernel(**inputs):
    x = np.asarray(inputs['x'])
    B, T, _ = x.shape
    cap = CAP_FULL if T == SEQ else max(64, int(T / 16 + 6 * math.sqrt(T / 16) + 16) // 32 * 32 + 32)
    key = (T, cap)
    if key not in _cache:
        _cache[key] = build(T, cap)[0]
    nc = _cache[key]
    shared = {n: np.ascontiguousarray(np.asarray(v, dtype=np.float32)) for n, v in inputs.items() if n != 'x'}
    in_maps = []
    for b in range(B):
        m = dict(shared)
        m['x'] = np.ascontiguousarray(x[b])
        in_maps.append(m)
    res = run_bass_kernel_spmd(nc, in_maps, core_ids=list(range(B)))
    return np.stack([np.asarray(res.results[b]['out']) for b in range(B)], axis=0).astype(np.float32)
```

```python
import math
from contextlib import ExitStack
import numpy as np
import concourse.bass as bass
import concourse.mybir as mybir
from concourse.bass_utils import run_bass_kernel_spmd

F32 = mybir.dt.float32
BF16 = mybir.dt.bfloat16
I32 = mybir.dt.int32
AF = mybir.ActivationFunctionType
ALU = mybir.AluOpType
AX = mybir.AxisListType

ENGS = ['tensor', 'vector', 'scalar', 'gpsimd', 'sync']
SEM_EPOCH = 30000
N_DMA_SEMS = 16


class Buf:
    __slots__ = ('name', 'w', 'r', 'excl')

    def __init__(self, name='', excl=False):
        self.name = name
        self.excl = excl
        self.w = None
        self.r = {}


class _Op:
    __slots__ = ('fn', 'waits', 'inc', 'incval', 'dma')

    def __init__(self, fn, waits, dma):
        self.fn = fn
        self.waits = waits
        self.inc = False
        self.incval = 0
        self.dma = dma


class K:
    def __init__(self, nc):
        self.nc = nc
        self.ops = {e: [] for e in ENGS}
        self.waited = {e: {} for e in ENGS}
        self.dma_rr = {e: 0 for e in ENGS}
        self.dma_cnt = {}

    def _need_wait(self, eng, t):
        key = (t[0], t[1])
        if self.waited[eng].get(key, -1) >= t[2]:
            return False
        self.waited[eng][key] = t[2]
        if t[0] == 'e':
            self.ops[t[1]][t[2]].inc = True
        return True

    def op(self, eng, fn, reads=(), writes=(), dma=False):
        idx = len(self.ops[eng])
        writes = list(writes) + [b for b in reads if b.excl]
        reads = [b for b in reads if not b.excl]
        deps = []
        for b in reads:
            if b.w is not None:
                deps.append(b.w)
        for b in writes:
            if b.w is not None:
                deps.append(b.w)
            deps.extend(b.r.values())
        waits = []
        for t in deps:
            if t[0] == 'e' and t[1] == eng and eng == 'tensor':
                continue
            if self._need_wait(eng, t):
                waits.append(t)
        dm = None
        if dma:
            slot = self.dma_rr[eng]
            self.dma_rr[eng] = (slot + 1) % N_DMA_SEMS
            cnt = self.dma_cnt.get((eng, slot), 0) + 1
            self.dma_cnt[(eng, slot)] = cnt
            dm = ((eng, slot), cnt * 16)
            if cnt > 1:
                t = ('d', (eng, slot), (cnt - 1) * 16)
                if self._need_wait(eng, t):
                    waits.append(t)
            tok = ('d', (eng, slot), cnt * 16)
        else:
            tok = ('e', eng, idx)
        self.ops[eng].append(_Op(fn, waits, dm))
        kk = (tok[0], tok[1])
        for b in reads:
            b.r[kk] = tok
        for b in writes:
            b.w = tok
            b.r = {}
        return tok

    def dma(self, eng, out, in_, reads=(), writes=(), **kw):
        return self.op(eng, lambda e: e.dma_start(out=out, in_=in_, **kw), reads=reads, writes=writes, dma=True)

    def wait_all(self, eng, tokens):
        waits = [t for t in tokens if self._need_wait(eng, t)]
        if waits:
            self.ops[eng].append(_Op(None, waits, None))

    def barrier(self):
        toks = []
        for e in ENGS:
            for i in range(len(self.ops[e]) - 1, -1, -1):
                o = self.ops[e][i]
                if o.fn is not None and o.dma is None:
                    toks.append(('e', e, i))
                    break
        for key, cnt in self.dma_cnt.items():
            toks.append(('d', key, cnt * 16))
        for e in ENGS:
            self.wait_all(e, toks)
        self.flush()

    def flush(self):
        nc = self.nc
        if not hasattr(self, 'flushed'):
            self.flushed = {e: 0 for e in ENGS}
            self.inccnt = {e: 0 for e in ENGS}
            self.esems = {e: [] for e in ENGS}
            self.dsems = {}
        start = dict(self.flushed)
        for e in ENGS:
            for o in self.ops[e][start[e]:]:
                if o.inc:
                    self.inccnt[e] += 1
                    o.incval = self.inccnt[e]
            need = (self.inccnt[e] + SEM_EPOCH - 1) // SEM_EPOCH
            while len(self.esems[e]) < max(need, 1):
                self.esems[e].append(nc.alloc_semaphore(f"es_{e}_{len(self.esems[e])}"))
        for key in self.dma_cnt:
            if key not in self.dsems:
                self.dsems[key] = nc.alloc_semaphore(f"ds_{key[0]}_{key[1]}")
        esems, dsems, ops = self.esems, self.dsems, self.ops

        def semval(e2, incval):
            return esems[e2][(incval - 1) // SEM_EPOCH], (incval - 1) % SEM_EPOCH + 1

        with nc.Block() as block:
            for eng in ENGS:
                todo = ops[eng][start[eng]:]

                def body(e, eng=eng, todo=todo):
                    for o in todo:
                        for t in o.waits:
                            if t[0] == 'e':
                                p = ops[t[1]][t[2]]
                                assert p.incval > 0, (eng, t)
                                s, v = semval(t[1], p.incval)
                                e.wait_ge(s, v)
                            else:
                                e.wait_ge(dsems[t[1]], t[2])
                        if o.fn is None:
                            continue
                        ins = o.fn(e)
                        if o.dma is not None:
                            ins.then_inc(dsems[o.dma[0]], 16)
                        elif o.inc:
                            s, v = semval(eng, o.incval)
                            ins.then_inc(s, 1)
                if todo:
                    getattr(block, eng)(body)
                self.flushed[eng] = len(ops[eng])

    def finish(self):
        self.flush()


_uid = [0]


def _un(name):
    _uid[0] += 1
    return f"{name}_u{_uid[0]}"


def interleave(gens, depth=2):
    active = []
    it = iter(gens)
    while True:
        while len(active) < depth:
            g = next(it, None)
            if g is None:
                break
            active.append(g)
        if not active:
            break
        for g in list(active):
            try:
                next(g)
            except StopIteration:
                active.remove(g)


_DONE = object()


class Ring:
    def __init__(self, es, nc, name, shape, dt, n):
        self.t = [es.enter_context(nc.sbuf_tensor(_un(f"{name}_{i}"), shape, dt)) for i in range(n)]
        self.b = [Buf(f"{name}_{i}") for i in range(n)]
        self.i = 0

    def next(self):
        i = self.i
        self.i = (i + 1) % len(self.t)
        return self.t[i], self.b[i]


D = 1024
NH = 8
ALPHA = 8 ** 0.25
LN_EPS = 1e-5
RMS_EPS = 1e-6
NEG = -1.0e30


def lambda_init(layer_idx):
    return 0.8 - 0.6 * math.exp(-0.3 * layer_idx)


class _Stop(Exception):
    pass


def build(T, CAP, stop=0, ksub=0, dumpflag=False):
    NT = T // 128
    NCH = T // 64
    NB = T // 512
    nc = bass.Bass("TRN2", target_bir_lowering=False)

    def din(name, shape, dt=F32):
        return nc.dram_tensor(name, shape, dt, kind="ExternalInput").ap()

    x = din("x", [T, D])
    a_w_in = din("a_w_in", [2, D, 4112])
    a_conv_w = din("a_conv_w", [2, 4, 3072])
    a_a_log = din("a_a_log", [2, 8])
    a_dt_bias = din("a_dt_bias", [2, 8])
    a_norm_w = din("a_norm_w", [2, 128])
    a_w_out = din("a_w_out", [2, D, D])
    kv_w = din("kv_w", [D, 2048])
    b_w_q = din("b_w_q", [2, D, D])
    b_lambda = din("b_lambda", [2, 4, 64])
    b_subln_w = din("b_subln_w", [2, 128])
    b_w_out = din("b_w_out", [2, D, D])
    ln_mix_g = din("ln_mix_g", [4, D])
    ln_mix_b = din("ln_mix_b", [4, D])
    ln_ffn_g = din("ln_ffn_g", [4, D])
    ln_ffn_b = din("ln_ffn_b", [4, D])
    moe_w_group = din("moe_w_group", [4, D, 4])
    moe_w_expert = din("moe_w_expert", [4, D, 32])
    moe_w13 = din("moe_w13", [4, 32, D, 1024])
    moe_w2 = din("moe_w2", [4, 32, 512, D])
    out = nc.dram_tensor("out", [T, D], F32, kind="ExternalOutput").ap()
    dbg = nc.dram_tensor("dbg", [T, D], BF16, kind="ExternalOutput").ap() if stop else None
    stage = [0]

    def checkpoint(noraise=False):
        stage[0] += 1
        if stop and stage[0] >= stop:
            if noraise:
                return True
            raise _Stop()
        return False

    h_d = nc.dram_tensor("h_d", [T, D], F32).ap()
    hT_d = nc.dram_tensor("hT_d", [D, T], BF16).ap()
    om_d = nc.dram_tensor("om_d", [T, D], BF16).ap()
    xb_d = nc.dram_tensor("xb_d", [T, D], BF16).ap()
    kT_d = nc.dram_tensor("kT_d", [NH, 2, 64, T], BF16).ap()
    va_d = nc.dram_tensor("va_d", [NH, T, 129], BF16).ap()
    NS = 32 * CAP
    xs_d = nc.dram_tensor("xs_d", [NS + 128, D], BF16).ap()
    ys_h = [nc.dram_tensor(f"ys_d{i}", [NS + 128, 512], F32).ap() for i in range(2)]
    hT_v = hT_d.rearrange("(k p) t -> p k t", p=128)

    k = K(nc)
    out_toks = []
    dumped = set()

    def dump(name, ap, B):
        if not dumpflag or name in dumped:
            return
        dumped.add(name)
        t = nc.dram_tensor("dump_" + name, list(ap.shape), F32, kind="ExternalOutput").ap()
        out_toks.append(k.dma('gpsimd', t, ap, reads=[B]))
    with ExitStack() as top:
        def sbt(es, name, shape, dt):
            return es.enter_context(nc.sbuf_tensor(_un(name), shape, dt))

        ps = [top.enter_context(nc.psum_tensor(f"ps{i}", [128, 512], F32)) for i in range(7)]
        psB = [Buf(f"ps{i}", excl=True) for i in range(7)]
        pb = top.enter_context(nc.psum_tensor("pb", [128, 1024], BF16))
        pbB = Buf("pb", excl=True)

        ident = sbt(top, "ident", [128, 128], F32)
        identb = sbt(top, "identb", [128, 128], BF16)
        ones_f = sbt(top, "ones_f", [128, 128], F32)
        ones_b = sbt(top, "ones_b", [128, 128], BF16)
        triu = sbt(top, "triu", [128, 128], F32)
        triub = sbt(top, "triub", [128, 128], BF16)
        sutri = sbt(top, "sutri", [128, 128], BF16)
        zero_b = sbt(top, "zero_b", [128, 1024], BF16)
        triu4 = sbt(top, "triu4", [64, 4, 64], F32)
        ident4 = sbt(top, "ident4", [64, 4, 64], F32)
        cB = Buf("consts")
        k.op('gpsimd', lambda e: e.iota(ones_f[:], [[1, 128]], base=0, channel_multiplier=-1,
                                        allow_small_or_imprecise_dtypes=True), writes=[cB])
        k.op('vector', lambda e: e.tensor_single_scalar(ident[:], ones_f[:], 0.0, ALU.is_equal), reads=[cB], writes=[cB])
        k.op('vector', lambda e: e.tensor_single_scalar(identb[:], ones_f[:], 0.0, ALU.is_equal), reads=[cB], writes=[cB])
        k.op('vector', lambda e: e.tensor_single_scalar(triu[:], ones_f[:], 0.0, ALU.is_ge), reads=[cB], writes=[cB])
        k.op('vector', lambda e: e.tensor_single_scalar(triub[:], ones_f[:], 0.0, ALU.is_ge), reads=[cB], writes=[cB])
        k.op('vector', lambda e: e.tensor_single_scalar(sutri[:], ones_f[:], 0.0, ALU.is_gt), reads=[cB], writes=[cB])
        for g4 in range(4):
            k.op('vector', lambda e, g4=g4: e.tensor_copy(triu4[:, g4, :], triu[0:64, 0:64]), reads=[cB], writes=[cB])
            k.op('vector', lambda e, g4=g4: e.tensor_copy(ident4[:, g4, :], ident[0:64, 0:64]), reads=[cB], writes=[cB])
        k.op('vector', lambda e: e.memset(ones_f[:], 1.0), reads=[cB], writes=[cB])
        k.op('vector', lambda e: e.memset(ones_b[:], 1.0), writes=[cB])
        k.op('vector', lambda e: e.memset(zero_b[:], 0.0), writes=[cB])
        zero_f = sbt(top, "zero_f", [128, 512], F32)
        k.op('vector', lambda e: e.memset(zero_f[:], 0.0), writes=[cB])
        for r0 in range(0, NS + 128, 128):
            k.dma('sync', xs_d[r0:r0 + 128, :], zero_b[:], reads=[cB])
        for hf in range(2):
            k.dma('sync', ys_h[hf][NS:NS + 128, :], zero_f[:], reads=[cB])
        k.barrier()

        def layer_norm(es_ring, t, tB, gbc, bbc, wBs, y, yB):
            st, stB = es_ring['st'].next()
            jk, jkB = es_ring['junk'].next()
            k.op('scalar', lambda e: e.activation(jk[:], t[:], AF.Copy, accum_out=st[:, 0:1]), reads=[tB], writes=[jkB, stB])
            k.op('scalar', lambda e: e.activation(jk[:], t[:], AF.Square, accum_out=st[:, 1:2]), reads=[tB], writes=[jkB, stB])
            k.op('vector', lambda e: e.tensor_scalar_mul(st[:, 2:3], st[:, 0:1], 1.0 / D), reads=[stB], writes=[stB])
            k.op('vector', lambda e: e.tensor_tensor(st[:, 3:4], st[:, 2:3], st[:, 2:3], ALU.mult), reads=[stB], writes=[stB])
            k.op('vector', lambda e: e.scalar_tensor_tensor(st[:, 4:5], st[:, 1:2], 1.0 / D, st[:, 3:4], ALU.mult, ALU.subtract),
                 reads=[stB], writes=[stB])
            k.op('scalar', lambda e: e.activation(st[:, 5:6], st[:, 4:5], AF.Sqrt, bias=LN_EPS), reads=[stB], writes=[stB])
            k.op('vector', lambda e: e.reciprocal(st[:, 6:7], st[:, 5:6]), reads=[stB], writes=[stB])
            k.op('vector', lambda e: e.tensor_scalar(t[:], t[:], st[:, 2:3], st[:, 6:7], ALU.subtract, ALU.mult), reads=[tB, stB], writes=[tB])
            k.op('gpsimd', lambda e: e.tensor_tensor(t[:], t[:], gbc[:], ALU.mult), reads=[tB] + wBs, writes=[tB])
            k.op('vector', lambda e: e.tensor_tensor(y[:], t[:], bbc[:], ALU.add), reads=[tB] + wBs, writes=[yB])

        def make_hT(rings, y, yB, tile):
            hb, hbB = rings['hTs'].next()
            for half in range(2):
                pp, ppB = ps[5 + half], psB[5 + half]
                for j in range(4):
                    kk = half * 4 + j
                    k.op('tensor', lambda e, pp=pp, j=j, kk=kk: e.transpose(pp[:, j * 128:(j + 1) * 128], y[:, kk * 128:(kk + 1) * 128], ident[:]),
                         reads=[yB, cB], writes=[ppB])
                eng = 'vector' if half == 0 else 'scalar'
                if eng == 'vector':
                    k.op('vector', lambda e, pp=pp, half=half: e.tensor_copy(hb[:, half * 4:(half + 1) * 4, :], pp[:].rearrange("p (a b) -> p a b", a=4)),
                         reads=[ppB], writes=[hbB])
                else:
                    k.op('scalar', lambda e, pp=pp, half=half: e.copy(hb[:, half * 4:(half + 1) * 4, :], pp[:].rearrange("p (a b) -> p a b", a=4)),
                         reads=[ppB], writes=[hbB])
            k.dma('sync', hT_v[:, :, tile * 128:(tile + 1) * 128], hb[:], reads=[hbB])

        with ExitStack() as es:
            rings = {'hTs': Ring(es, nc, "hTs", [128, 8, 128], BF16, 2)}
            xr = Ring(es, nc, "xin", [128, D], F32, 2)
            for t in range(NT):
                xt, xB = xr.next()
                k.dma('sync', xt[:], x[t * 128:(t + 1) * 128, :], writes=[xB])
                k.dma('gpsimd', h_d[t * 128:(t + 1) * 128, :], xt[:], reads=[xB])
                make_hT(rings, xt, xB, t)
            k.barrier()

        def load_w_bf16(es, name, src, cols):
            w = sbt(es, name, [128, 8, cols], BF16)
            wB = Buf(name)
            k.dma('gpsimd', w[:], src.rearrange("(k p) c -> p k c", p=128), writes=[wB])
            return w, wB

        def bcast_row(es, name, src_row, n, dt=F32):
            w = sbt(es, name, [128, n], dt)
            wB = Buf(name)
            k.dma('sync', w[:], src_row.partition_broadcast(128), writes=[wB])
            return w, wB

        def deltanet(l):
            with ExitStack() as es:
                cw = sbt(es, "cw", [128, 24, 4], F32)
                cwB = Buf("cw")
                cwn = sbt(es, "cwn", [4, 3072], F32)
                k.dma('sync', cwn[:], a_conv_w[l], writes=[cwB])
                for part in range(24):
                    k.op('tensor', lambda e, part=part: e.transpose(ps[0][:, part * 4:(part + 1) * 4], cwn[:, part * 128:(part + 1) * 128], ident[0:4, 0:4]),
                         reads=[cwB, cB], writes=[psB[0]])
                k.op('vector', lambda e: e.tensor_copy(cw[:].rearrange("p a b -> p (a b)"), ps[0][:, 0:96]), reads=[psB[0]], writes=[cwB])
                nw, nwB = bcast_row(es, "nw", a_norm_w[l:l + 1, :], 128)
                alog, alB = bcast_row(es, "alog", a_a_log[l:l + 1, :], 8)
                dtb, dtB = bcast_row(es, "dtb", a_dt_bias[l:l + 1, :], 8)
                nea = sbt(es, "nea", [128, 8], F32)
                k.op('scalar', lambda e: e.activation(nea[:], alog[:], AF.Exp), reads=[alB], writes=[alB])
                k.op('vector', lambda e: e.tensor_scalar_mul(nea[:], nea[:], -1.0), reads=[alB], writes=[alB])
                qT = sbt(es, "qT", [128, T], BF16)
                kT = sbt(es, "kT", [128, T], BF16)
                vT = sbt(es, "vT", [128, T], BF16)
                qkvB = [Buf("qT"), Buf("kT"), Buf("vT")]
                qkv = [qT, kT, vT]
                zs = sbt(es, "zs", [64, NCH, 128], BF16)
                zsB = Buf("zs")
                gl = sbt(es, "gl", [64, 8, NCH], F32)
                glB = Buf("gl")
                egl = sbt(es, "egl", [128, NCH], F32)
                eglB = Buf("egl")
                S = sbt(es, "S", [128, 128], F32)
                SB = Buf("S")
                hbr = Ring(es, nc, "hblk", [128, 8, 512], BF16, 2)
                raw = sbt(es, "raw", [128, 3, 515], F32)
                rawB = [Buf("raw0"), Buf("raw1"), Buf("raw2")]
                cvr = Ring(es, nc, "cv", [128, 512], F32, 2)
                sqr = Ring(es, nc, "sq", [128, 512], F32, 2)
                G = 4
                R = 2
                r_kg = Ring(es, nc, "kg", [64, G, 256], F32, R)
                r_kdec = Ring(es, nc, "kdec", [64, G, 128], F32, R)
                r_dm = Ring(es, nc, "dm", [64, G, 64], F32, R)
                r_dg = Ring(es, nc, "dg", [64, G, 64], F32, R)
                r_egb = Ring(es, nc, "egb", [128, G, 64], F32, R)
                r_qg = Ring(es, nc, "qg", [128, G, 64], F32, R)
                r_at = Ring(es, nc, "at", [64, G, 64], F32, R)
                r_X = Ring(es, nc, "X", [64, G, 64], F32, 4)
                r_Y = Ring(es, nc, "Y", [64, G, 64], F32, 4)
                r_P = Ring(es, nc, "P", [64, G, 64], F32, R)
                r_uw = Ring(es, nc, "uw", [64, G, 256], F32, R)
                r_wT = Ring(es, nc, "wT", [128, G, 64], F32, R)
                r_vn = Ring(es, nc, "vn", [64, 128], F32, 2)
                r_o = Ring(es, nc, "o", [64, 128], F32, 2)
                r_ob = Ring(es, nc, "ob", [64, 128], BF16, 2)
                r_st = Ring(es, nc, "dst", [64, 4], F32, 2)
                r_jk = Ring(es, nc, "djk", [64, 128], F32, 2)
                for h in range(NH):
                    wq3, wq3B = [], []
                    wcat = sbt(es, f"wcat{h}", [128, 8, 384], BF16) if h == 0 else wcat_keep[0]
                    wzg = sbt(es, f"wzg{h}", [128, 8, 128], BF16) if h == 0 else wcat_keep[1]
                    if h == 0:
                        wcat_keep = [wcat, wzg]
                        wcB = Buf("wcat")
                        wzB = Buf("wzg")
                    for part in range(3):
                        k.dma('gpsimd', wcat[:, :, part * 128:(part + 1) * 128],
                              a_w_in[l, :, part * 1024 + h * 128: part * 1024 + (h + 1) * 128].rearrange("(k p) c -> p k c", p=128), writes=[wcB])
                    k.dma('gpsimd', wzg[:], a_w_in[l, :, 3072 + h * 128:3072 + (h + 1) * 128].rearrange("(k p) c -> p k c", p=128), writes=[wzB])
                    if h == 0:
                        wg16 = sbt(es, "wg16", [128, 8, 16], BF16)
                        k.dma('gpsimd', wg16[:], a_w_in[l, :, 4096:4112].rearrange("(k p) c -> p k c", p=128), writes=[wzB])
                    for part in range(3):
                        k.op('vector', lambda e, part=part: e.memset(raw[:, part, 0:3], 0.0), writes=[rawB[part]])
                    if ksub == 5:
                        k.barrier()
                        return
                    for blk in range(NB):
                        hb, hbB = hbr.next()
                        k.dma('sync', hb[:], hT_v[:, :, blk * 512:(blk + 1) * 512], writes=[hbB])
                        for part in range(3):
                            pp, ppB = ps[part % 2], psB[part % 2]
                            for kc in range(8):
                                k.op('tensor', lambda e, pp=pp, kc=kc, part=part, hb=hb: e.matmul(pp[:, :], wcat[:, kc, part * 128:(part + 1) * 128], hb[:, kc, :],
                                                                                            start=(kc == 0), stop=(kc == 7)),
                                     reads=[wcB, hbB], writes=[ppB])
                            k.op('scalar', lambda e, pp=pp, part=part: e.copy(raw[:, part, 3:515], pp[:, :]), reads=[ppB], writes=[rawB[part]])
                            if ksub == 11:
                                k.barrier()
                                return
                            cv, cvB = cvr.next()
                            ci = part * 8 + h
                            k.op('vector', lambda e, cv=cv, part=part, ci=ci: e.tensor_scalar_mul(cv[:], raw[:, part, 0:512], cw[:, ci, 0:1]),
                                 reads=[rawB[part], cwB], writes=[cvB])
                            for j in range(1, 4):
                                k.op('vector', lambda e, cv=cv, part=part, ci=ci, j=j: e.scalar_tensor_tensor(cv[:], raw[:, part, j:j + 512], cw[:, ci, j:j + 1], cv[:],
                                                                                                        ALU.mult, ALU.add),
                                     reads=[rawB[part], cwB, cvB], writes=[cvB])
                            k.op('vector', lambda e, part=part: e.tensor_copy(raw[:, part, 0:3], raw[:, part, 512:515]), reads=[rawB[part]], writes=[rawB[part]])
                            if ksub == 12:
                                k.barrier()
                                return
                            dst = qkv[part][:, blk * 512:(blk + 1) * 512]
                            if part == 2:
                                k.op('scalar', lambda e, cv=cv, dst=dst: e.activation(dst, cv[:], AF.Silu), reads=[cvB], writes=[qkvB[part]])
                            else:
                                k.op('scalar', lambda e, cv=cv: e.activation(cv[:], cv[:], AF.Silu), reads=[cvB], writes=[cvB])
                                sq, sqB = sqr.next()
                                k.op('gpsimd', lambda e, cv=cv, sq=sq: e.tensor_tensor(sq[:], cv[:], cv[:], ALU.mult), reads=[cvB], writes=[sqB])
                                p2, p2B = ps[2], psB[2]
                                for hf in range(2):
                                    k.op('tensor', lambda e, sq=sq, p2=p2, hf=hf: e.matmul(p2[:, hf * 256:(hf + 1) * 256], ones_f[:], sq[:, hf * 256:(hf + 1) * 256], start=True, stop=True), reads=[sqB, cB], writes=[p2B])
                                k.op('scalar', lambda e, sq=sq, p2=p2: e.activation(sq[:], p2[:, :], AF.Sqrt, bias=RMS_EPS), reads=[p2B], writes=[sqB])
                                k.op('vector', lambda e, sq=sq: e.reciprocal(sq[:], sq[:]), reads=[sqB], writes=[sqB])
                                sc = (128 ** -0.5) if part == 0 else 1.0
                                k.op('vector', lambda e, cv=cv, sq=sq, dst=dst, sc=sc: e.scalar_tensor_tensor(dst, cv[:], sc, sq[:], ALU.mult, ALU.mult),
                                     reads=[cvB, sqB], writes=[qkvB[part]])
                            if ksub == 13:
                                k.barrier()
                                return
                        if ksub == 14:
                            k.barrier()
                            return
                        for cc in range(8):
                            c = blk * 8 + cc
                            pp, ppB = ps[3 + cc % 2], psB[3 + cc % 2]
                            for kc in range(8):
                                k.op('tensor', lambda e, pp=pp, kc=kc, cc=cc, hb=hb: e.matmul(pp[0:64, 0:128], hb[:, kc, cc * 64:(cc + 1) * 64], wzg[:, kc, :],
                                                                                       start=(kc == 0), stop=(kc == 7)),
                                     reads=[wzB, hbB], writes=[ppB])
                            for kc in range(8):
                                k.op('tensor', lambda e, pp=pp, kc=kc, cc=cc, hb=hb: e.matmul(pp[0:64, 128:144], hb[:, kc, cc * 64:(cc + 1) * 64], wg16[:, kc, :],
                                                                                       start=(kc == 0), stop=(kc == 7)),
                                     reads=[wzB, hbB], writes=[ppB])
                            k.op('scalar', lambda e, pp=pp, c=c: e.activation(zs[:, c, :], pp[0:64, 0:128], AF.Silu), reads=[ppB], writes=[zsB])
                            k.op('vector', lambda e, pp=pp, c=c, h=h: e.tensor_copy(gl[:, 0, c:c + 1], pp[0:64, 128 + h:129 + h]), reads=[ppB], writes=[glB])
                            k.op('vector', lambda e, pp=pp, c=c, h=h: e.tensor_copy(gl[:, 1, c:c + 1], pp[0:64, 136 + h:137 + h]), reads=[ppB], writes=[glB])
                    if ksub == 1:
                        k.barrier()
                        return
                    k.op('scalar', lambda e: e.activation(gl[:, 0, :], gl[:, 0, :], AF.Sigmoid), reads=[glB], writes=[glB])
                    k.op('vector', lambda e: e.tensor_scalar_mul(gl[:, 5, :], gl[:, 0, :], -1.0), reads=[glB], writes=[glB])
                    k.op('scalar', lambda e, h=h: e.activation(gl[:, 1, :], gl[:, 1, :], AF.Exp, bias=dtb[0:64, h:h + 1]), reads=[glB, dtB], writes=[glB])
                    k.op('scalar', lambda e: e.activation(gl[:, 1, :], gl[:, 1, :], AF.Ln, bias=1.0), reads=[glB], writes=[glB])
                    k.op('vector', lambda e, h=h: e.tensor_scalar_mul(gl[:, 1, :], gl[:, 1, :], nea[0:64, h:h + 1]), reads=[glB, alB], writes=[glB])
                    for c0 in range(0, NCH, 512):
                        n = min(512, NCH - c0)
                        k.op('tensor', lambda e, c0=c0, n=n: e.matmul(ps[0][0:64, 0:n], triu[0:64, 0:64], gl[:, 1, c0:c0 + n], start=True, stop=True),
                             reads=[glB, cB], writes=[psB[0]])
                        k.op('vector', lambda e, c0=c0, n=n: e.tensor_copy(gl[:, 2, c0:c0 + n], ps[0][0:64, 0:n]), reads=[psB[0]], writes=[glB])
                        k.op('tensor', lambda e, c0=c0, n=n: e.matmul(ps[1][:, 0:n], ones_f[0:64, :], gl[:, 1, c0:c0 + n], start=True, stop=True),
                             reads=[glB, cB], writes=[psB[1]])
                        k.op('scalar', lambda e, c0=c0, n=n: e.activation(egl[:, c0:c0 + n], ps[1][:, 0:n], AF.Exp), reads=[psB[1]], writes=[eglB])
                        k.op('vector', lambda e, c0=c0, n=n: e.tensor_copy(gl[:, 6, c0:c0 + n], ps[1][0:64, 0:n]), reads=[psB[1]], writes=[glB])
                    k.op('scalar', lambda e: e.activation(gl[:, 3, :], gl[:, 2, :], AF.Exp), reads=[glB], writes=[glB])
                    k.op('vector', lambda e: e.tensor_tensor(gl[:, 7, :], gl[:, 6, :], gl[:, 2, :], ALU.subtract), reads=[glB], writes=[glB])
                    k.op('scalar', lambda e: e.activation(gl[:, 4, :], gl[:, 7, :], AF.Exp), reads=[glB], writes=[glB])
                    k.op('vector', lambda e: e.memset(S[:], 0.0), writes=[SB])
                    if h == 0 and l == 0:
                        dump("gl", gl[:], glB)
                        dump("egl", egl[:], eglB)
                        dump("qT", qT[:, 0:128], qkvB[0])
                        dump("kT", kT[:, 0:128], qkvB[1])
                        dump("vT", vT[:, 0:128], qkvB[2])
                        dump("zs", zs[:, 0:2, :], zsB)
                    if ksub == 2:
                        k.barrier()
                        return

                    def bulk(gi, res):
                        c0 = gi * G
                        gcols = slice(c0 * 64, (c0 + G) * 64)
                        csl = [slice((c0 + g) * 64, (c0 + g + 1) * 64) for g in range(G)]
                        kg, kgB = r_kg.next()
                        kd, kdB = r_kdec.next()
                        for g in range(G):
                            k.op('tensor', lambda e, g=g: e.transpose(pb[0:64, g * 256:g * 256 + 128], kT[:, csl[g]], identb[:]), reads=[qkvB[1], cB], writes=[pbB])
                            k.op('tensor', lambda e, g=g: e.transpose(pb[0:64, g * 256 + 128:g * 256 + 256], vT[:, csl[g]], identb[:]), reads=[qkvB[2], cB], writes=[pbB])
                        yield
                        for g in range(G):
                            c = c0 + g
                            k.op('vector', lambda e, g=g, c=c: e.tensor_scalar_mul(kg[:, g, 128:256], pb[0:64, g * 256:g * 256 + 128], gl[:, 3, c:c + 1]), reads=[pbB, glB], writes=[kgB])
                            k.op('vector', lambda e, g=g, c=c: e.tensor_scalar_mul(kd[:, g, :], pb[0:64, g * 256:g * 256 + 128], gl[:, 4, c:c + 1]), reads=[pbB, glB], writes=[kdB])
                        k.op('scalar', lambda e: e.copy(kg[:, :, 0:128], pb[0:64, :].rearrange("p (g c) -> p g c", g=G)[:, :, 128:256]), reads=[pbB], writes=[kgB])
                        yield
                        p0, p0B = ps[0], psB[0]
                        for g in range(G):
                            k.op('tensor', lambda e, g=g: e.matmul(p0[0:64, g * 128:g * 128 + 64], kT[:, csl[g]], kT[:, csl[g]], start=True, stop=True), reads=[qkvB[1]], writes=[p0B])
                            k.op('tensor', lambda e, g=g: e.matmul(p0[0:64, g * 128 + 64:g * 128 + 128], kT[:, csl[g]], qT[:, csl[g]], start=True, stop=True), reads=[qkvB[1], qkvB[0]], writes=[p0B])
                        p0v = p0[0:64, :].rearrange("p (g c) -> p g c", g=G)
                        yield
                        dg, dgB = r_dg.next()
                        for g in range(G):
                            c = c0 + g
                            k.op('gpsimd', lambda e, g=g, c=c: e.tensor_scalar_mul(dg[:, g, :], ident[0:64, 0:64], gl[:, 2, c:c + 1]), reads=[glB, cB], writes=[dgB])
                        p1, p1B = ps[1], psB[1]
                        for g in range(G):
                            k.op('tensor', lambda e, g=g: e.matmul(p1[:, g * 64:(g + 1) * 64], ones_f[0:64, :], dg[:, g, :], start=True, stop=True), reads=[dgB, cB], writes=[p1B])
                        yield
                        dm, dmB = r_dm.next()
                        for g in range(G):
                            c = c0 + g
                            k.op('vector', lambda e, g=g, c=c: e.tensor_scalar(dm[:, g, :], p1[0:64, g * 64:(g + 1) * 64], gl[:, 2, c:c + 1], 0.0, ALU.subtract, ALU.min), reads=[p1B, glB], writes=[dmB])
                        yield
                        egb, egbB = r_egb.next()
                        k.op('scalar', lambda e: e.activation(egb[:].rearrange("p g c -> p (g c)"), p1[:, 0:G * 64], AF.Exp), reads=[p1B], writes=[egbB])
                        k.op('scalar', lambda e: e.activation(dm[:], dm[:], AF.Exp), reads=[dmB], writes=[dmB])
                        k.op('vector', lambda e: e.tensor_tensor(dm[:], dm[:], triu4[:], ALU.mult), reads=[dmB, cB], writes=[dmB])
                        qg, qgB = r_qg.next()
                        k.op('gpsimd', lambda e: e.tensor_tensor(qg[:].rearrange("p g c -> p (g c)"), qT[:, gcols], egb[:].rearrange("p g c -> p (g c)"), ALU.mult), reads=[qkvB[0], egbB], writes=[qgB])
                        yield
                        at, atB = r_at.next()
                        k.op('vector', lambda e: e.tensor_tensor(at[:], p0v[:, :, 64:128], dm[:], ALU.mult), reads=[p0B, dmB], writes=[atB])
                        k.op('vector', lambda e: e.tensor_tensor(dm[:], dm[:], ident4[:], ALU.subtract), reads=[dmB, cB], writes=[dmB])
                        X, XB = r_X.next()
                        for g in range(G):
                            c = c0 + g
                            k.op('vector', lambda e, X=X, g=g, c=c: e.scalar_tensor_tensor(X[:, g, :], p0[0:64, g * 128:g * 128 + 64], gl[:, 5, c:c + 1], dm[:, g, :], ALU.mult, ALU.mult),
                                 reads=[p0B, glB, dmB], writes=[XB])
                        yield
                        p2, p2B = ps[2], psB[2]
                        p3, p3B = ps[3], psB[3]
                        p4, p4B = ps[4], psB[4]
                        for g in range(G):
                            k.op('tensor', lambda e, X=X, g=g: e.transpose(p3[0:64, g * 64:(g + 1) * 64], X[:, g, :], ident[0:64, 0:64]), reads=[XB, cB], writes=[p3B])
                        yield
                        Y, YB = r_Y.next()
                        k.op('scalar', lambda e, Y=Y: e.copy(Y[:].rearrange("p g c -> p (g c)"), p3[0:64, 0:G * 64]), reads=[p3B], writes=[YB])
                        P, PB = r_P.next()
                        k.op('vector', lambda e, X=X: e.tensor_tensor(P[:], X[:], ident4[:], ALU.add), reads=[XB, cB], writes=[PB])
                        for s in range(1, 6):
                            if s < 5:
                                for g in range(G):
                                    k.op('tensor', lambda e, X=X, Y=Y, g=g: e.matmul(p2[0:64, g * 64:(g + 1) * 64], Y[:, g, :], X[:, g, :], start=True, stop=True), reads=[XB, YB], writes=[p2B])
                            for g in range(G):
                                k.op('tensor', lambda e, X=X, Y=Y, g=g: e.matmul(p3[0:64, g * 64:(g + 1) * 64], X[:, g, :], Y[:, g, :], start=True, stop=True), reads=[XB, YB], writes=[p3B])
                            yield
                            Yn, YnB = r_Y.next()
                            k.op('scalar', lambda e, Yn=Yn: e.copy(Yn[:].rearrange("p g c -> p (g c)"), p3[0:64, 0:G * 64]), reads=[p3B], writes=[YnB])
                            if s < 5:
                                Xn, XnB = r_X.next()
                                k.op('vector', lambda e, Xn=Xn: e.tensor_copy(Xn[:].rearrange("p g c -> p (g c)"), p2[0:64, 0:G * 64]), reads=[p2B], writes=[XnB])
                            yield
                            for g in range(G):
                                k.op('tensor', lambda e, Yn=Yn, g=g: e.matmul(p4[0:64, g * 64:(g + 1) * 64], Yn[:, g, :], P[:, g, :], start=True, stop=True), reads=[YnB, PB], writes=[p4B])
                            k.op('vector', lambda e: e.tensor_tensor(P[:].rearrange("p g c -> p (g c)"), P[:].rearrange("p g c -> p (g c)"), p4[0:64, 0:G * 64], ALU.add), reads=[p4B, PB], writes=[PB])
                            yield
                            Y, YB = Yn, YnB
                            if s < 5:
                                X, XB = Xn, XnB
                        yield
                        uw, uwB = r_uw.next()
                        for rr in range(G // 2):
                            for g2 in range(2):
                                g = rr * 2 + g2
                                k.op('tensor', lambda e, g=g, g2=g2: e.matmul(p4[0:64, g2 * 256:(g2 + 1) * 256], P[:, g, :], kg[:, g, :], start=True, stop=True), reads=[PB, kgB], writes=[p4B])
                            for g2 in range(2):
                                g = rr * 2 + g2
                                c = c0 + g
                                k.op('vector', lambda e, g=g, g2=g2, c=c: e.tensor_scalar_mul(uw[:, g, :], p4[0:64, g2 * 256:(g2 + 1) * 256], gl[:, 0, c:c + 1]), reads=[p4B, glB], writes=[uwB])
                        yield
                        for g in range(G):
                            k.op('tensor', lambda e, g=g: e.transpose(p1[:, g * 64:(g + 1) * 64], uw[:, g, 128:256], ident[0:64, 0:64]), reads=[uwB, cB], writes=[p1B])
                        wT, wTB = r_wT.next()
                        k.op('vector', lambda e: e.tensor_copy(wT[:].rearrange("p g c -> p (g c)"), p1[:, 0:G * 64]), reads=[p1B], writes=[wTB])
                        res.update(dict(uw=(uw, uwB), wT=(wT, wTB), qg=(qg, qgB), at=(at, atB), kd=(kd, kdB)))

                    def scan(c, r, g):
                        uw_, uwB = r['uw']
                        wT_, wTB = r['wT']
                        qg_, qgB = r['qg']
                        at_, atB = r['at']
                        kd_, kdB = r['kd']
                        uw, wT, qg, at, kd = uw_[:, g, :], wT_[:, g, :], qg_[:, g, :], at_[:, g, :], kd_[:, g, :]
                        p5, p5B = ps[5], psB[5]
                        k.op('tensor', lambda e: e.matmul(p5[0:64, 0:128], wT, S[:], start=True, stop=True), reads=[wTB, SB], writes=[p5B])
                        yield
                        vn, vnB = r_vn.next()
                        k.op('vector', lambda e: e.tensor_tensor(vn[:], uw[:, 0:128], p5[0:64, 0:128], ALU.subtract), reads=[uwB, p5B], writes=[vnB])
                        yield
                        k.op('tensor', lambda e: e.matmul(p5[0:64, 128:256], qg, S[:], start=True, stop=False), reads=[qgB, SB], writes=[p5B])
                        k.op('tensor', lambda e: e.matmul(p5[0:64, 128:256], at, vn[:], start=False, stop=True), reads=[atB, vnB], writes=[p5B])
                        p6, p6B = ps[6], psB[6]
                        k.op('tensor', lambda e: e.matmul(p6[:, 0:128], kd, vn[:], start=True, stop=True), reads=[kdB, vnB], writes=[p6B])
                        yield
                        k.op('vector', lambda e: e.scalar_tensor_tensor(S[:], S[:], egl[:, c:c + 1], p6[:, 0:128], ALU.mult, ALU.add),
                             reads=[SB, eglB, p6B], writes=[SB])
                        yield
                        o, oB = r_o.next()
                        st, stB = r_st.next()
                        jk, jkB = r_jk.next()
                        k.op('scalar', lambda e: e.copy(o[:], p5[0:64, 128:256]), reads=[p5B], writes=[oB])
                        if h == 0 and l == 0 and c <= 1:
                            dump(f"vn{c}", vn[:], vnB)
                            dump(f"o{c}", o[:], oB)
                            dump(f"S{c}", S[:], SB)
                        k.op('scalar', lambda e: e.activation(jk[:], o[:], AF.Square, accum_out=st[:, 0:1]), reads=[oB], writes=[jkB, stB])
                        k.op('scalar', lambda e: e.activation(st[:, 1:2], st[:, 0:1], AF.Sqrt, bias=RMS_EPS, scale=1.0 / 128), reads=[stB], writes=[stB])
                        yield
                        k.op('vector', lambda e: e.reciprocal(st[:, 2:3], st[:, 1:2]), reads=[stB], writes=[stB])
                        k.op('vector', lambda e: e.scalar_tensor_tensor(o[:], o[:], st[:, 2:3], nw[0:64, :], ALU.mult, ALU.mult), reads=[oB, stB, nwB], writes=[oB])
                        ob, obB = r_ob.next()
                        k.op('gpsimd', lambda e: e.tensor_tensor(ob[:], o[:], zs[:, c, :], ALU.mult), reads=[oB, zsB], writes=[obB])
                        k.dma('sync', om_d[c * 64:(c + 1) * 64, h * 128:(h + 1) * 128], ob[:], reads=[obB])

                    assert NCH % G == 0
                    pend = {}
                    for _ in bulk(0, pend):
                        pass
                    for gi in range(NCH // G):
                        nxt = {}
                        bg = bulk(gi + 1, nxt) if gi + 1 < NCH // G else None
                        for g in range(G):
                            for _ in scan(gi * G + g, pend, g):
                                for _r in range(2):
                                    if bg is not None and next(bg, _DONE) is _DONE:
                                        bg = None
                        if bg is not None:
                            for _ in bg:
                                pass
                        pend = nxt
                k.barrier()

        def shared_kv():
            with ExitStack() as es:
                hbr = Ring(es, nc, "khb", [128, 8, 512], BF16, 2)
                kr = Ring(es, nc, "kst", [64, 512], BF16, 3)
                vr = Ring(es, nc, "vst", [128, 129], BF16, 3)
                for h in range(NH):
                    wk, wkB = load_w_bf16(es, f"wk{h}", kv_w[:, h * 128:(h + 1) * 128], 128) if h == 0 else (wk_keep, wkB_keep)
                    wv, wvB = load_w_bf16(es, f"wv{h}", kv_w[:, 1024 + h * 128:1024 + (h + 1) * 128], 128) if h == 0 else (wv_keep, wvB_keep)
                    if h == 0:
                        wk_keep, wkB_keep, wv_keep, wvB_keep = wk, wkB, wv, wvB
                    else:
                        k.dma('gpsimd', wk[:], kv_w[:, h * 128:(h + 1) * 128].rearrange("(k p) c -> p k c", p=128), writes=[wkB])
                        k.dma('gpsimd', wv[:], kv_w[:, 1024 + h * 128:1024 + (h + 1) * 128].rearrange("(k p) c -> p k c", p=128), writes=[wvB])
                    for blk in range(NB):
                        hb, hbB = hbr.next()
                        k.dma('sync', hb[:], hT_v[:, :, blk * 512:(blk + 1) * 512], writes=[hbB])
                        for s in range(2):
                            pp, ppB = ps[s], psB[s]
                            for kc in range(8):
                                k.op('tensor', lambda e, pp=pp, kc=kc, s=s, hb=hb: e.matmul(pp[0:64, :], wk[:, kc, s * 64:(s + 1) * 64], hb[:, kc, :],
                                                                                     start=(kc == 0), stop=(kc == 7)), reads=[wkB, hbB], writes=[ppB])
                            kt, ktB = kr.next()
                            k.op('scalar' if s else 'vector', (lambda e, kt=kt, pp=pp: e.copy(kt[:], pp[0:64, :])) if s else
                                 (lambda e, kt=kt, pp=pp: e.tensor_copy(kt[:], pp[0:64, :])), reads=[ppB], writes=[ktB])
                            k.dma('sync', kT_d[h, s, :, blk * 512:(blk + 1) * 512], kt[:], reads=[ktB])
                        for tt in range(4):
                            pp, ppB = ps[2 + tt % 2], psB[2 + tt % 2]
                            for kc in range(8):
                                k.op('tensor', lambda e, pp=pp, kc=kc, tt=tt, hb=hb: e.matmul(pp[:, 0:128], hb[:, kc, tt * 128:(tt + 1) * 128], wv[:, kc, :],
                                                                                       start=(kc == 0), stop=(kc == 7)), reads=[wvB, hbB], writes=[ppB])
                            vt, vtB = vr.next()
                            k.op('vector', lambda e, vt=vt, pp=pp: e.tensor_copy(vt[:, 0:128], pp[:, 0:128]), reads=[ppB], writes=[vtB])
                            k.op('gpsimd', lambda e, vt=vt: e.memset(vt[:, 128:129], 1.0), writes=[vtB])
                            tok0 = blk * 512 + tt * 128
                            k.dma('sync', va_d[h, tok0:tok0 + 128, :], vt[:], reads=[vtB])
                k.barrier()

        def diffattn(j, layer):
            lam_init = lambda_init(layer)
            with ExitStack() as es:
                lp, lpB = bcast_row(es, "lp", b_lambda[j:j + 1].rearrange("o a b -> o (a b)"), 256)
                sw, swB = bcast_row(es, "sw", b_subln_w[j:j + 1, :], 128)
                lam = sbt(es, "lam", [128, 8], F32)
                lamB = Buf("lam")
                pr = sbt(es, "lpr", [128, 128], F32)
                k.op('vector', lambda e: e.tensor_tensor(pr[:, 0:64], lp[:, 0:64], lp[:, 64:128], ALU.mult), reads=[lpB], writes=[lamB])
                k.op('vector', lambda e: e.tensor_tensor(pr[:, 64:128], lp[:, 128:192], lp[:, 192:256], ALU.mult), reads=[lpB], writes=[lamB])
                k.op('vector', lambda e: e.reduce_sum(lam[:, 0:1], pr[:, 0:64], AX.X), reads=[lamB], writes=[lamB])
                k.op('vector', lambda e: e.reduce_sum(lam[:, 1:2], pr[:, 64:128], AX.X), reads=[lamB], writes=[lamB])
                k.op('scalar', lambda e: e.activation(lam[:, 2:4], lam[:, 0:2], AF.Exp), reads=[lamB], writes=[lamB])
                k.op('vector', lambda e: e.tensor_tensor(lam[:, 4:5], lam[:, 2:3], lam[:, 3:4], ALU.subtract), reads=[lamB], writes=[lamB])
                k.op('vector', lambda e: e.tensor_scalar(lam[:, 5:6], lam[:, 4:5], lam_init, -1.0, ALU.add, ALU.mult), reads=[lamB], writes=[lamB])
                k.op('vector', lambda e: e.tensor_scalar_mul(sw[:], sw[:], 1.0 - lam_init), reads=[swB], writes=[swB])
                qs = [sbt(es, f"qs{s}", [64, T], BF16) for s in range(2)]
                qsB = [Buf("qs0"), Buf("qs1")]
                kTs = [sbt(es, f"kTs{s}", [64, T], BF16) for s in range(2)]
                kTB = [Buf("kT0"), Buf("kT1")]
                va = sbt(es, "va", [128, NT, 144], BF16)
                vaB = Buf("va")
                hbr = Ring(es, nc, "ahb", [128, 8, 512], BF16, 2)
                ptr = Ring(es, nc, "pt", [128, 512], BF16, 6)
                r_o1 = Ring(es, nc, "ao1", [128, 128], F32, 2)
                r_st = Ring(es, nc, "ast", [128, 8], F32, 2)
                r_jk = Ring(es, nc, "ajk", [128, 128], F32, 2)
                r_ob = Ring(es, nc, "aob", [128, 128], BF16, 2)
                wq = sbt(es, "wq", [128, 8, 128], BF16)
                wqB = Buf("wq")
                acc = {}
                slots = [(4, 0), (4, 144), (4, 288), (5, 0), (5, 144), (5, 288), (6, 0), (6, 144)]
                for s in range(2):
                    for i in range(4):
                        acc[(s, i)] = slots[s * 4 + i]
                for h in range(NH):
                    k.dma('gpsimd', wq[:], b_w_q[j, :, h * 128:(h + 1) * 128].rearrange("(k p) c -> p k c", p=128), writes=[wqB])
                    for s in range(2):
                        k.dma('sync', kTs[s][:], kT_d[h, s], writes=[kTB[s]])
                    k.dma('sync', va[:, :, 0:129], va_d[h].rearrange("(t p) c -> p t c", p=128), writes=[vaB])
                    for blk in range(NB):
                        hb, hbB = hbr.next()
                        k.dma('sync', hb[:], hT_v[:, :, blk * 512:(blk + 1) * 512], writes=[hbB])
                        for s in range(2):
                            pp, ppB = ps[s], psB[s]
                            for kc in range(8):
                                k.op('tensor', lambda e, pp=pp, kc=kc, s=s, hb=hb: e.matmul(pp[0:64, :], wq[:, kc, s * 64:(s + 1) * 64], hb[:, kc, :],
                                                                                     start=(kc == 0), stop=(kc == 7)), reads=[wqB, hbB], writes=[ppB])
                            k.op('scalar', lambda e, pp=pp, s=s, blk=blk: e.activation(qs[s][:, blk * 512:(blk + 1) * 512], pp[0:64, :], AF.Copy, scale=0.125),
                                 reads=[ppB], writes=[qsB[s]])
                    for qb in range(NB):
                        nkt = 4 * qb + 4
                        steps = [(kt, s) for kt in range(nkt) for s in range(2)]
                        LA = 2

                        def front(idx, qb=qb):
                            kt, s = steps[idx]
                            jd = kt - 4 * qb
                            lo = 0 if jd < 0 else jd * 128
                            n = 512 - lo
                            pp, ppB = ps[idx % 4], psB[idx % 4]
                            k.op('tensor', lambda e, pp=pp, s=s, kt=kt, qb=qb, lo=lo, n=n: e.matmul(pp[:, 0:n], kTs[s][:, kt * 128:(kt + 1) * 128],
                                                                                             qs[s][:, qb * 512 + lo:(qb + 1) * 512], start=True, stop=True),
                                 reads=[kTB[s], qsB[s]], writes=[ppB])
                            pt, ptB = ptr.next()
                            k.op('scalar', lambda e, pt=pt, pp=pp, n=n: e.activation(pt[:, 0:n], pp[:, 0:n], AF.Exp), reads=[ppB], writes=[ptB])
                            if jd >= 0:
                                k.op('vector', lambda e, pt=pt: e.tensor_tensor(pt[:, 0:128], pt[:, 0:128], triub[:], ALU.mult), reads=[ptB, cB], writes=[ptB])
                            return pt, ptB, jd, lo

                        def back(idx, fr, qb=qb):
                            kt, s = steps[idx]
                            pt, ptB, jd, lo = fr
                            for i in range(max(jd, 0), 4):
                                bank, off = acc[(s, i)]
                                c0 = i * 128 - lo
                                st_flag = (kt == 0 and off == 0)
                                k.op('tensor', lambda e, pt=pt, bank=bank, off=off, c0=c0, kt=kt, i=i, qb=qb, st_flag=st_flag: e.matmul(
                                    ps[bank][:, off:off + 129], pt[:, c0:c0 + 128], va[:, kt, 0:129], start=st_flag, stop=(kt == 4 * qb + i),
                                    skip_group_check=True),
                                    reads=[ptB, vaB], writes=[psB[bank]])

                        fronts = {}
                        for idx in range(len(steps) + LA):
                            if idx < len(steps):
                                fronts[idx] = front(idx)
                            if idx - LA >= 0:
                                back(idx - LA, fronts.pop(idx - LA))
                        for i in range(4):
                            b1, f1 = acc[(0, i)]
                            b2, f2 = acc[(1, i)]
                            st, stB = r_st.next()
                            o1, o1B = r_o1.next()
                            jk, jkB = r_jk.next()
                            ob, obB = r_ob.next()
                            k.op('vector', lambda e, st=st, b1=b1, f1=f1: e.reciprocal(st[:, 0:1], ps[b1][:, f1 + 128:f1 + 129]), reads=[psB[b1]], writes=[stB])
                            k.op('vector', lambda e, st=st, b2=b2, f2=f2: e.reciprocal(st[:, 1:2], ps[b2][:, f2 + 128:f2 + 129]), reads=[psB[b2]], writes=[stB])
                            k.op('vector', lambda e, st=st: e.tensor_tensor(st[:, 2:3], st[:, 1:2], lam[:, 5:6], ALU.mult), reads=[stB, lamB], writes=[stB])
                            k.op('vector', lambda e, st=st, o1=o1, b1=b1, f1=f1: e.tensor_scalar_mul(o1[:], ps[b1][:, f1:f1 + 128], st[:, 0:1]),
                                 reads=[psB[b1], stB], writes=[o1B])
                            k.op('vector', lambda e, st=st, o1=o1, b2=b2, f2=f2: e.scalar_tensor_tensor(o1[:], ps[b2][:, f2:f2 + 128], st[:, 2:3], o1[:], ALU.mult, ALU.add),
                                 reads=[psB[b2], stB, o1B], writes=[o1B])
                            k.op('scalar', lambda e, st=st, o1=o1, jk=jk: e.activation(jk[:], o1[:], AF.Square, accum_out=st[:, 3:4]), reads=[o1B], writes=[jkB, stB])
                            k.op('scalar', lambda e, st=st: e.activation(st[:, 4:5], st[:, 3:4], AF.Sqrt, bias=RMS_EPS, scale=1.0 / 128), reads=[stB], writes=[stB])
                            k.op('vector', lambda e, st=st: e.reciprocal(st[:, 5:6], st[:, 4:5]), reads=[stB], writes=[stB])
                            k.op('vector', lambda e, st=st, o1=o1, ob=ob: e.scalar_tensor_tensor(ob[:], o1[:], st[:, 5:6], sw[:], ALU.mult, ALU.mult),
                                 reads=[o1B, stB, swB], writes=[obB])
                            t0 = qb * 512 + i * 128
                            k.dma('sync', om_d[t0:t0 + 128, h * 128:(h + 1) * 128], ob[:], reads=[obB])
                k.barrier()

        def tok_moe(layer, w_out_src, last):
            with ExitStack() as es:
                wo, woB = load_w_bf16(es, "wo", w_out_src, 1024)
                wr = sbt(es, "wr", [128, 8, 40], F32)
                wrB = Buf("wr")
                k.dma('sync', wr[:, :, 0:4], moe_w_group[layer].rearrange("(k p) c -> p k c", p=128), writes=[wrB])
                k.dma('sync', wr[:, :, 4:36], moe_w_expert[layer].rearrange("(k p) c -> p k c", p=128), writes=[wrB])
                g1, g1B = bcast_row(es, "lng1", ln_mix_g[layer:layer + 1, :], D)
                b1, b1B = bcast_row(es, "lnb1", ln_mix_b[layer:layer + 1, :], D)
                lnB1 = Buf("ln1")
                oh = [sbt(es, f"oh{i}", [128, NT, 32], F32) for i in range(2)]
                ohB = Buf("oh")
                gates = sbt(es, "gates", [128, NT, 2], F32)
                gB = Buf("gates")
                rings = {'hTs': Ring(es, nc, "hTs2", [128, 8, 128], BF16, 2), 'st': Ring(es, nc, "lst", [128, 8], F32, 2),
                         'junk': Ring(es, nc, "ljk", [128, D], F32, 1)}
                with ExitStack() as e1:
                    r_om = Ring(e1, nc, "om", [128, D], BF16, 2)
                    r_omT = Ring(e1, nc, "omT", [128, 8, 128], BF16, 2)
                    r_h = Ring(e1, nc, "hh", [128, D], F32, 2)
                    r_t = Ring(e1, nc, "tt", [128, D], F32, 2)
                    r_y = Ring(e1, nc, "yy", [128, D], F32, 2)
                    r_xb = Ring(e1, nc, "xb", [128, D], BF16, 2)
                    r_xT = Ring(e1, nc, "xT", [128, 8, 128], F32, 2)
                    r_rt = Ring(e1, nc, "rt", [128, 160], F32, 2)
                    def tok_body(t):
                        rows = slice(t * 128, (t + 1) * 128)
                        om, omB = r_om.next()
                        k.dma('sync', om[:], om_d[rows, :], writes=[omB])
                        hh, hhB = r_h.next()
                        k.dma('sync', hh[:], h_d[rows, :], writes=[hhB])
                        for kc in range(8):
                            k.op('tensor', lambda e, om=om, kc=kc: e.transpose(pb[:, kc * 128:(kc + 1) * 128], om[:, kc * 128:(kc + 1) * 128], identb[:]),
                                 reads=[omB, cB], writes=[pbB])
                        omT, omTB = r_omT.next()
                        k.op('vector', lambda e, omT=omT: e.tensor_copy(omT[:], pb[:].rearrange("p (a b) -> p a b", a=8)), reads=[pbB], writes=[omTB])
                        yield
                        tt, ttB = r_t.next()
                        for half in range(2):
                            pp, ppB = ps[half], psB[half]
                            for kc in range(8):
                                k.op('tensor', lambda e, pp=pp, kc=kc, half=half, omT=omT: e.matmul(pp[:, :], omT[:, kc, :], wo[:, kc, half * 512:(half + 1) * 512],
                                                                                             start=(kc == 0), stop=(kc == 7)), reads=[omTB, woB], writes=[ppB])
                            k.op('vector', lambda e, pp=pp, half=half, tt=tt, hh=hh: e.scalar_tensor_tensor(tt[:, half * 512:(half + 1) * 512], hh[:, half * 512:(half + 1) * 512],
                                                                                                      ALPHA, pp[:, :], ALU.mult, ALU.add),
                                 reads=[ppB, hhB], writes=[ttB])
                        yield
                        yy, yyB = r_y.next()
                        layer_norm(rings, tt, ttB, g1, b1, [g1B, b1B], yy, yyB)
                        yield
                        k.dma('sync', h_d[rows, :], yy[:], reads=[yyB, hhB])
                        xb, xbB = r_xb.next()
                        k.op('scalar', lambda e, xb=xb, yy=yy: e.copy(xb[:], yy[:]), reads=[yyB], writes=[xbB])
                        k.dma('sync', xb_d[rows, :], xb[:], reads=[xbB])
                        yield
                        xT, xTB = r_xT.next()
                        for half in range(2):
                            pp, ppB = ps[2 + half], psB[2 + half]
                            for jj in range(4):
                                kc = half * 4 + jj
                                k.op('tensor', lambda e, pp=pp, jj=jj, kc=kc, yy=yy: e.transpose(pp[:, jj * 128:(jj + 1) * 128], yy[:, kc * 128:(kc + 1) * 128], ident[:]),
                                     reads=[yyB, cB], writes=[ppB])
                            if half == 0:
                                k.op('vector', lambda e, pp=pp, xT=xT: e.tensor_copy(xT[:, 0:4, :], pp[:].rearrange("p (a b) -> p a b", a=4)), reads=[ppB], writes=[xTB])
                            else:
                                k.op('scalar', lambda e, pp=pp, xT=xT: e.copy(xT[:, 4:8, :], pp[:].rearrange("p (a b) -> p a b", a=4)), reads=[ppB], writes=[xTB])
                        yield
                        p4, p4B = ps[4], psB[4]
                        for kc in range(8):
                            k.op('tensor', lambda e, kc=kc, xT=xT: e.matmul(p4[:, 0:36], xT[:, kc, :], wr[:, kc, 0:36], start=(kc == 0), stop=(kc == 7)),
                                 reads=[xTB, wrB], writes=[p4B])
                        rt, rtB = r_rt.next()
                        V = lambda fn, rt=rt, rtB=rtB, extra_r=(), extra_w=(): k.op('vector', fn, reads=[rtB] + list(extra_r), writes=[rtB] + list(extra_w))
                        k.op('vector', lambda e, rt=rt: e.tensor_copy(rt[:, 0:36], p4[:, 0:36]), reads=[p4B], writes=[rtB])
                        V(lambda e, rt=rt: e.reduce_max(rt[:, 36:37], rt[:, 0:4], AX.X))
                        V(lambda e, rt=rt: e.tensor_scalar_mul(rt[:, 37:38], rt[:, 36:37], -1.0))
                        k.op('scalar', lambda e, rt=rt: e.activation(rt[:, 84:88], rt[:, 0:4], AF.Exp, bias=rt[:, 37:38], accum_out=rt[:, 38:39]), reads=[rtB], writes=[rtB])
                        V(lambda e, rt=rt: e.reciprocal(rt[:, 39:40], rt[:, 38:39]))
                        V(lambda e, rt=rt: e.tensor_scalar(rt[:, 40:44], rt[:, 0:4], rt[:, 36:37], None, ALU.is_ge))
                        V(lambda e, rt=rt: e.tensor_scalar(rt[:, 40:44], rt[:, 40:44], -NEG, NEG, ALU.mult, ALU.add))
                        for g in range(4):
                            V(lambda e, rt=rt, g=g: e.tensor_scalar(rt[:, 44 + g * 8:52 + g * 8], rt[:, 4 + g * 8:12 + g * 8], rt[:, 40 + g:41 + g], None, ALU.add))
                        yield
                        V(lambda e, rt=rt: e.reduce_max(rt[:, 76:77], rt[:, 44:76], AX.X))
                        V(lambda e, rt=rt, t=t: e.tensor_scalar(oh[0][:, t, :], rt[:, 44:76], rt[:, 76:77], None, ALU.is_ge), extra_w=[ohB])
                        V(lambda e, rt=rt, t=t: e.scalar_tensor_tensor(rt[:, 96:128], oh[0][:, t, :], NEG, rt[:, 44:76], ALU.mult, ALU.add), extra_r=[ohB])
                        V(lambda e, rt=rt: e.reduce_max(rt[:, 77:78], rt[:, 96:128], AX.X))
                        V(lambda e, rt=rt, t=t: e.tensor_scalar(oh[1][:, t, :], rt[:, 96:128], rt[:, 77:78], None, ALU.is_ge), extra_w=[ohB])
                        V(lambda e, rt=rt: e.tensor_tensor(rt[:, 78:79], rt[:, 77:78], rt[:, 76:77], ALU.subtract))
                        k.op('scalar', lambda e, rt=rt: e.activation(rt[:, 79:80], rt[:, 78:79], AF.Exp), reads=[rtB], writes=[rtB])
                        V(lambda e, rt=rt: e.tensor_scalar_add(rt[:, 80:81], rt[:, 79:80], 1.0))
                        V(lambda e, rt=rt: e.reciprocal(rt[:, 81:82], rt[:, 80:81]))
                        V(lambda e, rt=rt, t=t: e.tensor_tensor(gates[:, t, 0:1], rt[:, 39:40], rt[:, 81:82], ALU.mult), extra_w=[gB])
                        V(lambda e, rt=rt, t=t: e.tensor_tensor(gates[:, t, 1:2], rt[:, 39:40], gates[:, t, 0:1], ALU.subtract), extra_r=[gB], extra_w=[gB])
                    interleave((tok_body(t) for t in range(NT)), depth=2)
                    k.barrier()
                if checkpoint(noraise=True):
                    return True
                dest = sbt(es, "dest", [128, 2, NT], I32)
                destB = Buf("dest")
                with ExitStack() as e2:
                    selb = sbt(e2, "selb", [128, NT, 32], BF16)
                    cnt = sbt(e2, "cnt", [128, NT, 32], F32)
                    pref = sbt(e2, "pref", [128, NT, 32], F32)
                    slot = sbt(e2, "slot", [128, NT, 32], F32)
                    ebase = sbt(e2, "ebase", [128, 32], F32)
                    tmp = sbt(e2, "ptmp", [128, NT, 32], F32)
                    dfl = sbt(e2, "dfl", [128, 2, NT], F32)
                    pB = Buf("pos")
                    k.op('gpsimd', lambda e: e.iota(ebase[:], [[CAP, 32]], base=0, channel_multiplier=0, allow_small_or_imprecise_dtypes=True), writes=[pB])
                    k.op('vector', lambda e: e.tensor_tensor(selb[:], oh[0][:], oh[1][:], ALU.add), reads=[ohB], writes=[pB])
                    TPB = 16
                    for t0 in range(0, NT, TPB):
                        n = min(TPB, NT - t0)
                        k.op('tensor', lambda e, t0=t0, n=n: e.matmul(ps[0][:, 0:n * 32], ones_b[:], selb[:, t0:t0 + n, :].rearrange("p a b -> p (a b)"), start=True, stop=True),
                             reads=[pB, cB], writes=[psB[0]])
                        k.op('vector', lambda e, t0=t0, n=n: e.tensor_copy(cnt[:, t0:t0 + n, :].rearrange("p a b -> p (a b)"), ps[0][:, 0:n * 32]), reads=[psB[0]], writes=[pB])
                        k.op('tensor', lambda e, t0=t0, n=n: e.matmul(ps[1][:, 0:n * 32], sutri[:], selb[:, t0:t0 + n, :].rearrange("p a b -> p (a b)"), start=True, stop=True),
                             reads=[pB, cB], writes=[psB[1]])
                        k.op('vector', lambda e, t0=t0, n=n: e.tensor_copy(slot[:, t0:t0 + n, :].rearrange("p a b -> p (a b)"), ps[1][:, 0:n * 32]), reads=[psB[1]], writes=[pB])
                    k.op('vector', lambda e: e.memset(pref[:, 0, :], 0.0), reads=[pB], writes=[pB])
                    for t in range(1, NT):
                        k.op('vector', lambda e, t=t: e.tensor_tensor(pref[:, t, :], pref[:, t - 1, :], cnt[:, t - 1, :], ALU.add), reads=[pB], writes=[pB])
                    k.op('vector', lambda e: e.tensor_tensor(slot[:], slot[:], pref[:], ALU.add), reads=[pB], writes=[pB])
                    k.op('vector', lambda e: e.tensor_scalar(tmp[:], slot[:], float(CAP), 1.0e6, ALU.is_ge, ALU.mult), reads=[pB], writes=[pB])
                    k.op('vector', lambda e: e.tensor_tensor(slot[:], slot[:], tmp[:], ALU.add), reads=[pB], writes=[pB])
                    for t in range(NT):
                        k.op('gpsimd', lambda e, t=t: e.tensor_tensor(slot[:, t, :], slot[:, t, :], ebase[:], ALU.add), reads=[pB], writes=[pB])
                    for i in range(2):
                        k.op('vector', lambda e, i=i: e.tensor_tensor(tmp[:], slot[:], oh[i][:], ALU.mult), reads=[pB, ohB], writes=[pB])
                        k.op('vector', lambda e, i=i: e.reduce_sum(dfl[:, i, :], tmp[:], AX.X), reads=[pB], writes=[pB])
                    k.op('vector', lambda e: e.tensor_scalar_min(dfl[:], dfl[:], float(NS)), reads=[pB], writes=[pB])
                    k.op('vector', lambda e: e.tensor_copy(dest[:], dfl[:]), reads=[pB], writes=[destB])
                    r_xb2 = Ring(e2, nc, "xb2", [128, D], BF16, 3)
                    for t in range(NT):
                        xb, xbB = r_xb2.next()
                        k.dma('sync', xb[:], xb_d[t * 128:(t + 1) * 128, :], writes=[xbB])
                        for i in range(2):
                            k.op('gpsimd', lambda e, xb=xb, i=i, t=t: e.indirect_dma_start(
                                out=xs_d, out_offset=bass.IndirectOffsetOnAxis(ap=dest[:, i, t:t + 1], axis=0), in_=xb[:], in_offset=None),
                                reads=[xbB, destB], dma=True)
                    k.barrier()
                with ExitStack() as e3:
                    NG = (CAP + 127) // 128
                    r_w13 = Ring(e3, nc, "w13", [128, 8, 1024], BF16, 2)
                    r_w2 = Ring(e3, nc, "w2", [128, 4, 1024], BF16, 2)
                    r_xs = Ring(e3, nc, "xs", [128, NG, D], BF16, 2)
                    r_xsT = Ring(e3, nc, "xsT", [128, 8, CAP], BF16, 2)
                    r_sg = Ring(e3, nc, "sg", [128, 4, CAP], F32, 1)
                    r_hid = Ring(e3, nc, "hid", [128, 4, CAP], BF16, 2)
                    r_ys = Ring(e3, nc, "ys", [128, D], F32, 2)
                    for ex in range(32):
                        w13, w13B = r_w13.next()
                        k.dma('gpsimd', w13[:], moe_w13[layer, ex].rearrange("(k p) c -> p k c", p=128), writes=[w13B])
                        w2, w2B = r_w2.next()
                        k.dma('gpsimd', w2[:], moe_w2[layer, ex].rearrange("(k p) c -> p k c", p=128), writes=[w2B])
                        xs, xsB = r_xs.next()
                        xsT, xsTB = r_xsT.next()
                        for g in range(NG):
                            r0 = ex * CAP + g * 128
                            nr = min(128, CAP - g * 128)
                            k.dma('sync', xs[0:nr, g, :], xs_d[r0:r0 + nr, :], writes=[xsB])
                        for g in range(NG):
                            nr = min(128, CAP - g * 128)
                            for kc in range(8):
                                k.op('tensor', lambda e, xs=xs, g=g, kc=kc, nr=nr: e.transpose(pb[:, kc * 128:kc * 128 + nr], xs[0:nr, g, kc * 128:(kc + 1) * 128], identb[0:nr, 0:nr]),
                                     reads=[xsB, cB], writes=[pbB])
                            k.op('vector', lambda e, xsT=xsT, g=g, nr=nr: e.tensor_copy(xsT[:, :, g * 128:g * 128 + nr], pb[:].rearrange("p (a b) -> p a b", a=8)[:, :, 0:nr]),
                                 reads=[pbB], writes=[xsTB])
                        sg, sgB = r_sg.next()
                        hid, hidB = r_hid.next()
                        for n0 in range(0, CAP, 512):
                            n = min(512, CAP - n0)
                            for m in range(8):
                                pp, ppB = ps[m % 4], psB[m % 4]
                                for kc in range(8):
                                    k.op('tensor', lambda e, pp=pp, kc=kc, m=m, w13=w13, xsT=xsT, n0=n0, n=n: e.matmul(pp[:, 0:n], w13[:, kc, m * 128:(m + 1) * 128], xsT[:, kc, n0:n0 + n],
                                                                                                           start=(kc == 0), stop=(kc == 7)),
                                         reads=[w13B, xsTB], writes=[ppB])
                                if m < 4:
                                    k.op('scalar', lambda e, pp=pp, m=m, sg=sg, n0=n0, n=n: e.activation(sg[:, m, n0:n0 + n], pp[:, 0:n], AF.Silu), reads=[ppB], writes=[sgB])
                                else:
                                    k.op('vector', lambda e, pp=pp, m=m, sg=sg, hid=hid, n0=n0, n=n: e.tensor_tensor(hid[:, m - 4, n0:n0 + n], sg[:, m - 4, n0:n0 + n], pp[:, 0:n], ALU.mult),
                                         reads=[ppB, sgB], writes=[hidB])
                        for g in range(NG):
                            nr = min(128, CAP - g * 128)
                            ys, ysB = r_ys.next()
                            for half in range(2):
                                pp, ppB = ps[4 + half], psB[4 + half]
                                for f in range(4):
                                    k.op('tensor', lambda e, pp=pp, f=f, half=half, hid=hid, w2=w2, g=g, nr=nr: e.matmul(pp[0:nr, :], hid[:, f, g * 128:g * 128 + nr], w2[:, f, half * 512:(half + 1) * 512],
                                                                                                             start=(f == 0), stop=(f == 3)),
                                         reads=[hidB, w2B], writes=[ppB])
                                if half == 0:
                                    k.op('vector', lambda e, pp=pp, ys=ys, nr=nr: e.tensor_copy(ys[0:nr, 0:512], pp[0:nr, :]), reads=[ppB], writes=[ysB])
                                else:
                                    k.op('scalar', lambda e, pp=pp, ys=ys, nr=nr: e.copy(ys[0:nr, 512:1024], pp[0:nr, :]), reads=[ppB], writes=[ysB])
                            r0 = ex * CAP + g * 128
                            for hf in range(2):
                                k.dma('sync', ys_h[hf][r0:r0 + nr, :], ys[0:nr, hf * 512:(hf + 1) * 512], reads=[ysB])
                    k.barrier()
                with ExitStack() as e4:
                    g2, g2B = bcast_row(e4, "lng2", ln_ffn_g[layer:layer + 1, :], D)
                    b2, b2B = bcast_row(e4, "lnb2", ln_ffn_b[layer:layer + 1, :], D)
                    r_yq = [Ring(e4, nc, f"yq{q}", [128, 512], F32, 2) for q in range(4)]
                    r_h = Ring(e4, nc, "ch", [128, D], F32, 2)
                    r_o = Ring(e4, nc, "co", [128, D], F32, 2)
                    def comb_body(t):
                        rows = slice(t * 128, (t + 1) * 128)
                        hh, hhB = r_h.next()
                        k.dma('sync', hh[:], h_d[rows, :], writes=[hhB])
                        k.op('scalar', lambda e, hh=hh: e.mul(hh[:], hh[:], ALPHA), reads=[hhB], writes=[hhB])
                        for i in range(2):
                            for hf in range(2):
                                yq, yqB = r_yq[i * 2 + hf].next()
                                k.op('gpsimd', lambda e, yq=yq, i=i, t=t, hf=hf: e.indirect_dma_start(
                                    out=yq[:], out_offset=None, in_=ys_h[hf],
                                    in_offset=bass.IndirectOffsetOnAxis(ap=dest[:, i, t:t + 1], axis=0)), reads=[destB], writes=[yqB], dma=True)
                                k.op('vector', lambda e, hh=hh, yq=yq, t=t, i=i, hf=hf: e.scalar_tensor_tensor(
                                    hh[:, hf * 512:(hf + 1) * 512], yq[:], gates[:, t, i:i + 1], hh[:, hf * 512:(hf + 1) * 512], ALU.mult, ALU.add),
                                    reads=[hhB, yqB, gB], writes=[hhB])
                        yield
                        oo, ooB = r_o.next()
                        layer_norm(rings, hh, hhB, g2, b2, [g2B, b2B], oo, ooB)
                        yield
                        if last:
                            out_toks.append(k.dma('sync', out[rows, :], oo[:], reads=[ooB]))
                        else:
                            k.dma('sync', h_d[rows, :], oo[:], reads=[ooB, hhB])
                            make_hT(rings, oo, ooB, t)
                    interleave((comb_body(t) for t in range(NT)), depth=2)
                    k.barrier()

        try:
            checkpoint()
            for layer in range(4):
                if layer < 2:
                    deltanet(layer)
                    checkpoint()
                    if tok_moe(layer, a_w_out[layer], last=False):
                        raise _Stop()
                    checkpoint()
                else:
                    j = layer - 2
                    if j == 0:
                        shared_kv()
                    diffattn(j, layer)
                    checkpoint()
                    if tok_moe(layer, b_w_out[j], last=(layer == 3)):
                        raise _Stop()
                    checkpoint()
        except _Stop:
            out_toks.append(k.dma('sync', out, h_d))
            out_toks.append(k.dma('sync', dbg, om_d))
        k.wait_all('sync', out_toks)
        k.finish()
    return nc, k


SEQ = 8192
CAP_FULL = 640
_cache = {}


def kernel(**inputs):
    x = np.asarray(inputs['x'])
    B, T, _ = x.shape
    cap = CAP_FULL if T == SEQ else max(64, int(T / 16 + 6 * math.sqrt(T / 16) + 16) // 32 * 32 + 32)
    key = (T, cap)
    if key not in _cache:
        _cache[key] = build(T, cap)[0]
    nc = _cache[key]
    shared = {n: np.ascontiguousarray(np.asarray(v, dtype=np.float32)) for n, v in inputs.items() if n != 'x'}
    in_maps = []
    for b in range(B):
        m = dict(shared)
        m['x'] = np.ascontiguousarray(x[b])
        in_maps.append(m)
    res = run_bass_kernel_spmd(nc, in_maps, core_ids=list(range(B)))
    return np.stack([np.asarray(res.results[b]['out']) for b in range(B)], axis=0).astype(np.float32)
```

```python
import math
from contextlib import ExitStack
import numpy as np
import concourse.bass as bass
import concourse.mybir as mybir
from concourse.bass_utils import run_bass_kernel_spmd

F32 = mybir.dt.float32
BF16 = mybir.dt.bfloat16
I32 = mybir.dt.int32
AF = mybir.ActivationFunctionType
ALU = mybir.AluOpType
AX = mybir.AxisListType

ENGS = ['tensor', 'vector', 'scalar', 'gpsimd', 'sync']
SEM_EPOCH = 30000
N_DMA_SEMS = 16


class Buf:
    __slots__ = ('name', 'w', 'r', 'excl')

    def __init__(self, name='', excl=False):
        self.name = name
        self.excl = excl
        self.w = None
        self.r = {}


class _Op:
    __slots__ = ('fn', 'waits', 'inc', 'incval', 'dma')

    def __init__(self, fn, waits, dma):
        self.fn = fn
        self.waits = waits
        self.inc = False
        self.incval = 0
        self.dma = dma


class K:
    def __init__(self, nc):
        self.nc = nc
        self.ops = {e: [] for e in ENGS}
        self.waited = {e: {} for e in ENGS}
        self.dma_rr = {e: 0 for e in ENGS}
        self.dma_cnt = {}

    def _need_wait(self, eng, t):
        key = (t[0], t[1])
        if self.waited[eng].get(key, -1) >= t[2]:
            return False
        self.waited[eng][key] = t[2]
        if t[0] == 'e':
            self.ops[t[1]][t[2]].inc = True
        return True

    def op(self, eng, fn, reads=(), writes=(), dma=False):
        idx = len(self.ops[eng])
        writes = list(writes) + [b for b in reads if b.excl]
        reads = [b for b in reads if not b.excl]
        deps = []
        for b in reads:
            if b.w is not None:
                deps.append(b.w)
        for b in writes:
            if b.w is not None:
                deps.append(b.w)
            deps.extend(b.r.values())
        waits = []
        for t in deps:
            if t[0] == 'e' and t[1] == eng and eng == 'tensor':
                continue
            if self._need_wait(eng, t):
                waits.append(t)
        dm = None
        if dma:
            slot = self.dma_rr[eng]
            self.dma_rr[eng] = (slot + 1) % N_DMA_SEMS
            cnt = self.dma_cnt.get((eng, slot), 0) + 1
            self.dma_cnt[(eng, slot)] = cnt
            dm = ((eng, slot), cnt * 16)
            if cnt > 1:
                t = ('d', (eng, slot), (cnt - 1) * 16)
                if self._need_wait(eng, t):
                    waits.append(t)
            tok = ('d', (eng, slot), cnt * 16)
        else:
            tok = ('e', eng, idx)
        self.ops[eng].append(_Op(fn, waits, dm))
        kk = (tok[0], tok[1])
        for b in reads:
            b.r[kk] = tok
        for b in writes:
            b.w = tok
            b.r = {}
        return tok

    def dma(self, eng, out, in_, reads=(), writes=(), **kw):
        return self.op(eng, lambda e: e.dma_start(out=out, in_=in_, **kw), reads=reads, writes=writes, dma=True)

    def wait_all(self, eng, tokens):
        waits = [t for t in tokens if self._need_wait(eng, t)]
        if waits:
            self.ops[eng].append(_Op(None, waits, None))

    def barrier(self):
        toks = []
        for e in ENGS:
            for i in range(len(self.ops[e]) - 1, -1, -1):
                o = self.ops[e][i]
                if o.fn is not None and o.dma is None:
                    toks.append(('e', e, i))
                    break
        for key, cnt in self.dma_cnt.items():
            toks.append(('d', key, cnt * 16))
        for e in ENGS:
            self.wait_all(e, toks)
        self.flush()

    def flush(self):
        nc = self.nc
        if not hasattr(self, 'flushed'):
            self.flushed = {e: 0 for e in ENGS}
            self.inccnt = {e: 0 for e in ENGS}
            self.esems = {e: [] for e in ENGS}
            self.dsems = {}
        start = dict(self.flushed)
        for e in ENGS:
            for o in self.ops[e][start[e]:]:
                if o.inc:
                    self.inccnt[e] += 1
                    o.incval = self.inccnt[e]
            need = (self.inccnt[e] + SEM_EPOCH - 1) // SEM_EPOCH
            while len(self.esems[e]) < max(need, 1):
                self.esems[e].append(nc.alloc_semaphore(f"es_{e}_{len(self.esems[e])}"))
        for key in self.dma_cnt:
            if key not in self.dsems:
                self.dsems[key] = nc.alloc_semaphore(f"ds_{key[0]}_{key[1]}")
        esems, dsems, ops = self.esems, self.dsems, self.ops

        def semval(e2, incval):
            return esems[e2][(incval - 1) // SEM_EPOCH], (incval - 1) % SEM_EPOCH + 1

        with nc.Block() as block:
            for eng in ENGS:
                todo = ops[eng][start[eng]:]

                def body(e, eng=eng, todo=todo):
                    for o in todo:
                        for t in o.waits:
                            if t[0] == 'e':
                                p = ops[t[1]][t[2]]
                                assert p.incval > 0, (eng, t)
                                s, v = semval(t[1], p.incval)
                                e.wait_ge(s, v)
                            else:
                                e.wait_ge(dsems[t[1]], t[2])
                        if o.fn is None:
                            continue
                        ins = o.fn(e)
                        if o.dma is not None:
                            ins.then_inc(dsems[o.dma[0]], 16)
                        elif o.inc:
                            s, v = semval(eng, o.incval)
                            ins.then_inc(s, 1)
                if todo:
                    getattr(block, eng)(body)
                self.flushed[eng] = len(ops[eng])

    def finish(self):
        self.flush()


_uid = [0]


def _un(name):
    _uid[0] += 1
    return f"{name}_u{_uid[0]}"


def interleave(gens, depth=2):
    active = []
    it = iter(gens)
    while True:
        while len(active) < depth:
            g = next(it, None)
            if g is None:
                break
            active.append(g)
        if not active:
            break
        for g in list(active):
            try:
                next(g)
            except StopIteration:
                active.remove(g)


_DONE = object()


class Ring:
    def __init__(self, es, nc, name, shape, dt, n):
        self.t = [es.enter_context(nc.sbuf_tensor(_un(f"{name}_{i}"), shape, dt)) for i in range(n)]
        self.b = [Buf(f"{name}_{i}") for i in range(n)]
        self.i = 0

    def next(self):
        i = self.i
        self.i = (i + 1) % len(self.t)
        return self.t[i], self.b[i]


D = 1024
NH = 8
ALPHA = 8 ** 0.25
LN_EPS = 1e-5
RMS_EPS = 1e-6
NEG = -1.0e30


def lambda_init(layer_idx):
    return 0.8 - 0.6 * math.exp(-0.3 * layer_idx)


class _Stop(Exception):
    pass


def build(T, CAP, stop=0, ksub=0, dumpflag=False):
    NT = T // 128
    NCH = T // 64
    NB = T // 512
    nc = bass.Bass("TRN2", target_bir_lowering=False)

    def din(name, shape, dt=F32):
        return nc.dram_tensor(name, shape, dt, kind="ExternalInput").ap()

    x = din("x", [T, D])
    a_w_in = din("a_w_in", [2, D, 4112])
    a_conv_w = din("a_conv_w", [2, 4, 3072])
    a_a_log = din("a_a_log", [2, 8])
    a_dt_bias = din("a_dt_bias", [2, 8])
    a_norm_w = din("a_norm_w", [2, 128])
    a_w_out = din("a_w_out", [2, D, D])
    kv_w = din("kv_w", [D, 2048])
    b_w_q = din("b_w_q", [2, D, D])
    b_lambda = din("b_lambda", [2, 4, 64])
    b_subln_w = din("b_subln_w", [2, 128])
    b_w_out = din("b_w_out", [2, D, D])
    ln_mix_g = din("ln_mix_g", [4, D])
    ln_mix_b = din("ln_mix_b", [4, D])
    ln_ffn_g = din("ln_ffn_g", [4, D])
    ln_ffn_b = din("ln_ffn_b", [4, D])
    moe_w_group = din("moe_w_group", [4, D, 4])
    moe_w_expert = din("moe_w_expert", [4, D, 32])
    moe_w13 = din("moe_w13", [4, 32, D, 1024])
    moe_w2 = din("moe_w2", [4, 32, 512, D])
    out = nc.dram_tensor("out", [T, D], F32, kind="ExternalOutput").ap()
    dbg = nc.dram_tensor("dbg", [T, D], BF16, kind="ExternalOutput").ap() if stop else None
    stage = [0]

    def checkpoint(noraise=False):
        stage[0] += 1
        if stop and stage[0] >= stop:
            if noraise:
                return True
            raise _Stop()
        return False

    h_d = nc.dram_tensor("h_d", [T, D], F32).ap()
    hT_d = nc.dram_tensor("hT_d", [D, T], BF16).ap()
    om_d = nc.dram_tensor("om_d", [T, D], BF16).ap()
    xb_d = nc.dram_tensor("xb_d", [T, D], BF16).ap()
    kT_d = nc.dram_tensor("kT_d", [NH, 2, 64, T], BF16).ap()
    va_d = nc.dram_tensor("va_d", [NH, T, 129], BF16).ap()
    NS = 32 * CAP
    xs_d = nc.dram_tensor("xs_d", [NS + 128, D], BF16).ap()
    ys_h = [nc.dram_tensor(f"ys_d{i}", [NS + 128, 512], F32).ap() for i in range(2)]
    hT_v = hT_d.rearrange("(k p) t -> p k t", p=128)

    k = K(nc)
    out_toks = []
    dumped = set()

    def dump(name, ap, B):
        if not dumpflag or name in dumped:
            return
        dumped.add(name)
        t = nc.dram_tensor("dump_" + name, list(ap.shape), F32, kind="ExternalOutput").ap()
        out_toks.append(k.dma('gpsimd', t, ap, reads=[B]))
    with ExitStack() as top:
        def sbt(es, name, shape, dt):
            return es.enter_context(nc.sbuf_tensor(_un(name), shape, dt))

        ps = [top.enter_context(nc.psum_tensor(f"ps{i}", [128, 512], F32)) for i in range(7)]
        psB = [Buf(f"ps{i}", excl=True) for i in range(7)]
        pb = top.enter_context(nc.psum_tensor("pb", [128, 1024], BF16))
        pbB = Buf("pb", excl=True)

        ident = sbt(top, "ident", [128, 128], F32)
        identb = sbt(top, "identb", [128, 128], BF16)
        ones_f = sbt(top, "ones_f", [128, 128], F32)
        ones_b = sbt(top, "ones_b", [128, 128], BF16)
        triu = sbt(top, "triu", [128, 128], F32)
        triub = sbt(top, "triub", [128, 128], BF16)
        sutri = sbt(top, "sutri", [128, 128], BF16)
        zero_b = sbt(top, "zero_b", [128, 1024], BF16)
        triu4 = sbt(top, "triu4", [64, 4, 64], F32)
        ident4 = sbt(top, "ident4", [64, 4, 64], F32)
        cB = Buf("consts")
        k.op('gpsimd', lambda e: e.iota(ones_f[:], [[1, 128]], base=0, channel_multiplier=-1,
                                        allow_small_or_imprecise_dtypes=True), writes=[cB])
        k.op('vector', lambda e: e.tensor_single_scalar(ident[:], ones_f[:], 0.0, ALU.is_equal), reads=[cB], writes=[cB])
        k.op('vector', lambda e: e.tensor_single_scalar(identb[:], ones_f[:], 0.0, ALU.is_equal), reads=[cB], writes=[cB])
        k.op('vector', lambda e: e.tensor_single_scalar(triu[:], ones_f[:], 0.0, ALU.is_ge), reads=[cB], writes=[cB])
        k.op('vector', lambda e: e.tensor_single_scalar(triub[:], ones_f[:], 0.0, ALU.is_ge), reads=[cB], writes=[cB])
        k.op('vector', lambda e: e.tensor_single_scalar(sutri[:], ones_f[:], 0.0, ALU.is_gt), reads=[cB], writes=[cB])
        for g4 in range(4):
            k.op('vector', lambda e, g4=g4: e.tensor_copy(triu4[:, g4, :], triu[0:64, 0:64]), reads=[cB], writes=[cB])
            k.op('vector', lambda e, g4=g4: e.tensor_copy(ident4[:, g4, :], ident[0:64, 0:64]), reads=[cB], writes=[cB])
        k.op('vector', lambda e: e.memset(ones_f[:], 1.0), reads=[cB], writes=[cB])
        k.op('vector', lambda e: e.memset(ones_b[:], 1.0), writes=[cB])
        k.op('vector', lambda e: e.memset(zero_b[:], 0.0), writes=[cB])
        zero_f = sbt(top, "zero_f", [128, 512], F32)
        k.op('vector', lambda e: e.memset(zero_f[:], 0.0), writes=[cB])
        for r0 in range(0, NS + 128, 128):
            k.dma('sync', xs_d[r0:r0 + 128, :], zero_b[:], reads=[cB])
        for hf in range(2):
            k.dma('sync', ys_h[hf][NS:NS + 128, :], zero_f[:], reads=[cB])
        k.barrier()

        def layer_norm(es_ring, t, tB, gbc, bbc, wBs, y, yB):
            st, stB = es_ring['st'].next()
            jk, jkB = es_ring['junk'].next()
            k.op('scalar', lambda e: e.activation(jk[:], t[:], AF.Copy, accum_out=st[:, 0:1]), reads=[tB], writes=[jkB, stB])
            k.op('scalar', lambda e: e.activation(jk[:], t[:], AF.Square, accum_out=st[:, 1:2]), reads=[tB], writes=[jkB, stB])
            k.op('vector', lambda e: e.tensor_scalar_mul(st[:, 2:3], st[:, 0:1], 1.0 / D), reads=[stB], writes=[stB])
            k.op('vector', lambda e: e.tensor_tensor(st[:, 3:4], st[:, 2:3], st[:, 2:3], ALU.mult), reads=[stB], writes=[stB])
            k.op('vector', lambda e: e.scalar_tensor_tensor(st[:, 4:5], st[:, 1:2], 1.0 / D, st[:, 3:4], ALU.mult, ALU.subtract),
                 reads=[stB], writes=[stB])
            k.op('scalar', lambda e: e.activation(st[:, 5:6], st[:, 4:5], AF.Sqrt, bias=LN_EPS), reads=[stB], writes=[stB])
            k.op('vector', lambda e: e.reciprocal(st[:, 6:7], st[:, 5:6]), reads=[stB], writes=[stB])
            k.op('vector', lambda e: e.tensor_scalar(t[:], t[:], st[:, 2:3], st[:, 6:7], ALU.subtract, ALU.mult), reads=[tB, stB], writes=[tB])
            k.op('gpsimd', lambda e: e.tensor_tensor(t[:], t[:], gbc[:], ALU.mult), reads=[tB] + wBs, writes=[tB])
            k.op('vector', lambda e: e.tensor_tensor(y[:], t[:], bbc[:], ALU.add), reads=[tB] + wBs, writes=[yB])

        def make_hT(rings, y, yB, tile):
            hb, hbB = rings['hTs'].next()
            for half in range(2):
                pp, ppB = ps[5 + half], psB[5 + half]
                for j in range(4):
                    kk = half * 4 + j
                    k.op('tensor', lambda e, pp=pp, j=j, kk=kk: e.transpose(pp[:, j * 128:(j + 1) * 128], y[:, kk * 128:(kk + 1) * 128], ident[:]),
                         reads=[yB, cB], writes=[ppB])
                eng = 'vector' if half == 0 else 'scalar'
                if eng == 'vector':
                    k.op('vector', lambda e, pp=pp, half=half: e.tensor_copy(hb[:, half * 4:(half + 1) * 4, :], pp[:].rearrange("p (a b) -> p a b", a=4)),
                         reads=[ppB], writes=[hbB])
                else:
                    k.op('scalar', lambda e, pp=pp, half=half: e.copy(hb[:, half * 4:(half + 1) * 4, :], pp[:].rearrange("p (a b) -> p a b", a=4)),
                         reads=[ppB], writes=[hbB])
            k.dma('sync', hT_v[:, :, tile * 128:(tile + 1) * 128], hb[:], reads=[hbB])

        with ExitStack() as es:
            rings = {'hTs': Ring(es, nc, "hTs", [128, 8, 128], BF16, 2)}
            xr = Ring(es, nc, "xin", [128, D], F32, 2)
            for t in range(NT):
                xt, xB = xr.next()
                k.dma('sync', xt[:], x[t * 128:(t + 1) * 128, :], writes=[xB])
                k.dma('gpsimd', h_d[t * 128:(t + 1) * 128, :], xt[:], reads=[xB])
                make_hT(rings, xt, xB, t)
            k.barrier()

        def load_w_bf16(es, name, src, cols):
            w = sbt(es, name, [128, 8, cols], BF16)
            wB = Buf(name)
            k.dma('gpsimd', w[:], src.rearrange("(k p) c -> p k c", p=128), writes=[wB])
            return w, wB

        def bcast_row(es, name, src_row, n, dt=F32):
            w = sbt(es, name, [128, n], dt)
            wB = Buf(name)
            k.dma('sync', w[:], src_row.partition_broadcast(128), writes=[wB])
            return w, wB

        def deltanet(l):
            with ExitStack() as es:
                cw = sbt(es, "cw", [128, 24, 4], F32)
                cwB = Buf("cw")
                cwn = sbt(es, "cwn", [4, 3072], F32)
                k.dma('sync', cwn[:], a_conv_w[l], writes=[cwB])
                for part in range(24):
                    k.op('tensor', lambda e, part=part: e.transpose(ps[0][:, part * 4:(part + 1) * 4], cwn[:, part * 128:(part + 1) * 128], ident[0:4, 0:4]),
                         reads=[cwB, cB], writes=[psB[0]])
                k.op('vector', lambda e: e.tensor_copy(cw[:].rearrange("p a b -> p (a b)"), ps[0][:, 0:96]), reads=[psB[0]], writes=[cwB])
                nw, nwB = bcast_row(es, "nw", a_norm_w[l:l + 1, :], 128)
                alog, alB = bcast_row(es, "alog", a_a_log[l:l + 1, :], 8)
                dtb, dtB = bcast_row(es, "dtb", a_dt_bias[l:l + 1, :], 8)
                nea = sbt(es, "nea", [128, 8], F32)
                k.op('scalar', lambda e: e.activation(nea[:], alog[:], AF.Exp), reads=[alB], writes=[alB])
                k.op('vector', lambda e: e.tensor_scalar_mul(nea[:], nea[:], -1.0), reads=[alB], writes=[alB])
                qT = sbt(es, "qT", [128, T], BF16)
                kT = sbt(es, "kT", [128, T], BF16)
                vT = sbt(es, "vT", [128, T], BF16)
                qkvB = [Buf("qT"), Buf("kT"), Buf("vT")]
                qkv = [qT, kT, vT]
                zs = sbt(es, "zs", [64, NCH, 128], BF16)
                zsB = Buf("zs")
                gl = sbt(es, "gl", [64, 8, NCH], F32)
                glB = Buf("gl")
                egl = sbt(es, "egl", [128, NCH], F32)
                eglB = Buf("egl")
                S = sbt(es, "S", [128, 128], F32)
                SB = Buf("S")
                hbr = Ring(es, nc, "hblk", [128, 8, 512], BF16, 2)
                raw = sbt(es, "raw", [128, 3, 515], F32)
                rawB = [Buf("raw0"), Buf("raw1"), Buf("raw2")]
                cvr = Ring(es, nc, "cv", [128, 512], F32, 2)
                sqr = Ring(es, nc, "sq", [128, 512], F32, 2)
                G = 4
                R = 2
                r_kg = Ring(es, nc, "kg", [64, G, 256], F32, R)
                r_kdec = Ring(es, nc, "kdec", [64, G, 128], F32, R)
                r_dm = Ring(es, nc, "dm", [64, G, 64], F32, R)
                r_dg = Ring(es, nc, "dg", [64, G, 64], F32, R)
                r_egb = Ring(es, nc, "egb", [128, G, 64], F32, R)
                r_qg = Ring(es, nc, "qg", [128, G, 64], F32, R)
                r_at = Ring(es, nc, "at", [64, G, 64], F32, R)
                r_X = Ring(es, nc, "X", [64, G, 64], F32, 4)
                r_Y = Ring(es, nc, "Y", [64, G, 64], F32, 4)
                r_P = Ring(es, nc, "P", [64, G, 64], F32, R)
                r_uw = Ring(es, nc, "uw", [64, G, 256], F32, R)
                r_AT = Ring(es, nc, "AT", [128, G, 128], F32, R)
                r_Bc = Ring(es, nc, "Bc", [128, G, 128], F32, R)
                r_QpT = Ring(es, nc, "QpT", [128, G, 64], F32, R)
                r_O0 = Ring(es, nc, "O0", [64, G, 128], F32, R)
                r_vn = Ring(es, nc, "vn", [64, 128], F32, 2)
                r_o = Ring(es, nc, "o", [64, 128], F32, 2)
                r_ob = Ring(es, nc, "ob", [64, 128], BF16, 2)
                r_st = Ring(es, nc, "dst", [64, 4], F32, 2)
                r_jk = Ring(es, nc, "djk", [64, 128], F32, 2)
                for h in range(NH):
                    wq3, wq3B = [], []
                    wcat = sbt(es, f"wcat{h}", [128, 8, 384], BF16) if h == 0 else wcat_keep[0]
                    wzg = sbt(es, f"wzg{h}", [128, 8, 128], BF16) if h == 0 else wcat_keep[1]
                    if h == 0:
                        wcat_keep = [wcat, wzg]
                        wcB = Buf("wcat")
                        wzB = Buf("wzg")
                    for part in range(3):
                        k.dma('gpsimd', wcat[:, :, part * 128:(part + 1) * 128],
                              a_w_in[l, :, part * 1024 + h * 128: part * 1024 + (h + 1) * 128].rearrange("(k p) c -> p k c", p=128), writes=[wcB])
                    k.dma('gpsimd', wzg[:], a_w_in[l, :, 3072 + h * 128:3072 + (h + 1) * 128].rearrange("(k p) c -> p k c", p=128), writes=[wzB])
                    if h == 0:
                        wg16 = sbt(es, "wg16", [128, 8, 16], BF16)
                        k.dma('gpsimd', wg16[:], a_w_in[l, :, 4096:4112].rearrange("(k p) c -> p k c", p=128), writes=[wzB])
                    for part in range(3):
                        k.op('vector', lambda e, part=part: e.memset(raw[:, part, 0:3], 0.0), writes=[rawB[part]])
                    if ksub == 5:
                        k.barrier()
                        return
                    for blk in range(NB):
                        hb, hbB = hbr.next()
                        k.dma('sync', hb[:], hT_v[:, :, blk * 512:(blk + 1) * 512], writes=[hbB])
                        for part in range(3):
                            pp, ppB = ps[part % 2], psB[part % 2]
                            for kc in range(8):
                                k.op('tensor', lambda e, pp=pp, kc=kc, part=part, hb=hb: e.matmul(pp[:, :], wcat[:, kc, part * 128:(part + 1) * 128], hb[:, kc, :],
                                                                                            start=(kc == 0), stop=(kc == 7)),
                                     reads=[wcB, hbB], writes=[ppB])
                            k.op('scalar', lambda e, pp=pp, part=part: e.copy(raw[:, part, 3:515], pp[:, :]), reads=[ppB], writes=[rawB[part]])
                            if ksub == 11:
                                k.barrier()
                                return
                            cv, cvB = cvr.next()
                            ci = part * 8 + h
                            k.op('vector', lambda e, cv=cv, part=part, ci=ci: e.tensor_scalar_mul(cv[:], raw[:, part, 0:512], cw[:, ci, 0:1]),
                                 reads=[rawB[part], cwB], writes=[cvB])
                            for j in range(1, 4):
                                k.op('vector', lambda e, cv=cv, part=part, ci=ci, j=j: e.scalar_tensor_tensor(cv[:], raw[:, part, j:j + 512], cw[:, ci, j:j + 1], cv[:],
                                                                                                        ALU.mult, ALU.add),
                                     reads=[rawB[part], cwB, cvB], writes=[cvB])
                            k.op('vector', lambda e, part=part: e.tensor_copy(raw[:, part, 0:3], raw[:, part, 512:515]), reads=[rawB[part]], writes=[rawB[part]])
                            if ksub == 12:
                                k.barrier()
                                return
                            dst = qkv[part][:, blk * 512:(blk + 1) * 512]
                            if part == 2:
                                k.op('scalar', lambda e, cv=cv, dst=dst: e.activation(dst, cv[:], AF.Silu), reads=[cvB], writes=[qkvB[part]])
                            else:
                                k.op('scalar', lambda e, cv=cv: e.activation(cv[:], cv[:], AF.Silu), reads=[cvB], writes=[cvB])
                                sq, sqB = sqr.next()
                                k.op('gpsimd', lambda e, cv=cv, sq=sq: e.tensor_tensor(sq[:], cv[:], cv[:], ALU.mult), reads=[cvB], writes=[sqB])
                                p2, p2B = ps[2], psB[2]
                                for hf in range(2):
                                    k.op('tensor', lambda e, sq=sq, p2=p2, hf=hf: e.matmul(p2[:, hf * 256:(hf + 1) * 256], ones_f[:], sq[:, hf * 256:(hf + 1) * 256], start=True, stop=True), reads=[sqB, cB], writes=[p2B])
                                k.op('scalar', lambda e, sq=sq, p2=p2: e.activation(sq[:], p2[:, :], AF.Sqrt, bias=RMS_EPS), reads=[p2B], writes=[sqB])
                                k.op('vector', lambda e, sq=sq: e.reciprocal(sq[:], sq[:]), reads=[sqB], writes=[sqB])
                                sc = (128 ** -0.5) if part == 0 else 1.0
                                k.op('vector', lambda e, cv=cv, sq=sq, dst=dst, sc=sc: e.scalar_tensor_tensor(dst, cv[:], sc, sq[:], ALU.mult, ALU.mult),
                                     reads=[cvB, sqB], writes=[qkvB[part]])
                            if ksub == 13:
                                k.barrier()
                                return
                        if ksub == 14:
                            k.barrier()
                            return
                        for cc in range(8):
                            c = blk * 8 + cc
                            pp, ppB = ps[3 + cc % 2], psB[3 + cc % 2]
                            for kc in range(8):
                                k.op('tensor', lambda e, pp=pp, kc=kc, cc=cc, hb=hb: e.matmul(pp[0:64, 0:128], hb[:, kc, cc * 64:(cc + 1) * 64], wzg[:, kc, :],
                                                                                       start=(kc == 0), stop=(kc == 7)),
                                     reads=[wzB, hbB], writes=[ppB])
                            for kc in range(8):
                                k.op('tensor', lambda e, pp=pp, kc=kc, cc=cc, hb=hb: e.matmul(pp[0:64, 128:144], hb[:, kc, cc * 64:(cc + 1) * 64], wg16[:, kc, :],
                                                                                       start=(kc == 0), stop=(kc == 7)),
                                     reads=[wzB, hbB], writes=[ppB])
                            k.op('scalar', lambda e, pp=pp, c=c: e.activation(zs[:, c, :], pp[0:64, 0:128], AF.Silu), reads=[ppB], writes=[zsB])
                            k.op('vector', lambda e, pp=pp, c=c, h=h: e.tensor_copy(gl[:, 0, c:c + 1], pp[0:64, 128 + h:129 + h]), reads=[ppB], writes=[glB])
                            k.op('vector', lambda e, pp=pp, c=c, h=h: e.tensor_copy(gl[:, 1, c:c + 1], pp[0:64, 136 + h:137 + h]), reads=[ppB], writes=[glB])
                    if ksub == 1:
                        k.barrier()
                        return
                    k.op('scalar', lambda e: e.activation(gl[:, 0, :], gl[:, 0, :], AF.Sigmoid), reads=[glB], writes=[glB])
                    k.op('vector', lambda e: e.tensor_scalar_mul(gl[:, 5, :], gl[:, 0, :], -1.0), reads=[glB], writes=[glB])
                    k.op('scalar', lambda e, h=h: e.activation(gl[:, 1, :], gl[:, 1, :], AF.Exp, bias=dtb[0:64, h:h + 1]), reads=[glB, dtB], writes=[glB])
                    k.op('scalar', lambda e: e.activation(gl[:, 1, :], gl[:, 1, :], AF.Ln, bias=1.0), reads=[glB], writes=[glB])
                    k.op('vector', lambda e, h=h: e.tensor_scalar_mul(gl[:, 1, :], gl[:, 1, :], nea[0:64, h:h + 1]), reads=[glB, alB], writes=[glB])
                    for c0 in range(0, NCH, 512):
                        n = min(512, NCH - c0)
                        k.op('tensor', lambda e, c0=c0, n=n: e.matmul(ps[0][0:64, 0:n], triu[0:64, 0:64], gl[:, 1, c0:c0 + n], start=True, stop=True),
                             reads=[glB, cB], writes=[psB[0]])
                        k.op('vector', lambda e, c0=c0, n=n: e.tensor_copy(gl[:, 2, c0:c0 + n], ps[0][0:64, 0:n]), reads=[psB[0]], writes=[glB])
                        k.op('tensor', lambda e, c0=c0, n=n: e.matmul(ps[1][:, 0:n], ones_f[0:64, :], gl[:, 1, c0:c0 + n], start=True, stop=True),
                             reads=[glB, cB], writes=[psB[1]])
                        k.op('scalar', lambda e, c0=c0, n=n: e.activation(egl[:, c0:c0 + n], ps[1][:, 0:n], AF.Exp), reads=[psB[1]], writes=[eglB])
                        k.op('vector', lambda e, c0=c0, n=n: e.tensor_copy(gl[:, 6, c0:c0 + n], ps[1][0:64, 0:n]), reads=[psB[1]], writes=[glB])
                    k.op('scalar', lambda e: e.activation(gl[:, 3, :], gl[:, 2, :], AF.Exp), reads=[glB], writes=[glB])
                    k.op('vector', lambda e: e.tensor_tensor(gl[:, 7, :], gl[:, 6, :], gl[:, 2, :], ALU.subtract), reads=[glB], writes=[glB])
                    k.op('scalar', lambda e: e.activation(gl[:, 4, :], gl[:, 7, :], AF.Exp), reads=[glB], writes=[glB])
                    k.op('vector', lambda e: e.memset(S[:], 0.0), writes=[SB])
                    if h == 0 and l == 0:
                        dump("gl", gl[:], glB)
                        dump("egl", egl[:], eglB)
                        dump("qT", qT[:, 0:128], qkvB[0])
                        dump("kT", kT[:, 0:128], qkvB[1])
                        dump("vT", vT[:, 0:128], qkvB[2])
                        dump("zs", zs[:, 0:2, :], zsB)
                    if ksub == 2:
                        k.barrier()
                        return

                    def bulk(gi, res):
                        c0 = gi * G
                        gcols = slice(c0 * 64, (c0 + G) * 64)
                        csl = [slice((c0 + g) * 64, (c0 + g + 1) * 64) for g in range(G)]
                        kg, kgB = r_kg.next()
                        kd, kdB = r_kdec.next()
                        for g in range(G):
                            k.op('tensor', lambda e, g=g: e.transpose(pb[0:64, g * 256:g * 256 + 128], kT[:, csl[g]], identb[:]), reads=[qkvB[1], cB], writes=[pbB])
                            k.op('tensor', lambda e, g=g: e.transpose(pb[0:64, g * 256 + 128:g * 256 + 256], vT[:, csl[g]], identb[:]), reads=[qkvB[2], cB], writes=[pbB])
                        yield
                        for g in range(G):
                            c = c0 + g
                            k.op('vector', lambda e, g=g, c=c: e.tensor_scalar_mul(kg[:, g, 128:256], pb[0:64, g * 256:g * 256 + 128], gl[:, 3, c:c + 1]), reads=[pbB, glB], writes=[kgB])
                            k.op('vector', lambda e, g=g, c=c: e.tensor_scalar_mul(kd[:, g, :], pb[0:64, g * 256:g * 256 + 128], gl[:, 4, c:c + 1]), reads=[pbB, glB], writes=[kdB])
                        k.op('scalar', lambda e: e.copy(kg[:, :, 0:128], pb[0:64, :].rearrange("p (g c) -> p g c", g=G)[:, :, 128:256]), reads=[pbB], writes=[kgB])
                        yield
                        p0, p0B = ps[0], psB[0]
                        for g in range(G):
                            k.op('tensor', lambda e, g=g: e.matmul(p0[0:64, g * 128:g * 128 + 64], kT[:, csl[g]], kT[:, csl[g]], start=True, stop=True), reads=[qkvB[1]], writes=[p0B])
                            k.op('tensor', lambda e, g=g: e.matmul(p0[0:64, g * 128 + 64:g * 128 + 128], kT[:, csl[g]], qT[:, csl[g]], start=True, stop=True), reads=[qkvB[1], qkvB[0]], writes=[p0B])
                        p0v = p0[0:64, :].rearrange("p (g c) -> p g c", g=G)
                        yield
                        dg, dgB = r_dg.next()
                        for g in range(G):
                            c = c0 + g
                            k.op('gpsimd', lambda e, g=g, c=c: e.tensor_scalar_mul(dg[:, g, :], ident[0:64, 0:64], gl[:, 2, c:c + 1]), reads=[glB, cB], writes=[dgB])
                        p1, p1B = ps[1], psB[1]
                        for g in range(G):
                            k.op('tensor', lambda e, g=g: e.matmul(p1[:, g * 64:(g + 1) * 64], ones_f[0:64, :], dg[:, g, :], start=True, stop=True), reads=[dgB, cB], writes=[p1B])
                        yield
                        dm, dmB = r_dm.next()
                        for g in range(G):
                            c = c0 + g
                            k.op('vector', lambda e, g=g, c=c: e.tensor_scalar(dm[:, g, :], p1[0:64, g * 64:(g + 1) * 64], gl[:, 2, c:c + 1], 0.0, ALU.subtract, ALU.min), reads=[p1B, glB], writes=[dmB])
                        yield
                        egb, egbB = r_egb.next()
                        k.op('scalar', lambda e: e.activation(egb[:].rearrange("p g c -> p (g c)"), p1[:, 0:G * 64], AF.Exp), reads=[p1B], writes=[egbB])
                        k.op('scalar', lambda e: e.activation(dm[:], dm[:], AF.Exp), reads=[dmB], writes=[dmB])
                        k.op('vector', lambda e: e.tensor_tensor(dm[:], dm[:], triu4[:], ALU.mult), reads=[dmB, cB], writes=[dmB])
                        qg, qgB = r_qg.next()
                        k.op('gpsimd', lambda e: e.tensor_tensor(qg[:].rearrange("p g c -> p (g c)"), qT[:, gcols], egb[:].rearrange("p g c -> p (g c)"), ALU.mult), reads=[qkvB[0], egbB], writes=[qgB])
                        yield
                        at, atB = r_at.next()
                        k.op('vector', lambda e: e.tensor_tensor(at[:], p0v[:, :, 64:128], dm[:], ALU.mult), reads=[p0B, dmB], writes=[atB])
                        k.op('vector', lambda e: e.tensor_tensor(dm[:], dm[:], ident4[:], ALU.subtract), reads=[dmB, cB], writes=[dmB])
                        X, XB = r_X.next()
                        for g in range(G):
                            c = c0 + g
                            k.op('vector', lambda e, X=X, g=g, c=c: e.scalar_tensor_tensor(X[:, g, :], p0[0:64, g * 128:g * 128 + 64], gl[:, 5, c:c + 1], dm[:, g, :], ALU.mult, ALU.mult),
                                 reads=[p0B, glB, dmB], writes=[XB])
                        yield
                        p2, p2B = ps[2], psB[2]
                        p3, p3B = ps[3], psB[3]
                        p4, p4B = ps[4], psB[4]
                        for g in range(G):
                            k.op('tensor', lambda e, X=X, g=g: e.transpose(p3[0:64, g * 64:(g + 1) * 64], X[:, g, :], ident[0:64, 0:64]), reads=[XB, cB], writes=[p3B])
                        yield
                        Y, YB = r_Y.next()
                        k.op('scalar', lambda e, Y=Y: e.copy(Y[:].rearrange("p g c -> p (g c)"), p3[0:64, 0:G * 64]), reads=[p3B], writes=[YB])
                        P, PB = r_P.next()
                        k.op('vector', lambda e, X=X: e.tensor_tensor(P[:], X[:], ident4[:], ALU.add), reads=[XB, cB], writes=[PB])
                        for s in range(1, 6):
                            if s < 5:
                                for g in range(G):
                                    k.op('tensor', lambda e, X=X, Y=Y, g=g: e.matmul(p2[0:64, g * 64:(g + 1) * 64], Y[:, g, :], X[:, g, :], start=True, stop=True), reads=[XB, YB], writes=[p2B])
                            for g in range(G):
                                k.op('tensor', lambda e, X=X, Y=Y, g=g: e.matmul(p3[0:64, g * 64:(g + 1) * 64], X[:, g, :], Y[:, g, :], start=True, stop=True), reads=[XB, YB], writes=[p3B])
                            yield
                            Yn, YnB = r_Y.next()
                            k.op('scalar', lambda e, Yn=Yn: e.copy(Yn[:].rearrange("p g c -> p (g c)"), p3[0:64, 0:G * 64]), reads=[p3B], writes=[YnB])
                            if s < 5:
                                Xn, XnB = r_X.next()
                                k.op('vector', lambda e, Xn=Xn: e.tensor_copy(Xn[:].rearrange("p g c -> p (g c)"), p2[0:64, 0:G * 64]), reads=[p2B], writes=[XnB])
                            yield
                            for g in range(G):
                                k.op('tensor', lambda e, Yn=Yn, g=g: e.matmul(p4[0:64, g * 64:(g + 1) * 64], Yn[:, g, :], P[:, g, :], start=True, stop=True), reads=[YnB, PB], writes=[p4B])
                            k.op('vector', lambda e: e.tensor_tensor(P[:].rearrange("p g c -> p (g c)"), P[:].rearrange("p g c -> p (g c)"), p4[0:64, 0:G * 64], ALU.add), reads=[p4B, PB], writes=[PB])
                            yield
                            Y, YB = Yn, YnB
                            if s < 5:
                                X, XB = Xn, XnB
                        yield
                        uw, uwB = r_uw.next()
                        for rr in range(G // 2):
                            for g2 in range(2):
                                g = rr * 2 + g2
                                k.op('tensor', lambda e, g=g, g2=g2: e.matmul(p4[0:64, g2 * 256:(g2 + 1) * 256], P[:, g, :], kg[:, g, :], start=True, stop=True), reads=[PB, kgB], writes=[p4B])
                            for g2 in range(2):
                                g = rr * 2 + g2
                                c = c0 + g
                                k.op('vector', lambda e, g=g, g2=g2, c=c: e.tensor_scalar_mul(uw[:, g, :], p4[0:64, g2 * 256:(g2 + 1) * 256], gl[:, 0, c:c + 1]), reads=[p4B, glB], writes=[uwB])
                        yield
                        AT, ATB = r_AT.next()
                        Bc, BcB = r_Bc.next()
                        QpT, QpTB = r_QpT.next()
                        O0, O0B = r_O0.next()
                        for g in range(G):
                            k.op('tensor', lambda e, g=g: e.matmul(p2[:, g * 128:(g + 1) * 128], uw[:, g, 128:256], kd[:, g, :], start=True, stop=True), reads=[uwB, kdB], writes=[p2B])
                        for g in range(G):
                            k.op('tensor', lambda e, g=g: e.matmul(p3[:, g * 128:(g + 1) * 128], kd[:, g, :], uw[:, g, 0:128], start=True, stop=True), reads=[uwB, kdB], writes=[p3B])
                        yield
                        for g in range(G):
                            c = c0 + g
                            k.op('vector', lambda e, g=g, c=c: e.scalar_tensor_tensor(AT[:, g, :], ident[:], egl[:, c:c + 1], p2[:, g * 128:(g + 1) * 128], ALU.mult, ALU.subtract),
                                 reads=[p2B, eglB, cB], writes=[ATB])
                        k.op('scalar', lambda e: e.copy(Bc[:].rearrange("p g c -> p (g c)"), p3[:, 0:G * 128]), reads=[p3B], writes=[BcB])
                        yield
                        for g in range(G):
                            k.op('tensor', lambda e, g=g: e.matmul(p0[:, g * 64:(g + 1) * 64], uw[:, g, 128:256], at[:, g, :], start=True, stop=True), reads=[uwB, atB], writes=[p0B])
                        for g in range(G):
                            k.op('tensor', lambda e, g=g: e.matmul(p4[0:64, g * 128:(g + 1) * 128], at[:, g, :], uw[:, g, 0:128], start=True, stop=True), reads=[uwB, atB], writes=[p4B])
                        yield
                        k.op('vector', lambda e: e.tensor_tensor(QpT[:].rearrange("p g c -> p (g c)"), qg[:].rearrange("p g c -> p (g c)"), p0[:, 0:G * 64], ALU.subtract),
                             reads=[p0B, qgB], writes=[QpTB])
                        k.op('scalar', lambda e: e.copy(O0[:].rearrange("p g c -> p (g c)"), p4[0:64, 0:G * 128]), reads=[p4B], writes=[O0B])
                        res.update(dict(AT=(AT, ATB), Bc=(Bc, BcB), QpT=(QpT, QpTB), O0=(O0, O0B)))

                    def scan(c, r, g):
                        AT_, ATB = r['AT']
                        Bc_, BcB = r['Bc']
                        QpT_, QpTB = r['QpT']
                        O0_, O0B = r['O0']
                        p5, p5B = ps[5], psB[5]
                        p6, p6B = ps[6], psB[6]
                        k.op('tensor', lambda e: e.matmul(p5[:, 0:128], AT_[:, g, :], S[:], start=True, stop=True), reads=[ATB, SB], writes=[p5B])
                        k.op('tensor', lambda e: e.matmul(p6[0:64, 0:128], QpT_[:, g, :], S[:], start=True, stop=True), reads=[QpTB, SB], writes=[p6B])
                        yield
                        k.op('vector', lambda e: e.tensor_tensor(S[:], p5[:, 0:128], Bc_[:, g, :], ALU.add), reads=[p5B, BcB, SB], writes=[SB])
                        yield
                        o, oB = r_o.next()
                        st, stB = r_st.next()
                        jk, jkB = r_jk.next()
                        k.op('vector', lambda e: e.tensor_tensor(o[:], p6[0:64, 0:128], O0_[:, g, :], ALU.add), reads=[p6B, O0B], writes=[oB])
                        k.op('scalar', lambda e: e.activation(jk[:], o[:], AF.Square, accum_out=st[:, 0:1]), reads=[oB], writes=[jkB, stB])
                        k.op('scalar', lambda e: e.activation(st[:, 1:2], st[:, 0:1], AF.Sqrt, bias=RMS_EPS, scale=1.0 / 128), reads=[stB], writes=[stB])
                        yield
                        k.op('vector', lambda e: e.reciprocal(st[:, 2:3], st[:, 1:2]), reads=[stB], writes=[stB])
                        k.op('vector', lambda e: e.scalar_tensor_tensor(o[:], o[:], st[:, 2:3], nw[0:64, :], ALU.mult, ALU.mult), reads=[oB, stB, nwB], writes=[oB])
                        ob, obB = r_ob.next()
                        k.op('gpsimd', lambda e: e.tensor_tensor(ob[:], o[:], zs[:, c, :], ALU.mult), reads=[oB, zsB], writes=[obB])
                        k.dma('sync', om_d[c * 64:(c + 1) * 64, h * 128:(h + 1) * 128], ob[:], reads=[obB])

                    assert NCH % G == 0
                    pend = {}
                    for _ in bulk(0, pend):
                        pass
                    for gi in range(NCH // G):
                        nxt = {}
                        bg = bulk(gi + 1, nxt) if gi + 1 < NCH // G else None
                        for g in range(G):
                            for _ in scan(gi * G + g, pend, g):
                                for _r in range(2):
                                    if bg is not None and next(bg, _DONE) is _DONE:
                                        bg = None
                        if bg is not None:
                            for _ in bg:
                                pass
                        pend = nxt
                k.barrier()

        def shared_kv():
            with ExitStack() as es:
                hbr = Ring(es, nc, "khb", [128, 8, 512], BF16, 2)
                kr = Ring(es, nc, "kst", [64, 512], BF16, 3)
                vr = Ring(es, nc, "vst", [128, 129], BF16, 3)
                for h in range(NH):
                    wk, wkB = load_w_bf16(es, f"wk{h}", kv_w[:, h * 128:(h + 1) * 128], 128) if h == 0 else (wk_keep, wkB_keep)
                    wv, wvB = load_w_bf16(es, f"wv{h}", kv_w[:, 1024 + h * 128:1024 + (h + 1) * 128], 128) if h == 0 else (wv_keep, wvB_keep)
                    if h == 0:
                        wk_keep, wkB_keep, wv_keep, wvB_keep = wk, wkB, wv, wvB
                    else:
                        k.dma('gpsimd', wk[:], kv_w[:, h * 128:(h + 1) * 128].rearrange("(k p) c -> p k c", p=128), writes=[wkB])
                        k.dma('gpsimd', wv[:], kv_w[:, 1024 + h * 128:1024 + (h + 1) * 128].rearrange("(k p) c -> p k c", p=128), writes=[wvB])
                    for blk in range(NB):
                        hb, hbB = hbr.next()
                        k.dma('sync', hb[:], hT_v[:, :, blk * 512:(blk + 1) * 512], writes=[hbB])
                        for s in range(2):
                            pp, ppB = ps[s], psB[s]
                            for kc in range(8):
                                k.op('tensor', lambda e, pp=pp, kc=kc, s=s, hb=hb: e.matmul(pp[0:64, :], wk[:, kc, s * 64:(s + 1) * 64], hb[:, kc, :],
                                                                                     start=(kc == 0), stop=(kc == 7)), reads=[wkB, hbB], writes=[ppB])
                            kt, ktB = kr.next()
                            k.op('scalar' if s else 'vector', (lambda e, kt=kt, pp=pp: e.copy(kt[:], pp[0:64, :])) if s else
                                 (lambda e, kt=kt, pp=pp: e.tensor_copy(kt[:], pp[0:64, :])), reads=[ppB], writes=[ktB])
                            k.dma('sync', kT_d[h, s, :, blk * 512:(blk + 1) * 512], kt[:], reads=[ktB])
                        for tt in range(4):
                            pp, ppB = ps[2 + tt % 2], psB[2 + tt % 2]
                            for kc in range(8):
                                k.op('tensor', lambda e, pp=pp, kc=kc, tt=tt, hb=hb: e.matmul(pp[:, 0:128], hb[:, kc, tt * 128:(tt + 1) * 128], wv[:, kc, :],
                                                                                       start=(kc == 0), stop=(kc == 7)), reads=[wvB, hbB], writes=[ppB])
                            vt, vtB = vr.next()
                            k.op('vector', lambda e, vt=vt, pp=pp: e.tensor_copy(vt[:, 0:128], pp[:, 0:128]), reads=[ppB], writes=[vtB])
                            k.op('gpsimd', lambda e, vt=vt: e.memset(vt[:, 128:129], 1.0), writes=[vtB])
                            tok0 = blk * 512 + tt * 128
                            k.dma('sync', va_d[h, tok0:tok0 + 128, :], vt[:], reads=[vtB])
                k.barrier()

        def diffattn(j, layer):
            lam_init = lambda_init(layer)
            with ExitStack() as es:
                lp, lpB = bcast_row(es, "lp", b_lambda[j:j + 1].rearrange("o a b -> o (a b)"), 256)
                sw, swB = bcast_row(es, "sw", b_subln_w[j:j + 1, :], 128)
                lam = sbt(es, "lam", [128, 8], F32)
                lamB = Buf("lam")
                pr = sbt(es, "lpr", [128, 128], F32)
                k.op('vector', lambda e: e.tensor_tensor(pr[:, 0:64], lp[:, 0:64], lp[:, 64:128], ALU.mult), reads=[lpB], writes=[lamB])
                k.op('vector', lambda e: e.tensor_tensor(pr[:, 64:128], lp[:, 128:192], lp[:, 192:256], ALU.mult), reads=[lpB], writes=[lamB])
                k.op('vector', lambda e: e.reduce_sum(lam[:, 0:1], pr[:, 0:64], AX.X), reads=[lamB], writes=[lamB])
                k.op('vector', lambda e: e.reduce_sum(lam[:, 1:2], pr[:, 64:128], AX.X), reads=[lamB], writes=[lamB])
                k.op('scalar', lambda e: e.activation(lam[:, 2:4], lam[:, 0:2], AF.Exp), reads=[lamB], writes=[lamB])
                k.op('vector', lambda e: e.tensor_tensor(lam[:, 4:5], lam[:, 2:3], lam[:, 3:4], ALU.subtract), reads=[lamB], writes=[lamB])
                k.op('vector', lambda e: e.tensor_scalar(lam[:, 5:6], lam[:, 4:5], lam_init, -1.0, ALU.add, ALU.mult), reads=[lamB], writes=[lamB])
                k.op('vector', lambda e: e.tensor_scalar_mul(sw[:], sw[:], 1.0 - lam_init), reads=[swB], writes=[swB])
                qs = [sbt(es, f"qs{s}", [64, T], BF16) for s in range(2)]
                qsB = [Buf("qs0"), Buf("qs1")]
                kTs = [sbt(es, f"kTs{s}", [64, T], BF16) for s in range(2)]
                kTB = [Buf("kT0"), Buf("kT1")]
                va = sbt(es, "va", [128, NT, 144], BF16)
                vaB = Buf("va")
                hbr = Ring(es, nc, "ahb", [128, 8, 512], BF16, 2)
                ptr = Ring(es, nc, "pt", [128, 512], BF16, 6)
                r_o1 = Ring(es, nc, "ao1", [128, 128], F32, 2)
                r_st = Ring(es, nc, "ast", [128, 8], F32, 2)
                r_jk = Ring(es, nc, "ajk", [128, 128], F32, 2)
                r_ob = Ring(es, nc, "aob", [128, 128], BF16, 2)
                wq = sbt(es, "wq", [128, 8, 128], BF16)
                wqB = Buf("wq")
                acc = {}
                slots = [(4, 0), (4, 144), (4, 288), (5, 0), (5, 144), (5, 288), (6, 0), (6, 144)]
                for s in range(2):
                    for i in range(4):
                        acc[(s, i)] = slots[s * 4 + i]
                for h in range(NH):
                    k.dma('gpsimd', wq[:], b_w_q[j, :, h * 128:(h + 1) * 128].rearrange("(k p) c -> p k c", p=128), writes=[wqB])
                    for s in range(2):
                        k.dma('sync', kTs[s][:], kT_d[h, s], writes=[kTB[s]])
                    k.dma('sync', va[:, :, 0:129], va_d[h].rearrange("(t p) c -> p t c", p=128), writes=[vaB])
                    for blk in range(NB):
                        hb, hbB = hbr.next()
                        k.dma('sync', hb[:], hT_v[:, :, blk * 512:(blk + 1) * 512], writes=[hbB])
                        for s in range(2):
                            pp, ppB = ps[s], psB[s]
                            for kc in range(8):
                                k.op('tensor', lambda e, pp=pp, kc=kc, s=s, hb=hb: e.matmul(pp[0:64, :], wq[:, kc, s * 64:(s + 1) * 64], hb[:, kc, :],
                                                                                     start=(kc == 0), stop=(kc == 7)), reads=[wqB, hbB], writes=[ppB])
                            k.op('scalar', lambda e, pp=pp, s=s, blk=blk: e.activation(qs[s][:, blk * 512:(blk + 1) * 512], pp[0:64, :], AF.Copy, scale=0.125),
                                 reads=[ppB], writes=[qsB[s]])
                    for qb in range(NB):
                        nkt = 4 * qb + 4
                        steps = [(kt, s) for kt in range(nkt) for s in range(2)]
                        LA = 2

                        def front(idx, qb=qb):
                            kt, s = steps[idx]
                            jd = kt - 4 * qb
                            lo = 0 if jd < 0 else jd * 128
                            n = 512 - lo
                            pp, ppB = ps[idx % 4], psB[idx % 4]
                            k.op('tensor', lambda e, pp=pp, s=s, kt=kt, qb=qb, lo=lo, n=n: e.matmul(pp[:, 0:n], kTs[s][:, kt * 128:(kt + 1) * 128],
                                                                                             qs[s][:, qb * 512 + lo:(qb + 1) * 512], start=True, stop=True),
                                 reads=[kTB[s], qsB[s]], writes=[ppB])
                            pt, ptB = ptr.next()
                            k.op('scalar', lambda e, pt=pt, pp=pp, n=n: e.activation(pt[:, 0:n], pp[:, 0:n], AF.Exp), reads=[ppB], writes=[ptB])
                            if jd >= 0:
                                k.op('vector', lambda e, pt=pt: e.tensor_tensor(pt[:, 0:128], pt[:, 0:128], triub[:], ALU.mult), reads=[ptB, cB], writes=[ptB])
                            return pt, ptB, jd, lo

                        def back(idx, fr, qb=qb):
                            kt, s = steps[idx]
                            pt, ptB, jd, lo = fr
                            for i in range(max(jd, 0), 4):
                                bank, off = acc[(s, i)]
                                c0 = i * 128 - lo
                                st_flag = (kt == 0 and off == 0)
                                k.op('tensor', lambda e, pt=pt, bank=bank, off=off, c0=c0, kt=kt, i=i, qb=qb, st_flag=st_flag: e.matmul(
                                    ps[bank][:, off:off + 129], pt[:, c0:c0 + 128], va[:, kt, 0:129], start=st_flag, stop=(kt == 4 * qb + i),
                                    skip_group_check=True),
                                    reads=[ptB, vaB], writes=[psB[bank]])

                        fronts = {}
                        for idx in range(len(steps) + LA):
                            if idx < len(steps):
                                fronts[idx] = front(idx)
                            if idx - LA >= 0:
                                back(idx - LA, fronts.pop(idx - LA))
                        for i in range(4):
                            b1, f1 = acc[(0, i)]
                            b2, f2 = acc[(1, i)]
                            st, stB = r_st.next()
                            o1, o1B = r_o1.next()
                            jk, jkB = r_jk.next()
                            ob, obB = r_ob.next()
                            k.op('vector', lambda e, st=st, b1=b1, f1=f1: e.reciprocal(st[:, 0:1], ps[b1][:, f1 + 128:f1 + 129]), reads=[psB[b1]], writes=[stB])
                            k.op('vector', lambda e, st=st, b2=b2, f2=f2: e.reciprocal(st[:, 1:2], ps[b2][:, f2 + 128:f2 + 129]), reads=[psB[b2]], writes=[stB])
                            k.op('vector', lambda e, st=st: e.tensor_tensor(st[:, 2:3], st[:, 1:2], lam[:, 5:6], ALU.mult), reads=[stB, lamB], writes=[stB])
                            k.op('vector', lambda e, st=st, o1=o1, b1=b1, f1=f1: e.tensor_scalar_mul(o1[:], ps[b1][:, f1:f1 + 128], st[:, 0:1]),
                                 reads=[psB[b1], stB], writes=[o1B])
                            k.op('vector', lambda e, st=st, o1=o1, b2=b2, f2=f2: e.scalar_tensor_tensor(o1[:], ps[b2][:, f2:f2 + 128], st[:, 2:3], o1[:], ALU.mult, ALU.add),
                                 reads=[psB[b2], stB, o1B], writes=[o1B])
                            k.op('scalar', lambda e, st=st, o1=o1, jk=jk: e.activation(jk[:], o1[:], AF.Square, accum_out=st[:, 3:4]), reads=[o1B], writes=[jkB, stB])
                            k.op('scalar', lambda e, st=st: e.activation(st[:, 4:5], st[:, 3:4], AF.Sqrt, bias=RMS_EPS, scale=1.0 / 128), reads=[stB], writes=[stB])
                            k.op('vector', lambda e, st=st: e.reciprocal(st[:, 5:6], st[:, 4:5]), reads=[stB], writes=[stB])
                            k.op('vector', lambda e, st=st, o1=o1, ob=ob: e.scalar_tensor_tensor(ob[:], o1[:], st[:, 5:6], sw[:], ALU.mult, ALU.mult),
                                 reads=[o1B, stB, swB], writes=[obB])
                            t0 = qb * 512 + i * 128
                            k.dma('sync', om_d[t0:t0 + 128, h * 128:(h + 1) * 128], ob[:], reads=[obB])
                k.barrier()

        def tok_moe(layer, w_out_src, last):
            with ExitStack() as es:
                wo, woB = load_w_bf16(es, "wo", w_out_src, 1024)
                wr = sbt(es, "wr", [128, 8, 40], F32)
                wrB = Buf("wr")
                k.dma('sync', wr[:, :, 0:4], moe_w_group[layer].rearrange("(k p) c -> p k c", p=128), writes=[wrB])
                k.dma('sync', wr[:, :, 4:36], moe_w_expert[layer].rearrange("(k p) c -> p k c", p=128), writes=[wrB])
                g1, g1B = bcast_row(es, "lng1", ln_mix_g[layer:layer + 1, :], D)
                b1, b1B = bcast_row(es, "lnb1", ln_mix_b[layer:layer + 1, :], D)
                lnB1 = Buf("ln1")
                oh = [sbt(es, f"oh{i}", [128, NT, 32], F32) for i in range(2)]
                ohB = Buf("oh")
                gates = sbt(es, "gates", [128, NT, 2], F32)
                gB = Buf("gates")
                rings = {'hTs': Ring(es, nc, "hTs2", [128, 8, 128], BF16, 2), 'st': Ring(es, nc, "lst", [128, 8], F32, 2),
                         'junk': Ring(es, nc, "ljk", [128, D], F32, 1)}
                with ExitStack() as e1:
                    r_om = Ring(e1, nc, "om", [128, D], BF16, 2)
                    r_omT = Ring(e1, nc, "omT", [128, 8, 128], BF16, 2)
                    r_h = Ring(e1, nc, "hh", [128, D], F32, 2)
                    r_t = Ring(e1, nc, "tt", [128, D], F32, 2)
                    r_y = Ring(e1, nc, "yy", [128, D], F32, 2)
                    r_xb = Ring(e1, nc, "xb", [128, D], BF16, 2)
                    r_xT = Ring(e1, nc, "xT", [128, 8, 128], F32, 2)
                    r_rt = Ring(e1, nc, "rt", [128, 160], F32, 2)
                    def tok_body(t):
                        rows = slice(t * 128, (t + 1) * 128)
                        om, omB = r_om.next()
                        k.dma('sync', om[:], om_d[rows, :], writes=[omB])
                        hh, hhB = r_h.next()
                        k.dma('sync', hh[:], h_d[rows, :], writes=[hhB])
                        for kc in range(8):
                            k.op('tensor', lambda e, om=om, kc=kc: e.transpose(pb[:, kc * 128:(kc + 1) * 128], om[:, kc * 128:(kc + 1) * 128], identb[:]),
                                 reads=[omB, cB], writes=[pbB])
                        omT, omTB = r_omT.next()
                        k.op('vector', lambda e, omT=omT: e.tensor_copy(omT[:], pb[:].rearrange("p (a b) -> p a b", a=8)), reads=[pbB], writes=[omTB])
                        yield
                        tt, ttB = r_t.next()
                        for half in range(2):
                            pp, ppB = ps[half], psB[half]
                            for kc in range(8):
                                k.op('tensor', lambda e, pp=pp, kc=kc, half=half, omT=omT: e.matmul(pp[:, :], omT[:, kc, :], wo[:, kc, half * 512:(half + 1) * 512],
                                                                                             start=(kc == 0), stop=(kc == 7)), reads=[omTB, woB], writes=[ppB])
                            k.op('vector', lambda e, pp=pp, half=half, tt=tt, hh=hh: e.scalar_tensor_tensor(tt[:, half * 512:(half + 1) * 512], hh[:, half * 512:(half + 1) * 512],
                                                                                                      ALPHA, pp[:, :], ALU.mult, ALU.add),
                                 reads=[ppB, hhB], writes=[ttB])
                        yield
                        yy, yyB = r_y.next()
                        layer_norm(rings, tt, ttB, g1, b1, [g1B, b1B], yy, yyB)
                        yield
                        k.dma('sync', h_d[rows, :], yy[:], reads=[yyB, hhB])
                        xb, xbB = r_xb.next()
                        k.op('scalar', lambda e, xb=xb, yy=yy: e.copy(xb[:], yy[:]), reads=[yyB], writes=[xbB])
                        k.dma('sync', xb_d[rows, :], xb[:], reads=[xbB])
                        yield
                        xT, xTB = r_xT.next()
                        for half in range(2):
                            pp, ppB = ps[2 + half], psB[2 + half]
                            for jj in range(4):
                                kc = half * 4 + jj
                                k.op('tensor', lambda e, pp=pp, jj=jj, kc=kc, yy=yy: e.transpose(pp[:, jj * 128:(jj + 1) * 128], yy[:, kc * 128:(kc + 1) * 128], ident[:]),
                                     reads=[yyB, cB], writes=[ppB])
                            if half == 0:
                                k.op('vector', lambda e, pp=pp, xT=xT: e.tensor_copy(xT[:, 0:4, :], pp[:].rearrange("p (a b) -> p a b", a=4)), reads=[ppB], writes=[xTB])
                            else:
                                k.op('scalar', lambda e, pp=pp, xT=xT: e.copy(xT[:, 4:8, :], pp[:].rearrange("p (a b) -> p a b", a=4)), reads=[ppB], writes=[xTB])
                        yield
                        p4, p4B = ps[4], psB[4]
                        for kc in range(8):
                            k.op('tensor', lambda e, kc=kc, xT=xT: e.matmul(p4[:, 0:36], xT[:, kc, :], wr[:, kc, 0:36], start=(kc == 0), stop=(kc == 7)),
                                 reads=[xTB, wrB], writes=[p4B])
                        rt, rtB = r_rt.next()
                        V = lambda fn, rt=rt, rtB=rtB, extra_r=(), extra_w=(): k.op('vector', fn, reads=[rtB] + list(extra_r), writes=[rtB] + list(extra_w))
                        k.op('vector', lambda e, rt=rt: e.tensor_copy(rt[:, 0:36], p4[:, 0:36]), reads=[p4B], writes=[rtB])
                        V(lambda e, rt=rt: e.reduce_max(rt[:, 36:37], rt[:, 0:4], AX.X))
                        V(lambda e, rt=rt: e.tensor_scalar_mul(rt[:, 37:38], rt[:, 36:37], -1.0))
                        k.op('scalar', lambda e, rt=rt: e.activation(rt[:, 84:88], rt[:, 0:4], AF.Exp, bias=rt[:, 37:38], accum_out=rt[:, 38:39]), reads=[rtB], writes=[rtB])
                        V(lambda e, rt=rt: e.reciprocal(rt[:, 39:40], rt[:, 38:39]))
                        V(lambda e, rt=rt: e.tensor_scalar(rt[:, 40:44], rt[:, 0:4], rt[:, 36:37], None, ALU.is_ge))
                        V(lambda e, rt=rt: e.tensor_scalar(rt[:, 40:44], rt[:, 40:44], -NEG, NEG, ALU.mult, ALU.add))
                        for g in range(4):
                            V(lambda e, rt=rt, g=g: e.tensor_scalar(rt[:, 44 + g * 8:52 + g * 8], rt[:, 4 + g * 8:12 + g * 8], rt[:, 40 + g:41 + g], None, ALU.add))
                        yield
                        V(lambda e, rt=rt: e.reduce_max(rt[:, 76:77], rt[:, 44:76], AX.X))
                        V(lambda e, rt=rt, t=t: e.tensor_scalar(oh[0][:, t, :], rt[:, 44:76], rt[:, 76:77], None, ALU.is_ge), extra_w=[ohB])
                        V(lambda e, rt=rt, t=t: e.scalar_tensor_tensor(rt[:, 96:128], oh[0][:, t, :], NEG, rt[:, 44:76], ALU.mult, ALU.add), extra_r=[ohB])
                        V(lambda e, rt=rt: e.reduce_max(rt[:, 77:78], rt[:, 96:128], AX.X))
                        V(lambda e, rt=rt, t=t: e.tensor_scalar(oh[1][:, t, :], rt[:, 96:128], rt[:, 77:78], None, ALU.is_ge), extra_w=[ohB])
                        V(lambda e, rt=rt: e.tensor_tensor(rt[:, 78:79], rt[:, 77:78], rt[:, 76:77], ALU.subtract))
                        k.op('scalar', lambda e, rt=rt: e.activation(rt[:, 79:80], rt[:, 78:79], AF.Exp), reads=[rtB], writes=[rtB])
                        V(lambda e, rt=rt: e.tensor_scalar_add(rt[:, 80:81], rt[:, 79:80], 1.0))
                        V(lambda e, rt=rt: e.reciprocal(rt[:, 81:82], rt[:, 80:81]))
                        V(lambda e, rt=rt, t=t: e.tensor_tensor(gates[:, t, 0:1], rt[:, 39:40], rt[:, 81:82], ALU.mult), extra_w=[gB])
                        V(lambda e, rt=rt, t=t: e.tensor_tensor(gates[:, t, 1:2], rt[:, 39:40], gates[:, t, 0:1], ALU.subtract), extra_r=[gB], extra_w=[gB])
                    interleave((tok_body(t) for t in range(NT)), depth=2)
                    k.barrier()
                if checkpoint(noraise=True):
                    return True
                dest = sbt(es, "dest", [128, 2, NT], I32)
                destB = Buf("dest")
                with ExitStack() as e2:
                    selb = sbt(e2, "selb", [128, NT, 32], BF16)
                    cnt = sbt(e2, "cnt", [128, NT, 32], F32)
                    pref = sbt(e2, "pref", [128, NT, 32], F32)
                    slot = sbt(e2, "slot", [128, NT, 32], F32)
                    ebase = sbt(e2, "ebase", [128, 32], F32)
                    tmp = sbt(e2, "ptmp", [128, NT, 32], F32)
                    dfl = sbt(e2, "dfl", [128, 2, NT], F32)
                    pB = Buf("pos")
                    k.op('gpsimd', lambda e: e.iota(ebase[:], [[CAP, 32]], base=0, channel_multiplier=0, allow_small_or_imprecise_dtypes=True), writes=[pB])
                    k.op('vector', lambda e: e.tensor_tensor(selb[:], oh[0][:], oh[1][:], ALU.add), reads=[ohB], writes=[pB])
                    TPB = 16
                    for t0 in range(0, NT, TPB):
                        n = min(TPB, NT - t0)
                        k.op('tensor', lambda e, t0=t0, n=n: e.matmul(ps[0][:, 0:n * 32], ones_b[:], selb[:, t0:t0 + n, :].rearrange("p a b -> p (a b)"), start=True, stop=True),
                             reads=[pB, cB], writes=[psB[0]])
                        k.op('vector', lambda e, t0=t0, n=n: e.tensor_copy(cnt[:, t0:t0 + n, :].rearrange("p a b -> p (a b)"), ps[0][:, 0:n * 32]), reads=[psB[0]], writes=[pB])
                        k.op('tensor', lambda e, t0=t0, n=n: e.matmul(ps[1][:, 0:n * 32], sutri[:], selb[:, t0:t0 + n, :].rearrange("p a b -> p (a b)"), start=True, stop=True),
                             reads=[pB, cB], writes=[psB[1]])
                        k.op('vector', lambda e, t0=t0, n=n: e.tensor_copy(slot[:, t0:t0 + n, :].rearrange("p a b -> p (a b)"), ps[1][:, 0:n * 32]), reads=[psB[1]], writes=[pB])
                    k.op('vector', lambda e: e.memset(pref[:, 0, :], 0.0), reads=[pB], writes=[pB])
                    for t in range(1, NT):
                        k.op('vector', lambda e, t=t: e.tensor_tensor(pref[:, t, :], pref[:, t - 1, :], cnt[:, t - 1, :], ALU.add), reads=[pB], writes=[pB])
                    k.op('vector', lambda e: e.tensor_tensor(slot[:], slot[:], pref[:], ALU.add), reads=[pB], writes=[pB])
                    k.op('vector', lambda e: e.tensor_scalar(tmp[:], slot[:], float(CAP), 1.0e6, ALU.is_ge, ALU.mult), reads=[pB], writes=[pB])
                    k.op('vector', lambda e: e.tensor_tensor(slot[:], slot[:], tmp[:], ALU.add), reads=[pB], writes=[pB])
                    for t in range(NT):
                        k.op('gpsimd', lambda e, t=t: e.tensor_tensor(slot[:, t, :], slot[:, t, :], ebase[:], ALU.add), reads=[pB], writes=[pB])
                    for i in range(2):
                        k.op('vector', lambda e, i=i: e.tensor_tensor(tmp[:], slot[:], oh[i][:], ALU.mult), reads=[pB, ohB], writes=[pB])
                        k.op('vector', lambda e, i=i: e.reduce_sum(dfl[:, i, :], tmp[:], AX.X), reads=[pB], writes=[pB])
                    k.op('vector', lambda e: e.tensor_scalar_min(dfl[:], dfl[:], float(NS)), reads=[pB], writes=[pB])
                    k.op('vector', lambda e: e.tensor_copy(dest[:], dfl[:]), reads=[pB], writes=[destB])
                    r_xb2 = Ring(e2, nc, "xb2", [128, D], BF16, 3)
                    for t in range(NT):
                        xb, xbB = r_xb2.next()
                        k.dma('sync', xb[:], xb_d[t * 128:(t + 1) * 128, :], writes=[xbB])
                        for i in range(2):
                            k.op('gpsimd', lambda e, xb=xb, i=i, t=t: e.indirect_dma_start(
                                out=xs_d, out_offset=bass.IndirectOffsetOnAxis(ap=dest[:, i, t:t + 1], axis=0), in_=xb[:], in_offset=None),
                                reads=[xbB, destB], dma=True)
                    k.barrier()
                with ExitStack() as e3:
                    NG = (CAP + 127) // 128
                    r_w13 = Ring(e3, nc, "w13", [128, 8, 1024], BF16, 2)
                    r_w2 = Ring(e3, nc, "w2", [128, 4, 1024], BF16, 2)
                    r_xs = Ring(e3, nc, "xs", [128, NG, D], BF16, 2)
                    r_xsT = Ring(e3, nc, "xsT", [128, 8, CAP], BF16, 2)
                    r_sg = Ring(e3, nc, "sg", [128, 4, CAP], F32, 1)
                    r_hid = Ring(e3, nc, "hid", [128, 4, CAP], BF16, 2)
                    r_ys = Ring(e3, nc, "ys", [128, D], F32, 2)
                    for ex in range(32):
                        w13, w13B = r_w13.next()
                        k.dma('gpsimd', w13[:], moe_w13[layer, ex].rearrange("(k p) c -> p k c", p=128), writes=[w13B])
                        w2, w2B = r_w2.next()
                        k.dma('gpsimd', w2[:], moe_w2[layer, ex].rearrange("(k p) c -> p k c", p=128), writes=[w2B])
                        xs, xsB = r_xs.next()
                        xsT, xsTB = r_xsT.next()
                        for g in range(NG):
                            r0 = ex * CAP + g * 128
                            nr = min(128, CAP - g * 128)
                            k.dma('sync', xs[0:nr, g, :], xs_d[r0:r0 + nr, :], writes=[xsB])
                        for g in range(NG):
                            nr = min(128, CAP - g * 128)
                            for kc in range(8):
                                k.op('tensor', lambda e, xs=xs, g=g, kc=kc, nr=nr: e.transpose(pb[:, kc * 128:kc * 128 + nr], xs[0:nr, g, kc * 128:(kc + 1) * 128], identb[0:nr, 0:nr]),
                                     reads=[xsB, cB], writes=[pbB])
                            k.op('vector', lambda e, xsT=xsT, g=g, nr=nr: e.tensor_copy(xsT[:, :, g * 128:g * 128 + nr], pb[:].rearrange("p (a b) -> p a b", a=8)[:, :, 0:nr]),
                                 reads=[pbB], writes=[xsTB])
                        sg, sgB = r_sg.next()
                        hid, hidB = r_hid.next()
                        for n0 in range(0, CAP, 512):
                            n = min(512, CAP - n0)
                            for m in range(8):
                                pp, ppB = ps[m % 4], psB[m % 4]
                                for kc in range(8):
                                    k.op('tensor', lambda e, pp=pp, kc=kc, m=m, w13=w13, xsT=xsT, n0=n0, n=n: e.matmul(pp[:, 0:n], w13[:, kc, m * 128:(m + 1) * 128], xsT[:, kc, n0:n0 + n],
                                                                                                           start=(kc == 0), stop=(kc == 7)),
                                         reads=[w13B, xsTB], writes=[ppB])
                                if m < 4:
                                    k.op('scalar', lambda e, pp=pp, m=m, sg=sg, n0=n0, n=n: e.activation(sg[:, m, n0:n0 + n], pp[:, 0:n], AF.Silu), reads=[ppB], writes=[sgB])
                                else:
                                    k.op('vector', lambda e, pp=pp, m=m, sg=sg, hid=hid, n0=n0, n=n: e.tensor_tensor(hid[:, m - 4, n0:n0 + n], sg[:, m - 4, n0:n0 + n], pp[:, 0:n], ALU.mult),
                                         reads=[ppB, sgB], writes=[hidB])
                        for g in range(NG):
                            nr = min(128, CAP - g * 128)
                            ys, ysB = r_ys.next()
                            for half in range(2):
                                pp, ppB = ps[4 + half], psB[4 + half]
                                for f in range(4):
                                    k.op('tensor', lambda e, pp=pp, f=f, half=half, hid=hid, w2=w2, g=g, nr=nr: e.matmul(pp[0:nr, :], hid[:, f, g * 128:g * 128 + nr], w2[:, f, half * 512:(half + 1) * 512],
                                                                                                             start=(f == 0), stop=(f == 3)),
                                         reads=[hidB, w2B], writes=[ppB])
                                if half == 0:
                                    k.op('vector', lambda e, pp=pp, ys=ys, nr=nr: e.tensor_copy(ys[0:nr, 0:512], pp[0:nr, :]), reads=[ppB], writes=[ysB])
                                else:
                                    k.op('scalar', lambda e, pp=pp, ys=ys, nr=nr: e.copy(ys[0:nr, 512:1024], pp[0:nr, :]), reads=[ppB], writes=[ysB])
                            r0 = ex * CAP + g * 128
                            for hf in range(2):
                                k.dma('sync', ys_h[hf][r0:r0 + nr, :], ys[0:nr, hf * 512:(hf + 1) * 512], reads=[ysB])
                    k.barrier()
                with ExitStack() as e4:
                    g2, g2B = bcast_row(e4, "lng2", ln_ffn_g[layer:layer + 1, :], D)
                    b2, b2B = bcast_row(e4, "lnb2", ln_ffn_b[layer:layer + 1, :], D)
                    r_yq = [Ring(e4, nc, f"yq{q}", [128, 512], F32, 2) for q in range(4)]
                    r_h = Ring(e4, nc, "ch", [128, D], F32, 2)
                    r_o = Ring(e4, nc, "co", [128, D], F32, 2)
                    def comb_body(t):
                        rows = slice(t * 128, (t + 1) * 128)
                        hh, hhB = r_h.next()
                        k.dma('sync', hh[:], h_d[rows, :], writes=[hhB])
                        k.op('scalar', lambda e, hh=hh: e.mul(hh[:], hh[:], ALPHA), reads=[hhB], writes=[hhB])
                        for i in range(2):
                            for hf in range(2):
                                yq, yqB = r_yq[i * 2 + hf].next()
                                k.op('gpsimd', lambda e, yq=yq, i=i, t=t, hf=hf: e.indirect_dma_start(
                                    out=yq[:], out_offset=None, in_=ys_h[hf],
                                    in_offset=bass.IndirectOffsetOnAxis(ap=dest[:, i, t:t + 1], axis=0)), reads=[destB], writes=[yqB], dma=True)
                                k.op('vector', lambda e, hh=hh, yq=yq, t=t, i=i, hf=hf: e.scalar_tensor_tensor(
                                    hh[:, hf * 512:(hf + 1) * 512], yq[:], gates[:, t, i:i + 1], hh[:, hf * 512:(hf + 1) * 512], ALU.mult, ALU.add),
                                    reads=[hhB, yqB, gB], writes=[hhB])
                        yield
                        oo, ooB = r_o.next()
                        layer_norm(rings, hh, hhB, g2, b2, [g2B, b2B], oo, ooB)
                        yield
                        if last:
                            out_toks.append(k.dma('sync', out[rows, :], oo[:], reads=[ooB]))
                        else:
                            k.dma('sync', h_d[rows, :], oo[:], reads=[ooB, hhB])
                            make_hT(rings, oo, ooB, t)
                    interleave((comb_body(t) for t in range(NT)), depth=2)
                    k.barrier()

        try:
            checkpoint()
            for layer in range(4):
                if layer < 2:
                    deltanet(layer)
                    checkpoint()
                    if tok_moe(layer, a_w_out[layer], last=False):
                        raise _Stop()
                    checkpoint()
                else:
                    j = layer - 2
                    if j == 0:
                        shared_kv()
                    diffattn(j, layer)
                    checkpoint()
                    if tok_moe(layer, b_w_out[j], last=(layer == 3)):
                        raise _Stop()
                    checkpoint()
        except _Stop:
            out_toks.append(k.dma('sync', out, h_d))
            out_toks.append(k.dma('sync', dbg, om_d))
        k.wait_all('sync', out_toks)
        k.finish()
    return nc, k


SEQ = 8192
CAP_FULL = 640
_cache = {}


def kernel(**inputs):
    x = np.asarray(inputs['x'])
    B, T, _ = x.shape
    cap = CAP_FULL if T == SEQ else max(64, int(T / 16 + 6 * math.sqrt(T / 16) + 16) // 32 * 32 + 32)
    key = (T, cap)
    if key not in _cache:
        _cache[key] = build(T, cap)[0]
    nc = _cache[key]
    shared = {n: np.ascontiguousarray(np.asarray(v, dtype=np.float32)) for n, v in inputs.items() if n != 'x'}
    in_maps = []
    for b in range(B):
        m = dict(shared)
        m['x'] = np.ascontiguousarray(x[b])
        in_maps.append(m)
    res = run_bass_kernel_spmd(nc, in_maps, core_ids=list(range(B)))
    return np.stack([np.asarray(res.results[b]['out']) for b in range(B)], axis=0).astype(np.float32)
```

```python
import math
from contextlib import ExitStack
import numpy as np
import concourse.bass as bass
import concourse.mybir as mybir
from concourse.bass_utils import run_bass_kernel_spmd

F32 = mybir.dt.float32
BF16 = mybir.dt.bfloat16
I32 = mybir.dt.int32
AF = mybir.ActivationFunctionType
ALU = mybir.AluOpType
AX = mybir.AxisListType

ENGS = ['tensor', 'vector', 'scalar', 'gpsimd', 'sync']
SEM_EPOCH = 30000
N_DMA_SEMS = 16


class Buf:
    __slots__ = ('name', 'w', 'r', 'excl')

    def __init__(self, name='', excl=False):
        self.name = name
        self.excl = excl
        self.w = None
        self.r = {}


class _Op:
    __slots__ = ('fn', 'waits', 'inc', 'incval', 'dma')

    def __init__(self, fn, waits, dma):
        self.fn = fn
        self.waits = waits
        self.inc = False
        self.incval = 0
        self.dma = dma


class K:
    def __init__(self, nc):
        self.nc = nc
        self.ops = {e: [] for e in ENGS}
        self.waited = {e: {} for e in ENGS}
        self.dma_rr = {e: 0 for e in ENGS}
        self.dma_cnt = {}

    def _need_wait(self, eng, t):
        key = (t[0], t[1])
        if self.waited[eng].get(key, -1) >= t[2]:
            return False
        self.waited[eng][key] = t[2]
        if t[0] == 'e':
            self.ops[t[1]][t[2]].inc = True
        return True

    def op(self, eng, fn, reads=(), writes=(), dma=False):
        idx = len(self.ops[eng])
        writes = list(writes) + [b for b in reads if b.excl]
        reads = [b for b in reads if not b.excl]
        deps = []
        for b in reads:
            if b.w is not None:
                deps.append(b.w)
        for b in writes:
            if b.w is not None:
                deps.append(b.w)
            deps.extend(b.r.values())
        waits = []
        for t in deps:
            if t[0] == 'e' and t[1] == eng and eng == 'tensor':
                continue
            if self._need_wait(eng, t):
                waits.append(t)
        dm = None
        if dma:
            slot = self.dma_rr[eng]
            self.dma_rr[eng] = (slot + 1) % N_DMA_SEMS
            cnt = self.dma_cnt.get((eng, slot), 0) + 1
            self.dma_cnt[(eng, slot)] = cnt
            dm = ((eng, slot), cnt * 16)
            if cnt > 1:
                t = ('d', (eng, slot), (cnt - 1) * 16)
                if self._need_wait(eng, t):
                    waits.append(t)
            tok = ('d', (eng, slot), cnt * 16)
        else:
            tok = ('e', eng, idx)
        self.ops[eng].append(_Op(fn, waits, dm))
        kk = (tok[0], tok[1])
        for b in reads:
            b.r[kk] = tok
        for b in writes:
            b.w = tok
            b.r = {}
        return tok

    def dma(self, eng, out, in_, reads=(), writes=(), **kw):
        return self.op(eng, lambda e: e.dma_start(out=out, in_=in_, **kw), reads=reads, writes=writes, dma=True)

    def wait_all(self, eng, tokens):
        waits = [t for t in tokens if self._need_wait(eng, t)]
        if waits:
            self.ops[eng].append(_Op(None, waits, None))

    def barrier(self):
        toks = []
        for e in ENGS:
            for i in range(len(self.ops[e]) - 1, -1, -1):
                o = self.ops[e][i]
                if o.fn is not None and o.dma is None:
                    toks.append(('e', e, i))
                    break
        for key, cnt in self.dma_cnt.items():
            toks.append(('d', key, cnt * 16))
        for e in ENGS:
            self.wait_all(e, toks)
        self.flush()

    def flush(self):
        nc = self.nc
        if not hasattr(self, 'flushed'):
            self.flushed = {e: 0 for e in ENGS}
            self.inccnt = {e: 0 for e in ENGS}
            self.esems = {e: [] for e in ENGS}
            self.dsems = {}
        start = dict(self.flushed)
        for e in ENGS:
            for o in self.ops[e][start[e]:]:
                if o.inc:
                    self.inccnt[e] += 1
                    o.incval = self.inccnt[e]
            need = (self.inccnt[e] + SEM_EPOCH - 1) // SEM_EPOCH
            while len(self.esems[e]) < max(need, 1):
                self.esems[e].append(nc.alloc_semaphore(f"es_{e}_{len(self.esems[e])}"))
        for key in self.dma_cnt:
            if key not in self.dsems:
                self.dsems[key] = nc.alloc_semaphore(f"ds_{key[0]}_{key[1]}")
        esems, dsems, ops = self.esems, self.dsems, self.ops

        def semval(e2, incval):
            return esems[e2][(incval - 1) // SEM_EPOCH], (incval - 1) % SEM_EPOCH + 1

        with nc.Block() as block:
            for eng in ENGS:
                todo = ops[eng][start[eng]:]

                def body(e, eng=eng, todo=todo):
                    for o in todo:
                        for t in o.waits:
                            if t[0] == 'e':
                                p = ops[t[1]][t[2]]
                                assert p.incval > 0, (eng, t)
                                s, v = semval(t[1], p.incval)
                                e.wait_ge(s, v)
                            else:
                                e.wait_ge(dsems[t[1]], t[2])
                        if o.fn is None:
                            continue
                        ins = o.fn(e)
                        if o.dma is not None:
                            ins.then_inc(dsems[o.dma[0]], 16)
                        elif o.inc:
                            s, v = semval(eng, o.incval)
                            ins.then_inc(s, 1)
                if todo:
                    getattr(block, eng)(body)
                self.flushed[eng] = len(ops[eng])

    def finish(self):
        self.flush()


_uid = [0]


def _un(name):
    _uid[0] += 1
    return f"{name}_u{_uid[0]}"


def interleave(gens, depth=2):
    active = []
    it = iter(gens)
    while True:
        while len(active) < depth:
            g = next(it, None)
            if g is None:
                break
            active.append(g)
        if not active:
            break
        for g in list(active):
            try:
                next(g)
            except StopIteration:
                active.remove(g)


_DONE = object()


class Ring:
    def __init__(self, es, nc, name, shape, dt, n):
        self.t = [es.enter_context(nc.sbuf_tensor(_un(f"{name}_{i}"), shape, dt)) for i in range(n)]
        self.b = [Buf(f"{name}_{i}") for i in range(n)]
        self.i = 0

    def next(self):
        i = self.i
        self.i = (i + 1) % len(self.t)
        return self.t[i], self.b[i]


D = 1024
NH = 8
ALPHA = 8 ** 0.25
LN_EPS = 1e-5
RMS_EPS = 1e-6
NEG = -1.0e30


def lambda_init(layer_idx):
    return 0.8 - 0.6 * math.exp(-0.3 * layer_idx)


class _Stop(Exception):
    pass


def build(T, CAP, stop=0, ksub=0, dumpflag=False):
    NT = T // 128
    NCH = T // 64
    NB = T // 512
    nc = bass.Bass("TRN2", target_bir_lowering=False)

    def din(name, shape, dt=F32):
        return nc.dram_tensor(name, shape, dt, kind="ExternalInput").ap()

    x = din("x", [T, D])
    a_w_in = din("a_w_in", [2, D, 4112])
    a_conv_w = din("a_conv_w", [2, 4, 3072])
    a_a_log = din("a_a_log", [2, 8])
    a_dt_bias = din("a_dt_bias", [2, 8])
    a_norm_w = din("a_norm_w", [2, 128])
    a_w_out = din("a_w_out", [2, D, D])
    kv_w = din("kv_w", [D, 2048])
    b_w_q = din("b_w_q", [2, D, D])
    b_lambda = din("b_lambda", [2, 4, 64])
    b_subln_w = din("b_subln_w", [2, 128])
    b_w_out = din("b_w_out", [2, D, D])
    ln_mix_g = din("ln_mix_g", [4, D])
    ln_mix_b = din("ln_mix_b", [4, D])
    ln_ffn_g = din("ln_ffn_g", [4, D])
    ln_ffn_b = din("ln_ffn_b", [4, D])
    moe_w_group = din("moe_w_group", [4, D, 4])
    moe_w_expert = din("moe_w_expert", [4, D, 32])
    moe_w13 = din("moe_w13", [4, 32, D, 1024])
    moe_w2 = din("moe_w2", [4, 32, 512, D])
    out = nc.dram_tensor("out", [T, D], F32, kind="ExternalOutput").ap()
    dbg = nc.dram_tensor("dbg", [T, D], BF16, kind="ExternalOutput").ap() if stop else None
    stage = [0]

    def checkpoint(noraise=False):
        stage[0] += 1
        if stop and stage[0] >= stop:
            if noraise:
                return True
            raise _Stop()
        return False

    h_d = nc.dram_tensor("h_d", [T, D], F32).ap()
    hT_d = nc.dram_tensor("hT_d", [D, T], BF16).ap()
    om_d = nc.dram_tensor("om_d", [T, D], BF16).ap()
    xb_d = nc.dram_tensor("xb_d", [T, D], BF16).ap()
    kT_d = nc.dram_tensor("kT_d", [NH, 2, 64, T], BF16).ap()
    va_d = nc.dram_tensor("va_d", [NH, T, 129], BF16).ap()
    NS = 32 * CAP
    xs_d = nc.dram_tensor("xs_d", [NS + 128, D], BF16).ap()
    ys_h = [nc.dram_tensor(f"ys_d{i}", [NS + 128, 512], F32).ap() for i in range(2)]
    hT_v = hT_d.rearrange("(k p) t -> p k t", p=128)

    k = K(nc)
    out_toks = []
    dumped = set()

    def dump(name, ap, B):
        if not dumpflag or name in dumped:
            return
        dumped.add(name)
        t = nc.dram_tensor("dump_" + name, list(ap.shape), F32, kind="ExternalOutput").ap()
        out_toks.append(k.dma('gpsimd', t, ap, reads=[B]))
    with ExitStack() as top:
        def sbt(es, name, shape, dt):
            return es.enter_context(nc.sbuf_tensor(_un(name), shape, dt))

        ps = [top.enter_context(nc.psum_tensor(f"ps{i}", [128, 512], F32)) for i in range(7)]
        psB = [Buf(f"ps{i}", excl=True) for i in range(7)]
        pb = top.enter_context(nc.psum_tensor("pb", [128, 1024], BF16))
        pbB = Buf("pb", excl=True)

        ident = sbt(top, "ident", [128, 128], F32)
        identb = sbt(top, "identb", [128, 128], BF16)
        ones_f = sbt(top, "ones_f", [128, 128], F32)
        ones_b = sbt(top, "ones_b", [128, 128], BF16)
        triu = sbt(top, "triu", [128, 128], F32)
        triub = sbt(top, "triub", [128, 128], BF16)
        sutri = sbt(top, "sutri", [128, 128], BF16)
        zero_b = sbt(top, "zero_b", [128, 1024], BF16)
        triu4 = sbt(top, "triu4", [64, 4, 64], F32)
        ident4 = sbt(top, "ident4", [64, 4, 64], F32)
        cB = Buf("consts")
        k.op('gpsimd', lambda e: e.iota(ones_f[:], [[1, 128]], base=0, channel_multiplier=-1,
                                        allow_small_or_imprecise_dtypes=True), writes=[cB])
        k.op('vector', lambda e: e.tensor_single_scalar(ident[:], ones_f[:], 0.0, ALU.is_equal), reads=[cB], writes=[cB])
        k.op('vector', lambda e: e.tensor_single_scalar(identb[:], ones_f[:], 0.0, ALU.is_equal), reads=[cB], writes=[cB])
        k.op('vector', lambda e: e.tensor_single_scalar(triu[:], ones_f[:], 0.0, ALU.is_ge), reads=[cB], writes=[cB])
        k.op('vector', lambda e: e.tensor_single_scalar(triub[:], ones_f[:], 0.0, ALU.is_ge), reads=[cB], writes=[cB])
        k.op('vector', lambda e: e.tensor_single_scalar(sutri[:], ones_f[:], 0.0, ALU.is_gt), reads=[cB], writes=[cB])
        for g4 in range(4):
            k.op('vector', lambda e, g4=g4: e.tensor_copy(triu4[:, g4, :], triu[0:64, 0:64]), reads=[cB], writes=[cB])
            k.op('vector', lambda e, g4=g4: e.tensor_copy(ident4[:, g4, :], ident[0:64, 0:64]), reads=[cB], writes=[cB])
        k.op('vector', lambda e: e.memset(ones_f[:], 1.0), reads=[cB], writes=[cB])
        k.op('vector', lambda e: e.memset(ones_b[:], 1.0), writes=[cB])
        k.op('vector', lambda e: e.memset(zero_b[:], 0.0), writes=[cB])
        zero_f = sbt(top, "zero_f", [128, 512], F32)
        k.op('vector', lambda e: e.memset(zero_f[:], 0.0), writes=[cB])
        for r0 in range(0, NS + 128, 128):
            k.dma('sync', xs_d[r0:r0 + 128, :], zero_b[:], reads=[cB])
        for hf in range(2):
            k.dma('sync', ys_h[hf][NS:NS + 128, :], zero_f[:], reads=[cB])
        k.barrier()

        def layer_norm(es_ring, t, tB, gbc, bbc, wBs, y, yB):
            st, stB = es_ring['st'].next()
            jk, jkB = es_ring['junk'].next()
            k.op('scalar', lambda e: e.activation(jk[:], t[:], AF.Copy, accum_out=st[:, 0:1]), reads=[tB], writes=[jkB, stB])
            k.op('scalar', lambda e: e.activation(jk[:], t[:], AF.Square, accum_out=st[:, 1:2]), reads=[tB], writes=[jkB, stB])
            k.op('vector', lambda e: e.tensor_scalar_mul(st[:, 2:3], st[:, 0:1], 1.0 / D), reads=[stB], writes=[stB])
            k.op('vector', lambda e: e.tensor_tensor(st[:, 3:4], st[:, 2:3], st[:, 2:3], ALU.mult), reads=[stB], writes=[stB])
            k.op('vector', lambda e: e.scalar_tensor_tensor(st[:, 4:5], st[:, 1:2], 1.0 / D, st[:, 3:4], ALU.mult, ALU.subtract),
                 reads=[stB], writes=[stB])
            k.op('scalar', lambda e: e.activation(st[:, 5:6], st[:, 4:5], AF.Sqrt, bias=LN_EPS), reads=[stB], writes=[stB])
            k.op('vector', lambda e: e.reciprocal(st[:, 6:7], st[:, 5:6]), reads=[stB], writes=[stB])
            k.op('vector', lambda e: e.tensor_scalar(t[:], t[:], st[:, 2:3], st[:, 6:7], ALU.subtract, ALU.mult), reads=[tB, stB], writes=[tB])
            k.op('gpsimd', lambda e: e.tensor_tensor(t[:], t[:], gbc[:], ALU.mult), reads=[tB] + wBs, writes=[tB])
            k.op('vector', lambda e: e.tensor_tensor(y[:], t[:], bbc[:], ALU.add), reads=[tB] + wBs, writes=[yB])

        def make_hT(rings, y, yB, tile):
            hb, hbB = rings['hTs'].next()
            for half in range(2):
                pp, ppB = ps[5 + half], psB[5 + half]
                for j in range(4):
                    kk = half * 4 + j
                    k.op('tensor', lambda e, pp=pp, j=j, kk=kk: e.transpose(pp[:, j * 128:(j + 1) * 128], y[:, kk * 128:(kk + 1) * 128], ident[:]),
                         reads=[yB, cB], writes=[ppB])
                eng = 'vector' if half == 0 else 'scalar'
                if eng == 'vector':
                    k.op('vector', lambda e, pp=pp, half=half: e.tensor_copy(hb[:, half * 4:(half + 1) * 4, :], pp[:].rearrange("p (a b) -> p a b", a=4)),
                         reads=[ppB], writes=[hbB])
                else:
                    k.op('scalar', lambda e, pp=pp, half=half: e.copy(hb[:, half * 4:(half + 1) * 4, :], pp[:].rearrange("p (a b) -> p a b", a=4)),
                         reads=[ppB], writes=[hbB])
            k.dma('sync', hT_v[:, :, tile * 128:(tile + 1) * 128], hb[:], reads=[hbB])

        with ExitStack() as es:
            rings = {'hTs': Ring(es, nc, "hTs", [128, 8, 128], BF16, 2)}
            xr = Ring(es, nc, "xin", [128, D], F32, 2)
            for t in range(NT):
                xt, xB = xr.next()
                k.dma('sync', xt[:], x[t * 128:(t + 1) * 128, :], writes=[xB])
                k.dma('gpsimd', h_d[t * 128:(t + 1) * 128, :], xt[:], reads=[xB])
                make_hT(rings, xt, xB, t)
            k.barrier()

        def load_w_bf16(es, name, src, cols):
            w = sbt(es, name, [128, 8, cols], BF16)
            wB = Buf(name)
            k.dma('gpsimd', w[:], src.rearrange("(k p) c -> p k c", p=128), writes=[wB])
            return w, wB

        def bcast_row(es, name, src_row, n, dt=F32):
            w = sbt(es, name, [128, n], dt)
            wB = Buf(name)
            k.dma('sync', w[:], src_row.partition_broadcast(128), writes=[wB])
            return w, wB

        def deltanet(l):
            with ExitStack() as es:
                cw = sbt(es, "cw", [128, 24, 4], F32)
                cwB = Buf("cw")
                cwn = sbt(es, "cwn", [4, 3072], F32)
                k.dma('sync', cwn[:], a_conv_w[l], writes=[cwB])
                for part in range(24):
                    k.op('tensor', lambda e, part=part: e.transpose(ps[0][:, part * 4:(part + 1) * 4], cwn[:, part * 128:(part + 1) * 128], ident[0:4, 0:4]),
                         reads=[cwB, cB], writes=[psB[0]])
                k.op('vector', lambda e: e.tensor_copy(cw[:].rearrange("p a b -> p (a b)"), ps[0][:, 0:96]), reads=[psB[0]], writes=[cwB])
                nw, nwB = bcast_row(es, "nw", a_norm_w[l:l + 1, :], 128)
                alog, alB = bcast_row(es, "alog", a_a_log[l:l + 1, :], 8)
                dtb, dtB = bcast_row(es, "dtb", a_dt_bias[l:l + 1, :], 8)
                nea = sbt(es, "nea", [128, 8], F32)
                k.op('scalar', lambda e: e.activation(nea[:], alog[:], AF.Exp), reads=[alB], writes=[alB])
                k.op('vector', lambda e: e.tensor_scalar_mul(nea[:], nea[:], -1.0), reads=[alB], writes=[alB])
                qT = sbt(es, "qT", [128, T], BF16)
                kT = sbt(es, "kT", [128, T], BF16)
                vT = sbt(es, "vT", [128, T], BF16)
                qkvB = [Buf("qT"), Buf("kT"), Buf("vT")]
                qkv = [qT, kT, vT]
                zs = sbt(es, "zs", [64, NCH, 128], BF16)
                zsB = Buf("zs")
                gl = sbt(es, "gl", [64, 8, NCH], F32)
                glB = Buf("gl")
                egl = sbt(es, "egl", [128, NCH], F32)
                eglB = Buf("egl")
                S = sbt(es, "S", [128, 128], F32)
                SB = Buf("S")
                hbr = Ring(es, nc, "hblk", [128, 8, 512], BF16, 2)
                raw = sbt(es, "raw", [128, 3, 515], F32)
                rawB = [Buf("raw0"), Buf("raw1"), Buf("raw2")]
                cvr = Ring(es, nc, "cv", [128, 512], F32, 2)
                sqr = Ring(es, nc, "sq", [128, 512], F32, 2)
                G = 4
                R = 2
                r_kg = Ring(es, nc, "kg", [64, G, 256], F32, R)
                r_kdec = Ring(es, nc, "kdec", [64, G, 128], F32, R)
                r_dm = Ring(es, nc, "dm", [64, G, 64], F32, R)
                r_dg = Ring(es, nc, "dg", [64, G, 64], F32, R)
                r_egb = Ring(es, nc, "egb", [128, G, 64], F32, R)
                r_qg = Ring(es, nc, "qg", [128, G, 64], F32, R)
                r_at = Ring(es, nc, "at", [64, G, 64], F32, R)
                r_X = Ring(es, nc, "X", [64, G, 64], BF16, 4)
                r_Y = Ring(es, nc, "Y", [64, G, 64], BF16, 4)
                r_Pb = Ring(es, nc, "Pb", [64, G, 64], BF16, 2)
                r_P = Ring(es, nc, "P", [64, G, 64], F32, R)
                r_uw = Ring(es, nc, "uw", [64, G, 256], F32, R)
                r_AT = Ring(es, nc, "AT", [128, G, 128], F32, R)
                r_Bc = Ring(es, nc, "Bc", [128, G, 128], F32, R)
                r_QpT = Ring(es, nc, "QpT", [128, G, 64], F32, R)
                r_O0 = Ring(es, nc, "O0", [64, G, 128], F32, R)
                r_vn = Ring(es, nc, "vn", [64, 128], F32, 2)
                r_o = Ring(es, nc, "o", [64, 128], F32, 2)
                r_ob = Ring(es, nc, "ob", [64, 128], BF16, 2)
                r_st = Ring(es, nc, "dst", [64, 4], F32, 2)
                r_jk = Ring(es, nc, "djk", [64, 128], F32, 2)
                for h in range(NH):
                    wq3, wq3B = [], []
                    wcat = sbt(es, f"wcat{h}", [128, 8, 384], BF16) if h == 0 else wcat_keep[0]
                    wzg = sbt(es, f"wzg{h}", [128, 8, 128], BF16) if h == 0 else wcat_keep[1]
                    if h == 0:
                        wcat_keep = [wcat, wzg]
                        wcB = Buf("wcat")
                        wzB = Buf("wzg")
                    for part in range(3):
                        k.dma('gpsimd', wcat[:, :, part * 128:(part + 1) * 128],
                              a_w_in[l, :, part * 1024 + h * 128: part * 1024 + (h + 1) * 128].rearrange("(k p) c -> p k c", p=128), writes=[wcB])
                    k.dma('gpsimd', wzg[:], a_w_in[l, :, 3072 + h * 128:3072 + (h + 1) * 128].rearrange("(k p) c -> p k c", p=128), writes=[wzB])
                    if h == 0:
                        wg16 = sbt(es, "wg16", [128, 8, 16], BF16)
                        k.dma('gpsimd', wg16[:], a_w_in[l, :, 4096:4112].rearrange("(k p) c -> p k c", p=128), writes=[wzB])
                    for part in range(3):
                        k.op('vector', lambda e, part=part: e.memset(raw[:, part, 0:3], 0.0), writes=[rawB[part]])
                    if ksub == 5:
                        k.barrier()
                        return
                    for blk in range(NB):
                        hb, hbB = hbr.next()
                        k.dma('sync', hb[:], hT_v[:, :, blk * 512:(blk + 1) * 512], writes=[hbB])
                        for part in range(3):
                            pp, ppB = ps[part % 2], psB[part % 2]
                            for kc in range(8):
                                k.op('tensor', lambda e, pp=pp, kc=kc, part=part, hb=hb: e.matmul(pp[:, :], wcat[:, kc, part * 128:(part + 1) * 128], hb[:, kc, :],
                                                                                            start=(kc == 0), stop=(kc == 7)),
                                     reads=[wcB, hbB], writes=[ppB])
                            k.op('scalar', lambda e, pp=pp, part=part: e.copy(raw[:, part, 3:515], pp[:, :]), reads=[ppB], writes=[rawB[part]])
                            if ksub == 11:
                                k.barrier()
                                return
                            cv, cvB = cvr.next()
                            ci = part * 8 + h
                            k.op('vector', lambda e, cv=cv, part=part, ci=ci: e.tensor_scalar_mul(cv[:], raw[:, part, 0:512], cw[:, ci, 0:1]),
                                 reads=[rawB[part], cwB], writes=[cvB])
                            for j in range(1, 4):
                                k.op('vector', lambda e, cv=cv, part=part, ci=ci, j=j: e.scalar_tensor_tensor(cv[:], raw[:, part, j:j + 512], cw[:, ci, j:j + 1], cv[:],
                                                                                                        ALU.mult, ALU.add),
                                     reads=[rawB[part], cwB, cvB], writes=[cvB])
                            k.op('vector', lambda e, part=part: e.tensor_copy(raw[:, part, 0:3], raw[:, part, 512:515]), reads=[rawB[part]], writes=[rawB[part]])
                            if ksub == 12:
                                k.barrier()
                                return
                            dst = qkv[part][:, blk * 512:(blk + 1) * 512]
                            if part == 2:
                                k.op('scalar', lambda e, cv=cv, dst=dst: e.activation(dst, cv[:], AF.Silu), reads=[cvB], writes=[qkvB[part]])
                            else:
                                k.op('scalar', lambda e, cv=cv: e.activation(cv[:], cv[:], AF.Silu), reads=[cvB], writes=[cvB])
                                sq, sqB = sqr.next()
                                k.op('gpsimd', lambda e, cv=cv, sq=sq: e.tensor_tensor(sq[:], cv[:], cv[:], ALU.mult), reads=[cvB], writes=[sqB])
                                p2, p2B = ps[2], psB[2]
                                for hf in range(2):
                                    k.op('tensor', lambda e, sq=sq, p2=p2, hf=hf: e.matmul(p2[:, hf * 256:(hf + 1) * 256], ones_f[:], sq[:, hf * 256:(hf + 1) * 256], start=True, stop=True), reads=[sqB, cB], writes=[p2B])
                                k.op('scalar', lambda e, sq=sq, p2=p2: e.activation(sq[:], p2[:, :], AF.Sqrt, bias=RMS_EPS), reads=[p2B], writes=[sqB])
                                k.op('vector', lambda e, sq=sq: e.reciprocal(sq[:], sq[:]), reads=[sqB], writes=[sqB])
                                sc = (128 ** -0.5) if part == 0 else 1.0
                                k.op('vector', lambda e, cv=cv, sq=sq, dst=dst, sc=sc: e.scalar_tensor_tensor(dst, cv[:], sc, sq[:], ALU.mult, ALU.mult),
                                     reads=[cvB, sqB], writes=[qkvB[part]])
                            if ksub == 13:
                                k.barrier()
                                return
                        if ksub == 14:
                            k.barrier()
                            return
                        for cc in range(8):
                            c = blk * 8 + cc
                            pp, ppB = ps[3 + cc % 2], psB[3 + cc % 2]
                            for kc in range(8):
                                k.op('tensor', lambda e, pp=pp, kc=kc, cc=cc, hb=hb: e.matmul(pp[0:64, 0:128], hb[:, kc, cc * 64:(cc + 1) * 64], wzg[:, kc, :],
                                                                                       start=(kc == 0), stop=(kc == 7)),
                                     reads=[wzB, hbB], writes=[ppB])
                            for kc in range(8):
                                k.op('tensor', lambda e, pp=pp, kc=kc, cc=cc, hb=hb: e.matmul(pp[0:64, 128:144], hb[:, kc, cc * 64:(cc + 1) * 64], wg16[:, kc, :],
                                                                                       start=(kc == 0), stop=(kc == 7)),
                                     reads=[wzB, hbB], writes=[ppB])
                            k.op('scalar', lambda e, pp=pp, c=c: e.activation(zs[:, c, :], pp[0:64, 0:128], AF.Silu), reads=[ppB], writes=[zsB])
                            k.op('vector', lambda e, pp=pp, c=c, h=h: e.tensor_copy(gl[:, 0, c:c + 1], pp[0:64, 128 + h:129 + h]), reads=[ppB], writes=[glB])
                            k.op('vector', lambda e, pp=pp, c=c, h=h: e.tensor_copy(gl[:, 1, c:c + 1], pp[0:64, 136 + h:137 + h]), reads=[ppB], writes=[glB])
                    if ksub == 1:
                        k.barrier()
                        return
                    k.op('scalar', lambda e: e.activation(gl[:, 0, :], gl[:, 0, :], AF.Sigmoid), reads=[glB], writes=[glB])
                    k.op('vector', lambda e: e.tensor_scalar_mul(gl[:, 5, :], gl[:, 0, :], -1.0), reads=[glB], writes=[glB])
                    k.op('scalar', lambda e, h=h: e.activation(gl[:, 1, :], gl[:, 1, :], AF.Exp, bias=dtb[0:64, h:h + 1]), reads=[glB, dtB], writes=[glB])
                    k.op('scalar', lambda e: e.activation(gl[:, 1, :], gl[:, 1, :], AF.Ln, bias=1.0), reads=[glB], writes=[glB])
                    k.op('vector', lambda e, h=h: e.tensor_scalar_mul(gl[:, 1, :], gl[:, 1, :], nea[0:64, h:h + 1]), reads=[glB, alB], writes=[glB])
                    for c0 in range(0, NCH, 512):
                        n = min(512, NCH - c0)
                        k.op('tensor', lambda e, c0=c0, n=n: e.matmul(ps[0][0:64, 0:n], triu[0:64, 0:64], gl[:, 1, c0:c0 + n], start=True, stop=True),
                             reads=[glB, cB], writes=[psB[0]])
                        k.op('vector', lambda e, c0=c0, n=n: e.tensor_copy(gl[:, 2, c0:c0 + n], ps[0][0:64, 0:n]), reads=[psB[0]], writes=[glB])
                        k.op('tensor', lambda e, c0=c0, n=n: e.matmul(ps[1][:, 0:n], ones_f[0:64, :], gl[:, 1, c0:c0 + n], start=True, stop=True),
                             reads=[glB, cB], writes=[psB[1]])
                        k.op('scalar', lambda e, c0=c0, n=n: e.activation(egl[:, c0:c0 + n], ps[1][:, 0:n], AF.Exp), reads=[psB[1]], writes=[eglB])
                        k.op('vector', lambda e, c0=c0, n=n: e.tensor_copy(gl[:, 6, c0:c0 + n], ps[1][0:64, 0:n]), reads=[psB[1]], writes=[glB])
                    k.op('scalar', lambda e: e.activation(gl[:, 3, :], gl[:, 2, :], AF.Exp), reads=[glB], writes=[glB])
                    k.op('vector', lambda e: e.tensor_tensor(gl[:, 7, :], gl[:, 6, :], gl[:, 2, :], ALU.subtract), reads=[glB], writes=[glB])
                    k.op('scalar', lambda e: e.activation(gl[:, 4, :], gl[:, 7, :], AF.Exp), reads=[glB], writes=[glB])
                    k.op('vector', lambda e: e.memset(S[:], 0.0), writes=[SB])
                    if h == 0 and l == 0:
                        dump("gl", gl[:], glB)
                        dump("egl", egl[:], eglB)
                        dump("qT", qT[:, 0:128], qkvB[0])
                        dump("kT", kT[:, 0:128], qkvB[1])
                        dump("vT", vT[:, 0:128], qkvB[2])
                        dump("zs", zs[:, 0:2, :], zsB)
                    if ksub == 2:
                        k.barrier()
                        return

                    def bulk(gi, res):
                        c0 = gi * G
                        gcols = slice(c0 * 64, (c0 + G) * 64)
                        csl = [slice((c0 + g) * 64, (c0 + g + 1) * 64) for g in range(G)]
                        kg, kgB = r_kg.next()
                        kd, kdB = r_kdec.next()
                        for g in range(G):
                            k.op('tensor', lambda e, g=g: e.transpose(pb[0:64, g * 256:g * 256 + 128], kT[:, csl[g]], identb[:]), reads=[qkvB[1], cB], writes=[pbB])
                            k.op('tensor', lambda e, g=g: e.transpose(pb[0:64, g * 256 + 128:g * 256 + 256], vT[:, csl[g]], identb[:]), reads=[qkvB[2], cB], writes=[pbB])
                        yield
                        for g in range(G):
                            c = c0 + g
                            k.op('vector', lambda e, g=g, c=c: e.tensor_scalar_mul(kg[:, g, 128:256], pb[0:64, g * 256:g * 256 + 128], gl[:, 3, c:c + 1]), reads=[pbB, glB], writes=[kgB])
                            k.op('vector', lambda e, g=g, c=c: e.tensor_scalar_mul(kd[:, g, :], pb[0:64, g * 256:g * 256 + 128], gl[:, 4, c:c + 1]), reads=[pbB, glB], writes=[kdB])
                        k.op('scalar', lambda e: e.copy(kg[:, :, 0:128], pb[0:64, :].rearrange("p (g c) -> p g c", g=G)[:, :, 128:256]), reads=[pbB], writes=[kgB])
                        yield
                        p0, p0B = ps[0], psB[0]
                        for g in range(G):
                            k.op('tensor', lambda e, g=g: e.matmul(p0[0:64, g * 128:g * 128 + 64], kT[:, csl[g]], kT[:, csl[g]], start=True, stop=True), reads=[qkvB[1]], writes=[p0B])
                            k.op('tensor', lambda e, g=g: e.matmul(p0[0:64, g * 128 + 64:g * 128 + 128], kT[:, csl[g]], qT[:, csl[g]], start=True, stop=True), reads=[qkvB[1], qkvB[0]], writes=[p0B])
                        p0v = p0[0:64, :].rearrange("p (g c) -> p g c", g=G)
                        yield
                        dg, dgB = r_dg.next()
                        for g in range(G):
                            c = c0 + g
                            k.op('gpsimd', lambda e, g=g, c=c: e.tensor_scalar_mul(dg[:, g, :], ident[0:64, 0:64], gl[:, 2, c:c + 1]), reads=[glB, cB], writes=[dgB])
                        p1, p1B = ps[1], psB[1]
                        for g in range(G):
                            k.op('tensor', lambda e, g=g: e.matmul(p1[:, g * 64:(g + 1) * 64], ones_f[0:64, :], dg[:, g, :], start=True, stop=True), reads=[dgB, cB], writes=[p1B])
                        yield
                        dm, dmB = r_dm.next()
                        for g in range(G):
                            c = c0 + g
                            k.op('vector', lambda e, g=g, c=c: e.tensor_scalar(dm[:, g, :], p1[0:64, g * 64:(g + 1) * 64], gl[:, 2, c:c + 1], 0.0, ALU.subtract, ALU.min), reads=[p1B, glB], writes=[dmB])
                        yield
                        egb, egbB = r_egb.next()
                        k.op('scalar', lambda e: e.activation(egb[:].rearrange("p g c -> p (g c)"), p1[:, 0:G * 64], AF.Exp), reads=[p1B], writes=[egbB])
                        k.op('scalar', lambda e: e.activation(dm[:], dm[:], AF.Exp), reads=[dmB], writes=[dmB])
                        k.op('vector', lambda e: e.tensor_tensor(dm[:], dm[:], triu4[:], ALU.mult), reads=[dmB, cB], writes=[dmB])
                        qg, qgB = r_qg.next()
                        k.op('gpsimd', lambda e: e.tensor_tensor(qg[:].rearrange("p g c -> p (g c)"), qT[:, gcols], egb[:].rearrange("p g c -> p (g c)"), ALU.mult), reads=[qkvB[0], egbB], writes=[qgB])
                        yield
                        at, atB = r_at.next()
                        k.op('vector', lambda e: e.tensor_tensor(at[:], p0v[:, :, 64:128], dm[:], ALU.mult), reads=[p0B, dmB], writes=[atB])
                        k.op('vector', lambda e: e.tensor_tensor(dm[:], dm[:], ident4[:], ALU.subtract), reads=[dmB, cB], writes=[dmB])
                        X, XB = r_X.next()
                        for g in range(G):
                            c = c0 + g
                            k.op('vector', lambda e, X=X, g=g, c=c: e.scalar_tensor_tensor(X[:, g, :], p0[0:64, g * 128:g * 128 + 64], gl[:, 5, c:c + 1], dm[:, g, :], ALU.mult, ALU.mult),
                                 reads=[p0B, glB, dmB], writes=[XB])
                        yield
                        p2, p2B = ps[2], psB[2]
                        p3, p3B = ps[3], psB[3]
                        p4, p4B = ps[4], psB[4]
                        for g in range(G):
                            k.op('tensor', lambda e, X=X, g=g: e.transpose(pb[0:64, g * 64:(g + 1) * 64], X[:, g, :], identb[0:64, 0:64]), reads=[XB, cB], writes=[pbB])
                        yield
                        Y, YB = r_Y.next()
                        k.op('scalar', lambda e, Y=Y: e.copy(Y[:].rearrange("p g c -> p (g c)"), pb[0:64, 0:G * 64]), reads=[pbB], writes=[YB])
                        P, PB = r_P.next()
                        k.op('vector', lambda e, X=X: e.tensor_tensor(P[:], X[:], ident4[:], ALU.add), reads=[XB, cB], writes=[PB])
                        Pb, PbB = r_Pb.next()
                        k.op('gpsimd', lambda e, Pb=Pb: e.tensor_copy(Pb[:], P[:]), reads=[PB], writes=[PbB])
                        for s in range(1, 6):
                            if s < 5:
                                for g in range(G):
                                    k.op('tensor', lambda e, X=X, Y=Y, g=g: e.matmul(p2[0:64, g * 64:(g + 1) * 64], Y[:, g, :], X[:, g, :], start=True, stop=True), reads=[XB, YB], writes=[p2B])
                            for g in range(G):
                                k.op('tensor', lambda e, X=X, Y=Y, g=g: e.matmul(p3[0:64, g * 64:(g + 1) * 64], X[:, g, :], Y[:, g, :], start=True, stop=True), reads=[XB, YB], writes=[p3B])
                            yield
                            Yn, YnB = r_Y.next()
                            k.op('scalar', lambda e, Yn=Yn: e.copy(Yn[:].rearrange("p g c -> p (g c)"), p3[0:64, 0:G * 64]), reads=[p3B], writes=[YnB])
                            if s < 5:
                                Xn, XnB = r_X.next()
                                k.op('vector', lambda e, Xn=Xn: e.tensor_copy(Xn[:].rearrange("p g c -> p (g c)"), p2[0:64, 0:G * 64]), reads=[p2B], writes=[XnB])
                            yield
                            for g in range(G):
                                k.op('tensor', lambda e, Yn=Yn, Pb=Pb, g=g: e.matmul(p4[0:64, g * 64:(g + 1) * 64], Yn[:, g, :], Pb[:, g, :], start=True, stop=True), reads=[YnB, PbB], writes=[p4B])
                            k.op('vector', lambda e: e.tensor_tensor(P[:].rearrange("p g c -> p (g c)"), P[:].rearrange("p g c -> p (g c)"), p4[0:64, 0:G * 64], ALU.add), reads=[p4B, PB], writes=[PB])
                            if s < 5:
                                Pb, PbB = r_Pb.next()
                                k.op('gpsimd', lambda e, Pb=Pb: e.tensor_copy(Pb[:], P[:]), reads=[PB], writes=[PbB])
                            yield
                            Y, YB = Yn, YnB
                            if s < 5:
                                X, XB = Xn, XnB
                        yield
                        uw, uwB = r_uw.next()
                        for rr in range(G // 2):
                            for g2 in range(2):
                                g = rr * 2 + g2
                                k.op('tensor', lambda e, g=g, g2=g2: e.matmul(p4[0:64, g2 * 256:(g2 + 1) * 256], P[:, g, :], kg[:, g, :], start=True, stop=True), reads=[PB, kgB], writes=[p4B])
                            for g2 in range(2):
                                g = rr * 2 + g2
                                c = c0 + g
                                k.op('vector', lambda e, g=g, g2=g2, c=c: e.tensor_scalar_mul(uw[:, g, :], p4[0:64, g2 * 256:(g2 + 1) * 256], gl[:, 0, c:c + 1]), reads=[p4B, glB], writes=[uwB])
                        yield
                        AT, ATB = r_AT.next()
                        Bc, BcB = r_Bc.next()
                        QpT, QpTB = r_QpT.next()
                        O0, O0B = r_O0.next()
                        for g in range(G):
                            k.op('tensor', lambda e, g=g: e.matmul(p2[:, g * 128:(g + 1) * 128], uw[:, g, 128:256], kd[:, g, :], start=True, stop=True), reads=[uwB, kdB], writes=[p2B])
                        for g in range(G):
                            k.op('tensor', lambda e, g=g: e.matmul(p3[:, g * 128:(g + 1) * 128], kd[:, g, :], uw[:, g, 0:128], start=True, stop=True), reads=[uwB, kdB], writes=[p3B])
                        yield
                        for g in range(G):
                            c = c0 + g
                            k.op('vector', lambda e, g=g, c=c: e.scalar_tensor_tensor(AT[:, g, :], ident[:], egl[:, c:c + 1], p2[:, g * 128:(g + 1) * 128], ALU.mult, ALU.subtract),
                                 reads=[p2B, eglB, cB], writes=[ATB])
                        k.op('scalar', lambda e: e.copy(Bc[:].rearrange("p g c -> p (g c)"), p3[:, 0:G * 128]), reads=[p3B], writes=[BcB])
                        yield
                        for g in range(G):
                            k.op('tensor', lambda e, g=g: e.matmul(p0[:, g * 64:(g + 1) * 64], uw[:, g, 128:256], at[:, g, :], start=True, stop=True), reads=[uwB, atB], writes=[p0B])
                        for g in range(G):
                            k.op('tensor', lambda e, g=g: e.matmul(p4[0:64, g * 128:(g + 1) * 128], at[:, g, :], uw[:, g, 0:128], start=True, stop=True), reads=[uwB, atB], writes=[p4B])
                        yield
                        k.op('vector', lambda e: e.tensor_tensor(QpT[:].rearrange("p g c -> p (g c)"), qg[:].rearrange("p g c -> p (g c)"), p0[:, 0:G * 64], ALU.subtract),
                             reads=[p0B, qgB], writes=[QpTB])
                        k.op('scalar', lambda e: e.copy(O0[:].rearrange("p g c -> p (g c)"), p4[0:64, 0:G * 128]), reads=[p4B], writes=[O0B])
                        res.update(dict(AT=(AT, ATB), Bc=(Bc, BcB), QpT=(QpT, QpTB), O0=(O0, O0B)))

                    def scan(c, r, g):
                        AT_, ATB = r['AT']
                        Bc_, BcB = r['Bc']
                        QpT_, QpTB = r['QpT']
                        O0_, O0B = r['O0']
                        p5, p5B = ps[5], psB[5]
                        p6, p6B = ps[6], psB[6]
                        k.op('tensor', lambda e: e.matmul(p5[:, 0:128], AT_[:, g, :], S[:], start=True, stop=True), reads=[ATB, SB], writes=[p5B])
                        k.op('tensor', lambda e: e.matmul(p6[0:64, 0:128], QpT_[:, g, :], S[:], start=True, stop=True), reads=[QpTB, SB], writes=[p6B])
                        yield
                        k.op('vector', lambda e: e.tensor_tensor(S[:], p5[:, 0:128], Bc_[:, g, :], ALU.add), reads=[p5B, BcB, SB], writes=[SB])
                        yield
                        o, oB = r_o.next()
                        st, stB = r_st.next()
                        jk, jkB = r_jk.next()
                        k.op('vector', lambda e: e.tensor_tensor(o[:], p6[0:64, 0:128], O0_[:, g, :], ALU.add), reads=[p6B, O0B], writes=[oB])
                        k.op('scalar', lambda e: e.activation(jk[:], o[:], AF.Square, accum_out=st[:, 0:1]), reads=[oB], writes=[jkB, stB])
                        k.op('scalar', lambda e: e.activation(st[:, 1:2], st[:, 0:1], AF.Sqrt, bias=RMS_EPS, scale=1.0 / 128), reads=[stB], writes=[stB])
                        yield
                        k.op('vector', lambda e: e.reciprocal(st[:, 2:3], st[:, 1:2]), reads=[stB], writes=[stB])
                        k.op('vector', lambda e: e.scalar_tensor_tensor(o[:], o[:], st[:, 2:3], nw[0:64, :], ALU.mult, ALU.mult), reads=[oB, stB, nwB], writes=[oB])
                        ob, obB = r_ob.next()
                        k.op('gpsimd', lambda e: e.tensor_tensor(ob[:], o[:], zs[:, c, :], ALU.mult), reads=[oB, zsB], writes=[obB])
                        k.dma('sync', om_d[c * 64:(c + 1) * 64, h * 128:(h + 1) * 128], ob[:], reads=[obB])

                    assert NCH % G == 0
                    pend = {}
                    for _ in bulk(0, pend):
                        pass
                    for gi in range(NCH // G):
                        nxt = {}
                        bg = bulk(gi + 1, nxt) if gi + 1 < NCH // G else None
                        for g in range(G):
                            for _ in scan(gi * G + g, pend, g):
                                for _r in range(2):
                                    if bg is not None and next(bg, _DONE) is _DONE:
                                        bg = None
                        if bg is not None:
                            for _ in bg:
                                pass
                        pend = nxt
                k.barrier()

        def shared_kv():
            with ExitStack() as es:
                hbr = Ring(es, nc, "khb", [128, 8, 512], BF16, 2)
                kr = Ring(es, nc, "kst", [64, 512], BF16, 3)
                vr = Ring(es, nc, "vst", [128, 129], BF16, 3)
                for h in range(NH):
                    wk, wkB = load_w_bf16(es, f"wk{h}", kv_w[:, h * 128:(h + 1) * 128], 128) if h == 0 else (wk_keep, wkB_keep)
                    wv, wvB = load_w_bf16(es, f"wv{h}", kv_w[:, 1024 + h * 128:1024 + (h + 1) * 128], 128) if h == 0 else (wv_keep, wvB_keep)
                    if h == 0:
                        wk_keep, wkB_keep, wv_keep, wvB_keep = wk, wkB, wv, wvB
                    else:
                        k.dma('gpsimd', wk[:], kv_w[:, h * 128:(h + 1) * 128].rearrange("(k p) c -> p k c", p=128), writes=[wkB])
                        k.dma('gpsimd', wv[:], kv_w[:, 1024 + h * 128:1024 + (h + 1) * 128].rearrange("(k p) c -> p k c", p=128), writes=[wvB])
                    for blk in range(NB):
                        hb, hbB = hbr.next()
                        k.dma('sync', hb[:], hT_v[:, :, blk * 512:(blk + 1) * 512], writes=[hbB])
                        for s in range(2):
                            pp, ppB = ps[s], psB[s]
                            for kc in range(8):
                                k.op('tensor', lambda e, pp=pp, kc=kc, s=s, hb=hb: e.matmul(pp[0:64, :], wk[:, kc, s * 64:(s + 1) * 64], hb[:, kc, :],
                                                                                     start=(kc == 0), stop=(kc == 7)), reads=[wkB, hbB], writes=[ppB])
                            kt, ktB = kr.next()
                            k.op('scalar' if s else 'vector', (lambda e, kt=kt, pp=pp: e.copy(kt[:], pp[0:64, :])) if s else
                                 (lambda e, kt=kt, pp=pp: e.tensor_copy(kt[:], pp[0:64, :])), reads=[ppB], writes=[ktB])
                            k.dma('sync', kT_d[h, s, :, blk * 512:(blk + 1) * 512], kt[:], reads=[ktB])
                        for tt in range(4):
                            pp, ppB = ps[2 + tt % 2], psB[2 + tt % 2]
                            for kc in range(8):
                                k.op('tensor', lambda e, pp=pp, kc=kc, tt=tt, hb=hb: e.matmul(pp[:, 0:128], hb[:, kc, tt * 128:(tt + 1) * 128], wv[:, kc, :],
                                                                                       start=(kc == 0), stop=(kc == 7)), reads=[wvB, hbB], writes=[ppB])
                            vt, vtB = vr.next()
                            k.op('vector', lambda e, vt=vt, pp=pp: e.tensor_copy(vt[:, 0:128], pp[:, 0:128]), reads=[ppB], writes=[vtB])
                            k.op('gpsimd', lambda e, vt=vt: e.memset(vt[:, 128:129], 1.0), writes=[vtB])
                            tok0 = blk * 512 + tt * 128
                            k.dma('sync', va_d[h, tok0:tok0 + 128, :], vt[:], reads=[vtB])
                k.barrier()

        def diffattn(j, layer):
            lam_init = lambda_init(layer)
            with ExitStack() as es:
                lp, lpB = bcast_row(es, "lp", b_lambda[j:j + 1].rearrange("o a b -> o (a b)"), 256)
                sw, swB = bcast_row(es, "sw", b_subln_w[j:j + 1, :], 128)
                lam = sbt(es, "lam", [128, 8], F32)
                lamB = Buf("lam")
                pr = sbt(es, "lpr", [128, 128], F32)
                k.op('vector', lambda e: e.tensor_tensor(pr[:, 0:64], lp[:, 0:64], lp[:, 64:128], ALU.mult), reads=[lpB], writes=[lamB])
                k.op('vector', lambda e: e.tensor_tensor(pr[:, 64:128], lp[:, 128:192], lp[:, 192:256], ALU.mult), reads=[lpB], writes=[lamB])
                k.op('vector', lambda e: e.reduce_sum(lam[:, 0:1], pr[:, 0:64], AX.X), reads=[lamB], writes=[lamB])
                k.op('vector', lambda e: e.reduce_sum(lam[:, 1:2], pr[:, 64:128], AX.X), reads=[lamB], writes=[lamB])
                k.op('scalar', lambda e: e.activation(lam[:, 2:4], lam[:, 0:2], AF.Exp), reads=[lamB], writes=[lamB])
                k.op('vector', lambda e: e.tensor_tensor(lam[:, 4:5], lam[:, 2:3], lam[:, 3:4], ALU.subtract), reads=[lamB], writes=[lamB])
                k.op('vector', lambda e: e.tensor_scalar(lam[:, 5:6], lam[:, 4:5], lam_init, -1.0, ALU.add, ALU.mult), reads=[lamB], writes=[lamB])
                k.op('vector', lambda e: e.tensor_scalar_mul(sw[:], sw[:], 1.0 - lam_init), reads=[swB], writes=[swB])
                qs = [sbt(es, f"qs{s}", [64, T], BF16) for s in range(2)]
                qsB = [Buf("qs0"), Buf("qs1")]
                kTs = [sbt(es, f"kTs{s}", [64, T], BF16) for s in range(2)]
                kTB = [Buf("kT0"), Buf("kT1")]
                va = sbt(es, "va", [128, NT, 144], BF16)
                vaB = Buf("va")
                hbr = Ring(es, nc, "ahb", [128, 8, 512], BF16, 2)
                ptr = Ring(es, nc, "pt", [128, 512], BF16, 6)
                r_o1 = Ring(es, nc, "ao1", [128, 128], F32, 2)
                r_st = Ring(es, nc, "ast", [128, 8], F32, 2)
                r_jk = Ring(es, nc, "ajk", [128, 128], F32, 2)
                r_ob = Ring(es, nc, "aob", [128, 128], BF16, 2)
                wq = sbt(es, "wq", [128, 8, 128], BF16)
                wqB = Buf("wq")
                acc = {}
                slots = [(4, 0), (4, 144), (4, 288), (5, 0), (5, 144), (5, 288), (6, 0), (6, 144)]
                for s in range(2):
                    for i in range(4):
                        acc[(s, i)] = slots[s * 4 + i]
                for h in range(NH):
                    k.dma('gpsimd', wq[:], b_w_q[j, :, h * 128:(h + 1) * 128].rearrange("(k p) c -> p k c", p=128), writes=[wqB])
                    for s in range(2):
                        k.dma('sync', kTs[s][:], kT_d[h, s], writes=[kTB[s]])
                    k.dma('sync', va[:, :, 0:129], va_d[h].rearrange("(t p) c -> p t c", p=128), writes=[vaB])
                    for blk in range(NB):
                        hb, hbB = hbr.next()
                        k.dma('sync', hb[:], hT_v[:, :, blk * 512:(blk + 1) * 512], writes=[hbB])
                        for s in range(2):
                            pp, ppB = ps[s], psB[s]
                            for kc in range(8):
                                k.op('tensor', lambda e, pp=pp, kc=kc, s=s, hb=hb: e.matmul(pp[0:64, :], wq[:, kc, s * 64:(s + 1) * 64], hb[:, kc, :],
                                                                                     start=(kc == 0), stop=(kc == 7)), reads=[wqB, hbB], writes=[ppB])
                            k.op('scalar', lambda e, pp=pp, s=s, blk=blk: e.activation(qs[s][:, blk * 512:(blk + 1) * 512], pp[0:64, :], AF.Copy, scale=0.125),
                                 reads=[ppB], writes=[qsB[s]])
                    for qb in range(NB):
                        nkt = 4 * qb + 4
                        steps = [(kt, s) for kt in range(nkt) for s in range(2)]
                        LA = 2

                        def front(idx, qb=qb):
                            kt, s = steps[idx]
                            jd = kt - 4 * qb
                            lo = 0 if jd < 0 else jd * 128
                            n = 512 - lo
                            pp, ppB = ps[idx % 4], psB[idx % 4]
                            k.op('tensor', lambda e, pp=pp, s=s, kt=kt, qb=qb, lo=lo, n=n: e.matmul(pp[:, 0:n], kTs[s][:, kt * 128:(kt + 1) * 128],
                                                                                             qs[s][:, qb * 512 + lo:(qb + 1) * 512], start=True, stop=True),
                                 reads=[kTB[s], qsB[s]], writes=[ppB])
                            pt, ptB = ptr.next()
                            k.op('scalar', lambda e, pt=pt, pp=pp, n=n: e.activation(pt[:, 0:n], pp[:, 0:n], AF.Exp), reads=[ppB], writes=[ptB])
                            if jd >= 0:
                                k.op('vector', lambda e, pt=pt: e.tensor_tensor(pt[:, 0:128], pt[:, 0:128], triub[:], ALU.mult), reads=[ptB, cB], writes=[ptB])
                            return pt, ptB, jd, lo

                        def back(idx, fr, qb=qb):
                            kt, s = steps[idx]
                            pt, ptB, jd, lo = fr
                            for i in range(max(jd, 0), 4):
                                bank, off = acc[(s, i)]
                                c0 = i * 128 - lo
                                st_flag = (kt == 0 and off == 0)
                                k.op('tensor', lambda e, pt=pt, bank=bank, off=off, c0=c0, kt=kt, i=i, qb=qb, st_flag=st_flag: e.matmul(
                                    ps[bank][:, off:off + 129], pt[:, c0:c0 + 128], va[:, kt, 0:129], start=st_flag, stop=(kt == 4 * qb + i),
                                    skip_group_check=True),
                                    reads=[ptB, vaB], writes=[psB[bank]])

                        fronts = {}
                        for idx in range(len(steps) + LA):
                            if idx < len(steps):
                                fronts[idx] = front(idx)
                            if idx - LA >= 0:
                                back(idx - LA, fronts.pop(idx - LA))
                        for i in range(4):
                            b1, f1 = acc[(0, i)]
                            b2, f2 = acc[(1, i)]
                            st, stB = r_st.next()
                            o1, o1B = r_o1.next()
                            jk, jkB = r_jk.next()
                            ob, obB = r_ob.next()
                            k.op('vector', lambda e, st=st, b1=b1, f1=f1: e.reciprocal(st[:, 0:1], ps[b1][:, f1 + 128:f1 + 129]), reads=[psB[b1]], writes=[stB])
                            k.op('vector', lambda e, st=st, b2=b2, f2=f2: e.reciprocal(st[:, 1:2], ps[b2][:, f2 + 128:f2 + 129]), reads=[psB[b2]], writes=[stB])
                            k.op('vector', lambda e, st=st: e.tensor_tensor(st[:, 2:3], st[:, 1:2], lam[:, 5:6], ALU.mult), reads=[stB, lamB], writes=[stB])
                            k.op('vector', lambda e, st=st, o1=o1, b1=b1, f1=f1: e.tensor_scalar_mul(o1[:], ps[b1][:, f1:f1 + 128], st[:, 0:1]),
                                 reads=[psB[b1], stB], writes=[o1B])
                            k.op('vector', lambda e, st=st, o1=o1, b2=b2, f2=f2: e.scalar_tensor_tensor(o1[:], ps[b2][:, f2:f2 + 128], st[:, 2:3], o1[:], ALU.mult, ALU.add),
                                 reads=[psB[b2], stB, o1B], writes=[o1B])
                            k.op('scalar', lambda e, st=st, o1=o1, jk=jk: e.activation(jk[:], o1[:], AF.Square, accum_out=st[:, 3:4]), reads=[o1B], writes=[jkB, stB])
                            k.op('scalar', lambda e, st=st: e.activation(st[:, 4:5], st[:, 3:4], AF.Sqrt, bias=RMS_EPS, scale=1.0 / 128), reads=[stB], writes=[stB])
                            k.op('vector', lambda e, st=st: e.reciprocal(st[:, 5:6], st[:, 4:5]), reads=[stB], writes=[stB])
                            k.op('vector', lambda e, st=st, o1=o1, ob=ob: e.scalar_tensor_tensor(ob[:], o1[:], st[:, 5:6], sw[:], ALU.mult, ALU.mult),
                                 reads=[o1B, stB, swB], writes=[obB])
                            t0 = qb * 512 + i * 128
                            k.dma('sync', om_d[t0:t0 + 128, h * 128:(h + 1) * 128], ob[:], reads=[obB])
                k.barrier()

        def tok_moe(layer, w_out_src, last):
            with ExitStack() as es:
                wo, woB = load_w_bf16(es, "wo", w_out_src, 1024)
                wr = sbt(es, "wr", [128, 8, 40], F32)
                wrB = Buf("wr")
                k.dma('sync', wr[:, :, 0:4], moe_w_group[layer].rearrange("(k p) c -> p k c", p=128), writes=[wrB])
                k.dma('sync', wr[:, :, 4:36], moe_w_expert[layer].rearrange("(k p) c -> p k c", p=128), writes=[wrB])
                g1, g1B = bcast_row(es, "lng1", ln_mix_g[layer:layer + 1, :], D)
                b1, b1B = bcast_row(es, "lnb1", ln_mix_b[layer:layer + 1, :], D)
                lnB1 = Buf("ln1")
                oh = [sbt(es, f"oh{i}", [128, NT, 32], F32) for i in range(2)]
                ohB = Buf("oh")
                gates = sbt(es, "gates", [128, NT, 2], F32)
                gB = Buf("gates")
                rings = {'hTs': Ring(es, nc, "hTs2", [128, 8, 128], BF16, 2), 'st': Ring(es, nc, "lst", [128, 8], F32, 2),
                         'junk': Ring(es, nc, "ljk", [128, D], F32, 1)}
                with ExitStack() as e1:
                    r_om = Ring(e1, nc, "om", [128, D], BF16, 2)
                    r_omT = Ring(e1, nc, "omT", [128, 8, 128], BF16, 2)
                    r_h = Ring(e1, nc, "hh", [128, D], F32, 2)
                    r_t = Ring(e1, nc, "tt", [128, D], F32, 2)
                    r_y = Ring(e1, nc, "yy", [128, D], F32, 2)
                    r_xb = Ring(e1, nc, "xb", [128, D], BF16, 2)
                    r_xT = Ring(e1, nc, "xT", [128, 8, 128], F32, 2)
                    r_rt = Ring(e1, nc, "rt", [128, 160], F32, 2)
                    def tok_body(t):
                        rows = slice(t * 128, (t + 1) * 128)
                        om, omB = r_om.next()
                        k.dma('sync', om[:], om_d[rows, :], writes=[omB])
                        hh, hhB = r_h.next()
                        k.dma('sync', hh[:], h_d[rows, :], writes=[hhB])
                        for kc in range(8):
                            k.op('tensor', lambda e, om=om, kc=kc: e.transpose(pb[:, kc * 128:(kc + 1) * 128], om[:, kc * 128:(kc + 1) * 128], identb[:]),
                                 reads=[omB, cB], writes=[pbB])
                        omT, omTB = r_omT.next()
                        k.op('vector', lambda e, omT=omT: e.tensor_copy(omT[:], pb[:].rearrange("p (a b) -> p a b", a=8)), reads=[pbB], writes=[omTB])
                        yield
                        tt, ttB = r_t.next()
                        for half in range(2):
                            pp, ppB = ps[half], psB[half]
                            for kc in range(8):
                                k.op('tensor', lambda e, pp=pp, kc=kc, half=half, omT=omT: e.matmul(pp[:, :], omT[:, kc, :], wo[:, kc, half * 512:(half + 1) * 512],
                                                                                             start=(kc == 0), stop=(kc == 7)), reads=[omTB, woB], writes=[ppB])
                            k.op('vector', lambda e, pp=pp, half=half, tt=tt, hh=hh: e.scalar_tensor_tensor(tt[:, half * 512:(half + 1) * 512], hh[:, half * 512:(half + 1) * 512],
                                                                                                      ALPHA, pp[:, :], ALU.mult, ALU.add),
                                 reads=[ppB, hhB], writes=[ttB])
                        yield
                        yy, yyB = r_y.next()
                        layer_norm(rings, tt, ttB, g1, b1, [g1B, b1B], yy, yyB)
                        yield
                        k.dma('sync', h_d[rows, :], yy[:], reads=[yyB, hhB])
                        xb, xbB = r_xb.next()
                        k.op('scalar', lambda e, xb=xb, yy=yy: e.copy(xb[:], yy[:]), reads=[yyB], writes=[xbB])
                        k.dma('sync', xb_d[rows, :], xb[:], reads=[xbB])
                        yield
                        xT, xTB = r_xT.next()
                        for half in range(2):
                            pp, ppB = ps[2 + half], psB[2 + half]
                            for jj in range(4):
                                kc = half * 4 + jj
                                k.op('tensor', lambda e, pp=pp, jj=jj, kc=kc, yy=yy: e.transpose(pp[:, jj * 128:(jj + 1) * 128], yy[:, kc * 128:(kc + 1) * 128], ident[:]),
                                     reads=[yyB, cB], writes=[ppB])
                            if half == 0:
                                k.op('vector', lambda e, pp=pp, xT=xT: e.tensor_copy(xT[:, 0:4, :], pp[:].rearrange("p (a b) -> p a b", a=4)), reads=[ppB], writes=[xTB])
                            else:
                                k.op('scalar', lambda e, pp=pp, xT=xT: e.copy(xT[:, 4:8, :], pp[:].rearrange("p (a b) -> p a b", a=4)), reads=[ppB], writes=[xTB])
                        yield
                        p4, p4B = ps[4], psB[4]
                        for kc in range(8):
                            k.op('tensor', lambda e, kc=kc, xT=xT: e.matmul(p4[:, 0:36], xT[:, kc, :], wr[:, kc, 0:36], start=(kc == 0), stop=(kc == 7)),
                                 reads=[xTB, wrB], writes=[p4B])
                        rt, rtB = r_rt.next()
                        V = lambda fn, rt=rt, rtB=rtB, extra_r=(), extra_w=(): k.op('vector', fn, reads=[rtB] + list(extra_r), writes=[rtB] + list(extra_w))
                        k.op('vector', lambda e, rt=rt: e.tensor_copy(rt[:, 0:36], p4[:, 0:36]), reads=[p4B], writes=[rtB])
                        V(lambda e, rt=rt: e.reduce_max(rt[:, 36:37], rt[:, 0:4], AX.X))
                        V(lambda e, rt=rt: e.tensor_scalar_mul(rt[:, 37:38], rt[:, 36:37], -1.0))
                        k.op('scalar', lambda e, rt=rt: e.activation(rt[:, 84:88], rt[:, 0:4], AF.Exp, bias=rt[:, 37:38], accum_out=rt[:, 38:39]), reads=[rtB], writes=[rtB])
                        V(lambda e, rt=rt: e.reciprocal(rt[:, 39:40], rt[:, 38:39]))
                        V(lambda e, rt=rt: e.tensor_scalar(rt[:, 40:44], rt[:, 0:4], rt[:, 36:37], None, ALU.is_ge))
                        V(lambda e, rt=rt: e.tensor_scalar(rt[:, 40:44], rt[:, 40:44], -NEG, NEG, ALU.mult, ALU.add))
                        for g in range(4):
                            V(lambda e, rt=rt, g=g: e.tensor_scalar(rt[:, 44 + g * 8:52 + g * 8], rt[:, 4 + g * 8:12 + g * 8], rt[:, 40 + g:41 + g], None, ALU.add))
                        yield
                        V(lambda e, rt=rt: e.reduce_max(rt[:, 76:77], rt[:, 44:76], AX.X))
                        V(lambda e, rt=rt, t=t: e.tensor_scalar(oh[0][:, t, :], rt[:, 44:76], rt[:, 76:77], None, ALU.is_ge), extra_w=[ohB])
                        V(lambda e, rt=rt, t=t: e.scalar_tensor_tensor(rt[:, 96:128], oh[0][:, t, :], NEG, rt[:, 44:76], ALU.mult, ALU.add), extra_r=[ohB])
                        V(lambda e, rt=rt: e.reduce_max(rt[:, 77:78], rt[:, 96:128], AX.X))
                        V(lambda e, rt=rt, t=t: e.tensor_scalar(oh[1][:, t, :], rt[:, 96:128], rt[:, 77:78], None, ALU.is_ge), extra_w=[ohB])
                        V(lambda e, rt=rt: e.tensor_tensor(rt[:, 78:79], rt[:, 77:78], rt[:, 76:77], ALU.subtract))
                        k.op('scalar', lambda e, rt=rt: e.activation(rt[:, 79:80], rt[:, 78:79], AF.Exp), reads=[rtB], writes=[rtB])
                        V(lambda e, rt=rt: e.tensor_scalar_add(rt[:, 80:81], rt[:, 79:80], 1.0))
                        V(lambda e, rt=rt: e.reciprocal(rt[:, 81:82], rt[:, 80:81]))
                        V(lambda e, rt=rt, t=t: e.tensor_tensor(gates[:, t, 0:1], rt[:, 39:40], rt[:, 81:82], ALU.mult), extra_w=[gB])
                        V(lambda e, rt=rt, t=t: e.tensor_tensor(gates[:, t, 1:2], rt[:, 39:40], gates[:, t, 0:1], ALU.subtract), extra_r=[gB], extra_w=[gB])
                    interleave((tok_body(t) for t in range(NT)), depth=2)
                    k.barrier()
                if checkpoint(noraise=True):
                    return True
                dest = sbt(es, "dest", [128, 2, NT], I32)
                destB = Buf("dest")
                with ExitStack() as e2:
                    selb = sbt(e2, "selb", [128, NT, 32], BF16)
                    cnt = sbt(e2, "cnt", [128, NT, 32], F32)
                    pref = sbt(e2, "pref", [128, NT, 32], F32)
                    slot = sbt(e2, "slot", [128, NT, 32], F32)
                    ebase = sbt(e2, "ebase", [128, 32], F32)
                    tmp = sbt(e2, "ptmp", [128, NT, 32], F32)
                    dfl = sbt(e2, "dfl", [128, 2, NT], F32)
                    pB = Buf("pos")
                    k.op('gpsimd', lambda e: e.iota(ebase[:], [[CAP, 32]], base=0, channel_multiplier=0, allow_small_or_imprecise_dtypes=True), writes=[pB])
                    k.op('vector', lambda e: e.tensor_tensor(selb[:], oh[0][:], oh[1][:], ALU.add), reads=[ohB], writes=[pB])
                    TPB = 16
                    for t0 in range(0, NT, TPB):
                        n = min(TPB, NT - t0)
                        k.op('tensor', lambda e, t0=t0, n=n: e.matmul(ps[0][:, 0:n * 32], ones_b[:], selb[:, t0:t0 + n, :].rearrange("p a b -> p (a b)"), start=True, stop=True),
                             reads=[pB, cB], writes=[psB[0]])
                        k.op('vector', lambda e, t0=t0, n=n: e.tensor_copy(cnt[:, t0:t0 + n, :].rearrange("p a b -> p (a b)"), ps[0][:, 0:n * 32]), reads=[psB[0]], writes=[pB])
                        k.op('tensor', lambda e, t0=t0, n=n: e.matmul(ps[1][:, 0:n * 32], sutri[:], selb[:, t0:t0 + n, :].rearrange("p a b -> p (a b)"), start=True, stop=True),
                             reads=[pB, cB], writes=[psB[1]])
                        k.op('vector', lambda e, t0=t0, n=n: e.tensor_copy(slot[:, t0:t0 + n, :].rearrange("p a b -> p (a b)"), ps[1][:, 0:n * 32]), reads=[psB[1]], writes=[pB])
                    k.op('vector', lambda e: e.memset(pref[:, 0, :], 0.0), reads=[pB], writes=[pB])
                    for t in range(1, NT):
                        k.op('vector', lambda e, t=t: e.tensor_tensor(pref[:, t, :], pref[:, t - 1, :], cnt[:, t - 1, :], ALU.add), reads=[pB], writes=[pB])
                    k.op('vector', lambda e: e.tensor_tensor(slot[:], slot[:], pref[:], ALU.add), reads=[pB], writes=[pB])
                    k.op('vector', lambda e: e.tensor_scalar(tmp[:], slot[:], float(CAP), 1.0e6, ALU.is_ge, ALU.mult), reads=[pB], writes=[pB])
                    k.op('vector', lambda e: e.tensor_tensor(slot[:], slot[:], tmp[:], ALU.add), reads=[pB], writes=[pB])
                    for t in range(NT):
                        k.op('gpsimd', lambda e, t=t: e.tensor_tensor(slot[:, t, :], slot[:, t, :], ebase[:], ALU.add), reads=[pB], writes=[pB])
                    for i in range(2):
                        k.op('vector', lambda e, i=i: e.tensor_tensor(tmp[:], slot[:], oh[i][:], ALU.mult), reads=[pB, ohB], writes=[pB])
                        k.op('vector', lambda e, i=i: e.reduce_sum(dfl[:, i, :], tmp[:], AX.X), reads=[pB], writes=[pB])
                    k.op('vector', lambda e: e.tensor_scalar_min(dfl[:], dfl[:], float(NS)), reads=[pB], writes=[pB])
                    k.op('vector', lambda e: e.tensor_copy(dest[:], dfl[:]), reads=[pB], writes=[destB])
                    r_xb2 = Ring(e2, nc, "xb2", [128, D], BF16, 3)
                    for t in range(NT):
                        xb, xbB = r_xb2.next()
                        k.dma('sync', xb[:], xb_d[t * 128:(t + 1) * 128, :], writes=[xbB])
                        for i in range(2):
                            k.op('gpsimd', lambda e, xb=xb, i=i, t=t: e.indirect_dma_start(
                                out=xs_d, out_offset=bass.IndirectOffsetOnAxis(ap=dest[:, i, t:t + 1], axis=0), in_=xb[:], in_offset=None),
                                reads=[xbB, destB], dma=True)
                    k.barrier()
                with ExitStack() as e3:
                    NG = (CAP + 127) // 128
                    r_w13 = Ring(e3, nc, "w13", [128, 8, 1024], BF16, 2)
                    r_w2 = Ring(e3, nc, "w2", [128, 4, 1024], BF16, 2)
                    r_xs = Ring(e3, nc, "xs", [128, NG, D], BF16, 2)
                    r_xsT = Ring(e3, nc, "xsT", [128, 8, CAP], BF16, 2)
                    r_sg = Ring(e3, nc, "sg", [128, 4, CAP], F32, 1)
                    r_hid = Ring(e3, nc, "hid", [128, 4, CAP], BF16, 2)
                    r_ys = Ring(e3, nc, "ys", [128, D], F32, 2)
                    for ex in range(32):
                        w13, w13B = r_w13.next()
                        k.dma('gpsimd', w13[:], moe_w13[layer, ex].rearrange("(k p) c -> p k c", p=128), writes=[w13B])
                        w2, w2B = r_w2.next()
                        k.dma('gpsimd', w2[:], moe_w2[layer, ex].rearrange("(k p) c -> p k c", p=128), writes=[w2B])
                        xs, xsB = r_xs.next()
                        xsT, xsTB = r_xsT.next()
                        for g in range(NG):
                            r0 = ex * CAP + g * 128
                            nr = min(128, CAP - g * 128)
                            k.dma('sync', xs[0:nr, g, :], xs_d[r0:r0 + nr, :], writes=[xsB])
                        for g in range(NG):
                            nr = min(128, CAP - g * 128)
                            for kc in range(8):
                                k.op('tensor', lambda e, xs=xs, g=g, kc=kc, nr=nr: e.transpose(pb[:, kc * 128:kc * 128 + nr], xs[0:nr, g, kc * 128:(kc + 1) * 128], identb[0:nr, 0:nr]),
                                     reads=[xsB, cB], writes=[pbB])
                            k.op('vector', lambda e, xsT=xsT, g=g, nr=nr: e.tensor_copy(xsT[:, :, g * 128:g * 128 + nr], pb[:].rearrange("p (a b) -> p a b", a=8)[:, :, 0:nr]),
                                 reads=[pbB], writes=[xsTB])
                        sg, sgB = r_sg.next()
                        hid, hidB = r_hid.next()
                        for n0 in range(0, CAP, 512):
                            n = min(512, CAP - n0)
                            for m in range(8):
                                pp, ppB = ps[m % 4], psB[m % 4]
                                for kc in range(8):
                                    k.op('tensor', lambda e, pp=pp, kc=kc, m=m, w13=w13, xsT=xsT, n0=n0, n=n: e.matmul(pp[:, 0:n], w13[:, kc, m * 128:(m + 1) * 128], xsT[:, kc, n0:n0 + n],
                                                                                                           start=(kc == 0), stop=(kc == 7)),
                                         reads=[w13B, xsTB], writes=[ppB])
                                if m < 4:
                                    k.op('scalar', lambda e, pp=pp, m=m, sg=sg, n0=n0, n=n: e.activation(sg[:, m, n0:n0 + n], pp[:, 0:n], AF.Silu), reads=[ppB], writes=[sgB])
                                else:
                                    k.op('vector', lambda e, pp=pp, m=m, sg=sg, hid=hid, n0=n0, n=n: e.tensor_tensor(hid[:, m - 4, n0:n0 + n], sg[:, m - 4, n0:n0 + n], pp[:, 0:n], ALU.mult),
                                         reads=[ppB, sgB], writes=[hidB])
                        for g in range(NG):
                            nr = min(128, CAP - g * 128)
                            ys, ysB = r_ys.next()
                            for half in range(2):
                                pp, ppB = ps[4 + half], psB[4 + half]
                                for f in range(4):
                                    k.op('tensor', lambda e, pp=pp, f=f, half=half, hid=hid, w2=w2, g=g, nr=nr: e.matmul(pp[0:nr, :], hid[:, f, g * 128:g * 128 + nr], w2[:, f, half * 512:(half + 1) * 512],
                                                                                                             start=(f == 0), stop=(f == 3)),
                                         reads=[hidB, w2B], writes=[ppB])
                                if half == 0:
                                    k.op('vector', lambda e, pp=pp, ys=ys, nr=nr: e.tensor_copy(ys[0:nr, 0:512], pp[0:nr, :]), reads=[ppB], writes=[ysB])
                                else:
                                    k.op('scalar', lambda e, pp=pp, ys=ys, nr=nr: e.copy(ys[0:nr, 512:1024], pp[0:nr, :]), reads=[ppB], writes=[ysB])
                            r0 = ex * CAP + g * 128
                            for hf in range(2):
                                k.dma('sync', ys_h[hf][r0:r0 + nr, :], ys[0:nr, hf * 512:(hf + 1) * 512], reads=[ysB])
                    k.barrier()
                with ExitStack() as e4:
                    g2, g2B = bcast_row(e4, "lng2", ln_ffn_g[layer:layer + 1, :], D)
                    b2, b2B = bcast_row(e4, "lnb2", ln_ffn_b[layer:layer + 1, :], D)
                    r_yq = [Ring(e4, nc, f"yq{q}", [128, 512], F32, 2) for q in range(4)]
                    r_h = Ring(e4, nc, "ch", [128, D], F32, 2)
                    r_o = Ring(e4, nc, "co", [128, D], F32, 2)
                    def comb_body(t):
                        rows = slice(t * 128, (t + 1) * 128)
                        hh, hhB = r_h.next()
                        k.dma('sync', hh[:], h_d[rows, :], writes=[hhB])
                        k.op('scalar', lambda e, hh=hh: e.mul(hh[:], hh[:], ALPHA), reads=[hhB], writes=[hhB])
                        for i in range(2):
                            for hf in range(2):
                                yq, yqB = r_yq[i * 2 + hf].next()
                                k.op('gpsimd', lambda e, yq=yq, i=i, t=t, hf=hf: e.indirect_dma_start(
                                    out=yq[:], out_offset=None, in_=ys_h[hf],
                                    in_offset=bass.IndirectOffsetOnAxis(ap=dest[:, i, t:t + 1], axis=0)), reads=[destB], writes=[yqB], dma=True)
                                k.op('vector', lambda e, hh=hh, yq=yq, t=t, i=i, hf=hf: e.scalar_tensor_tensor(
                                    hh[:, hf * 512:(hf + 1) * 512], yq[:], gates[:, t, i:i + 1], hh[:, hf * 512:(hf + 1) * 512], ALU.mult, ALU.add),
                                    reads=[hhB, yqB, gB], writes=[hhB])
                        yield
                        oo, ooB = r_o.next()
                        layer_norm(rings, hh, hhB, g2, b2, [g2B, b2B], oo, ooB)
                        yield
                        if last:
                            out_toks.append(k.dma('sync', out[rows, :], oo[:], reads=[ooB]))
                        else:
                            k.dma('sync', h_d[rows, :], oo[:], reads=[ooB, hhB])
                            make_hT(rings, oo, ooB, t)
                    interleave((comb_body(t) for t in range(NT)), depth=2)
                    k.barrier()

        try:
            checkpoint()
            for layer in range(4):
                if layer < 2:
                    deltanet(layer)
                    checkpoint()
                    if tok_moe(layer, a_w_out[layer], last=False):
                        raise _Stop()
                    checkpoint()
                else:
                    j = layer - 2
                    if j == 0:
                        shared_kv()
                    diffattn(j, layer)
                    checkpoint()
                    if tok_moe(layer, b_w_out[j], last=(layer == 3)):
                        raise _Stop()
                    checkpoint()
        except _Stop:
            out_toks.append(k.dma('sync', out, h_d))
            out_toks.append(k.dma('sync', dbg, om_d))
        k.wait_all('sync', out_toks)
        k.finish()
    return nc, k


SEQ = 8192
CAP_FULL = 640
_cache = {}


def kernel(**inputs):
    x = np.asarray(inputs['x'])
    B, T, _ = x.shape
    cap = CAP_FULL if T == SEQ else max(64, int(T / 16 + 6 * math.sqrt(T / 16) + 16) // 32 * 32 + 32)
    key = (T, cap)
    if key not in _cache:
        _cache[key] = build(T, cap)[0]
    nc = _cache[key]
    shared = {n: np.ascontiguousarray(np.asarray(v, dtype=np.float32)) for n, v in inputs.items() if n != 'x'}
    in_maps = []
    for b in range(B):
        m = dict(shared)
        m['x'] = np.ascontiguousarray(x[b])
        in_maps.append(m)
    res = run_bass_kernel_spmd(nc, in_maps, core_ids=list(range(B)))
    return np.stack([np.asarray(res.results[b]['out']) for b in range(B)], axis=0).astype(np.float32)
```

```python
import math
from contextlib import ExitStack
import numpy as np
import concourse.bass as bass
import concourse.mybir as mybir
from concourse.bass_utils import run_bass_kernel_spmd

F32 = mybir.dt.float32
BF16 = mybir.dt.bfloat16
I32 = mybir.dt.int32
AF = mybir.ActivationFunctionType
ALU = mybir.AluOpType
AX = mybir.AxisListType

ENGS = ['tensor', 'vector', 'scalar', 'gpsimd', 'sync']
SEM_EPOCH = 30000
N_DMA_SEMS = 16


class Buf:
    __slots__ = ('name', 'w', 'r', 'excl')

    def __init__(self, name='', excl=False):
        self.name = name
        self.excl = excl
        self.w = None
        self.r = {}


class _Op:
    __slots__ = ('fn', 'waits', 'inc', 'incval', 'dma')

    def __init__(self, fn, waits, dma):
        self.fn = fn
        self.waits = waits
        self.inc = False
        self.incval = 0
        self.dma = dma


class K:
    def __init__(self, nc):
        self.nc = nc
        self.ops = {e: [] for e in ENGS}
        self.waited = {e: {} for e in ENGS}
        self.dma_rr = {e: 0 for e in ENGS}
        self.dma_cnt = {}

    def _need_wait(self, eng, t):
        key = (t[0], t[1])
        if self.waited[eng].get(key, -1) >= t[2]:
            return False
        self.waited[eng][key] = t[2]
        if t[0] == 'e':
            self.ops[t[1]][t[2]].inc = True
        return True

    def op(self, eng, fn, reads=(), writes=(), dma=False):
        idx = len(self.ops[eng])
        writes = list(writes) + [b for b in reads if b.excl]
        reads = [b for b in reads if not b.excl]
        deps = []
        for b in reads:
            if b.w is not None:
                deps.append(b.w)
        for b in writes:
            if b.w is not None:
                deps.append(b.w)
            deps.extend(b.r.values())
        waits = []
        for t in deps:
            if t[0] == 'e' and t[1] == eng and eng == 'tensor':
                continue
            if self._need_wait(eng, t):
                waits.append(t)
        dm = None
        if dma:
            slot = self.dma_rr[eng]
            self.dma_rr[eng] = (slot + 1) % N_DMA_SEMS
            cnt = self.dma_cnt.get((eng, slot), 0) + 1
            self.dma_cnt[(eng, slot)] = cnt
            dm = ((eng, slot), cnt * 16)
            if cnt > 1:
                t = ('d', (eng, slot), (cnt - 1) * 16)
                if self._need_wait(eng, t):
                    waits.append(t)
            tok = ('d', (eng, slot), cnt * 16)
        else:
            tok = ('e', eng, idx)
        self.ops[eng].append(_Op(fn, waits, dm))
        kk = (tok[0], tok[1])
        for b in reads:
            b.r[kk] = tok
        for b in writes:
            b.w = tok
            b.r = {}
        return tok

    def dma(self, eng, out, in_, reads=(), writes=(), **kw):
        return self.op(eng, lambda e: e.dma_start(out=out, in_=in_, **kw), reads=reads, writes=writes, dma=True)

    def wait_all(self, eng, tokens):
        waits = [t for t in tokens if self._need_wait(eng, t)]
        if waits:
            self.ops[eng].append(_Op(None, waits, None))

    def barrier(self):
        toks = []
        for e in ENGS:
            for i in range(len(self.ops[e]) - 1, -1, -1):
                o = self.ops[e][i]
                if o.fn is not None and o.dma is None:
                    toks.append(('e', e, i))
                    break
        for key, cnt in self.dma_cnt.items():
            toks.append(('d', key, cnt * 16))
        for e in ENGS:
            self.wait_all(e, toks)
        self.flush()

    def flush(self):
        nc = self.nc
        if not hasattr(self, 'flushed'):
            self.flushed = {e: 0 for e in ENGS}
            self.inccnt = {e: 0 for e in ENGS}
            self.esems = {e: [] for e in ENGS}
            self.dsems = {}
        start = dict(self.flushed)
        for e in ENGS:
            for o in self.ops[e][start[e]:]:
                if o.inc:
                    self.inccnt[e] += 1
                    o.incval = self.inccnt[e]
            need = (self.inccnt[e] + SEM_EPOCH - 1) // SEM_EPOCH
            while len(self.esems[e]) < max(need, 1):
                self.esems[e].append(nc.alloc_semaphore(f"es_{e}_{len(self.esems[e])}"))
        for key in self.dma_cnt:
            if key not in self.dsems:
                self.dsems[key] = nc.alloc_semaphore(f"ds_{key[0]}_{key[1]}")
        esems, dsems, ops = self.esems, self.dsems, self.ops

        def semval(e2, incval):
            return esems[e2][(incval - 1) // SEM_EPOCH], (incval - 1) % SEM_EPOCH + 1

        with nc.Block() as block:
            for eng in ENGS:
                todo = ops[eng][start[eng]:]

                def body(e, eng=eng, todo=todo):
                    for o in todo:
                        for t in o.waits:
                            if t[0] == 'e':
                                p = ops[t[1]][t[2]]
                                assert p.incval > 0, (eng, t)
                                s, v = semval(t[1], p.incval)
                                e.wait_ge(s, v)
                            else:
                                e.wait_ge(dsems[t[1]], t[2])
                        if o.fn is None:
                            continue
                        ins = o.fn(e)
                        if o.dma is not None:
                            ins.then_inc(dsems[o.dma[0]], 16)
                        elif o.inc:
                            s, v = semval(eng, o.incval)
                            ins.then_inc(s, 1)
                if todo:
                    getattr(block, eng)(body)
                self.flushed[eng] = len(ops[eng])

    def finish(self):
        self.flush()


_uid = [0]


def _un(name):
    _uid[0] += 1
    return f"{name}_u{_uid[0]}"


def interleave(gens, depth=2):
    active = []
    it = iter(gens)
    while True:
        while len(active) < depth:
            g = next(it, None)
            if g is None:
                break
            active.append(g)
        if not active:
            break
        for g in list(active):
            try:
                next(g)
            except StopIteration:
                active.remove(g)


_DONE = object()


class Ring:
    def __init__(self, es, nc, name, shape, dt, n):
        self.t = [es.enter_context(nc.sbuf_tensor(_un(f"{name}_{i}"), shape, dt)) for i in range(n)]
        self.b = [Buf(f"{name}_{i}") for i in range(n)]
        self.i = 0

    def next(self):
        i = self.i
        self.i = (i + 1) % len(self.t)
        return self.t[i], self.b[i]


D = 1024
NH = 8
ALPHA = 8 ** 0.25
LN_EPS = 1e-5
RMS_EPS = 1e-6
NEG = -1.0e30


def lambda_init(layer_idx):
    return 0.8 - 0.6 * math.exp(-0.3 * layer_idx)


class _Stop(Exception):
    pass


def build(T, CAP, stop=0, ksub=0, dumpflag=False):
    NT = T // 128
    NCH = T // 64
    NB = T // 512
    nc = bass.Bass("TRN2", target_bir_lowering=False)

    def din(name, shape, dt=F32):
        return nc.dram_tensor(name, shape, dt, kind="ExternalInput").ap()

    x = din("x", [T, D])
    a_w_in = din("a_w_in", [2, D, 4112])
    a_conv_w = din("a_conv_w", [2, 4, 3072])
    a_a_log = din("a_a_log", [2, 8])
    a_dt_bias = din("a_dt_bias", [2, 8])
    a_norm_w = din("a_norm_w", [2, 128])
    a_w_out = din("a_w_out", [2, D, D])
    kv_w = din("kv_w", [D, 2048])
    b_w_q = din("b_w_q", [2, D, D])
    b_lambda = din("b_lambda", [2, 4, 64])
    b_subln_w = din("b_subln_w", [2, 128])
    b_w_out = din("b_w_out", [2, D, D])
    ln_mix_g = din("ln_mix_g", [4, D])
    ln_mix_b = din("ln_mix_b", [4, D])
    ln_ffn_g = din("ln_ffn_g", [4, D])
    ln_ffn_b = din("ln_ffn_b", [4, D])
    moe_w_group = din("moe_w_group", [4, D, 4])
    moe_w_expert = din("moe_w_expert", [4, D, 32])
    moe_w13 = din("moe_w13", [4, 32, D, 1024])
    moe_w2 = din("moe_w2", [4, 32, 512, D])
    out = nc.dram_tensor("out", [T, D], F32, kind="ExternalOutput").ap()
    dbg = nc.dram_tensor("dbg", [T, D], BF16, kind="ExternalOutput").ap() if stop else None
    stage = [0]

    def checkpoint(noraise=False):
        stage[0] += 1
        if stop and stage[0] >= stop:
            if noraise:
                return True
            raise _Stop()
        return False

    h_d = nc.dram_tensor("h_d", [T, D], F32).ap()
    hT_d = nc.dram_tensor("hT_d", [D, T], BF16).ap()
    om_d = nc.dram_tensor("om_d", [T, D], BF16).ap()
    xb_d = nc.dram_tensor("xb_d", [T, D], BF16).ap()
    kT_d = nc.dram_tensor("kT_d", [NH, 2, 64, T], BF16).ap()
    va_d = nc.dram_tensor("va_d", [NH, T, 129], BF16).ap()
    NS = 32 * CAP
    xs_d = nc.dram_tensor("xs_d", [NS + 128, D], BF16).ap()
    ys_h = [nc.dram_tensor(f"ys_d{i}", [NS + 128, 512], F32).ap() for i in range(2)]
    hT_v = hT_d.rearrange("(k p) t -> p k t", p=128)

    k = K(nc)
    out_toks = []
    dumped = set()

    def dump(name, ap, B):
        if not dumpflag or name in dumped:
            return
        dumped.add(name)
        t = nc.dram_tensor("dump_" + name, list(ap.shape), F32, kind="ExternalOutput").ap()
        out_toks.append(k.dma('gpsimd', t, ap, reads=[B]))
    with ExitStack() as top:
        def sbt(es, name, shape, dt):
            return es.enter_context(nc.sbuf_tensor(_un(name), shape, dt))

        ps = [top.enter_context(nc.psum_tensor(f"ps{i}", [128, 512], F32)) for i in range(7)]
        psB = [Buf(f"ps{i}", excl=True) for i in range(7)]
        pb = top.enter_context(nc.psum_tensor("pb", [128, 1024], BF16))
        pbB = Buf("pb", excl=True)

        ident = sbt(top, "ident", [128, 128], F32)
        identb = sbt(top, "identb", [128, 128], BF16)
        ones_f = sbt(top, "ones_f", [128, 128], F32)
        ones_b = sbt(top, "ones_b", [128, 128], BF16)
        triu = sbt(top, "triu", [128, 128], F32)
        triub = sbt(top, "triub", [128, 128], BF16)
        sutri = sbt(top, "sutri", [128, 128], BF16)
        zero_b = sbt(top, "zero_b", [128, 1024], BF16)
        triu4 = sbt(top, "triu4", [64, 4, 64], F32)
        ident4 = sbt(top, "ident4", [64, 4, 64], F32)
        cB = Buf("consts")
        k.op('gpsimd', lambda e: e.iota(ones_f[:], [[1, 128]], base=0, channel_multiplier=-1,
                                        allow_small_or_imprecise_dtypes=True), writes=[cB])
        k.op('vector', lambda e: e.tensor_single_scalar(ident[:], ones_f[:], 0.0, ALU.is_equal), reads=[cB], writes=[cB])
        k.op('vector', lambda e: e.tensor_single_scalar(identb[:], ones_f[:], 0.0, ALU.is_equal), reads=[cB], writes=[cB])
        k.op('vector', lambda e: e.tensor_single_scalar(triu[:], ones_f[:], 0.0, ALU.is_ge), reads=[cB], writes=[cB])
        k.op('vector', lambda e: e.tensor_single_scalar(triub[:], ones_f[:], 0.0, ALU.is_ge), reads=[cB], writes=[cB])
        k.op('vector', lambda e: e.tensor_single_scalar(sutri[:], ones_f[:], 0.0, ALU.is_gt), reads=[cB], writes=[cB])
        for g4 in range(4):
            k.op('vector', lambda e, g4=g4: e.tensor_copy(triu4[:, g4, :], triu[0:64, 0:64]), reads=[cB], writes=[cB])
            k.op('vector', lambda e, g4=g4: e.tensor_copy(ident4[:, g4, :], ident[0:64, 0:64]), reads=[cB], writes=[cB])
        k.op('vector', lambda e: e.memset(ones_f[:], 1.0), reads=[cB], writes=[cB])
        k.op('vector', lambda e: e.memset(ones_b[:], 1.0), writes=[cB])
        k.op('vector', lambda e: e.memset(zero_b[:], 0.0), writes=[cB])
        zero_f = sbt(top, "zero_f", [128, 512], F32)
        k.op('vector', lambda e: e.memset(zero_f[:], 0.0), writes=[cB])
        for r0 in range(0, NS + 128, 128):
            k.dma('sync', xs_d[r0:r0 + 128, :], zero_b[:], reads=[cB])
        for hf in range(2):
            k.dma('sync', ys_h[hf][NS:NS + 128, :], zero_f[:], reads=[cB])
        k.barrier()

        def layer_norm(es_ring, t, tB, gbc, bbc, wBs, y, yB):
            st, stB = es_ring['st'].next()
            jk, jkB = es_ring['junk'].next()
            k.op('scalar', lambda e: e.activation(jk[:], t[:], AF.Copy, accum_out=st[:, 0:1]), reads=[tB], writes=[jkB, stB])
            k.op('scalar', lambda e: e.activation(jk[:], t[:], AF.Square, accum_out=st[:, 1:2]), reads=[tB], writes=[jkB, stB])
            k.op('vector', lambda e: e.tensor_scalar_mul(st[:, 2:3], st[:, 0:1], 1.0 / D), reads=[stB], writes=[stB])
            k.op('vector', lambda e: e.tensor_tensor(st[:, 3:4], st[:, 2:3], st[:, 2:3], ALU.mult), reads=[stB], writes=[stB])
            k.op('vector', lambda e: e.scalar_tensor_tensor(st[:, 4:5], st[:, 1:2], 1.0 / D, st[:, 3:4], ALU.mult, ALU.subtract),
                 reads=[stB], writes=[stB])
            k.op('scalar', lambda e: e.activation(st[:, 5:6], st[:, 4:5], AF.Sqrt, bias=LN_EPS), reads=[stB], writes=[stB])
            k.op('vector', lambda e: e.reciprocal(st[:, 6:7], st[:, 5:6]), reads=[stB], writes=[stB])
            k.op('vector', lambda e: e.tensor_scalar(t[:], t[:], st[:, 2:3], st[:, 6:7], ALU.subtract, ALU.mult), reads=[tB, stB], writes=[tB])
            k.op('gpsimd', lambda e: e.tensor_tensor(t[:], t[:], gbc[:], ALU.mult), reads=[tB] + wBs, writes=[tB])
            k.op('vector', lambda e: e.tensor_tensor(y[:], t[:], bbc[:], ALU.add), reads=[tB] + wBs, writes=[yB])

        def make_hT(rings, y, yB, tile):
            hb, hbB = rings['hTs'].next()
            for half in range(2):
                pp, ppB = ps[5 + half], psB[5 + half]
                for j in range(4):
                    kk = half * 4 + j
                    k.op('tensor', lambda e, pp=pp, j=j, kk=kk: e.transpose(pp[:, j * 128:(j + 1) * 128], y[:, kk * 128:(kk + 1) * 128], ident[:]),
                         reads=[yB, cB], writes=[ppB])
                eng = 'vector' if half == 0 else 'scalar'
                if eng == 'vector':
                    k.op('vector', lambda e, pp=pp, half=half: e.tensor_copy(hb[:, half * 4:(half + 1) * 4, :], pp[:].rearrange("p (a b) -> p a b", a=4)),
                         reads=[ppB], writes=[hbB])
                else:
                    k.op('scalar', lambda e, pp=pp, half=half: e.copy(hb[:, half * 4:(half + 1) * 4, :], pp[:].rearrange("p (a b) -> p a b", a=4)),
                         reads=[ppB], writes=[hbB])
            k.dma('sync', hT_v[:, :, tile * 128:(tile + 1) * 128], hb[:], reads=[hbB])

        with ExitStack() as es:
            rings = {'hTs': Ring(es, nc, "hTs", [128, 8, 128], BF16, 2)}
            xr = Ring(es, nc, "xin", [128, D], F32, 2)
            for t in range(NT):
                xt, xB = xr.next()
                k.dma('sync', xt[:], x[t * 128:(t + 1) * 128, :], writes=[xB])
                k.dma('gpsimd', h_d[t * 128:(t + 1) * 128, :], xt[:], reads=[xB])
                make_hT(rings, xt, xB, t)
            k.barrier()

        def load_w_bf16(es, name, src, cols):
            w = sbt(es, name, [128, 8, cols], BF16)
            wB = Buf(name)
            k.dma('gpsimd', w[:], src.rearrange("(k p) c -> p k c", p=128), writes=[wB])
            return w, wB

        def bcast_row(es, name, src_row, n, dt=F32):
            w = sbt(es, name, [128, n], dt)
            wB = Buf(name)
            k.dma('sync', w[:], src_row.partition_broadcast(128), writes=[wB])
            return w, wB

        def deltanet(l):
            with ExitStack() as es:
                cw = sbt(es, "cw", [128, 24, 4], F32)
                cwB = Buf("cw")
                cwn = sbt(es, "cwn", [4, 3072], F32)
                k.dma('sync', cwn[:], a_conv_w[l], writes=[cwB])
                for part in range(24):
                    k.op('tensor', lambda e, part=part: e.transpose(ps[0][:, part * 4:(part + 1) * 4], cwn[:, part * 128:(part + 1) * 128], ident[0:4, 0:4]),
                         reads=[cwB, cB], writes=[psB[0]])
                k.op('vector', lambda e: e.tensor_copy(cw[:].rearrange("p a b -> p (a b)"), ps[0][:, 0:96]), reads=[psB[0]], writes=[cwB])
                nw, nwB = bcast_row(es, "nw", a_norm_w[l:l + 1, :], 128)
                alog, alB = bcast_row(es, "alog", a_a_log[l:l + 1, :], 8)
                dtb, dtB = bcast_row(es, "dtb", a_dt_bias[l:l + 1, :], 8)
                nea = sbt(es, "nea", [128, 8], F32)
                k.op('scalar', lambda e: e.activation(nea[:], alog[:], AF.Exp), reads=[alB], writes=[alB])
                k.op('vector', lambda e: e.tensor_scalar_mul(nea[:], nea[:], -1.0), reads=[alB], writes=[alB])
                qT = sbt(es, "qT", [128, T], BF16)
                kT = sbt(es, "kT", [128, T], BF16)
                vT = sbt(es, "vT", [128, T], BF16)
                qkvB = [Buf("qT"), Buf("kT"), Buf("vT")]
                qkv = [qT, kT, vT]
                zT = sbt(es, "zT", [128, T], BF16)
                zTB = Buf("zT")
                glT = sbt(es, "glT", [64, NCH, 16], F32)
                glTB = Buf("glT")
                wg16 = sbt(es, "wg16", [128, 8, 16], BF16)
                wgB = Buf("wg16")
                gl = sbt(es, "gl", [64, 8, NCH], F32)
                glB = Buf("gl")
                egl = sbt(es, "egl", [128, NCH], F32)
                eglB = Buf("egl")
                S = sbt(es, "S", [128, 128], F32)
                SB = Buf("S")
                hbr = Ring(es, nc, "hblk", [128, 8, 512], BF16, 2)
                raw = sbt(es, "raw", [128, 3, 515], F32)
                rawB = [Buf("raw0"), Buf("raw1"), Buf("raw2")]
                cvr = Ring(es, nc, "cv", [128, 512], F32, 2)
                sqr = Ring(es, nc, "sq", [128, 512], F32, 2)
                G = 4
                R = 2
                r_kg = Ring(es, nc, "kg", [64, G, 256], F32, R)
                r_kdec = Ring(es, nc, "kdec", [64, G, 128], F32, R)
                r_dm = Ring(es, nc, "dm", [64, G, 64], F32, R)
                r_dg = Ring(es, nc, "dg", [64, G, 64], F32, R)
                r_egb = Ring(es, nc, "egb", [128, G, 64], F32, R)
                r_qg = Ring(es, nc, "qg", [128, G, 64], F32, R)
                r_at = Ring(es, nc, "at", [64, G, 64], F32, R)
                r_X = Ring(es, nc, "X", [64, G, 64], BF16, 4)
                r_Y = Ring(es, nc, "Y", [64, G, 64], BF16, 4)
                r_Pb = Ring(es, nc, "Pb", [64, G, 64], BF16, 2)
                r_zs4 = Ring(es, nc, "zs4", [64, G, 128], BF16, R)
                r_P = Ring(es, nc, "P", [64, G, 64], F32, R)
                r_uw = Ring(es, nc, "uw", [64, G, 256], F32, R)
                r_AT = Ring(es, nc, "AT", [128, G, 128], F32, R)
                r_Bc = Ring(es, nc, "Bc", [128, G, 128], F32, R)
                r_QpT = Ring(es, nc, "QpT", [128, G, 64], F32, R)
                r_O0 = Ring(es, nc, "O0", [64, G, 128], F32, R)
                r_vn = Ring(es, nc, "vn", [64, 128], F32, 2)
                r_o = Ring(es, nc, "o", [64, 128], F32, 2)
                r_ob = Ring(es, nc, "ob", [64, 128], BF16, 2)
                r_st = Ring(es, nc, "dst", [64, 4], F32, 2)
                r_jk = Ring(es, nc, "djk", [64, 128], F32, 2)
                k.dma('gpsimd', wg16[:], a_w_in[l, :, 4096:4112].rearrange("(k p) c -> p k c", p=128), writes=[wgB])
                glFr = Ring(es, nc, "glF", [16, 512], F32, 2)
                for blk in range(NB):
                    hb, hbB = hbr.next()
                    k.dma('sync', hb[:], hT_v[:, :, blk * 512:(blk + 1) * 512], writes=[hbB])
                    for kc in range(8):
                        k.op('tensor', lambda e, kc=kc, hb=hb: e.matmul(ps[3][0:16, :], wg16[:, kc, :], hb[:, kc, :], start=(kc == 0), stop=(kc == 7)),
                             reads=[wgB, hbB], writes=[psB[3]])
                    glF, glFB = glFr.next()
                    k.op('scalar', lambda e, glF=glF: e.copy(glF[:], ps[3][0:16, :]), reads=[psB[3]], writes=[glFB])
                    for cc in range(8):
                        k.op('tensor', lambda e, cc=cc, glF=glF: e.transpose(ps[4][0:64, cc * 16:(cc + 1) * 16], glF[:, cc * 64:(cc + 1) * 64], ident[0:16, 0:16]),
                             reads=[glFB, cB], writes=[psB[4]])
                    k.op('vector', lambda e, blk=blk: e.tensor_copy(glT[:, blk * 8:(blk + 1) * 8, :].rearrange("p a b -> p (a b)"), ps[4][0:64, 0:128]),
                         reads=[psB[4]], writes=[glTB])
                for h in range(NH):
                    wq3, wq3B = [], []
                    wcat = sbt(es, f"wcat{h}", [128, 8, 512], BF16) if h == 0 else wcat_keep[0]
                    if h == 0:
                        wcat_keep = [wcat]
                        wcB = Buf("wcat")
                    for part in range(3):
                        k.dma('gpsimd', wcat[:, :, part * 128:(part + 1) * 128],
                              a_w_in[l, :, part * 1024 + h * 128: part * 1024 + (h + 1) * 128].rearrange("(k p) c -> p k c", p=128), writes=[wcB])
                    k.dma('gpsimd', wcat[:, :, 384:512], a_w_in[l, :, 3072 + h * 128:3072 + (h + 1) * 128].rearrange("(k p) c -> p k c", p=128), writes=[wcB])
                    for part in range(3):
                        k.op('vector', lambda e, part=part: e.memset(raw[:, part, 0:3], 0.0), writes=[rawB[part]])
                    if ksub == 5:
                        k.barrier()
                        return
                    for blk in range(NB):
                        hb, hbB = hbr.next()
                        k.dma('sync', hb[:], hT_v[:, :, blk * 512:(blk + 1) * 512], writes=[hbB])
                        for part in range(3):
                            pp, ppB = ps[part % 2], psB[part % 2]
                            for kc in range(8):
                                k.op('tensor', lambda e, pp=pp, kc=kc, part=part, hb=hb: e.matmul(pp[:, :], wcat[:, kc, part * 128:(part + 1) * 128], hb[:, kc, :],
                                                                                            start=(kc == 0), stop=(kc == 7)),
                                     reads=[wcB, hbB], writes=[ppB])
                            k.op('scalar', lambda e, pp=pp, part=part: e.copy(raw[:, part, 3:515], pp[:, :]), reads=[ppB], writes=[rawB[part]])
                            if ksub == 11:
                                k.barrier()
                                return
                            cv, cvB = cvr.next()
                            ci = part * 8 + h
                            k.op('vector', lambda e, cv=cv, part=part, ci=ci: e.tensor_scalar_mul(cv[:], raw[:, part, 0:512], cw[:, ci, 0:1]),
                                 reads=[rawB[part], cwB], writes=[cvB])
                            for j in range(1, 4):
                                k.op('vector', lambda e, cv=cv, part=part, ci=ci, j=j: e.scalar_tensor_tensor(cv[:], raw[:, part, j:j + 512], cw[:, ci, j:j + 1], cv[:],
                                                                                                        ALU.mult, ALU.add),
                                     reads=[rawB[part], cwB, cvB], writes=[cvB])
                            k.op('vector', lambda e, part=part: e.tensor_copy(raw[:, part, 0:3], raw[:, part, 512:515]), reads=[rawB[part]], writes=[rawB[part]])
                            if ksub == 12:
                                k.barrier()
                                return
                            dst = qkv[part][:, blk * 512:(blk + 1) * 512]
                            if part == 2:
                                k.op('scalar', lambda e, cv=cv, dst=dst: e.activation(dst, cv[:], AF.Silu), reads=[cvB], writes=[qkvB[part]])
                            else:
                                k.op('scalar', lambda e, cv=cv: e.activation(cv[:], cv[:], AF.Silu), reads=[cvB], writes=[cvB])
                                sq, sqB = sqr.next()
                                k.op('gpsimd', lambda e, cv=cv, sq=sq: e.tensor_tensor(sq[:], cv[:], cv[:], ALU.mult), reads=[cvB], writes=[sqB])
                                p2, p2B = ps[2], psB[2]
                                for hf in range(2):
                                    k.op('tensor', lambda e, sq=sq, p2=p2, hf=hf: e.matmul(p2[:, hf * 256:(hf + 1) * 256], ones_f[:], sq[:, hf * 256:(hf + 1) * 256], start=True, stop=True), reads=[sqB, cB], writes=[p2B])
                                k.op('scalar', lambda e, sq=sq, p2=p2: e.activation(sq[:], p2[:, :], AF.Sqrt, bias=RMS_EPS), reads=[p2B], writes=[sqB])
                                k.op('vector', lambda e, sq=sq: e.reciprocal(sq[:], sq[:]), reads=[sqB], writes=[sqB])
                                sc = (128 ** -0.5) if part == 0 else 1.0
                                k.op('vector', lambda e, cv=cv, sq=sq, dst=dst, sc=sc: e.scalar_tensor_tensor(dst, cv[:], sc, sq[:], ALU.mult, ALU.mult),
                                     reads=[cvB, sqB], writes=[qkvB[part]])
                            if ksub == 13:
                                k.barrier()
                                return
                        if ksub == 14:
                            k.barrier()
                            return
                        pz, pzB = ps[3], psB[3]
                        for kc in range(8):
                            k.op('tensor', lambda e, kc=kc, hb=hb: e.matmul(pz[:, :], wcat[:, kc, 384:512], hb[:, kc, :], start=(kc == 0), stop=(kc == 7)),
                                 reads=[wcB, hbB], writes=[pzB])
                        k.op('scalar', lambda e, blk=blk: e.activation(zT[:, blk * 512:(blk + 1) * 512], pz[:, :], AF.Silu), reads=[pzB], writes=[zTB])
                    k.op('vector', lambda e, h=h: e.tensor_copy(gl[:, 0, :], glT[:, :, h]), reads=[glTB], writes=[glB])
                    k.op('vector', lambda e, h=h: e.tensor_copy(gl[:, 1, :], glT[:, :, 8 + h]), reads=[glTB], writes=[glB])
                    if ksub == 1:
                        k.barrier()
                        return
                    k.op('scalar', lambda e: e.activation(gl[:, 0, :], gl[:, 0, :], AF.Sigmoid), reads=[glB], writes=[glB])
                    k.op('vector', lambda e: e.tensor_scalar_mul(gl[:, 5, :], gl[:, 0, :], -1.0), reads=[glB], writes=[glB])
                    k.op('scalar', lambda e, h=h: e.activation(gl[:, 1, :], gl[:, 1, :], AF.Exp, bias=dtb[0:64, h:h + 1]), reads=[glB, dtB], writes=[glB])
                    k.op('scalar', lambda e: e.activation(gl[:, 1, :], gl[:, 1, :], AF.Ln, bias=1.0), reads=[glB], writes=[glB])
                    k.op('vector', lambda e, h=h: e.tensor_scalar_mul(gl[:, 1, :], gl[:, 1, :], nea[0:64, h:h + 1]), reads=[glB, alB], writes=[glB])
                    for c0 in range(0, NCH, 512):
                        n = min(512, NCH - c0)
                        k.op('tensor', lambda e, c0=c0, n=n: e.matmul(ps[0][0:64, 0:n], triu[0:64, 0:64], gl[:, 1, c0:c0 + n], start=True, stop=True),
                             reads=[glB, cB], writes=[psB[0]])
                        k.op('vector', lambda e, c0=c0, n=n: e.tensor_copy(gl[:, 2, c0:c0 + n], ps[0][0:64, 0:n]), reads=[psB[0]], writes=[glB])
                        k.op('tensor', lambda e, c0=c0, n=n: e.matmul(ps[1][:, 0:n], ones_f[0:64, :], gl[:, 1, c0:c0 + n], start=True, stop=True),
                             reads=[glB, cB], writes=[psB[1]])
                        k.op('scalar', lambda e, c0=c0, n=n: e.activation(egl[:, c0:c0 + n], ps[1][:, 0:n], AF.Exp), reads=[psB[1]], writes=[eglB])
                        k.op('vector', lambda e, c0=c0, n=n: e.tensor_copy(gl[:, 6, c0:c0 + n], ps[1][0:64, 0:n]), reads=[psB[1]], writes=[glB])
                    k.op('scalar', lambda e: e.activation(gl[:, 3, :], gl[:, 2, :], AF.Exp), reads=[glB], writes=[glB])
                    k.op('vector', lambda e: e.tensor_tensor(gl[:, 7, :], gl[:, 6, :], gl[:, 2, :], ALU.subtract), reads=[glB], writes=[glB])
                    k.op('scalar', lambda e: e.activation(gl[:, 4, :], gl[:, 7, :], AF.Exp), reads=[glB], writes=[glB])
                    k.op('vector', lambda e: e.memset(S[:], 0.0), writes=[SB])
                    if h == 0 and l == 0:
                        dump("gl", gl[:], glB)
                        dump("egl", egl[:], eglB)
                        dump("qT", qT[:, 0:128], qkvB[0])
                        dump("kT", kT[:, 0:128], qkvB[1])
                        dump("vT", vT[:, 0:128], qkvB[2])
                    if ksub == 2:
                        k.barrier()
                        return

                    def bulk(gi, res):
                        c0 = gi * G
                        gcols = slice(c0 * 64, (c0 + G) * 64)
                        csl = [slice((c0 + g) * 64, (c0 + g + 1) * 64) for g in range(G)]
                        kg, kgB = r_kg.next()
                        kd, kdB = r_kdec.next()
                        for g in range(G):
                            k.op('tensor', lambda e, g=g: e.transpose(pb[0:64, g * 256:g * 256 + 128], kT[:, csl[g]], identb[:]), reads=[qkvB[1], cB], writes=[pbB])
                            k.op('tensor', lambda e, g=g: e.transpose(pb[0:64, g * 256 + 128:g * 256 + 256], vT[:, csl[g]], identb[:]), reads=[qkvB[2], cB], writes=[pbB])
                        yield
                        for g in range(G):
                            c = c0 + g
                            k.op('vector', lambda e, g=g, c=c: e.tensor_scalar_mul(kg[:, g, 128:256], pb[0:64, g * 256:g * 256 + 128], gl[:, 3, c:c + 1]), reads=[pbB, glB], writes=[kgB])
                            k.op('vector', lambda e, g=g, c=c: e.tensor_scalar_mul(kd[:, g, :], pb[0:64, g * 256:g * 256 + 128], gl[:, 4, c:c + 1]), reads=[pbB, glB], writes=[kdB])
                        k.op('scalar', lambda e: e.copy(kg[:, :, 0:128], pb[0:64, :].rearrange("p (g c) -> p g c", g=G)[:, :, 128:256]), reads=[pbB], writes=[kgB])
                        yield
                        p0, p0B = ps[0], psB[0]
                        for g in range(G):
                            k.op('tensor', lambda e, g=g: e.matmul(p0[0:64, g * 128:g * 128 + 64], kT[:, csl[g]], kT[:, csl[g]], start=True, stop=True), reads=[qkvB[1]], writes=[p0B])
                            k.op('tensor', lambda e, g=g: e.matmul(p0[0:64, g * 128 + 64:g * 128 + 128], kT[:, csl[g]], qT[:, csl[g]], start=True, stop=True), reads=[qkvB[1], qkvB[0]], writes=[p0B])
                        p0v = p0[0:64, :].rearrange("p (g c) -> p g c", g=G)
                        yield
                        dg, dgB = r_dg.next()
                        for g in range(G):
                            c = c0 + g
                            k.op('gpsimd', lambda e, g=g, c=c: e.tensor_scalar_mul(dg[:, g, :], ident[0:64, 0:64], gl[:, 2, c:c + 1]), reads=[glB, cB], writes=[dgB])
                        p1, p1B = ps[1], psB[1]
                        for g in range(G):
                            k.op('tensor', lambda e, g=g: e.matmul(p1[:, g * 64:(g + 1) * 64], ones_f[0:64, :], dg[:, g, :], start=True, stop=True), reads=[dgB, cB], writes=[p1B])
                        yield
                        dm, dmB = r_dm.next()
                        for g in range(G):
                            c = c0 + g
                            k.op('vector', lambda e, g=g, c=c: e.tensor_scalar(dm[:, g, :], p1[0:64, g * 64:(g + 1) * 64], gl[:, 2, c:c + 1], 0.0, ALU.subtract, ALU.min), reads=[p1B, glB], writes=[dmB])
                        yield
                        egb, egbB = r_egb.next()
                        k.op('scalar', lambda e: e.activation(egb[:].rearrange("p g c -> p (g c)"), p1[:, 0:G * 64], AF.Exp), reads=[p1B], writes=[egbB])
                        k.op('scalar', lambda e: e.activation(dm[:], dm[:], AF.Exp), reads=[dmB], writes=[dmB])
                        k.op('vector', lambda e: e.tensor_tensor(dm[:], dm[:], triu4[:], ALU.mult), reads=[dmB, cB], writes=[dmB])
                        qg, qgB = r_qg.next()
                        k.op('gpsimd', lambda e: e.tensor_tensor(qg[:].rearrange("p g c -> p (g c)"), qT[:, gcols], egb[:].rearrange("p g c -> p (g c)"), ALU.mult), reads=[qkvB[0], egbB], writes=[qgB])
                        yield
                        at, atB = r_at.next()
                        k.op('vector', lambda e: e.tensor_tensor(at[:], p0v[:, :, 64:128], dm[:], ALU.mult), reads=[p0B, dmB], writes=[atB])
                        k.op('vector', lambda e: e.tensor_tensor(dm[:], dm[:], ident4[:], ALU.subtract), reads=[dmB, cB], writes=[dmB])
                        X, XB = r_X.next()
                        for g in range(G):
                            c = c0 + g
                            k.op('vector', lambda e, X=X, g=g, c=c: e.scalar_tensor_tensor(X[:, g, :], p0[0:64, g * 128:g * 128 + 64], gl[:, 5, c:c + 1], dm[:, g, :], ALU.mult, ALU.mult),
                                 reads=[p0B, glB, dmB], writes=[XB])
                        yield
                        p2, p2B = ps[2], psB[2]
                        p3, p3B = ps[3], psB[3]
                        p4, p4B = ps[4], psB[4]
                        for g in range(G):
                            k.op('tensor', lambda e, X=X, g=g: e.transpose(pb[0:64, g * 64:(g + 1) * 64], X[:, g, :], identb[0:64, 0:64]), reads=[XB, cB], writes=[pbB])
                        yield
                        Y, YB = r_Y.next()
                        k.op('scalar', lambda e, Y=Y: e.copy(Y[:].rearrange("p g c -> p (g c)"), pb[0:64, 0:G * 64]), reads=[pbB], writes=[YB])
                        P, PB = r_P.next()
                        k.op('vector', lambda e, X=X: e.tensor_tensor(P[:], X[:], ident4[:], ALU.add), reads=[XB, cB], writes=[PB])
                        Pb, PbB = r_Pb.next()
                        k.op('gpsimd', lambda e, Pb=Pb: e.tensor_copy(Pb[:], P[:]), reads=[PB], writes=[PbB])
                        for s in range(1, 6):
                            if s < 5:
                                for g in range(G):
                                    k.op('tensor', lambda e, X=X, Y=Y, g=g: e.matmul(p2[0:64, g * 64:(g + 1) * 64], Y[:, g, :], X[:, g, :], start=True, stop=True), reads=[XB, YB], writes=[p2B])
                            for g in range(G):
                                k.op('tensor', lambda e, X=X, Y=Y, g=g: e.matmul(p3[0:64, g * 64:(g + 1) * 64], X[:, g, :], Y[:, g, :], start=True, stop=True), reads=[XB, YB], writes=[p3B])
                            yield
                            Yn, YnB = r_Y.next()
                            k.op('scalar', lambda e, Yn=Yn: e.copy(Yn[:].rearrange("p g c -> p (g c)"), p3[0:64, 0:G * 64]), reads=[p3B], writes=[YnB])
                            if s < 5:
                                Xn, XnB = r_X.next()
                                k.op('vector', lambda e, Xn=Xn: e.tensor_copy(Xn[:].rearrange("p g c -> p (g c)"), p2[0:64, 0:G * 64]), reads=[p2B], writes=[XnB])
                            yield
                            for g in range(G):
                                k.op('tensor', lambda e, Yn=Yn, Pb=Pb, g=g: e.matmul(p4[0:64, g * 64:(g + 1) * 64], Yn[:, g, :], Pb[:, g, :], start=True, stop=True), reads=[YnB, PbB], writes=[p4B])
                            k.op('vector', lambda e: e.tensor_tensor(P[:].rearrange("p g c -> p (g c)"), P[:].rearrange("p g c -> p (g c)"), p4[0:64, 0:G * 64], ALU.add), reads=[p4B, PB], writes=[PB])
                            if s < 5:
                                Pb, PbB = r_Pb.next()
                                k.op('gpsimd', lambda e, Pb=Pb: e.tensor_copy(Pb[:], P[:]), reads=[PB], writes=[PbB])
                            yield
                            Y, YB = Yn, YnB
                            if s < 5:
                                X, XB = Xn, XnB
                        yield
                        uw, uwB = r_uw.next()
                        for rr in range(G // 2):
                            for g2 in range(2):
                                g = rr * 2 + g2
                                k.op('tensor', lambda e, g=g, g2=g2: e.matmul(p4[0:64, g2 * 256:(g2 + 1) * 256], P[:, g, :], kg[:, g, :], start=True, stop=True), reads=[PB, kgB], writes=[p4B])
                            for g2 in range(2):
                                g = rr * 2 + g2
                                c = c0 + g
                                k.op('vector', lambda e, g=g, g2=g2, c=c: e.tensor_scalar_mul(uw[:, g, :], p4[0:64, g2 * 256:(g2 + 1) * 256], gl[:, 0, c:c + 1]), reads=[p4B, glB], writes=[uwB])
                        yield
                        AT, ATB = r_AT.next()
                        Bc, BcB = r_Bc.next()
                        QpT, QpTB = r_QpT.next()
                        O0, O0B = r_O0.next()
                        for g in range(G):
                            k.op('tensor', lambda e, g=g: e.matmul(p2[:, g * 128:(g + 1) * 128], uw[:, g, 128:256], kd[:, g, :], start=True, stop=True), reads=[uwB, kdB], writes=[p2B])
                        for g in range(G):
                            k.op('tensor', lambda e, g=g: e.matmul(p3[:, g * 128:(g + 1) * 128], kd[:, g, :], uw[:, g, 0:128], start=True, stop=True), reads=[uwB, kdB], writes=[p3B])
                        yield
                        for g in range(G):
                            c = c0 + g
                            k.op('vector', lambda e, g=g, c=c: e.scalar_tensor_tensor(AT[:, g, :], ident[:], egl[:, c:c + 1], p2[:, g * 128:(g + 1) * 128], ALU.mult, ALU.subtract),
                                 reads=[p2B, eglB, cB], writes=[ATB])
                        k.op('scalar', lambda e: e.copy(Bc[:].rearrange("p g c -> p (g c)"), p3[:, 0:G * 128]), reads=[p3B], writes=[BcB])
                        yield
                        for g in range(G):
                            k.op('tensor', lambda e, g=g: e.matmul(p0[:, g * 64:(g + 1) * 64], uw[:, g, 128:256], at[:, g, :], start=True, stop=True), reads=[uwB, atB], writes=[p0B])
                        for g in range(G):
                            k.op('tensor', lambda e, g=g: e.matmul(p4[0:64, g * 128:(g + 1) * 128], at[:, g, :], uw[:, g, 0:128], start=True, stop=True), reads=[uwB, atB], writes=[p4B])
                        yield
                        k.op('vector', lambda e: e.tensor_tensor(QpT[:].rearrange("p g c -> p (g c)"), qg[:].rearrange("p g c -> p (g c)"), p0[:, 0:G * 64], ALU.subtract),
                             reads=[p0B, qgB], writes=[QpTB])
                        k.op('scalar', lambda e: e.copy(O0[:].rearrange("p g c -> p (g c)"), p4[0:64, 0:G * 128]), reads=[p4B], writes=[O0B])
                        yield
                        for g in range(G):
                            k.op('tensor', lambda e, g=g: e.transpose(pb[0:64, g * 128:(g + 1) * 128], zT[:, csl[g]], identb[:]), reads=[zTB, cB], writes=[pbB])
                        zs4, zs4B = r_zs4.next()
                        k.op('scalar', lambda e: e.copy(zs4[:].rearrange("p g c -> p (g c)"), pb[0:64, 0:G * 128]), reads=[pbB], writes=[zs4B])
                        res.update(dict(AT=(AT, ATB), Bc=(Bc, BcB), QpT=(QpT, QpTB), O0=(O0, O0B), zs=(zs4, zs4B)))

                    def scan(c, r, g):
                        AT_, ATB = r['AT']
                        Bc_, BcB = r['Bc']
                        QpT_, QpTB = r['QpT']
                        O0_, O0B = r['O0']
                        zs4_, zs4B_ = r['zs']
                        p5, p5B = ps[5], psB[5]
                        p6, p6B = ps[6], psB[6]
                        k.op('tensor', lambda e: e.matmul(p5[:, 0:128], AT_[:, g, :], S[:], start=True, stop=True), reads=[ATB, SB], writes=[p5B])
                        k.op('tensor', lambda e: e.matmul(p6[0:64, 0:128], QpT_[:, g, :], S[:], start=True, stop=True), reads=[QpTB, SB], writes=[p6B])
                        yield
                        k.op('vector', lambda e: e.tensor_tensor(S[:], p5[:, 0:128], Bc_[:, g, :], ALU.add), reads=[p5B, BcB, SB], writes=[SB])
                        yield
                        o, oB = r_o.next()
                        st, stB = r_st.next()
                        jk, jkB = r_jk.next()
                        k.op('vector', lambda e: e.tensor_tensor(o[:], p6[0:64, 0:128], O0_[:, g, :], ALU.add), reads=[p6B, O0B], writes=[oB])
                        k.op('scalar', lambda e: e.activation(jk[:], o[:], AF.Square, accum_out=st[:, 0:1]), reads=[oB], writes=[jkB, stB])
                        k.op('scalar', lambda e: e.activation(st[:, 1:2], st[:, 0:1], AF.Sqrt, bias=RMS_EPS, scale=1.0 / 128), reads=[stB], writes=[stB])
                        yield
                        k.op('vector', lambda e: e.reciprocal(st[:, 2:3], st[:, 1:2]), reads=[stB], writes=[stB])
                        k.op('vector', lambda e: e.scalar_tensor_tensor(o[:], o[:], st[:, 2:3], nw[0:64, :], ALU.mult, ALU.mult), reads=[oB, stB, nwB], writes=[oB])
                        ob, obB = r_ob.next()
                        k.op('gpsimd', lambda e: e.tensor_tensor(ob[:], o[:], zs4_[:, g, :], ALU.mult), reads=[oB, zs4B_], writes=[obB])
                        k.dma('sync', om_d[c * 64:(c + 1) * 64, h * 128:(h + 1) * 128], ob[:], reads=[obB])

                    assert NCH % G == 0
                    pend = {}
                    for _ in bulk(0, pend):
                        pass
                    for gi in range(NCH // G):
                        nxt = {}
                        bg = bulk(gi + 1, nxt) if gi + 1 < NCH // G else None
                        for g in range(G):
                            for _ in scan(gi * G + g, pend, g):
                                for _r in range(2):
                                    if bg is not None and next(bg, _DONE) is _DONE:
                                        bg = None
                        if bg is not None:
                            for _ in bg:
                                pass
                        pend = nxt
                k.barrier()

        def shared_kv():
            with ExitStack() as es:
                hbr = Ring(es, nc, "khb", [128, 8, 512], BF16, 2)
                kr = Ring(es, nc, "kst", [64, 512], BF16, 3)
                vr = Ring(es, nc, "vst", [128, 129], BF16, 3)
                for h in range(NH):
                    wk, wkB = load_w_bf16(es, f"wk{h}", kv_w[:, h * 128:(h + 1) * 128], 128) if h == 0 else (wk_keep, wkB_keep)
                    wv, wvB = load_w_bf16(es, f"wv{h}", kv_w[:, 1024 + h * 128:1024 + (h + 1) * 128], 128) if h == 0 else (wv_keep, wvB_keep)
                    if h == 0:
                        wk_keep, wkB_keep, wv_keep, wvB_keep = wk, wkB, wv, wvB
                    else:
                        k.dma('gpsimd', wk[:], kv_w[:, h * 128:(h + 1) * 128].rearrange("(k p) c -> p k c", p=128), writes=[wkB])
                        k.dma('gpsimd', wv[:], kv_w[:, 1024 + h * 128:1024 + (h + 1) * 128].rearrange("(k p) c -> p k c", p=128), writes=[wvB])
                    for blk in range(NB):
                        hb, hbB = hbr.next()
                        k.dma('sync', hb[:], hT_v[:, :, blk * 512:(blk + 1) * 512], writes=[hbB])
                        for s in range(2):
                            pp, ppB = ps[s], psB[s]
                            for kc in range(8):
                                k.op('tensor', lambda e, pp=pp, kc=kc, s=s, hb=hb: e.matmul(pp[0:64, :], wk[:, kc, s * 64:(s + 1) * 64], hb[:, kc, :],
                                                                                     start=(kc == 0), stop=(kc == 7)), reads=[wkB, hbB], writes=[ppB])
                            kt, ktB = kr.next()
                            k.op('scalar' if s else 'vector', (lambda e, kt=kt, pp=pp: e.copy(kt[:], pp[0:64, :])) if s else
                                 (lambda e, kt=kt, pp=pp: e.tensor_copy(kt[:], pp[0:64, :])), reads=[ppB], writes=[ktB])
                            k.dma('sync', kT_d[h, s, :, blk * 512:(blk + 1) * 512], kt[:], reads=[ktB])
                        for tt in range(4):
                            pp, ppB = ps[2 + tt % 2], psB[2 + tt % 2]
                            for kc in range(8):
                                k.op('tensor', lambda e, pp=pp, kc=kc, tt=tt, hb=hb: e.matmul(pp[:, 0:128], hb[:, kc, tt * 128:(tt + 1) * 128], wv[:, kc, :],
                                                                                       start=(kc == 0), stop=(kc == 7)), reads=[wvB, hbB], writes=[ppB])
                            vt, vtB = vr.next()
                            k.op('vector', lambda e, vt=vt, pp=pp: e.tensor_copy(vt[:, 0:128], pp[:, 0:128]), reads=[ppB], writes=[vtB])
                            k.op('gpsimd', lambda e, vt=vt: e.memset(vt[:, 128:129], 1.0), writes=[vtB])
                            tok0 = blk * 512 + tt * 128
                            k.dma('sync', va_d[h, tok0:tok0 + 128, :], vt[:], reads=[vtB])
                k.barrier()

        def diffattn(j, layer):
            lam_init = lambda_init(layer)
            with ExitStack() as es:
                lp, lpB = bcast_row(es, "lp", b_lambda[j:j + 1].rearrange("o a b -> o (a b)"), 256)
                sw, swB = bcast_row(es, "sw", b_subln_w[j:j + 1, :], 128)
                lam = sbt(es, "lam", [128, 8], F32)
                lamB = Buf("lam")
                pr = sbt(es, "lpr", [128, 128], F32)
                k.op('vector', lambda e: e.tensor_tensor(pr[:, 0:64], lp[:, 0:64], lp[:, 64:128], ALU.mult), reads=[lpB], writes=[lamB])
                k.op('vector', lambda e: e.tensor_tensor(pr[:, 64:128], lp[:, 128:192], lp[:, 192:256], ALU.mult), reads=[lpB], writes=[lamB])
                k.op('vector', lambda e: e.reduce_sum(lam[:, 0:1], pr[:, 0:64], AX.X), reads=[lamB], writes=[lamB])
                k.op('vector', lambda e: e.reduce_sum(lam[:, 1:2], pr[:, 64:128], AX.X), reads=[lamB], writes=[lamB])
                k.op('scalar', lambda e: e.activation(lam[:, 2:4], lam[:, 0:2], AF.Exp), reads=[lamB], writes=[lamB])
                k.op('vector', lambda e: e.tensor_tensor(lam[:, 4:5], lam[:, 2:3], lam[:, 3:4], ALU.subtract), reads=[lamB], writes=[lamB])
                k.op('vector', lambda e: e.tensor_scalar(lam[:, 5:6], lam[:, 4:5], lam_init, -1.0, ALU.add, ALU.mult), reads=[lamB], writes=[lamB])
                k.op('vector', lambda e: e.tensor_scalar_mul(sw[:], sw[:], 1.0 - lam_init), reads=[swB], writes=[swB])
                qs = [sbt(es, f"qs{s}", [64, T], BF16) for s in range(2)]
                qsB = [Buf("qs0"), Buf("qs1")]
                kTs = [sbt(es, f"kTs{s}", [64, T], BF16) for s in range(2)]
                kTB = [Buf("kT0"), Buf("kT1")]
                va = sbt(es, "va", [128, NT, 144], BF16)
                vaB = Buf("va")
                hbr = Ring(es, nc, "ahb", [128, 8, 512], BF16, 2)
                ptr = Ring(es, nc, "pt", [128, 512], BF16, 6)
                r_o1 = Ring(es, nc, "ao1", [128, 128], F32, 2)
                r_st = Ring(es, nc, "ast", [128, 8], F32, 2)
                r_jk = Ring(es, nc, "ajk", [128, 128], F32, 2)
                r_ob = Ring(es, nc, "aob", [128, 128], BF16, 2)
                wq = sbt(es, "wq", [128, 8, 128], BF16)
                wqB = Buf("wq")
                acc = {}
                slots = [(4, 0), (4, 144), (4, 288), (5, 0), (5, 144), (5, 288), (6, 0), (6, 144)]
                for s in range(2):
                    for i in range(4):
                        acc[(s, i)] = slots[s * 4 + i]
                for h in range(NH):
                    k.dma('gpsimd', wq[:], b_w_q[j, :, h * 128:(h + 1) * 128].rearrange("(k p) c -> p k c", p=128), writes=[wqB])
                    for s in range(2):
                        k.dma('sync', kTs[s][:], kT_d[h, s], writes=[kTB[s]])
                    k.dma('sync', va[:, :, 0:129], va_d[h].rearrange("(t p) c -> p t c", p=128), writes=[vaB])
                    for blk in range(NB):
                        hb, hbB = hbr.next()
                        k.dma('sync', hb[:], hT_v[:, :, blk * 512:(blk + 1) * 512], writes=[hbB])
                        for s in range(2):
                            pp, ppB = ps[s], psB[s]
                            for kc in range(8):
                                k.op('tensor', lambda e, pp=pp, kc=kc, s=s, hb=hb: e.matmul(pp[0:64, :], wq[:, kc, s * 64:(s + 1) * 64], hb[:, kc, :],
                                                                                     start=(kc == 0), stop=(kc == 7)), reads=[wqB, hbB], writes=[ppB])
                            k.op('scalar', lambda e, pp=pp, s=s, blk=blk: e.activation(qs[s][:, blk * 512:(blk + 1) * 512], pp[0:64, :], AF.Copy, scale=0.125),
                                 reads=[ppB], writes=[qsB[s]])
                    for qb in range(NB):
                        nkt = 4 * qb + 4
                        steps = [(kt, s) for kt in range(nkt) for s in range(2)]
                        LA = 2

                        def front(idx, qb=qb):
                            kt, s = steps[idx]
                            jd = kt - 4 * qb
                            lo = 0 if jd < 0 else jd * 128
                            n = 512 - lo
                            pp, ppB = ps[idx % 4], psB[idx % 4]
                            k.op('tensor', lambda e, pp=pp, s=s, kt=kt, qb=qb, lo=lo, n=n: e.matmul(pp[:, 0:n], kTs[s][:, kt * 128:(kt + 1) * 128],
                                                                                             qs[s][:, qb * 512 + lo:(qb + 1) * 512], start=True, stop=True),
                                 reads=[kTB[s], qsB[s]], writes=[ppB])
                            pt, ptB = ptr.next()
                            k.op('scalar', lambda e, pt=pt, pp=pp, n=n: e.activation(pt[:, 0:n], pp[:, 0:n], AF.Exp), reads=[ppB], writes=[ptB])
                            if jd >= 0:
                                k.op('vector', lambda e, pt=pt: e.tensor_tensor(pt[:, 0:128], pt[:, 0:128], triub[:], ALU.mult), reads=[ptB, cB], writes=[ptB])
                            return pt, ptB, jd, lo

                        def back(idx, fr, qb=qb):
                            kt, s = steps[idx]
                            pt, ptB, jd, lo = fr
                            for i in range(max(jd, 0), 4):
                                bank, off = acc[(s, i)]
                                c0 = i * 128 - lo
                                st_flag = (kt == 0 and off == 0)
                                k.op('tensor', lambda e, pt=pt, bank=bank, off=off, c0=c0, kt=kt, i=i, qb=qb, st_flag=st_flag: e.matmul(
                                    ps[bank][:, off:off + 129], pt[:, c0:c0 + 128], va[:, kt, 0:129], start=st_flag, stop=(kt == 4 * qb + i),
                                    skip_group_check=True),
                                    reads=[ptB, vaB], writes=[psB[bank]])

                        fronts = {}
                        for idx in range(len(steps) + LA):
                            if idx < len(steps):
                                fronts[idx] = front(idx)
                            if idx - LA >= 0:
                                back(idx - LA, fronts.pop(idx - LA))
                        for i in range(4):
                            b1, f1 = acc[(0, i)]
                            b2, f2 = acc[(1, i)]
                            st, stB = r_st.next()
                            o1, o1B = r_o1.next()
                            jk, jkB = r_jk.next()
                            ob, obB = r_ob.next()
                            k.op('vector', lambda e, st=st, b1=b1, f1=f1: e.reciprocal(st[:, 0:1], ps[b1][:, f1 + 128:f1 + 129]), reads=[psB[b1]], writes=[stB])
                            k.op('vector', lambda e, st=st, b2=b2, f2=f2: e.reciprocal(st[:, 1:2], ps[b2][:, f2 + 128:f2 + 129]), reads=[psB[b2]], writes=[stB])
                            k.op('vector', lambda e, st=st: e.tensor_tensor(st[:, 2:3], st[:, 1:2], lam[:, 5:6], ALU.mult), reads=[stB, lamB], writes=[stB])
                            k.op('vector', lambda e, st=st, o1=o1, b1=b1, f1=f1: e.tensor_scalar_mul(o1[:], ps[b1][:, f1:f1 + 128], st[:, 0:1]),
                                 reads=[psB[b1], stB], writes=[o1B])
                            k.op('vector', lambda e, st=st, o1=o1, b2=b2, f2=f2: e.scalar_tensor_tensor(o1[:], ps[b2][:, f2:f2 + 128], st[:, 2:3], o1[:], ALU.mult, ALU.add),
                                 reads=[psB[b2], stB, o1B], writes=[o1B])
                            k.op('scalar', lambda e, st=st, o1=o1, jk=jk: e.activation(jk[:], o1[:], AF.Square, accum_out=st[:, 3:4]), reads=[o1B], writes=[jkB, stB])
                            k.op('scalar', lambda e, st=st: e.activation(st[:, 4:5], st[:, 3:4], AF.Sqrt, bias=RMS_EPS, scale=1.0 / 128), reads=[stB], writes=[stB])
                            k.op('vector', lambda e, st=st: e.reciprocal(st[:, 5:6], st[:, 4:5]), reads=[stB], writes=[stB])
                            k.op('vector', lambda e, st=st, o1=o1, ob=ob: e.scalar_tensor_tensor(ob[:], o1[:], st[:, 5:6], sw[:], ALU.mult, ALU.mult),
                                 reads=[o1B, stB, swB], writes=[obB])
                            t0 = qb * 512 + i * 128
                            k.dma('sync', om_d[t0:t0 + 128, h * 128:(h + 1) * 128], ob[:], reads=[obB])
                k.barrier()

        def tok_moe(layer, w_out_src, last):
            with ExitStack() as es:
                wo, woB = load_w_bf16(es, "wo", w_out_src, 1024)
                wr = sbt(es, "wr", [128, 8, 40], F32)
                wrB = Buf("wr")
                k.dma('sync', wr[:, :, 0:4], moe_w_group[layer].rearrange("(k p) c -> p k c", p=128), writes=[wrB])
                k.dma('sync', wr[:, :, 4:36], moe_w_expert[layer].rearrange("(k p) c -> p k c", p=128), writes=[wrB])
                g1, g1B = bcast_row(es, "lng1", ln_mix_g[layer:layer + 1, :], D)
                b1, b1B = bcast_row(es, "lnb1", ln_mix_b[layer:layer + 1, :], D)
                lnB1 = Buf("ln1")
                oh = [sbt(es, f"oh{i}", [128, NT, 32], F32) for i in range(2)]
                ohB = Buf("oh")
                gates = sbt(es, "gates", [128, NT, 2], F32)
                gB = Buf("gates")
                rings = {'hTs': Ring(es, nc, "hTs2", [128, 8, 128], BF16, 2), 'st': Ring(es, nc, "lst", [128, 8], F32, 2),
                         'junk': Ring(es, nc, "ljk", [128, D], F32, 1)}
                with ExitStack() as e1:
                    r_om = Ring(e1, nc, "om", [128, D], BF16, 2)
                    r_omT = Ring(e1, nc, "omT", [128, 8, 128], BF16, 2)
                    r_h = Ring(e1, nc, "hh", [128, D], F32, 2)
                    r_t = Ring(e1, nc, "tt", [128, D], F32, 2)
                    r_y = Ring(e1, nc, "yy", [128, D], F32, 2)
                    r_xb = Ring(e1, nc, "xb", [128, D], BF16, 2)
                    r_xT = Ring(e1, nc, "xT", [128, 8, 128], F32, 2)
                    r_rt = Ring(e1, nc, "rt", [128, 160], F32, 2)
                    def tok_body(t):
                        rows = slice(t * 128, (t + 1) * 128)
                        om, omB = r_om.next()
                        k.dma('sync', om[:], om_d[rows, :], writes=[omB])
                        hh, hhB = r_h.next()
                        k.dma('sync', hh[:], h_d[rows, :], writes=[hhB])
                        for kc in range(8):
                            k.op('tensor', lambda e, om=om, kc=kc: e.transpose(pb[:, kc * 128:(kc + 1) * 128], om[:, kc * 128:(kc + 1) * 128], identb[:]),
                                 reads=[omB, cB], writes=[pbB])
                        omT, omTB = r_omT.next()
                        k.op('vector', lambda e, omT=omT: e.tensor_copy(omT[:], pb[:].rearrange("p (a b) -> p a b", a=8)), reads=[pbB], writes=[omTB])
                        yield
                        tt, ttB = r_t.next()
                        for half in range(2):
                            pp, ppB = ps[half], psB[half]
                            for kc in range(8):
                                k.op('tensor', lambda e, pp=pp, kc=kc, half=half, omT=omT: e.matmul(pp[:, :], omT[:, kc, :], wo[:, kc, half * 512:(half + 1) * 512],
                                                                                             start=(kc == 0), stop=(kc == 7)), reads=[omTB, woB], writes=[ppB])
                            k.op('vector', lambda e, pp=pp, half=half, tt=tt, hh=hh: e.scalar_tensor_tensor(tt[:, half * 512:(half + 1) * 512], hh[:, half * 512:(half + 1) * 512],
                                                                                                      ALPHA, pp[:, :], ALU.mult, ALU.add),
                                 reads=[ppB, hhB], writes=[ttB])
                        yield
                        yy, yyB = r_y.next()
                        layer_norm(rings, tt, ttB, g1, b1, [g1B, b1B], yy, yyB)
                        yield
                        k.dma('sync', h_d[rows, :], yy[:], reads=[yyB, hhB])
                        xb, xbB = r_xb.next()
                        k.op('scalar', lambda e, xb=xb, yy=yy: e.copy(xb[:], yy[:]), reads=[yyB], writes=[xbB])
                        k.dma('sync', xb_d[rows, :], xb[:], reads=[xbB])
                        yield
                        xT, xTB = r_xT.next()
                        for half in range(2):
                            pp, ppB = ps[2 + half], psB[2 + half]
                            for jj in range(4):
                                kc = half * 4 + jj
                                k.op('tensor', lambda e, pp=pp, jj=jj, kc=kc, yy=yy: e.transpose(pp[:, jj * 128:(jj + 1) * 128], yy[:, kc * 128:(kc + 1) * 128], ident[:]),
                                     reads=[yyB, cB], writes=[ppB])
                            if half == 0:
                                k.op('vector', lambda e, pp=pp, xT=xT: e.tensor_copy(xT[:, 0:4, :], pp[:].rearrange("p (a b) -> p a b", a=4)), reads=[ppB], writes=[xTB])
                            else:
                                k.op('scalar', lambda e, pp=pp, xT=xT: e.copy(xT[:, 4:8, :], pp[:].rearrange("p (a b) -> p a b", a=4)), reads=[ppB], writes=[xTB])
                        yield
                        p4, p4B = ps[4], psB[4]
                        for kc in range(8):
                            k.op('tensor', lambda e, kc=kc, xT=xT: e.matmul(p4[:, 0:36], xT[:, kc, :], wr[:, kc, 0:36], start=(kc == 0), stop=(kc == 7)),
                                 reads=[xTB, wrB], writes=[p4B])
                        rt, rtB = r_rt.next()
                        V = lambda fn, rt=rt, rtB=rtB, extra_r=(), extra_w=(): k.op('vector', fn, reads=[rtB] + list(extra_r), writes=[rtB] + list(extra_w))
                        k.op('vector', lambda e, rt=rt: e.tensor_copy(rt[:, 0:36], p4[:, 0:36]), reads=[p4B], writes=[rtB])
                        V(lambda e, rt=rt: e.reduce_max(rt[:, 36:37], rt[:, 0:4], AX.X))
                        V(lambda e, rt=rt: e.tensor_scalar_mul(rt[:, 37:38], rt[:, 36:37], -1.0))
                        k.op('scalar', lambda e, rt=rt: e.activation(rt[:, 84:88], rt[:, 0:4], AF.Exp, bias=rt[:, 37:38], accum_out=rt[:, 38:39]), reads=[rtB], writes=[rtB])
                        V(lambda e, rt=rt: e.reciprocal(rt[:, 39:40], rt[:, 38:39]))
                        V(lambda e, rt=rt: e.tensor_scalar(rt[:, 40:44], rt[:, 0:4], rt[:, 36:37], None, ALU.is_ge))
                        V(lambda e, rt=rt: e.tensor_scalar(rt[:, 40:44], rt[:, 40:44], -NEG, NEG, ALU.mult, ALU.add))
                        for g in range(4):
                            V(lambda e, rt=rt, g=g: e.tensor_scalar(rt[:, 44 + g * 8:52 + g * 8], rt[:, 4 + g * 8:12 + g * 8], rt[:, 40 + g:41 + g], None, ALU.add))
                        yield
                        V(lambda e, rt=rt: e.reduce_max(rt[:, 76:77], rt[:, 44:76], AX.X))
                        V(lambda e, rt=rt, t=t: e.tensor_scalar(oh[0][:, t, :], rt[:, 44:76], rt[:, 76:77], None, ALU.is_ge), extra_w=[ohB])
                        V(lambda e, rt=rt, t=t: e.scalar_tensor_tensor(rt[:, 96:128], oh[0][:, t, :], NEG, rt[:, 44:76], ALU.mult, ALU.add), extra_r=[ohB])
                        V(lambda e, rt=rt: e.reduce_max(rt[:, 77:78], rt[:, 96:128], AX.X))
                        V(lambda e, rt=rt, t=t: e.tensor_scalar(oh[1][:, t, :], rt[:, 96:128], rt[:, 77:78], None, ALU.is_ge), extra_w=[ohB])
                        V(lambda e, rt=rt: e.tensor_tensor(rt[:, 78:79], rt[:, 77:78], rt[:, 76:77], ALU.subtract))
                        k.op('scalar', lambda e, rt=rt: e.activation(rt[:, 79:80], rt[:, 78:79], AF.Exp), reads=[rtB], writes=[rtB])
                        V(lambda e, rt=rt: e.tensor_scalar_add(rt[:, 80:81], rt[:, 79:80], 1.0))
                        V(lambda e, rt=rt: e.reciprocal(rt[:, 81:82], rt[:, 80:81]))
                        V(lambda e, rt=rt, t=t: e.tensor_tensor(gates[:, t, 0:1], rt[:, 39:40], rt[:, 81:82], ALU.mult), extra_w=[gB])
                        V(lambda e, rt=rt, t=t: e.tensor_tensor(gates[:, t, 1:2], rt[:, 39:40], gates[:, t, 0:1], ALU.subtract), extra_r=[gB], extra_w=[gB])
                    interleave((tok_body(t) for t in range(NT)), depth=2)
                    k.barrier()
                if checkpoint(noraise=True):
                    return True
                dest = sbt(es, "dest", [128, 2, NT], I32)
                destB = Buf("dest")
                with ExitStack() as e2:
                    selb = sbt(e2, "selb", [128, NT, 32], BF16)
                    cnt = sbt(e2, "cnt", [128, NT, 32], F32)
                    pref = sbt(e2, "pref", [128, NT, 32], F32)
                    slot = sbt(e2, "slot", [128, NT, 32], F32)
                    ebase = sbt(e2, "ebase", [128, 32], F32)
                    tmp = sbt(e2, "ptmp", [128, NT, 32], F32)
                    dfl = sbt(e2, "dfl", [128, 2, NT], F32)
                    pB = Buf("pos")
                    k.op('gpsimd', lambda e: e.iota(ebase[:], [[CAP, 32]], base=0, channel_multiplier=0, allow_small_or_imprecise_dtypes=True), writes=[pB])
                    k.op('vector', lambda e: e.tensor_tensor(selb[:], oh[0][:], oh[1][:], ALU.add), reads=[ohB], writes=[pB])
                    TPB = 16
                    for t0 in range(0, NT, TPB):
                        n = min(TPB, NT - t0)
                        k.op('tensor', lambda e, t0=t0, n=n: e.matmul(ps[0][:, 0:n * 32], ones_b[:], selb[:, t0:t0 + n, :].rearrange("p a b -> p (a b)"), start=True, stop=True),
                             reads=[pB, cB], writes=[psB[0]])
                        k.op('vector', lambda e, t0=t0, n=n: e.tensor_copy(cnt[:, t0:t0 + n, :].rearrange("p a b -> p (a b)"), ps[0][:, 0:n * 32]), reads=[psB[0]], writes=[pB])
                        k.op('tensor', lambda e, t0=t0, n=n: e.matmul(ps[1][:, 0:n * 32], sutri[:], selb[:, t0:t0 + n, :].rearrange("p a b -> p (a b)"), start=True, stop=True),
                             reads=[pB, cB], writes=[psB[1]])
                        k.op('vector', lambda e, t0=t0, n=n: e.tensor_copy(slot[:, t0:t0 + n, :].rearrange("p a b -> p (a b)"), ps[1][:, 0:n * 32]), reads=[psB[1]], writes=[pB])
                    k.op('vector', lambda e: e.memset(pref[:, 0, :], 0.0), reads=[pB], writes=[pB])
                    for t in range(1, NT):
                        k.op('vector', lambda e, t=t: e.tensor_tensor(pref[:, t, :], pref[:, t - 1, :], cnt[:, t - 1, :], ALU.add), reads=[pB], writes=[pB])
                    k.op('vector', lambda e: e.tensor_tensor(slot[:], slot[:], pref[:], ALU.add), reads=[pB], writes=[pB])
                    k.op('vector', lambda e: e.tensor_scalar(tmp[:], slot[:], float(CAP), 1.0e6, ALU.is_ge, ALU.mult), reads=[pB], writes=[pB])
                    k.op('vector', lambda e: e.tensor_tensor(slot[:], slot[:], tmp[:], ALU.add), reads=[pB], writes=[pB])
                    for t in range(NT):
                        k.op('gpsimd', lambda e, t=t: e.tensor_tensor(slot[:, t, :], slot[:, t, :], ebase[:], ALU.add), reads=[pB], writes=[pB])
                    for i in range(2):
                        k.op('vector', lambda e, i=i: e.tensor_tensor(tmp[:], slot[:], oh[i][:], ALU.mult), reads=[pB, ohB], writes=[pB])
                        k.op('vector', lambda e, i=i: e.reduce_sum(dfl[:, i, :], tmp[:], AX.X), reads=[pB], writes=[pB])
                    k.op('vector', lambda e: e.tensor_scalar_min(dfl[:], dfl[:], float(NS)), reads=[pB], writes=[pB])
                    k.op('vector', lambda e: e.tensor_copy(dest[:], dfl[:]), reads=[pB], writes=[destB])
                    r_xb2 = Ring(e2, nc, "xb2", [128, D], BF16, 3)
                    for t in range(NT):
                        xb, xbB = r_xb2.next()
                        k.dma('sync', xb[:], xb_d[t * 128:(t + 1) * 128, :], writes=[xbB])
                        for i in range(2):
                            k.op('gpsimd', lambda e, xb=xb, i=i, t=t: e.indirect_dma_start(
                                out=xs_d, out_offset=bass.IndirectOffsetOnAxis(ap=dest[:, i, t:t + 1], axis=0), in_=xb[:], in_offset=None),
                                reads=[xbB, destB], dma=True)
                    k.barrier()
                with ExitStack() as e3:
                    NG = (CAP + 127) // 128
                    r_w13 = Ring(e3, nc, "w13", [128, 8, 1024], BF16, 2)
                    r_w2 = Ring(e3, nc, "w2", [128, 4, 1024], BF16, 2)
                    r_xs = Ring(e3, nc, "xs", [128, NG, D], BF16, 2)
                    r_xsT = Ring(e3, nc, "xsT", [128, 8, CAP], BF16, 2)
                    r_sg = Ring(e3, nc, "sg", [128, 4, CAP], F32, 1)
                    r_hid = Ring(e3, nc, "hid", [128, 4, CAP], BF16, 2)
                    r_ys = Ring(e3, nc, "ys", [128, D], F32, 2)
                    for ex in range(32):
                        w13, w13B = r_w13.next()
                        k.dma('gpsimd', w13[:], moe_w13[layer, ex].rearrange("(k p) c -> p k c", p=128), writes=[w13B])
                        w2, w2B = r_w2.next()
                        k.dma('gpsimd', w2[:], moe_w2[layer, ex].rearrange("(k p) c -> p k c", p=128), writes=[w2B])
                        xs, xsB = r_xs.next()
                        xsT, xsTB = r_xsT.next()
                        for g in range(NG):
                            r0 = ex * CAP + g * 128
                            nr = min(128, CAP - g * 128)
                            k.dma('sync', xs[0:nr, g, :], xs_d[r0:r0 + nr, :], writes=[xsB])
                        for g in range(NG):
                            nr = min(128, CAP - g * 128)
                            for kc in range(8):
                                k.op('tensor', lambda e, xs=xs, g=g, kc=kc, nr=nr: e.transpose(pb[:, kc * 128:kc * 128 + nr], xs[0:nr, g, kc * 128:(kc + 1) * 128], identb[0:nr, 0:nr]),
                                     reads=[xsB, cB], writes=[pbB])
                            k.op('vector', lambda e, xsT=xsT, g=g, nr=nr: e.tensor_copy(xsT[:, :, g * 128:g * 128 + nr], pb[:].rearrange("p (a b) -> p a b", a=8)[:, :, 0:nr]),
                                 reads=[pbB], writes=[xsTB])
                        sg, sgB = r_sg.next()
                        hid, hidB = r_hid.next()
                        for n0 in range(0, CAP, 512):
                            n = min(512, CAP - n0)
                            for m in range(8):
                                pp, ppB = ps[m % 4], psB[m % 4]
                                for kc in range(8):
                                    k.op('tensor', lambda e, pp=pp, kc=kc, m=m, w13=w13, xsT=xsT, n0=n0, n=n: e.matmul(pp[:, 0:n], w13[:, kc, m * 128:(m + 1) * 128], xsT[:, kc, n0:n0 + n],
                                                                                                           start=(kc == 0), stop=(kc == 7)),
                                         reads=[w13B, xsTB], writes=[ppB])
                                if m < 4:
                                    k.op('scalar', lambda e, pp=pp, m=m, sg=sg, n0=n0, n=n: e.activation(sg[:, m, n0:n0 + n], pp[:, 0:n], AF.Silu), reads=[ppB], writes=[sgB])
                                else:
                                    k.op('vector', lambda e, pp=pp, m=m, sg=sg, hid=hid, n0=n0, n=n: e.tensor_tensor(hid[:, m - 4, n0:n0 + n], sg[:, m - 4, n0:n0 + n], pp[:, 0:n], ALU.mult),
                                         reads=[ppB, sgB], writes=[hidB])
                        for g in range(NG):
                            nr = min(128, CAP - g * 128)
                            ys, ysB = r_ys.next()
                            for half in range(2):
                                pp, ppB = ps[4 + half], psB[4 + half]
                                for f in range(4):
                                    k.op('tensor', lambda e, pp=pp, f=f, half=half, hid=hid, w2=w2, g=g, nr=nr: e.matmul(pp[0:nr, :], hid[:, f, g * 128:g * 128 + nr], w2[:, f, half * 512:(half + 1) * 512],
                                                                                                             start=(f == 0), stop=(f == 3)),
                                         reads=[hidB, w2B], writes=[ppB])
                                if half == 0:
                                    k.op('vector', lambda e, pp=pp, ys=ys, nr=nr: e.tensor_copy(ys[0:nr, 0:512], pp[0:nr, :]), reads=[ppB], writes=[ysB])
                                else:
                                    k.op('scalar', lambda e, pp=pp, ys=ys, nr=nr: e.copy(ys[0:nr, 512:1024], pp[0:nr, :]), reads=[ppB], writes=[ysB])
                            r0 = ex * CAP + g * 128
                            for hf in range(2):
                                k.dma('sync', ys_h[hf][r0:r0 + nr, :], ys[0:nr, hf * 512:(hf + 1) * 512], reads=[ysB])
                    k.barrier()
                with ExitStack() as e4:
                    g2, g2B = bcast_row(e4, "lng2", ln_ffn_g[layer:layer + 1, :], D)
                    b2, b2B = bcast_row(e4, "lnb2", ln_ffn_b[layer:layer + 1, :], D)
                    r_yq = [Ring(e4, nc, f"yq{q}", [128, 512], F32, 2) for q in range(4)]
                    r_h = Ring(e4, nc, "ch", [128, D], F32, 2)
                    r_o = Ring(e4, nc, "co", [128, D], F32, 2)
                    def comb_body(t):
                        rows = slice(t * 128, (t + 1) * 128)
                        hh, hhB = r_h.next()
                        k.dma('sync', hh[:], h_d[rows, :], writes=[hhB])
                        k.op('scalar', lambda e, hh=hh: e.mul(hh[:], hh[:], ALPHA), reads=[hhB], writes=[hhB])
                        for i in range(2):
                            for hf in range(2):
                                yq, yqB = r_yq[i * 2 + hf].next()
                                k.op('gpsimd', lambda e, yq=yq, i=i, t=t, hf=hf: e.indirect_dma_start(
                                    out=yq[:], out_offset=None, in_=ys_h[hf],
                                    in_offset=bass.IndirectOffsetOnAxis(ap=dest[:, i, t:t + 1], axis=0)), reads=[destB], writes=[yqB], dma=True)
                                k.op('vector', lambda e, hh=hh, yq=yq, t=t, i=i, hf=hf: e.scalar_tensor_tensor(
                                    hh[:, hf * 512:(hf + 1) * 512], yq[:], gates[:, t, i:i + 1], hh[:, hf * 512:(hf + 1) * 512], ALU.mult, ALU.add),
                                    reads=[hhB, yqB, gB], writes=[hhB])
                        yield
                        oo, ooB = r_o.next()
                        layer_norm(rings, hh, hhB, g2, b2, [g2B, b2B], oo, ooB)
                        yield
                        if last:
                            out_toks.append(k.dma('sync', out[rows, :], oo[:], reads=[ooB]))
                        else:
                            k.dma('sync', h_d[rows, :], oo[:], reads=[ooB, hhB])
                            make_hT(rings, oo, ooB, t)
                    interleave((comb_body(t) for t in range(NT)), depth=2)
                    k.barrier()

        try:
            checkpoint()
            for layer in range(4):
                if layer < 2:
                    deltanet(layer)
                    checkpoint()
                    if tok_moe(layer, a_w_out[layer], last=False):
                        raise _Stop()
                    checkpoint()
                else:
                    j = layer - 2
                    if j == 0:
                        shared_kv()
                    diffattn(j, layer)
                    checkpoint()
                    if tok_moe(layer, b_w_out[j], last=(layer == 3)):
                        raise _Stop()
                    checkpoint()
        except _Stop:
            out_toks.append(k.dma('sync', out, h_d))
            out_toks.append(k.dma('sync', dbg, om_d))
        k.wait_all('sync', out_toks)
        k.finish()
    return nc, k


SEQ = 8192
CAP_FULL = 640
_cache = {}


def kernel(**inputs):
    x = np.asarray(inputs['x'])
    B, T, _ = x.shape
    cap = CAP_FULL if T == SEQ else max(64, int(T / 16 + 6 * math.sqrt(T / 16) + 16) // 32 * 32 + 32)
    key = (T, cap)
    if key not in _cache:
        _cache[key] = build(T, cap)[0]
    nc = _cache[key]
    shared = {n: np.ascontiguousarray(np.asarray(v, dtype=np.float32)) for n, v in inputs.items() if n != 'x'}
    in_maps = []
    for b in range(B):
        m = dict(shared)
        m['x'] = np.ascontiguousarray(x[b])
        in_maps.append(m)
    res = run_bass_kernel_spmd(nc, in_maps, core_ids=list(range(B)))
    return np.stack([np.asarray(res.results[b]['out']) for b in range(B)], axis=0).astype(np.float32)
```

```python
import math
from contextlib import ExitStack
import numpy as np
import concourse.bass as bass
import concourse.mybir as mybir
from concourse.bass_utils import run_bass_kernel_spmd

F32 = mybir.dt.float32
BF16 = mybir.dt.bfloat16
I32 = mybir.dt.int32
AF = mybir.ActivationFunctionType
ALU = mybir.AluOpType
AX = mybir.AxisListType

ENGS = ['tensor', 'vector', 'scalar', 'gpsimd', 'sync']
SEM_EPOCH = 30000
N_DMA_SEMS = 16


class Buf:
    __slots__ = ('name', 'w', 'r', 'excl')

    def __init__(self, name='', excl=False):
        self.name = name
        self.excl = excl
        self.w = None
        self.r = {}


class _Op:
    __slots__ = ('fn', 'waits', 'inc', 'incval', 'dma')

    def __init__(self, fn, waits, dma):
        self.fn = fn
        self.waits = waits
        self.inc = False
        self.incval = 0
        self.dma = dma


class K:
    def __init__(self, nc):
        self.nc = nc
        self.ops = {e: [] for e in ENGS}
        self.waited = {e: {} for e in ENGS}
        self.dma_rr = {e: 0 for e in ENGS}
        self.dma_cnt = {}

    def _need_wait(self, eng, t):
        key = (t[0], t[1])
        if self.waited[eng].get(key, -1) >= t[2]:
            return False
        self.waited[eng][key] = t[2]
        if t[0] == 'e':
            self.ops[t[1]][t[2]].inc = True
        return True

    def op(self, eng, fn, reads=(), writes=(), dma=False):
        idx = len(self.ops[eng])
        writes = list(writes) + [b for b in reads if b.excl]
        reads = [b for b in reads if not b.excl]
        deps = []
        for b in reads:
            if b.w is not None:
                deps.append(b.w)
        for b in writes:
            if b.w is not None:
                deps.append(b.w)
            deps.extend(b.r.values())
        waits = []
        for t in deps:
            if t[0] == 'e' and t[1] == eng and eng == 'tensor':
                continue
            if self._need_wait(eng, t):
                waits.append(t)
        dm = None
        if dma:
            slot = self.dma_rr[eng]
            self.dma_rr[eng] = (slot + 1) % N_DMA_SEMS
            cnt = self.dma_cnt.get((eng, slot), 0) + 1
            self.dma_cnt[(eng, slot)] = cnt
            dm = ((eng, slot), cnt * 16)
            if cnt > 1:
                t = ('d', (eng, slot), (cnt - 1) * 16)
                if self._need_wait(eng, t):
                    waits.append(t)
            tok = ('d', (eng, slot), cnt * 16)
        else:
            tok = ('e', eng, idx)
        self.ops[eng].append(_Op(fn, waits, dm))
        kk = (tok[0], tok[1])
        for b in reads:
            b.r[kk] = tok
        for b in writes:
            b.w = tok
            b.r = {}
        return tok

    def dma(self, eng, out, in_, reads=(), writes=(), **kw):
        return self.op(eng, lambda e: e.dma_start(out=out, in_=in_, **kw), reads=reads, writes=writes, dma=True)

    def wait_all(self, eng, tokens):
        waits = [t for t in tokens if self._need_wait(eng, t)]
        if waits:
            self.ops[eng].append(_Op(None, waits, None))

    def barrier(self):
        toks = []
        for e in ENGS:
            for i in range(len(self.ops[e]) - 1, -1, -1):
                o = self.ops[e][i]
                if o.fn is not None and o.dma is None:
                    toks.append(('e', e, i))
                    break
        for key, cnt in self.dma_cnt.items():
            toks.append(('d', key, cnt * 16))
        for e in ENGS:
            self.wait_all(e, toks)
        self.flush()

    def flush(self):
        nc = self.nc
        if not hasattr(self, 'flushed'):
            self.flushed = {e: 0 for e in ENGS}
            self.inccnt = {e: 0 for e in ENGS}
            self.esems = {e: [] for e in ENGS}
            self.dsems = {}
        start = dict(self.flushed)
        for e in ENGS:
            for o in self.ops[e][start[e]:]:
                if o.inc:
                    self.inccnt[e] += 1
                    o.incval = self.inccnt[e]
            need = (self.inccnt[e] + SEM_EPOCH - 1) // SEM_EPOCH
            while len(self.esems[e]) < max(need, 1):
                self.esems[e].append(nc.alloc_semaphore(f"es_{e}_{len(self.esems[e])}"))
        for key in self.dma_cnt:
            if key not in self.dsems:
                self.dsems[key] = nc.alloc_semaphore(f"ds_{key[0]}_{key[1]}")
        esems, dsems, ops = self.esems, self.dsems, self.ops

        def semval(e2, incval):
            return esems[e2][(incval - 1) // SEM_EPOCH], (incval - 1) % SEM_EPOCH + 1

        with nc.Block() as block:
            for eng in ENGS:
                todo = ops[eng][start[eng]:]

                def body(e, eng=eng, todo=todo):
                    for o in todo:
                        for t in o.waits:
                            if t[0] == 'e':
                                p = ops[t[1]][t[2]]
                                assert p.incval > 0, (eng, t)
                                s, v = semval(t[1], p.incval)
                                e.wait_ge(s, v)
                            else:
                                e.wait_ge(dsems[t[1]], t[2])
                        if o.fn is None:
                            continue
                        ins = o.fn(e)
                        if o.dma is not None:
                            ins.then_inc(dsems[o.dma[0]], 16)
                        elif o.inc:
                            s, v = semval(eng, o.incval)
                            ins.then_inc(s, 1)
                if todo:
                    getattr(block, eng)(body)
                self.flushed[eng] = len(ops[eng])

    def finish(self):
        self.flush()


_uid = [0]


def _un(name):
    _uid[0] += 1
    return f"{name}_u{_uid[0]}"


def interleave(gens, depth=2):
    active = []
    it = iter(gens)
    while True:
        while len(active) < depth:
            g = next(it, None)
            if g is None:
                break
            active.append(g)
        if not active:
            break
        for g in list(active):
            try:
                next(g)
            except StopIteration:
                active.remove(g)


_DONE = object()


class Ring:
    def __init__(self, es, nc, name, shape, dt, n):
        self.t = [es.enter_context(nc.sbuf_tensor(_un(f"{name}_{i}"), shape, dt)) for i in range(n)]
        self.b = [Buf(f"{name}_{i}") for i in range(n)]
        self.i = 0

    def next(self):
        i = self.i
        self.i = (i + 1) % len(self.t)
        return self.t[i], self.b[i]


D = 1024
NH = 8
ALPHA = 8 ** 0.25
LN_EPS = 1e-5
RMS_EPS = 1e-6
NEG = -1.0e30


def lambda_init(layer_idx):
    return 0.8 - 0.6 * math.exp(-0.3 * layer_idx)


class _Stop(Exception):
    pass


def build(T, CAP, stop=0, ksub=0, dumpflag=False):
    NT = T // 128
    NCH = T // 64
    NB = T // 512
    nc = bass.Bass("TRN2", target_bir_lowering=False)

    def din(name, shape, dt=F32):
        return nc.dram_tensor(name, shape, dt, kind="ExternalInput").ap()

    x = din("x", [T, D])
    a_w_in = din("a_w_in", [2, D, 4112])
    a_conv_w = din("a_conv_w", [2, 4, 3072])
    a_a_log = din("a_a_log", [2, 8])
    a_dt_bias = din("a_dt_bias", [2, 8])
    a_norm_w = din("a_norm_w", [2, 128])
    a_w_out = din("a_w_out", [2, D, D])
    kv_w = din("kv_w", [D, 2048])
    b_w_q = din("b_w_q", [2, D, D])
    b_lambda = din("b_lambda", [2, 4, 64])
    b_subln_w = din("b_subln_w", [2, 128])
    b_w_out = din("b_w_out", [2, D, D])
    ln_mix_g = din("ln_mix_g", [4, D])
    ln_mix_b = din("ln_mix_b", [4, D])
    ln_ffn_g = din("ln_ffn_g", [4, D])
    ln_ffn_b = din("ln_ffn_b", [4, D])
    moe_w_group = din("moe_w_group", [4, D, 4])
    moe_w_expert = din("moe_w_expert", [4, D, 32])
    moe_w13 = din("moe_w13", [4, 32, D, 1024])
    moe_w2 = din("moe_w2", [4, 32, 512, D])
    out = nc.dram_tensor("out", [T, D], F32, kind="ExternalOutput").ap()
    dbg = nc.dram_tensor("dbg", [T, D], BF16, kind="ExternalOutput").ap() if stop else None
    stage = [0]

    def checkpoint(noraise=False):
        stage[0] += 1
        if stop and stage[0] >= stop:
            if noraise:
                return True
            raise _Stop()
        return False

    h_d = nc.dram_tensor("h_d", [T, D], F32).ap()
    hT_d = nc.dram_tensor("hT_d", [D, T], BF16).ap()
    om_d = nc.dram_tensor("om_d", [T, D], BF16).ap()
    xb_d = nc.dram_tensor("xb_d", [T, D], BF16).ap()
    kT_d = nc.dram_tensor("kT_d", [NH, 2, 64, T], BF16).ap()
    va_d = nc.dram_tensor("va_d", [NH, T, 129], BF16).ap()
    NS = 32 * CAP
    xs_d = nc.dram_tensor("xs_d", [NS + 128, D], BF16).ap()
    ys_h = [nc.dram_tensor(f"ys_d{i}", [NS + 128, 512], F32).ap() for i in range(2)]
    hT_v = hT_d.rearrange("(k p) t -> p k t", p=128)

    k = K(nc)
    out_toks = []
    dumped = set()

    def dump(name, ap, B):
        if not dumpflag or name in dumped:
            return
        dumped.add(name)
        t = nc.dram_tensor("dump_" + name, list(ap.shape), F32, kind="ExternalOutput").ap()
        out_toks.append(k.dma('gpsimd', t, ap, reads=[B]))
    with ExitStack() as top:
        def sbt(es, name, shape, dt):
            return es.enter_context(nc.sbuf_tensor(_un(name), shape, dt))

        ps = [top.enter_context(nc.psum_tensor(f"ps{i}", [128, 512], F32)) for i in range(7)]
        psB = [Buf(f"ps{i}", excl=True) for i in range(7)]
        pb = top.enter_context(nc.psum_tensor("pb", [128, 1024], BF16))
        pbB = Buf("pb", excl=True)

        ident = sbt(top, "ident", [128, 128], F32)
        identb = sbt(top, "identb", [128, 128], BF16)
        ones_f = sbt(top, "ones_f", [128, 128], F32)
        ones_b = sbt(top, "ones_b", [128, 128], BF16)
        triu = sbt(top, "triu", [128, 128], F32)
        triub = sbt(top, "triub", [128, 128], BF16)
        sutri = sbt(top, "sutri", [128, 128], BF16)
        zero_b = sbt(top, "zero_b", [128, 1024], BF16)
        triu4 = sbt(top, "triu4", [64, 4, 64], F32)
        ident4 = sbt(top, "ident4", [64, 4, 64], F32)
        cB = Buf("consts")
        k.op('gpsimd', lambda e: e.iota(ones_f[:], [[1, 128]], base=0, channel_multiplier=-1,
                                        allow_small_or_imprecise_dtypes=True), writes=[cB])
        k.op('vector', lambda e: e.tensor_single_scalar(ident[:], ones_f[:], 0.0, ALU.is_equal), reads=[cB], writes=[cB])
        k.op('vector', lambda e: e.tensor_single_scalar(identb[:], ones_f[:], 0.0, ALU.is_equal), reads=[cB], writes=[cB])
        k.op('vector', lambda e: e.tensor_single_scalar(triu[:], ones_f[:], 0.0, ALU.is_ge), reads=[cB], writes=[cB])
        k.op('vector', lambda e: e.tensor_single_scalar(triub[:], ones_f[:], 0.0, ALU.is_ge), reads=[cB], writes=[cB])
        k.op('vector', lambda e: e.tensor_single_scalar(sutri[:], ones_f[:], 0.0, ALU.is_gt), reads=[cB], writes=[cB])
        for g4 in range(4):
            k.op('vector', lambda e, g4=g4: e.tensor_copy(triu4[:, g4, :], triu[0:64, 0:64]), reads=[cB], writes=[cB])
            k.op('vector', lambda e, g4=g4: e.tensor_copy(ident4[:, g4, :], ident[0:64, 0:64]), reads=[cB], writes=[cB])
        k.op('vector', lambda e: e.memset(ones_f[:], 1.0), reads=[cB], writes=[cB])
        k.op('vector', lambda e: e.memset(ones_b[:], 1.0), writes=[cB])
        k.op('vector', lambda e: e.memset(zero_b[:], 0.0), writes=[cB])
        zero_f = sbt(top, "zero_f", [128, 512], F32)
        k.op('vector', lambda e: e.memset(zero_f[:], 0.0), writes=[cB])
        for r0 in range(0, NS + 128, 128):
            k.dma('sync', xs_d[r0:r0 + 128, :], zero_b[:], reads=[cB])
        for hf in range(2):
            k.dma('sync', ys_h[hf][NS:NS + 128, :], zero_f[:], reads=[cB])
        k.barrier()

        def layer_norm(es_ring, t, tB, gbc, bbc, wBs, y, yB):
            st, stB = es_ring['st'].next()
            jk, jkB = es_ring['junk'].next()
            k.op('scalar', lambda e: e.activation(jk[:], t[:], AF.Copy, accum_out=st[:, 0:1]), reads=[tB], writes=[jkB, stB])
            k.op('scalar', lambda e: e.activation(jk[:], t[:], AF.Square, accum_out=st[:, 1:2]), reads=[tB], writes=[jkB, stB])
            k.op('vector', lambda e: e.tensor_scalar_mul(st[:, 2:3], st[:, 0:1], 1.0 / D), reads=[stB], writes=[stB])
            k.op('vector', lambda e: e.tensor_tensor(st[:, 3:4], st[:, 2:3], st[:, 2:3], ALU.mult), reads=[stB], writes=[stB])
            k.op('vector', lambda e: e.scalar_tensor_tensor(st[:, 4:5], st[:, 1:2], 1.0 / D, st[:, 3:4], ALU.mult, ALU.subtract),
                 reads=[stB], writes=[stB])
            k.op('scalar', lambda e: e.activation(st[:, 5:6], st[:, 4:5], AF.Sqrt, bias=LN_EPS), reads=[stB], writes=[stB])
            k.op('vector', lambda e: e.reciprocal(st[:, 6:7], st[:, 5:6]), reads=[stB], writes=[stB])
            k.op('vector', lambda e: e.tensor_scalar(t[:], t[:], st[:, 2:3], st[:, 6:7], ALU.subtract, ALU.mult), reads=[tB, stB], writes=[tB])
            k.op('gpsimd', lambda e: e.tensor_tensor(t[:], t[:], gbc[:], ALU.mult), reads=[tB] + wBs, writes=[tB])
            k.op('vector', lambda e: e.tensor_tensor(y[:], t[:], bbc[:], ALU.add), reads=[tB] + wBs, writes=[yB])

        def make_hT(rings, y, yB, tile):
            hb, hbB = rings['hTs'].next()
            for half in range(2):
                pp, ppB = ps[5 + half], psB[5 + half]
                for j in range(4):
                    kk = half * 4 + j
                    k.op('tensor', lambda e, pp=pp, j=j, kk=kk: e.transpose(pp[:, j * 128:(j + 1) * 128], y[:, kk * 128:(kk + 1) * 128], ident[:]),
                         reads=[yB, cB], writes=[ppB])
                eng = 'vector' if half == 0 else 'scalar'
                if eng == 'vector':
                    k.op('vector', lambda e, pp=pp, half=half: e.tensor_copy(hb[:, half * 4:(half + 1) * 4, :], pp[:].rearrange("p (a b) -> p a b", a=4)),
                         reads=[ppB], writes=[hbB])
                else:
                    k.op('scalar', lambda e, pp=pp, half=half: e.copy(hb[:, half * 4:(half + 1) * 4, :], pp[:].rearrange("p (a b) -> p a b", a=4)),
                         reads=[ppB], writes=[hbB])
            k.dma('sync', hT_v[:, :, tile * 128:(tile + 1) * 128], hb[:], reads=[hbB])

        with ExitStack() as es:
            rings = {'hTs': Ring(es, nc, "hTs", [128, 8, 128], BF16, 2)}
            xr = Ring(es, nc, "xin", [128, D], F32, 2)
            for t in range(NT):
                xt, xB = xr.next()
                k.dma('sync', xt[:], x[t * 128:(t + 1) * 128, :], writes=[xB])
                k.dma('gpsimd', h_d[t * 128:(t + 1) * 128, :], xt[:], reads=[xB])
                make_hT(rings, xt, xB, t)
            k.barrier()

        def load_w_bf16(es, name, src, cols):
            w = sbt(es, name, [128, 8, cols], BF16)
            wB = Buf(name)
            k.dma('gpsimd', w[:], src.rearrange("(k p) c -> p k c", p=128), writes=[wB])
            return w, wB

        def bcast_row(es, name, src_row, n, dt=F32):
            w = sbt(es, name, [128, n], dt)
            wB = Buf(name)
            k.dma('sync', w[:], src_row.partition_broadcast(128), writes=[wB])
            return w, wB

        def deltanet(l):
            with ExitStack() as es:
                cw = sbt(es, "cw", [128, 24, 4], F32)
                cwB = Buf("cw")
                cwn = sbt(es, "cwn", [4, 3072], F32)
                k.dma('sync', cwn[:], a_conv_w[l], writes=[cwB])
                for part in range(24):
                    k.op('tensor', lambda e, part=part: e.transpose(ps[0][:, part * 4:(part + 1) * 4], cwn[:, part * 128:(part + 1) * 128], ident[0:4, 0:4]),
                         reads=[cwB, cB], writes=[psB[0]])
                k.op('vector', lambda e: e.tensor_copy(cw[:].rearrange("p a b -> p (a b)"), ps[0][:, 0:96]), reads=[psB[0]], writes=[cwB])
                nw, nwB = bcast_row(es, "nw", a_norm_w[l:l + 1, :], 128)
                alog, alB = bcast_row(es, "alog", a_a_log[l:l + 1, :], 8)
                dtb, dtB = bcast_row(es, "dtb", a_dt_bias[l:l + 1, :], 8)
                nea = sbt(es, "nea", [128, 8], F32)
                k.op('scalar', lambda e: e.activation(nea[:], alog[:], AF.Exp), reads=[alB], writes=[alB])
                k.op('vector', lambda e: e.tensor_scalar_mul(nea[:], nea[:], -1.0), reads=[alB], writes=[alB])
                qT = sbt(es, "qT", [128, T], BF16)
                kT = sbt(es, "kT", [128, T], BF16)
                vT = sbt(es, "vT", [128, T], BF16)
                qkvB = [Buf("qT"), Buf("kT"), Buf("vT")]
                qkv = [qT, kT, vT]
                zT = sbt(es, "zT", [128, T], BF16)
                zTB = Buf("zT")
                glT = sbt(es, "glT", [64, NCH, 16], F32)
                glTB = Buf("glT")
                wg16 = sbt(es, "wg16", [128, 8, 16], BF16)
                wgB = Buf("wg16")
                gl = sbt(es, "gl", [64, 8, NCH], F32)
                glB = Buf("gl")
                egl = sbt(es, "egl", [128, NCH], F32)
                eglB = Buf("egl")
                S = sbt(es, "S", [128, 128], F32)
                SB = Buf("S")
                hbr = Ring(es, nc, "hblk", [128, 8, 512], BF16, 2)
                raw = sbt(es, "raw", [128, 3, 515], F32)
                rawB = [Buf("raw0"), Buf("raw1"), Buf("raw2")]
                cvr = Ring(es, nc, "cv", [128, 512], F32, 2)
                sqr = Ring(es, nc, "sq", [128, 512], F32, 2)
                G = 4
                R = 2
                r_kg = Ring(es, nc, "kg", [64, G, 256], F32, R)
                r_kdec = Ring(es, nc, "kdec", [64, G, 128], F32, R)
                r_dm = Ring(es, nc, "dm", [64, G, 64], F32, R)
                r_dg = Ring(es, nc, "dg", [64, G, 64], F32, R)
                r_egb = Ring(es, nc, "egb", [128, G, 64], F32, R)
                r_qg = Ring(es, nc, "qg", [128, G, 64], F32, R)
                r_at = Ring(es, nc, "at", [64, G, 64], F32, R)
                r_X = Ring(es, nc, "X", [64, G, 64], BF16, 4)
                r_Y = Ring(es, nc, "Y", [64, G, 64], BF16, 4)
                r_Pb = Ring(es, nc, "Pb", [64, G, 64], BF16, 2)
                r_zs4 = Ring(es, nc, "zs4", [64, G, 128], BF16, R)
                r_P = Ring(es, nc, "P", [64, G, 64], F32, R)
                r_uw = Ring(es, nc, "uw", [64, G, 256], F32, R)
                r_AT = Ring(es, nc, "AT", [128, G, 128], F32, R)
                r_Bc = Ring(es, nc, "Bc", [128, G, 128], F32, R)
                r_QpT = Ring(es, nc, "QpT", [128, G, 64], F32, R)
                r_O0 = Ring(es, nc, "O0", [64, G, 128], F32, R)
                r_vn = Ring(es, nc, "vn", [64, 128], F32, 2)
                r_o = Ring(es, nc, "o", [64, 128], F32, 2)
                r_ob = Ring(es, nc, "ob", [64, 128], BF16, 2)
                r_st = Ring(es, nc, "dst", [64, 4], F32, 2)
                r_jk = Ring(es, nc, "djk", [64, 128], F32, 2)
                k.dma('gpsimd', wg16[:], a_w_in[l, :, 4096:4112].rearrange("(k p) c -> p k c", p=128), writes=[wgB])
                glFr = Ring(es, nc, "glF", [16, 512], F32, 2)
                for blk in range(NB):
                    hb, hbB = hbr.next()
                    k.dma('sync', hb[:], hT_v[:, :, blk * 512:(blk + 1) * 512], writes=[hbB])
                    for kc in range(8):
                        k.op('tensor', lambda e, kc=kc, hb=hb: e.matmul(ps[3][0:16, :], wg16[:, kc, :], hb[:, kc, :], start=(kc == 0), stop=(kc == 7)),
                             reads=[wgB, hbB], writes=[psB[3]])
                    glF, glFB = glFr.next()
                    k.op('scalar', lambda e, glF=glF: e.copy(glF[:], ps[3][0:16, :]), reads=[psB[3]], writes=[glFB])
                    for cc in range(8):
                        k.op('tensor', lambda e, cc=cc, glF=glF: e.transpose(ps[4][0:64, cc * 16:(cc + 1) * 16], glF[:, cc * 64:(cc + 1) * 64], ident[0:16, 0:16]),
                             reads=[glFB, cB], writes=[psB[4]])
                    k.op('vector', lambda e, blk=blk: e.tensor_copy(glT[:, blk * 8:(blk + 1) * 8, :].rearrange("p a b -> p (a b)"), ps[4][0:64, 0:128]),
                         reads=[psB[4]], writes=[glTB])
                for h in range(NH):
                    wq3, wq3B = [], []
                    wcat = sbt(es, f"wcat{h}", [128, 8, 512], BF16) if h == 0 else wcat_keep[0]
                    if h == 0:
                        wcat_keep = [wcat]
                        wcB = Buf("wcat")
                    for part in range(3):
                        k.dma('gpsimd', wcat[:, :, part * 128:(part + 1) * 128],
                              a_w_in[l, :, part * 1024 + h * 128: part * 1024 + (h + 1) * 128].rearrange("(k p) c -> p k c", p=128), writes=[wcB])
                    k.dma('gpsimd', wcat[:, :, 384:512], a_w_in[l, :, 3072 + h * 128:3072 + (h + 1) * 128].rearrange("(k p) c -> p k c", p=128), writes=[wcB])
                    for part in range(3):
                        k.op('vector', lambda e, part=part: e.memset(raw[:, part, 0:3], 0.0), writes=[rawB[part]])
                    if ksub == 5:
                        k.barrier()
                        return
                    for blk in range(NB):
                        hb, hbB = hbr.next()
                        k.dma('sync', hb[:], hT_v[:, :, blk * 512:(blk + 1) * 512], writes=[hbB])
                        for part in range(3):
                            pp, ppB = ps[part % 2], psB[part % 2]
                            for kc in range(8):
                                k.op('tensor', lambda e, pp=pp, kc=kc, part=part, hb=hb: e.matmul(pp[:, :], wcat[:, kc, part * 128:(part + 1) * 128], hb[:, kc, :],
                                                                                            start=(kc == 0), stop=(kc == 7)),
                                     reads=[wcB, hbB], writes=[ppB])
                            k.op('scalar', lambda e, pp=pp, part=part: e.copy(raw[:, part, 3:515], pp[:, :]), reads=[ppB], writes=[rawB[part]])
                            if ksub == 11:
                                k.barrier()
                                return
                            cv, cvB = cvr.next()
                            ci = part * 8 + h
                            k.op('vector', lambda e, cv=cv, part=part, ci=ci: e.tensor_scalar_mul(cv[:], raw[:, part, 0:512], cw[:, ci, 0:1]),
                                 reads=[rawB[part], cwB], writes=[cvB])
                            for j in range(1, 4):
                                k.op('vector', lambda e, cv=cv, part=part, ci=ci, j=j: e.scalar_tensor_tensor(cv[:], raw[:, part, j:j + 512], cw[:, ci, j:j + 1], cv[:],
                                                                                                        ALU.mult, ALU.add),
                                     reads=[rawB[part], cwB, cvB], writes=[cvB])
                            k.op('vector', lambda e, part=part: e.tensor_copy(raw[:, part, 0:3], raw[:, part, 512:515]), reads=[rawB[part]], writes=[rawB[part]])
                            if ksub == 12:
                                k.barrier()
                                return
                            dst = qkv[part][:, blk * 512:(blk + 1) * 512]
                            if part == 2:
                                k.op('scalar', lambda e, cv=cv, dst=dst: e.activation(dst, cv[:], AF.Silu), reads=[cvB], writes=[qkvB[part]])
                            else:
                                k.op('scalar', lambda e, cv=cv: e.activation(cv[:], cv[:], AF.Silu), reads=[cvB], writes=[cvB])
                                sq, sqB = sqr.next()
                                k.op('gpsimd', lambda e, cv=cv, sq=sq: e.tensor_tensor(sq[:], cv[:], cv[:], ALU.mult), reads=[cvB], writes=[sqB])
                                p2, p2B = ps[2], psB[2]
                                for hf in range(2):
                                    k.op('tensor', lambda e, sq=sq, p2=p2, hf=hf: e.matmul(p2[:, hf * 256:(hf + 1) * 256], ones_f[:], sq[:, hf * 256:(hf + 1) * 256], start=True, stop=True), reads=[sqB, cB], writes=[p2B])
                                k.op('scalar', lambda e, sq=sq, p2=p2: e.activation(sq[:], p2[:, :], AF.Sqrt, bias=RMS_EPS), reads=[p2B], writes=[sqB])
                                k.op('vector', lambda e, sq=sq: e.reciprocal(sq[:], sq[:]), reads=[sqB], writes=[sqB])
                                sc = (128 ** -0.5) if part == 0 else 1.0
                                k.op('vector', lambda e, cv=cv, sq=sq, dst=dst, sc=sc: e.scalar_tensor_tensor(dst, cv[:], sc, sq[:], ALU.mult, ALU.mult),
                                     reads=[cvB, sqB], writes=[qkvB[part]])
                            if ksub == 13:
                                k.barrier()
                                return
                        if ksub == 14:
                            k.barrier()
                            return
                        pz, pzB = ps[3], psB[3]
                        for kc in range(8):
                            k.op('tensor', lambda e, kc=kc, hb=hb: e.matmul(pz[:, :], wcat[:, kc, 384:512], hb[:, kc, :], start=(kc == 0), stop=(kc == 7)),
                                 reads=[wcB, hbB], writes=[pzB])
                        k.op('scalar', lambda e, blk=blk: e.activation(zT[:, blk * 512:(blk + 1) * 512], pz[:, :], AF.Silu), reads=[pzB], writes=[zTB])
                    k.op('vector', lambda e, h=h: e.tensor_copy(gl[:, 0, :], glT[:, :, h]), reads=[glTB], writes=[glB])
                    k.op('vector', lambda e, h=h: e.tensor_copy(gl[:, 1, :], glT[:, :, 8 + h]), reads=[glTB], writes=[glB])
                    if ksub == 1:
                        k.barrier()
                        return
                    k.op('scalar', lambda e: e.activation(gl[:, 0, :], gl[:, 0, :], AF.Sigmoid), reads=[glB], writes=[glB])
                    k.op('vector', lambda e: e.tensor_scalar_mul(gl[:, 5, :], gl[:, 0, :], -1.0), reads=[glB], writes=[glB])
                    k.op('scalar', lambda e, h=h: e.activation(gl[:, 1, :], gl[:, 1, :], AF.Exp, bias=dtb[0:64, h:h + 1]), reads=[glB, dtB], writes=[glB])
                    k.op('scalar', lambda e: e.activation(gl[:, 1, :], gl[:, 1, :], AF.Ln, bias=1.0), reads=[glB], writes=[glB])
                    k.op('vector', lambda e, h=h: e.tensor_scalar_mul(gl[:, 1, :], gl[:, 1, :], nea[0:64, h:h + 1]), reads=[glB, alB], writes=[glB])
                    for c0 in range(0, NCH, 512):
                        n = min(512, NCH - c0)
                        k.op('tensor', lambda e, c0=c0, n=n: e.matmul(ps[0][0:64, 0:n], triu[0:64, 0:64], gl[:, 1, c0:c0 + n], start=True, stop=True),
                             reads=[glB, cB], writes=[psB[0]])
                        k.op('vector', lambda e, c0=c0, n=n: e.tensor_copy(gl[:, 2, c0:c0 + n], ps[0][0:64, 0:n]), reads=[psB[0]], writes=[glB])
                        k.op('tensor', lambda e, c0=c0, n=n: e.matmul(ps[1][:, 0:n], ones_f[0:64, :], gl[:, 1, c0:c0 + n], start=True, stop=True),
                             reads=[glB, cB], writes=[psB[1]])
                        k.op('scalar', lambda e, c0=c0, n=n: e.activation(egl[:, c0:c0 + n], ps[1][:, 0:n], AF.Exp), reads=[psB[1]], writes=[eglB])
                        k.op('vector', lambda e, c0=c0, n=n: e.tensor_copy(gl[:, 6, c0:c0 + n], ps[1][0:64, 0:n]), reads=[psB[1]], writes=[glB])
                    k.op('scalar', lambda e: e.activation(gl[:, 3, :], gl[:, 2, :], AF.Exp), reads=[glB], writes=[glB])
                    k.op('vector', lambda e: e.tensor_tensor(gl[:, 7, :], gl[:, 6, :], gl[:, 2, :], ALU.subtract), reads=[glB], writes=[glB])
                    k.op('scalar', lambda e: e.activation(gl[:, 4, :], gl[:, 7, :], AF.Exp), reads=[glB], writes=[glB])
                    k.op('vector', lambda e: e.memset(S[:], 0.0), writes=[SB])
                    if h == 0 and l == 0:
                        dump("gl", gl[:], glB)
                        dump("egl", egl[:], eglB)
                        dump("qT", qT[:, 0:128], qkvB[0])
                        dump("kT", kT[:, 0:128], qkvB[1])
                        dump("vT", vT[:, 0:128], qkvB[2])
                    if ksub == 2:
                        k.barrier()
                        return

                    def bulk(gi, res):
                        c0 = gi * G
                        gcols = slice(c0 * 64, (c0 + G) * 64)
                        csl = [slice((c0 + g) * 64, (c0 + g + 1) * 64) for g in range(G)]
                        kg, kgB = r_kg.next()
                        kd, kdB = r_kdec.next()
                        for g in range(G):
                            k.op('tensor', lambda e, g=g: e.transpose(pb[0:64, g * 256:g * 256 + 128], kT[:, csl[g]], identb[:]), reads=[qkvB[1], cB], writes=[pbB])
                            k.op('tensor', lambda e, g=g: e.transpose(pb[0:64, g * 256 + 128:g * 256 + 256], vT[:, csl[g]], identb[:]), reads=[qkvB[2], cB], writes=[pbB])
                        yield
                        for g in range(G):
                            c = c0 + g
                            k.op('vector', lambda e, g=g, c=c: e.tensor_scalar_mul(kg[:, g, 128:256], pb[0:64, g * 256:g * 256 + 128], gl[:, 3, c:c + 1]), reads=[pbB, glB], writes=[kgB])
                            k.op('vector', lambda e, g=g, c=c: e.tensor_scalar_mul(kd[:, g, :], pb[0:64, g * 256:g * 256 + 128], gl[:, 4, c:c + 1]), reads=[pbB, glB], writes=[kdB])
                        k.op('scalar', lambda e: e.copy(kg[:, :, 0:128], pb[0:64, :].rearrange("p (g c) -> p g c", g=G)[:, :, 128:256]), reads=[pbB], writes=[kgB])
                        yield
                        p0, p0B = ps[0], psB[0]
                        for g in range(G):
                            k.op('tensor', lambda e, g=g: e.matmul(p0[0:64, g * 128:g * 128 + 64], kT[:, csl[g]], kT[:, csl[g]], start=True, stop=True), reads=[qkvB[1]], writes=[p0B])
                            k.op('tensor', lambda e, g=g: e.matmul(p0[0:64, g * 128 + 64:g * 128 + 128], kT[:, csl[g]], qT[:, csl[g]], start=True, stop=True), reads=[qkvB[1], qkvB[0]], writes=[p0B])
                        p0v = p0[0:64, :].rearrange("p (g c) -> p g c", g=G)
                        yield
                        dg, dgB = r_dg.next()
                        for g in range(G):
                            c = c0 + g
                            k.op('gpsimd', lambda e, g=g, c=c: e.tensor_scalar_mul(dg[:, g, :], ident[0:64, 0:64], gl[:, 2, c:c + 1]), reads=[glB, cB], writes=[dgB])
                        p1, p1B = ps[1], psB[1]
                        for g in range(G):
                            k.op('tensor', lambda e, g=g: e.matmul(p1[:, g * 64:(g + 1) * 64], ones_f[0:64, :], dg[:, g, :], start=True, stop=True), reads=[dgB, cB], writes=[p1B])
                        yield
                        dm, dmB = r_dm.next()
                        for g in range(G):
                            c = c0 + g
                            k.op('vector', lambda e, g=g, c=c: e.tensor_scalar(dm[:, g, :], p1[0:64, g * 64:(g + 1) * 64], gl[:, 2, c:c + 1], 0.0, ALU.subtract, ALU.min), reads=[p1B, glB], writes=[dmB])
                        yield
                        egb, egbB = r_egb.next()
                        k.op('scalar', lambda e: e.activation(egb[:].rearrange("p g c -> p (g c)"), p1[:, 0:G * 64], AF.Exp), reads=[p1B], writes=[egbB])
                        k.op('scalar', lambda e: e.activation(dm[:], dm[:], AF.Exp), reads=[dmB], writes=[dmB])
                        k.op('vector', lambda e: e.tensor_tensor(dm[:], dm[:], triu4[:], ALU.mult), reads=[dmB, cB], writes=[dmB])
                        qg, qgB = r_qg.next()
                        k.op('gpsimd', lambda e: e.tensor_tensor(qg[:].rearrange("p g c -> p (g c)"), qT[:, gcols], egb[:].rearrange("p g c -> p (g c)"), ALU.mult), reads=[qkvB[0], egbB], writes=[qgB])
                        yield
                        at, atB = r_at.next()
                        k.op('vector', lambda e: e.tensor_tensor(at[:], p0v[:, :, 64:128], dm[:], ALU.mult), reads=[p0B, dmB], writes=[atB])
                        k.op('vector', lambda e: e.tensor_tensor(dm[:], dm[:], ident4[:], ALU.subtract), reads=[dmB, cB], writes=[dmB])
                        X, XB = r_X.next()
                        for g in range(G):
                            c = c0 + g
                            k.op('vector', lambda e, X=X, g=g, c=c: e.scalar_tensor_tensor(X[:, g, :], p0[0:64, g * 128:g * 128 + 64], gl[:, 5, c:c + 1], dm[:, g, :], ALU.mult, ALU.mult),
                                 reads=[p0B, glB, dmB], writes=[XB])
                        yield
                        p2, p2B = ps[2], psB[2]
                        p3, p3B = ps[3], psB[3]
                        p4, p4B = ps[4], psB[4]
                        for g in range(G):
                            k.op('tensor', lambda e, X=X, g=g: e.transpose(pb[0:64, g * 64:(g + 1) * 64], X[:, g, :], identb[0:64, 0:64]), reads=[XB, cB], writes=[pbB])
                        yield
                        Y, YB = r_Y.next()
                        k.op('scalar', lambda e, Y=Y: e.copy(Y[:].rearrange("p g c -> p (g c)"), pb[0:64, 0:G * 64]), reads=[pbB], writes=[YB])
                        P, PB = r_P.next()
                        k.op('vector', lambda e, X=X: e.tensor_tensor(P[:], X[:], ident4[:], ALU.add), reads=[XB, cB], writes=[PB])
                        Pb, PbB = r_Pb.next()
                        k.op('gpsimd', lambda e, Pb=Pb: e.tensor_copy(Pb[:], P[:]), reads=[PB], writes=[PbB])
                        for s in range(1, 6):
                            if s < 5:
                                for g in range(G):
                                    k.op('tensor', lambda e, X=X, Y=Y, g=g: e.matmul(p2[0:64, g * 64:(g + 1) * 64], Y[:, g, :], X[:, g, :], start=True, stop=True), reads=[XB, YB], writes=[p2B])
                            for g in range(G):
                                k.op('tensor', lambda e, X=X, Y=Y, g=g: e.matmul(p3[0:64, g * 64:(g + 1) * 64], X[:, g, :], Y[:, g, :], start=True, stop=True), reads=[XB, YB], writes=[p3B])
                            yield
                            Yn, YnB = r_Y.next()
                            k.op('scalar', lambda e, Yn=Yn: e.copy(Yn[:].rearrange("p g c -> p (g c)"), p3[0:64, 0:G * 64]), reads=[p3B], writes=[YnB])
                            if s < 5:
                                Xn, XnB = r_X.next()
                                k.op('vector', lambda e, Xn=Xn: e.tensor_copy(Xn[:].rearrange("p g c -> p (g c)"), p2[0:64, 0:G * 64]), reads=[p2B], writes=[XnB])
                            yield
                            for g in range(G):
                                k.op('tensor', lambda e, Yn=Yn, Pb=Pb, g=g: e.matmul(p4[0:64, g * 64:(g + 1) * 64], Yn[:, g, :], Pb[:, g, :], start=True, stop=True), reads=[YnB, PbB], writes=[p4B])
                            k.op('vector', lambda e: e.tensor_tensor(P[:].rearrange("p g c -> p (g c)"), P[:].rearrange("p g c -> p (g c)"), p4[0:64, 0:G * 64], ALU.add), reads=[p4B, PB], writes=[PB])
                            if s < 5:
                                Pb, PbB = r_Pb.next()
                                k.op('gpsimd', lambda e, Pb=Pb: e.tensor_copy(Pb[:], P[:]), reads=[PB], writes=[PbB])
                            yield
                            Y, YB = Yn, YnB
                            if s < 5:
                                X, XB = Xn, XnB
                        yield
                        uw, uwB = r_uw.next()
                        for rr in range(G // 2):
                            for g2 in range(2):
                                g = rr * 2 + g2
                                k.op('tensor', lambda e, g=g, g2=g2: e.matmul(p4[0:64, g2 * 256:(g2 + 1) * 256], P[:, g, :], kg[:, g, :], start=True, stop=True), reads=[PB, kgB], writes=[p4B])
                            for g2 in range(2):
                                g = rr * 2 + g2
                                c = c0 + g
                                k.op('vector', lambda e, g=g, g2=g2, c=c: e.tensor_scalar_mul(uw[:, g, :], p4[0:64, g2 * 256:(g2 + 1) * 256], gl[:, 0, c:c + 1]), reads=[p4B, glB], writes=[uwB])
                        yield
                        AT, ATB = r_AT.next()
                        Bc, BcB = r_Bc.next()
                        QpT, QpTB = r_QpT.next()
                        O0, O0B = r_O0.next()
                        for g in range(G):
                            k.op('tensor', lambda e, g=g: e.matmul(p2[:, g * 128:(g + 1) * 128], uw[:, g, 128:256], kd[:, g, :], start=True, stop=True), reads=[uwB, kdB], writes=[p2B])
                        for g in range(G):
                            k.op('tensor', lambda e, g=g: e.matmul(p3[:, g * 128:(g + 1) * 128], kd[:, g, :], uw[:, g, 0:128], start=True, stop=True), reads=[uwB, kdB], writes=[p3B])
                        yield
                        for g in range(G):
                            c = c0 + g
                            k.op('vector', lambda e, g=g, c=c: e.scalar_tensor_tensor(AT[:, g, :], ident[:], egl[:, c:c + 1], p2[:, g * 128:(g + 1) * 128], ALU.mult, ALU.subtract),
                                 reads=[p2B, eglB, cB], writes=[ATB])
                        k.op('scalar', lambda e: e.copy(Bc[:].rearrange("p g c -> p (g c)"), p3[:, 0:G * 128]), reads=[p3B], writes=[BcB])
                        yield
                        for g in range(G):
                            k.op('tensor', lambda e, g=g: e.matmul(p0[:, g * 64:(g + 1) * 64], uw[:, g, 128:256], at[:, g, :], start=True, stop=True), reads=[uwB, atB], writes=[p0B])
                        for g in range(G):
                            k.op('tensor', lambda e, g=g: e.matmul(p4[0:64, g * 128:(g + 1) * 128], at[:, g, :], uw[:, g, 0:128], start=True, stop=True), reads=[uwB, atB], writes=[p4B])
                        yield
                        k.op('vector', lambda e: e.tensor_tensor(QpT[:].rearrange("p g c -> p (g c)"), qg[:].rearrange("p g c -> p (g c)"), p0[:, 0:G * 64], ALU.subtract),
                             reads=[p0B, qgB], writes=[QpTB])
                        k.op('scalar', lambda e: e.copy(O0[:].rearrange("p g c -> p (g c)"), p4[0:64, 0:G * 128]), reads=[p4B], writes=[O0B])
                        yield
                        for g in range(G):
                            k.op('tensor', lambda e, g=g: e.transpose(pb[0:64, g * 128:(g + 1) * 128], zT[:, csl[g]], identb[:]), reads=[zTB, cB], writes=[pbB])
                        zs4, zs4B = r_zs4.next()
                        k.op('scalar', lambda e: e.copy(zs4[:].rearrange("p g c -> p (g c)"), pb[0:64, 0:G * 128]), reads=[pbB], writes=[zs4B])
                        res.update(dict(AT=(AT, ATB), Bc=(Bc, BcB), QpT=(QpT, QpTB), O0=(O0, O0B), zs=(zs4, zs4B)))

                    def scan(c, r, g):
                        AT_, ATB = r['AT']
                        Bc_, BcB = r['Bc']
                        QpT_, QpTB = r['QpT']
                        O0_, O0B = r['O0']
                        zs4_, zs4B_ = r['zs']
                        p5, p5B = ps[5], psB[5]
                        p6, p6B = ps[6], psB[6]
                        k.op('tensor', lambda e: e.matmul(p5[:, 0:128], AT_[:, g, :], S[:], start=True, stop=True), reads=[ATB, SB], writes=[p5B])
                        k.op('tensor', lambda e: e.matmul(p6[0:64, 0:128], QpT_[:, g, :], S[:], start=True, stop=True), reads=[QpTB, SB], writes=[p6B])
                        yield
                        k.op('vector', lambda e: e.tensor_tensor(S[:], p5[:, 0:128], Bc_[:, g, :], ALU.add), reads=[p5B, BcB, SB], writes=[SB])
                        yield
                        o, oB = r_o.next()
                        st, stB = r_st.next()
                        jk, jkB = r_jk.next()
                        k.op('vector', lambda e: e.tensor_tensor(o[:], p6[0:64, 0:128], O0_[:, g, :], ALU.add), reads=[p6B, O0B], writes=[oB])
                        k.op('scalar', lambda e: e.activation(jk[:], o[:], AF.Square, accum_out=st[:, 0:1]), reads=[oB], writes=[jkB, stB])
                        k.op('scalar', lambda e: e.activation(st[:, 1:2], st[:, 0:1], AF.Sqrt, bias=RMS_EPS, scale=1.0 / 128), reads=[stB], writes=[stB])
                        yield
                        k.op('vector', lambda e: e.reciprocal(st[:, 2:3], st[:, 1:2]), reads=[stB], writes=[stB])
                        k.op('vector', lambda e: e.scalar_tensor_tensor(o[:], o[:], st[:, 2:3], nw[0:64, :], ALU.mult, ALU.mult), reads=[oB, stB, nwB], writes=[oB])
                        ob, obB = r_ob.next()
                        k.op('gpsimd', lambda e: e.tensor_tensor(ob[:], o[:], zs4_[:, g, :], ALU.mult), reads=[oB, zs4B_], writes=[obB])
                        k.dma('sync', om_d[c * 64:(c + 1) * 64, h * 128:(h + 1) * 128], ob[:], reads=[obB])

                    assert NCH % G == 0
                    pend = {}
                    for _ in bulk(0, pend):
                        pass
                    for gi in range(NCH // G):
                        nxt = {}
                        bg = bulk(gi + 1, nxt) if gi + 1 < NCH // G else None
                        for g in range(G):
                            for _ in scan(gi * G + g, pend, g):
                                for _r in range(2):
                                    if bg is not None and next(bg, _DONE) is _DONE:
                                        bg = None
                        if bg is not None:
                            for _ in bg:
                                pass
                        pend = nxt
                k.barrier()

        def shared_kv():
            with ExitStack() as es:
                hbr = Ring(es, nc, "khb", [128, 8, 512], BF16, 2)
                kr = Ring(es, nc, "kst", [64, 512], BF16, 3)
                vr = Ring(es, nc, "vst", [128, 129], BF16, 3)
                for h in range(NH):
                    wk, wkB = load_w_bf16(es, f"wk{h}", kv_w[:, h * 128:(h + 1) * 128], 128) if h == 0 else (wk_keep, wkB_keep)
                    wv, wvB = load_w_bf16(es, f"wv{h}", kv_w[:, 1024 + h * 128:1024 + (h + 1) * 128], 128) if h == 0 else (wv_keep, wvB_keep)
                    if h == 0:
                        wk_keep, wkB_keep, wv_keep, wvB_keep = wk, wkB, wv, wvB
                    else:
                        k.dma('gpsimd', wk[:], kv_w[:, h * 128:(h + 1) * 128].rearrange("(k p) c -> p k c", p=128), writes=[wkB])
                        k.dma('gpsimd', wv[:], kv_w[:, 1024 + h * 128:1024 + (h + 1) * 128].rearrange("(k p) c -> p k c", p=128), writes=[wvB])
                    for blk in range(NB):
                        hb, hbB = hbr.next()
                        k.dma('sync', hb[:], hT_v[:, :, blk * 512:(blk + 1) * 512], writes=[hbB])
                        for s in range(2):
                            pp, ppB = ps[s], psB[s]
                            for kc in range(8):
                                k.op('tensor', lambda e, pp=pp, kc=kc, s=s, hb=hb: e.matmul(pp[0:64, :], wk[:, kc, s * 64:(s + 1) * 64], hb[:, kc, :],
                                                                                     start=(kc == 0), stop=(kc == 7)), reads=[wkB, hbB], writes=[ppB])
                            kt, ktB = kr.next()
                            k.op('scalar' if s else 'vector', (lambda e, kt=kt, pp=pp: e.copy(kt[:], pp[0:64, :])) if s else
                                 (lambda e, kt=kt, pp=pp: e.tensor_copy(kt[:], pp[0:64, :])), reads=[ppB], writes=[ktB])
                            k.dma('sync', kT_d[h, s, :, blk * 512:(blk + 1) * 512], kt[:], reads=[ktB])
                        for tt in range(4):
                            pp, ppB = ps[2 + tt % 2], psB[2 + tt % 2]
                            for kc in range(8):
                                k.op('tensor', lambda e, pp=pp, kc=kc, tt=tt, hb=hb: e.matmul(pp[:, 0:128], hb[:, kc, tt * 128:(tt + 1) * 128], wv[:, kc, :],
                                                                                       start=(kc == 0), stop=(kc == 7)), reads=[wvB, hbB], writes=[ppB])
                            vt, vtB = vr.next()
                            k.op('vector', lambda e, vt=vt, pp=pp: e.tensor_copy(vt[:, 0:128], pp[:, 0:128]), reads=[ppB], writes=[vtB])
                            k.op('gpsimd', lambda e, vt=vt: e.memset(vt[:, 128:129], 1.0), writes=[vtB])
                            tok0 = blk * 512 + tt * 128
                            k.dma('sync', va_d[h, tok0:tok0 + 128, :], vt[:], reads=[vtB])
                k.barrier()

        def diffattn(j, layer):
            lam_init = lambda_init(layer)
            with ExitStack() as es:
                lp, lpB = bcast_row(es, "lp", b_lambda[j:j + 1].rearrange("o a b -> o (a b)"), 256)
                sw, swB = bcast_row(es, "sw", b_subln_w[j:j + 1, :], 128)
                lam = sbt(es, "lam", [128, 8], F32)
                lamB = Buf("lam")
                pr = sbt(es, "lpr", [128, 128], F32)
                k.op('vector', lambda e: e.tensor_tensor(pr[:, 0:64], lp[:, 0:64], lp[:, 64:128], ALU.mult), reads=[lpB], writes=[lamB])
                k.op('vector', lambda e: e.tensor_tensor(pr[:, 64:128], lp[:, 128:192], lp[:, 192:256], ALU.mult), reads=[lpB], writes=[lamB])
                k.op('vector', lambda e: e.reduce_sum(lam[:, 0:1], pr[:, 0:64], AX.X), reads=[lamB], writes=[lamB])
                k.op('vector', lambda e: e.reduce_sum(lam[:, 1:2], pr[:, 64:128], AX.X), reads=[lamB], writes=[lamB])
                k.op('scalar', lambda e: e.activation(lam[:, 2:4], lam[:, 0:2], AF.Exp), reads=[lamB], writes=[lamB])
                k.op('vector', lambda e: e.tensor_tensor(lam[:, 4:5], lam[:, 2:3], lam[:, 3:4], ALU.subtract), reads=[lamB], writes=[lamB])
                k.op('vector', lambda e: e.tensor_scalar(lam[:, 5:6], lam[:, 4:5], lam_init, -1.0, ALU.add, ALU.mult), reads=[lamB], writes=[lamB])
                k.op('vector', lambda e: e.tensor_scalar_mul(sw[:], sw[:], 1.0 - lam_init), reads=[swB], writes=[swB])
                qs = [sbt(es, f"qs{s}", [64, T], BF16) for s in range(2)]
                qsB = [Buf("qs0"), Buf("qs1")]
                kTs = [sbt(es, f"kTs{s}", [64, T], BF16) for s in range(2)]
                kTB = [Buf("kT0"), Buf("kT1")]
                va = sbt(es, "va", [128, NT, 144], BF16)
                vaB = Buf("va")
                hbr = Ring(es, nc, "ahb", [128, 8, 512], BF16, 2)
                ptr = Ring(es, nc, "pt", [128, 512], BF16, 8)
                r_o1 = Ring(es, nc, "ao1", [128, 128], F32, 2)
                r_st = Ring(es, nc, "ast", [128, 8], F32, 2)
                r_jk = Ring(es, nc, "ajk", [128, 128], F32, 2)
                r_ob = Ring(es, nc, "aob", [128, 128], BF16, 2)
                wq = sbt(es, "wq", [128, 8, 128], BF16)
                wqB = Buf("wq")
                acc = {}
                slots = [(4, 0), (4, 144), (4, 288), (5, 0), (5, 144), (5, 288), (6, 0), (6, 144)]
                for s in range(2):
                    for i in range(4):
                        acc[(s, i)] = slots[s * 4 + i]
                for h in range(NH):
                    k.dma('gpsimd', wq[:], b_w_q[j, :, h * 128:(h + 1) * 128].rearrange("(k p) c -> p k c", p=128), writes=[wqB])
                    for s in range(2):
                        k.dma('sync', kTs[s][:], kT_d[h, s], writes=[kTB[s]])
                    k.dma('sync', va[:, :, 0:129], va_d[h].rearrange("(t p) c -> p t c", p=128), writes=[vaB])
                    for blk in range(NB):
                        hb, hbB = hbr.next()
                        k.dma('sync', hb[:], hT_v[:, :, blk * 512:(blk + 1) * 512], writes=[hbB])
                        for s in range(2):
                            pp, ppB = ps[s], psB[s]
                            for kc in range(8):
                                k.op('tensor', lambda e, pp=pp, kc=kc, s=s, hb=hb: e.matmul(pp[0:64, :], wq[:, kc, s * 64:(s + 1) * 64], hb[:, kc, :],
                                                                                     start=(kc == 0), stop=(kc == 7)), reads=[wqB, hbB], writes=[ppB])
                            k.op('scalar', lambda e, pp=pp, s=s, blk=blk: e.activation(qs[s][:, blk * 512:(blk + 1) * 512], pp[0:64, :], AF.Copy, scale=0.125),
                                 reads=[ppB], writes=[qsB[s]])
                    for qb in range(NB):
                        nkt = 4 * qb + 4
                        steps = [(kt, s) for kt in range(nkt) for s in range(2)]
                        LA = 3

                        def front(idx, qb=qb):
                            kt, s = steps[idx]
                            jd = kt - 4 * qb
                            lo = 0 if jd < 0 else jd * 128
                            n = 512 - lo
                            pp, ppB = ps[idx % 4], psB[idx % 4]
                            k.op('tensor', lambda e, pp=pp, s=s, kt=kt, qb=qb, lo=lo, n=n: e.matmul(pp[:, 0:n], kTs[s][:, kt * 128:(kt + 1) * 128],
                                                                                             qs[s][:, qb * 512 + lo:(qb + 1) * 512], start=True, stop=True),
                                 reads=[kTB[s], qsB[s]], writes=[ppB])
                            pt, ptB = ptr.next()
                            k.op('scalar', lambda e, pt=pt, pp=pp, n=n: e.activation(pt[:, 0:n], pp[:, 0:n], AF.Exp), reads=[ppB], writes=[ptB])
                            if jd >= 0:
                                k.op('vector', lambda e, pt=pt: e.tensor_tensor(pt[:, 0:128], pt[:, 0:128], triub[:], ALU.mult), reads=[ptB, cB], writes=[ptB])
                            return pt, ptB, jd, lo

                        def back(idx, fr, qb=qb):
                            kt, s = steps[idx]
                            pt, ptB, jd, lo = fr
                            for i in range(max(jd, 0), 4):
                                bank, off = acc[(s, i)]
                                c0 = i * 128 - lo
                                st_flag = (kt == 0 and off == 0)
                                k.op('tensor', lambda e, pt=pt, bank=bank, off=off, c0=c0, kt=kt, i=i, qb=qb, st_flag=st_flag: e.matmul(
                                    ps[bank][:, off:off + 129], pt[:, c0:c0 + 128], va[:, kt, 0:129], start=st_flag, stop=(kt == 4 * qb + i),
                                    skip_group_check=True),
                                    reads=[ptB, vaB], writes=[psB[bank]])

                        fronts = {}
                        for idx in range(len(steps) + LA):
                            if idx < len(steps):
                                fronts[idx] = front(idx)
                            if idx - LA >= 0:
                                back(idx - LA, fronts.pop(idx - LA))
                        for i in range(4):
                            b1, f1 = acc[(0, i)]
                            b2, f2 = acc[(1, i)]
                            st, stB = r_st.next()
                            o1, o1B = r_o1.next()
                            jk, jkB = r_jk.next()
                            ob, obB = r_ob.next()
                            k.op('vector', lambda e, st=st, b1=b1, f1=f1: e.reciprocal(st[:, 0:1], ps[b1][:, f1 + 128:f1 + 129]), reads=[psB[b1]], writes=[stB])
                            k.op('vector', lambda e, st=st, b2=b2, f2=f2: e.reciprocal(st[:, 1:2], ps[b2][:, f2 + 128:f2 + 129]), reads=[psB[b2]], writes=[stB])
                            k.op('vector', lambda e, st=st: e.tensor_tensor(st[:, 2:3], st[:, 1:2], lam[:, 5:6], ALU.mult), reads=[stB, lamB], writes=[stB])
                            k.op('vector', lambda e, st=st, o1=o1, b1=b1, f1=f1: e.tensor_scalar_mul(o1[:], ps[b1][:, f1:f1 + 128], st[:, 0:1]),
                                 reads=[psB[b1], stB], writes=[o1B])
                            k.op('vector', lambda e, st=st, o1=o1, b2=b2, f2=f2: e.scalar_tensor_tensor(o1[:], ps[b2][:, f2:f2 + 128], st[:, 2:3], o1[:], ALU.mult, ALU.add),
                                 reads=[psB[b2], stB, o1B], writes=[o1B])
                            k.op('scalar', lambda e, st=st, o1=o1, jk=jk: e.activation(jk[:], o1[:], AF.Square, accum_out=st[:, 3:4]), reads=[o1B], writes=[jkB, stB])
                            k.op('scalar', lambda e, st=st: e.activation(st[:, 4:5], st[:, 3:4], AF.Sqrt, bias=RMS_EPS, scale=1.0 / 128), reads=[stB], writes=[stB])
                            k.op('vector', lambda e, st=st: e.reciprocal(st[:, 5:6], st[:, 4:5]), reads=[stB], writes=[stB])
                            k.op('vector', lambda e, st=st, o1=o1, ob=ob: e.scalar_tensor_tensor(ob[:], o1[:], st[:, 5:6], sw[:], ALU.mult, ALU.mult),
                                 reads=[o1B, stB, swB], writes=[obB])
                            t0 = qb * 512 + i * 128
                            k.dma('sync', om_d[t0:t0 + 128, h * 128:(h + 1) * 128], ob[:], reads=[obB])
                k.barrier()

        def tok_moe(layer, w_out_src, last):
            with ExitStack() as es:
                wo, woB = load_w_bf16(es, "wo", w_out_src, 1024)
                wr = sbt(es, "wr", [128, 8, 40], F32)
                wrB = Buf("wr")
                k.dma('sync', wr[:, :, 0:4], moe_w_group[layer].rearrange("(k p) c -> p k c", p=128), writes=[wrB])
                k.dma('sync', wr[:, :, 4:36], moe_w_expert[layer].rearrange("(k p) c -> p k c", p=128), writes=[wrB])
                g1, g1B = bcast_row(es, "lng1", ln_mix_g[layer:layer + 1, :], D)
                b1, b1B = bcast_row(es, "lnb1", ln_mix_b[layer:layer + 1, :], D)
                lnB1 = Buf("ln1")
                oh = [sbt(es, f"oh{i}", [128, NT, 32], F32) for i in range(2)]
                ohB = Buf("oh")
                gates = sbt(es, "gates", [128, NT, 2], F32)
                gB = Buf("gates")
                rings = {'hTs': Ring(es, nc, "hTs2", [128, 8, 128], BF16, 3), 'st': Ring(es, nc, "lst", [128, 8], F32, 3),
                         'junk': Ring(es, nc, "ljk", [128, D], F32, 2)}
                with ExitStack() as e1:
                    r_om = Ring(e1, nc, "om", [128, D], BF16, 3)
                    r_omT = Ring(e1, nc, "omT", [128, 8, 128], BF16, 3)
                    r_h = Ring(e1, nc, "hh", [128, D], F32, 3)
                    r_t = Ring(e1, nc, "tt", [128, D], F32, 3)
                    r_y = Ring(e1, nc, "yy", [128, D], F32, 3)
                    r_xb = Ring(e1, nc, "xb", [128, D], BF16, 3)
                    r_xT = Ring(e1, nc, "xT", [128, 8, 128], F32, 3)
                    r_rt = Ring(e1, nc, "rt", [128, 160], F32, 3)
                    def tok_body(t):
                        rows = slice(t * 128, (t + 1) * 128)
                        om, omB = r_om.next()
                        k.dma('sync', om[:], om_d[rows, :], writes=[omB])
                        hh, hhB = r_h.next()
                        k.dma('sync', hh[:], h_d[rows, :], writes=[hhB])
                        for kc in range(8):
                            k.op('tensor', lambda e, om=om, kc=kc: e.transpose(pb[:, kc * 128:(kc + 1) * 128], om[:, kc * 128:(kc + 1) * 128], identb[:]),
                                 reads=[omB, cB], writes=[pbB])
                        omT, omTB = r_omT.next()
                        k.op('vector', lambda e, omT=omT: e.tensor_copy(omT[:], pb[:].rearrange("p (a b) -> p a b", a=8)), reads=[pbB], writes=[omTB])
                        yield
                        tt, ttB = r_t.next()
                        for half in range(2):
                            pp, ppB = ps[half], psB[half]
                            for kc in range(8):
                                k.op('tensor', lambda e, pp=pp, kc=kc, half=half, omT=omT: e.matmul(pp[:, :], omT[:, kc, :], wo[:, kc, half * 512:(half + 1) * 512],
                                                                                             start=(kc == 0), stop=(kc == 7)), reads=[omTB, woB], writes=[ppB])
                            k.op('vector', lambda e, pp=pp, half=half, tt=tt, hh=hh: e.scalar_tensor_tensor(tt[:, half * 512:(half + 1) * 512], hh[:, half * 512:(half + 1) * 512],
                                                                                                      ALPHA, pp[:, :], ALU.mult, ALU.add),
                                 reads=[ppB, hhB], writes=[ttB])
                        yield
                        yy, yyB = r_y.next()
                        layer_norm(rings, tt, ttB, g1, b1, [g1B, b1B], yy, yyB)
                        yield
                        k.dma('sync', h_d[rows, :], yy[:], reads=[yyB, hhB])
                        xb, xbB = r_xb.next()
                        k.op('scalar', lambda e, xb=xb, yy=yy: e.copy(xb[:], yy[:]), reads=[yyB], writes=[xbB])
                        k.dma('sync', xb_d[rows, :], xb[:], reads=[xbB])
                        yield
                        xT, xTB = r_xT.next()
                        for half in range(2):
                            pp, ppB = ps[2 + half], psB[2 + half]
                            for jj in range(4):
                                kc = half * 4 + jj
                                k.op('tensor', lambda e, pp=pp, jj=jj, kc=kc, yy=yy: e.transpose(pp[:, jj * 128:(jj + 1) * 128], yy[:, kc * 128:(kc + 1) * 128], ident[:]),
                                     reads=[yyB, cB], writes=[ppB])
                            if half == 0:
                                k.op('vector', lambda e, pp=pp, xT=xT: e.tensor_copy(xT[:, 0:4, :], pp[:].rearrange("p (a b) -> p a b", a=4)), reads=[ppB], writes=[xTB])
                            else:
                                k.op('scalar', lambda e, pp=pp, xT=xT: e.copy(xT[:, 4:8, :], pp[:].rearrange("p (a b) -> p a b", a=4)), reads=[ppB], writes=[xTB])
                        yield
                        p4, p4B = ps[4], psB[4]
                        for kc in range(8):
                            k.op('tensor', lambda e, kc=kc, xT=xT: e.matmul(p4[:, 0:36], xT[:, kc, :], wr[:, kc, 0:36], start=(kc == 0), stop=(kc == 7)),
                                 reads=[xTB, wrB], writes=[p4B])
                        rt, rtB = r_rt.next()
                        V = lambda fn, rt=rt, rtB=rtB, extra_r=(), extra_w=(): k.op('vector', fn, reads=[rtB] + list(extra_r), writes=[rtB] + list(extra_w))
                        k.op('vector', lambda e, rt=rt: e.tensor_copy(rt[:, 0:36], p4[:, 0:36]), reads=[p4B], writes=[rtB])
                        V(lambda e, rt=rt: e.reduce_max(rt[:, 36:37], rt[:, 0:4], AX.X))
                        V(lambda e, rt=rt: e.tensor_scalar_mul(rt[:, 37:38], rt[:, 36:37], -1.0))
                        k.op('scalar', lambda e, rt=rt: e.activation(rt[:, 84:88], rt[:, 0:4], AF.Exp, bias=rt[:, 37:38], accum_out=rt[:, 38:39]), reads=[rtB], writes=[rtB])
                        V(lambda e, rt=rt: e.reciprocal(rt[:, 39:40], rt[:, 38:39]))
                        V(lambda e, rt=rt: e.tensor_scalar(rt[:, 40:44], rt[:, 0:4], rt[:, 36:37], None, ALU.is_ge))
                        V(lambda e, rt=rt: e.tensor_scalar(rt[:, 40:44], rt[:, 40:44], -NEG, NEG, ALU.mult, ALU.add))
                        for g in range(4):
                            V(lambda e, rt=rt, g=g: e.tensor_scalar(rt[:, 44 + g * 8:52 + g * 8], rt[:, 4 + g * 8:12 + g * 8], rt[:, 40 + g:41 + g], None, ALU.add))
                        yield
                        V(lambda e, rt=rt: e.reduce_max(rt[:, 76:77], rt[:, 44:76], AX.X))
                        V(lambda e, rt=rt, t=t: e.tensor_scalar(oh[0][:, t, :], rt[:, 44:76], rt[:, 76:77], None, ALU.is_ge), extra_w=[ohB])
                        V(lambda e, rt=rt, t=t: e.scalar_tensor_tensor(rt[:, 96:128], oh[0][:, t, :], NEG, rt[:, 44:76], ALU.mult, ALU.add), extra_r=[ohB])
                        V(lambda e, rt=rt: e.reduce_max(rt[:, 77:78], rt[:, 96:128], AX.X))
                        V(lambda e, rt=rt, t=t: e.tensor_scalar(oh[1][:, t, :], rt[:, 96:128], rt[:, 77:78], None, ALU.is_ge), extra_w=[ohB])
                        V(lambda e, rt=rt: e.tensor_tensor(rt[:, 78:79], rt[:, 77:78], rt[:, 76:77], ALU.subtract))
                        k.op('scalar', lambda e, rt=rt: e.activation(rt[:, 79:80], rt[:, 78:79], AF.Exp), reads=[rtB], writes=[rtB])
                        V(lambda e, rt=rt: e.tensor_scalar_add(rt[:, 80:81], rt[:, 79:80], 1.0))
                        V(lambda e, rt=rt: e.reciprocal(rt[:, 81:82], rt[:, 80:81]))
                        V(lambda e, rt=rt, t=t: e.tensor_tensor(gates[:, t, 0:1], rt[:, 39:40], rt[:, 81:82], ALU.mult), extra_w=[gB])
                        V(lambda e, rt=rt, t=t: e.tensor_tensor(gates[:, t, 1:2], rt[:, 39:40], gates[:, t, 0:1], ALU.subtract), extra_r=[gB], extra_w=[gB])
                    interleave((tok_body(t) for t in range(NT)), depth=3)
                    k.barrier()
                if checkpoint(noraise=True):
                    return True
                dest = sbt(es, "dest", [128, 2, NT], I32)
                destB = Buf("dest")
                with ExitStack() as e2:
                    selb = sbt(e2, "selb", [128, NT, 32], BF16)
                    cnt = sbt(e2, "cnt", [128, NT, 32], F32)
                    pref = sbt(e2, "pref", [128, NT, 32], F32)
                    slot = sbt(e2, "slot", [128, NT, 32], F32)
                    ebase = sbt(e2, "ebase", [128, 32], F32)
                    tmp = sbt(e2, "ptmp", [128, NT, 32], F32)
                    dfl = sbt(e2, "dfl", [128, 2, NT], F32)
                    pB = Buf("pos")
                    k.op('gpsimd', lambda e: e.iota(ebase[:], [[CAP, 32]], base=0, channel_multiplier=0, allow_small_or_imprecise_dtypes=True), writes=[pB])
                    k.op('vector', lambda e: e.tensor_tensor(selb[:], oh[0][:], oh[1][:], ALU.add), reads=[ohB], writes=[pB])
                    TPB = 16
                    for t0 in range(0, NT, TPB):
                        n = min(TPB, NT - t0)
                        k.op('tensor', lambda e, t0=t0, n=n: e.matmul(ps[0][:, 0:n * 32], ones_b[:], selb[:, t0:t0 + n, :].rearrange("p a b -> p (a b)"), start=True, stop=True),
                             reads=[pB, cB], writes=[psB[0]])
                        k.op('vector', lambda e, t0=t0, n=n: e.tensor_copy(cnt[:, t0:t0 + n, :].rearrange("p a b -> p (a b)"), ps[0][:, 0:n * 32]), reads=[psB[0]], writes=[pB])
                        k.op('tensor', lambda e, t0=t0, n=n: e.matmul(ps[1][:, 0:n * 32], sutri[:], selb[:, t0:t0 + n, :].rearrange("p a b -> p (a b)"), start=True, stop=True),
                             reads=[pB, cB], writes=[psB[1]])
                        k.op('vector', lambda e, t0=t0, n=n: e.tensor_copy(slot[:, t0:t0 + n, :].rearrange("p a b -> p (a b)"), ps[1][:, 0:n * 32]), reads=[psB[1]], writes=[pB])
                    k.op('vector', lambda e: e.memset(pref[:, 0, :], 0.0), reads=[pB], writes=[pB])
                    for t in range(1, NT):
                        k.op('vector', lambda e, t=t: e.tensor_tensor(pref[:, t, :], pref[:, t - 1, :], cnt[:, t - 1, :], ALU.add), reads=[pB], writes=[pB])
                    k.op('vector', lambda e: e.tensor_tensor(slot[:], slot[:], pref[:], ALU.add), reads=[pB], writes=[pB])
                    k.op('vector', lambda e: e.tensor_scalar(tmp[:], slot[:], float(CAP), 1.0e6, ALU.is_ge, ALU.mult), reads=[pB], writes=[pB])
                    k.op('vector', lambda e: e.tensor_tensor(slot[:], slot[:], tmp[:], ALU.add), reads=[pB], writes=[pB])
                    for t in range(NT):
                        k.op('gpsimd', lambda e, t=t: e.tensor_tensor(slot[:, t, :], slot[:, t, :], ebase[:], ALU.add), reads=[pB], writes=[pB])
                    for i in range(2):
                        k.op('vector', lambda e, i=i: e.tensor_tensor(tmp[:], slot[:], oh[i][:], ALU.mult), reads=[pB, ohB], writes=[pB])
                        k.op('vector', lambda e, i=i: e.reduce_sum(dfl[:, i, :], tmp[:], AX.X), reads=[pB], writes=[pB])
                    k.op('vector', lambda e: e.tensor_scalar_min(dfl[:], dfl[:], float(NS)), reads=[pB], writes=[pB])
                    k.op('vector', lambda e: e.tensor_copy(dest[:], dfl[:]), reads=[pB], writes=[destB])
                    r_xb2 = Ring(e2, nc, "xb2", [128, D], BF16, 3)
                    for t in range(NT):
                        xb, xbB = r_xb2.next()
                        k.dma('sync', xb[:], xb_d[t * 128:(t + 1) * 128, :], writes=[xbB])
                        for i in range(2):
                            k.op('gpsimd', lambda e, xb=xb, i=i, t=t: e.indirect_dma_start(
                                out=xs_d, out_offset=bass.IndirectOffsetOnAxis(ap=dest[:, i, t:t + 1], axis=0), in_=xb[:], in_offset=None),
                                reads=[xbB, destB], dma=True)
                    k.barrier()
                with ExitStack() as e3:
                    NG = (CAP + 127) // 128
                    r_w13 = Ring(e3, nc, "w13", [128, 8, 1024], BF16, 2)
                    r_w2 = Ring(e3, nc, "w2", [128, 4, 1024], BF16, 2)
                    r_xs = Ring(e3, nc, "xs", [128, NG, D], BF16, 2)
                    r_xsT = Ring(e3, nc, "xsT", [128, 8, CAP], BF16, 2)
                    r_sg = Ring(e3, nc, "sg", [128, 4, CAP], F32, 1)
                    r_hid = Ring(e3, nc, "hid", [128, 4, CAP], BF16, 2)
                    r_ys = Ring(e3, nc, "ys", [128, D], F32, 2)
                    for ex in range(32):
                        w13, w13B = r_w13.next()
                        k.dma('gpsimd', w13[:], moe_w13[layer, ex].rearrange("(k p) c -> p k c", p=128), writes=[w13B])
                        w2, w2B = r_w2.next()
                        k.dma('gpsimd', w2[:], moe_w2[layer, ex].rearrange("(k p) c -> p k c", p=128), writes=[w2B])
                        xs, xsB = r_xs.next()
                        xsT, xsTB = r_xsT.next()
                        for g in range(NG):
                            r0 = ex * CAP + g * 128
                            nr = min(128, CAP - g * 128)
                            k.dma('sync', xs[0:nr, g, :], xs_d[r0:r0 + nr, :], writes=[xsB])
                        for g in range(NG):
                            nr = min(128, CAP - g * 128)
                            for kc in range(8):
                                k.op('tensor', lambda e, xs=xs, g=g, kc=kc, nr=nr: e.transpose(pb[:, kc * 128:kc * 128 + nr], xs[0:nr, g, kc * 128:(kc + 1) * 128], identb[0:nr, 0:nr]),
                                     reads=[xsB, cB], writes=[pbB])
                            k.op('vector', lambda e, xsT=xsT, g=g, nr=nr: e.tensor_copy(xsT[:, :, g * 128:g * 128 + nr], pb[:].rearrange("p (a b) -> p a b", a=8)[:, :, 0:nr]),
                                 reads=[pbB], writes=[xsTB])
                        sg, sgB = r_sg.next()
                        hid, hidB = r_hid.next()
                        for n0 in range(0, CAP, 512):
                            n = min(512, CAP - n0)
                            for m in range(8):
                                pp, ppB = ps[m % 4], psB[m % 4]
                                for kc in range(8):
                                    k.op('tensor', lambda e, pp=pp, kc=kc, m=m, w13=w13, xsT=xsT, n0=n0, n=n: e.matmul(pp[:, 0:n], w13[:, kc, m * 128:(m + 1) * 128], xsT[:, kc, n0:n0 + n],
                                                                                                           start=(kc == 0), stop=(kc == 7)),
                                         reads=[w13B, xsTB], writes=[ppB])
                                if m < 4:
                                    k.op('scalar', lambda e, pp=pp, m=m, sg=sg, n0=n0, n=n: e.activation(sg[:, m, n0:n0 + n], pp[:, 0:n], AF.Silu), reads=[ppB], writes=[sgB])
                                else:
                                    k.op('vector', lambda e, pp=pp, m=m, sg=sg, hid=hid, n0=n0, n=n: e.tensor_tensor(hid[:, m - 4, n0:n0 + n], sg[:, m - 4, n0:n0 + n], pp[:, 0:n], ALU.mult),
                                         reads=[ppB, sgB], writes=[hidB])
                        for g in range(NG):
                            nr = min(128, CAP - g * 128)
                            ys, ysB = r_ys.next()
                            for half in range(2):
                                pp, ppB = ps[4 + half], psB[4 + half]
                                for f in range(4):
                                    k.op('tensor', lambda e, pp=pp, f=f, half=half, hid=hid, w2=w2, g=g, nr=nr: e.matmul(pp[0:nr, :], hid[:, f, g * 128:g * 128 + nr], w2[:, f, half * 512:(half + 1) * 512],
                                                                                                             start=(f == 0), stop=(f == 3)),
                                         reads=[hidB, w2B], writes=[ppB])
                                if half == 0:
                                    k.op('vector', lambda e, pp=pp, ys=ys, nr=nr: e.tensor_copy(ys[0:nr, 0:512], pp[0:nr, :]), reads=[ppB], writes=[ysB])
                                else:
                                    k.op('scalar', lambda e, pp=pp, ys=ys, nr=nr: e.copy(ys[0:nr, 512:1024], pp[0:nr, :]), reads=[ppB], writes=[ysB])
                            r0 = ex * CAP + g * 128
                            for hf in range(2):
                                k.dma('sync', ys_h[hf][r0:r0 + nr, :], ys[0:nr, hf * 512:(hf + 1) * 512], reads=[ysB])
                    k.barrier()
                with ExitStack() as e4:
                    g2, g2B = bcast_row(e4, "lng2", ln_ffn_g[layer:layer + 1, :], D)
                    b2, b2B = bcast_row(e4, "lnb2", ln_ffn_b[layer:layer + 1, :], D)
                    r_yq = [Ring(e4, nc, f"yq{q}", [128, 512], F32, 3) for q in range(4)]
                    r_h = Ring(e4, nc, "ch", [128, D], F32, 3)
                    r_o = Ring(e4, nc, "co", [128, D], F32, 3)
                    def comb_body(t):
                        rows = slice(t * 128, (t + 1) * 128)
                        hh, hhB = r_h.next()
                        k.dma('sync', hh[:], h_d[rows, :], writes=[hhB])
                        k.op('scalar', lambda e, hh=hh: e.mul(hh[:], hh[:], ALPHA), reads=[hhB], writes=[hhB])
                        for i in range(2):
                            for hf in range(2):
                                yq, yqB = r_yq[i * 2 + hf].next()
                                k.op('gpsimd', lambda e, yq=yq, i=i, t=t, hf=hf: e.indirect_dma_start(
                                    out=yq[:], out_offset=None, in_=ys_h[hf],
                                    in_offset=bass.IndirectOffsetOnAxis(ap=dest[:, i, t:t + 1], axis=0)), reads=[destB], writes=[yqB], dma=True)
                                k.op('vector', lambda e, hh=hh, yq=yq, t=t, i=i, hf=hf: e.scalar_tensor_tensor(
                                    hh[:, hf * 512:(hf + 1) * 512], yq[:], gates[:, t, i:i + 1], hh[:, hf * 512:(hf + 1) * 512], ALU.mult, ALU.add),
                                    reads=[hhB, yqB, gB], writes=[hhB])
                        yield
                        oo, ooB = r_o.next()
                        layer_norm(rings, hh, hhB, g2, b2, [g2B, b2B], oo, ooB)
                        yield
                        if last:
                            out_toks.append(k.dma('sync', out[rows, :], oo[:], reads=[ooB]))
                        else:
                            k.dma('sync', h_d[rows, :], oo[:], reads=[ooB, hhB])
                            make_hT(rings, oo, ooB, t)
                    interleave((comb_body(t) for t in range(NT)), depth=3)
                    k.barrier()

        try:
            checkpoint()
            for layer in range(4):
                if layer < 2:
                    deltanet(layer)
                    checkpoint()
                    if tok_moe(layer, a_w_out[layer], last=False):
                        raise _Stop()
                    checkpoint()
                else:
                    j = layer - 2
                    if j == 0:
                        shared_kv()
                    diffattn(j, layer)
                    checkpoint()
                    if tok_moe(layer, b_w_out[j], last=(layer == 3)):
                        raise _Stop()
                    checkpoint()
        except _Stop:
            out_toks.append(k.dma('sync', out, h_d))
            out_toks.append(k.dma('sync', dbg, om_d))
        k.wait_all('sync', out_toks)
        k.finish()
    return nc, k


SEQ = 8192
CAP_FULL = 640
_cache = {}


def kernel(**inputs):
    x = np.asarray(inputs['x'])
    B, T, _ = x.shape
    cap = CAP_FULL if T == SEQ else max(64, int(T / 16 + 6 * math.sqrt(T / 16) + 16) // 32 * 32 + 32)
    key = (T, cap)
    if key not in _cache:
        _cache[key] = build(T, cap)[0]
    nc = _cache[key]
    shared = {n: np.ascontiguousarray(np.asarray(v, dtype=np.float32)) for n, v in inputs.items() if n != 'x'}
    in_maps = []
    for b in range(B):
        m = dict(shared)
        m['x'] = np.ascontiguousarray(x[b])
        in_maps.append(m)
    res = run_bass_kernel_spmd(nc, in_maps, core_ids=list(range(B)))
    return np.stack([np.asarray(res.results[b]['out']) for b in range(B)], axis=0).astype(np.float32)
```
